# Optimizing a Trainium2 kernel written in Bass

```python
import math
import jax, jax.numpy as jnp
from jax import lax
import numpy as np

D_MODEL = 2048
BATCH = 4
SEQ = 4096
DEPTH = 1

M_HEADS = 4
M_DH = D_MODEL // M_HEADS
M_WIDTH = M_HEADS * M_DH
M_CHUNK = 64
A_HEADS = 16
A_KV_HEADS = 4
A_GROUP = A_HEADS // A_KV_HEADS
A_DH = D_MODEL // A_HEADS
A_WIDTH = A_HEADS * A_DH
WINDOW = 128
A_BLOCK = WINDOW
REL_BUCKETS = 32
REL_MAX_DIST = 128
PEER_HEADS = 8
PEER_NKEYS = 128
PEER_TOPK = 16
PEER_QDIM = 256
PEER_EXPERTS = PEER_NKEYS * PEER_NKEYS
PEER_TOKEN_BLOCK = 128
EPS = 1e-6

IN_SPLITS = (M_WIDTH, M_WIDTH, M_WIDTH, M_WIDTH, 4 * M_HEADS,
             A_WIDTH, A_KV_HEADS * A_DH, A_KV_HEADS * A_DH, D_MODEL, D_MODEL)
IN_COLS = sum(IN_SPLITS)
IN_OFFSETS = tuple(int(v) for v in np.cumsum(IN_SPLITS)[:-1])

kernel_name = "hybrid_mlstm_swa_peer_block"


def rmsnorm(x, g):
    xf = x.astype(jnp.float32)
    y = xf * lax.rsqrt(jnp.mean(xf * xf, axis=-1, keepdims=True) + EPS)
    return (y * g.astype(jnp.float32)).astype(x.dtype)


def mlstm_chunkwise(q, k, v, log_i, log_f):
    B, H, S, dk = q.shape
    dv = v.shape[-1]
    nc = S // M_CHUNK

    def to_chunks(a):
        a = a.reshape((B, H, nc, M_CHUNK) + a.shape[3:])
        return jnp.moveaxis(a, 2, 0)

    causal_in_chunk = jnp.tril(jnp.ones((M_CHUNK, M_CHUNK), dtype=bool))

    def step(carry, inp):
        C, n, m = carry
        q_, k_, v_, li, lf = inp
        b = jnp.cumsum(lf, axis=-1)
        dmat = b[..., :, None] - b[..., None, :] + li[..., None, :]
        dmat = jnp.where(causal_in_chunk, dmat, -jnp.inf)
        inter = b + m[..., None]
        m_row = jnp.maximum(inter, jnp.max(dmat, axis=-1))
        w_inter = jnp.exp(inter - m_row)
        s = jnp.einsum('bhld,bhsd->bhls', q_, k_) * jnp.exp(dmat - m_row[..., None])
        num = (w_inter[..., None] * jnp.einsum('bhld,bhdv->bhlv', q_, C)
               + jnp.einsum('bhls,bhsv->bhlv', s, v_))
        den = w_inter * jnp.einsum('bhld,bhd->bhl', q_, n) + jnp.sum(s, axis=-1)
        h = num / jnp.maximum(jnp.abs(den), jnp.exp(-m_row))[..., None]
        b_last = b[..., -1]
        g = b_last[..., None] - b + li
        m_new = jnp.maximum(b_last + m, jnp.max(g, axis=-1))
        decay = jnp.exp(b_last + m - m_new)
        wk = jnp.exp(g - m_new[..., None])
        kw = k_ * wk[..., None]
        C_new = decay[..., None, None] * C + jnp.einsum('bhld,bhlv->bhdv', kw, v_)
        n_new = decay[..., None] * n + jnp.sum(kw, axis=2)
        return (C_new, n_new, m_new), h

    init = (jnp.zeros((B, H, dk, dv), jnp.float32),
            jnp.zeros((B, H, dk), jnp.float32),
            jnp.zeros((B, H), jnp.float32))
    _, hs = lax.scan(step, init, (to_chunks(q), to_chunks(k), to_chunks(v),
                                  to_chunks(log_i), to_chunks(log_f)))
    return jnp.moveaxis(hs, 0, 2).reshape(B, H, S, dv)


def t5_bucket(rel):
    half = REL_BUCKETS // 2
    max_exact = half // 2
    ret = jnp.where(rel > 0, half, 0)
    n = jnp.abs(rel)
    nf = jnp.maximum(n, 1).astype(jnp.float32)
    large = max_exact + (jnp.log(nf / max_exact) / math.log(REL_MAX_DIST / max_exact)
                         * (half - max_exact)).astype(jnp.int32)
    large = jnp.minimum(large, half - 1)
    return ret + jnp.where(n < max_exact, n, large)


def banded_window_attention(q, k, v, sink, rel_bias):
    B, S = q.shape[0], q.shape[1]
    nb = S // A_BLOCK
    qb = q.reshape(B, nb, A_BLOCK, A_KV_HEADS, A_GROUP, A_DH)
    pad = ((0, 0), (A_BLOCK, A_BLOCK), (0, 0), (0, 0))

    def band(t):
        tp = jnp.pad(t, pad).reshape(B, nb + 2, A_BLOCK, A_KV_HEADS, A_DH)
        return jnp.concatenate([tp[:, :-2], tp[:, 1:-1], tp[:, 2:]], axis=2)

    k_band, v_band = band(k), band(v)
    q_local = jnp.arange(A_BLOCK)
    k_local = jnp.arange(3 * A_BLOCK) - A_BLOCK
    rel = k_local[None, :] - q_local[:, None]
    in_window = jnp.abs(rel) <= WINDOW
    bias = rel_bias.astype(jnp.float32)[t5_bucket(rel)]
    bias = bias.transpose(2, 0, 1).reshape(A_KV_HEADS, A_GROUP, A_BLOCK, 3 * A_BLOCK)
    k_pos = jnp.arange(nb)[:, None] * A_BLOCK + k_local[None, :]
    valid = (k_pos >= 0) & (k_pos < S)
    mask = in_window[None] & valid[:, None, :]
    sink_f = sink.astype(jnp.float32).reshape(A_KV_HEADS, A_GROUP, 1, 1)

    def block_fn(args):
        qb_, kb_, vb_, mb_ = args
        s = jnp.einsum('bqhgd,bkhd->bhgqk', qb_, kb_).astype(jnp.float32) + bias
        s = jnp.where(mb_, s, -jnp.inf)
        sink_col = jnp.broadcast_to(sink_f, s.shape[:-1] + (1,))
        p = jax.nn.softmax(jnp.concatenate([s, sink_col], axis=-1), axis=-1)[..., :-1]
        return jnp.einsum('bhgqk,bkhd->bqhgd', p.astype(vb_.dtype), vb_)

    out = lax.map(block_fn, (jnp.moveaxis(qb, 1, 0), jnp.moveaxis(k_band, 1, 0),
                             jnp.moveaxis(v_band, 1, 0), mask))
    return jnp.moveaxis(out, 0, 1).reshape(B, S, A_WIDTH)


def peer_ffn(xt, peer_wq, peer_keys, peer_u, peer_v):
    T = xt.shape[0]
    K = PEER_TOPK
    pq = (xt @ peer_wq).reshape(T, PEER_HEADS, 2, PEER_QDIM // 2)
    sub = jnp.einsum('thpd,hpnd->thpn', pq, peer_keys).astype(jnp.float32)
    sv, si = lax.top_k(sub, K)
    cand = (sv[:, :, 0, :, None] + sv[:, :, 1, None, :]).reshape(T, PEER_HEADS, K * K)
    cv, ci = lax.top_k(cand, K)
    e1 = jnp.take_along_axis(si[:, :, 0], ci // K, axis=-1)
    e2 = jnp.take_along_axis(si[:, :, 1], ci % K, axis=-1)
    experts = (e1 * PEER_NKEYS + e2).reshape(T, PEER_HEADS * K)
    gates = jax.nn.softmax(cv, axis=-1).reshape(T, PEER_HEADS * K).astype(xt.dtype)
    nblk = T // PEER_TOKEN_BLOCK

    def expert_block(args):
        xb, eb, gb = args
        u = peer_u[eb]
        act = jax.nn.gelu(jnp.einsum('tkd,td->tk', u, xb), approximate=False) * gb
        return jnp.einsum('tk,tkd->td', act, peer_v[eb])

    out = lax.map(expert_block, (xt.reshape(nblk, PEER_TOKEN_BLOCK, -1),
                                 experts.reshape(nblk, PEER_TOKEN_BLOCK, -1),
                                 gates.reshape(nblk, PEER_TOKEN_BLOCK, -1)))
    return out.reshape(T, -1)


def setup_inputs(seed: int = 0) -> dict:
    key = jax.random.key(seed)
    ks = jax.random.split(key, 20)
    nrm = lambda k, shape, scale: jax.random.normal(k, shape, jnp.float32) * scale
    x = nrm(ks[0], (BATCH, SEQ, D_MODEL), 1.0)
    norm1_g = 1.0 + nrm(ks[1], (D_MODEL,), 0.05)
    w_in = nrm(ks[2], (D_MODEL, IN_COLS), D_MODEL ** -0.5)
    ib = nrm(ks[3], (2, M_HEADS), 0.1)
    fb = jnp.linspace(3.0, 6.0, M_HEADS, dtype=jnp.float32)[None, :] + nrm(ks[4], (2, M_HEADS), 0.1)
    mlstm_gate_b = jnp.stack([ib, fb], axis=1)
    mlstm_norm_g = 1.0 + nrm(ks[5], (M_HEADS, M_DH), 0.05)
    w_m_proj = nrm(ks[6], (M_WIDTH, D_MODEL), M_WIDTH ** -0.5)
    attn_q_norm_g = 1.0 + nrm(ks[7], (A_DH,), 0.05)
    attn_k_norm_g = 1.0 + nrm(ks[8], (A_DH,), 0.05)
    attn_sink = nrm(ks[9], (A_HEADS,), 0.5)
    rel_bias = nrm(ks[10], (REL_BUCKETS, A_HEADS), 0.5)
    w_a_proj = nrm(ks[11], (A_WIDTH, D_MODEL), A_WIDTH ** -0.5)
    w_out = nrm(ks[12], (D_MODEL, D_MODEL), D_MODEL ** -0.5)
    norm2_g = 1.0 + nrm(ks[13], (D_MODEL,), 0.05)
    peer_wq = nrm(ks[14], (D_MODEL, PEER_HEADS * PEER_QDIM), D_MODEL ** -0.5)
    peer_keys = nrm(ks[15], (PEER_HEADS, 2, PEER_NKEYS, PEER_QDIM // 2), (PEER_QDIM // 2) ** -0.5)
    peer_u = nrm(ks[16], (PEER_EXPERTS, D_MODEL), D_MODEL ** -0.5)
    peer_v = nrm(ks[17], (PEER_EXPERTS, D_MODEL), PEER_HEADS ** -0.5)
    return {"x": x, "norm1_g": norm1_g, "w_in": w_in, "mlstm_gate_b": mlstm_gate_b,
            "mlstm_norm_g": mlstm_norm_g, "w_m_proj": w_m_proj,
            "attn_q_norm_g": attn_q_norm_g, "attn_k_norm_g": attn_k_norm_g,
            "attn_sink": attn_sink, "rel_bias": rel_bias, "w_a_proj": w_a_proj,
            "w_out": w_out, "norm2_g": norm2_g, "peer_wq": peer_wq,
            "peer_keys": peer_keys, "peer_u": peer_u, "peer_v": peer_v}


def reference(x, norm1_g, w_in, mlstm_gate_b, mlstm_norm_g, w_m_proj,
              attn_q_norm_g, attn_k_norm_g, attn_sink, rel_bias, w_a_proj,
              w_out, norm2_g, peer_wq, peer_keys, peer_u, peer_v):
    B, S, D = x.shape
    f32 = jnp.float32
    for _layer in range(DEPTH):
        xn = rmsnorm(x, norm1_g)
        proj = xn @ w_in
        mq, mk, mv, mo, mg, aq, ak, av, gm, ga = jnp.split(proj, IN_OFFSETS, axis=-1)

        heads_first = lambda t: t.reshape(B, S, M_HEADS, M_DH).transpose(0, 2, 1, 3).astype(f32)
        q_m = heads_first(mq)
        k_m = heads_first(mk) * (M_DH ** -0.5)
        v_m = heads_first(mv)
        gates = (mg.reshape(B, S, 2, 2, M_HEADS).astype(f32) + mlstm_gate_b.astype(f32))
        gates = gates.transpose(2, 3, 0, 4, 1)
        h_fwd = mlstm_chunkwise(q_m, k_m, v_m, gates[0, 0], jax.nn.log_sigmoid(gates[0, 1]))
        flip = lambda t: jnp.flip(t, axis=2)
        h_bwd = flip(mlstm_chunkwise(flip(q_m), flip(k_m), flip(v_m),
                                     flip(gates[1, 0]), flip(jax.nn.log_sigmoid(gates[1, 1]))))
        h_m = (h_fwd + h_bwd).transpose(0, 2, 1, 3)
        h_m = h_m * lax.rsqrt(jnp.mean(h_m * h_m, axis=-1, keepdims=True) + EPS) * mlstm_norm_g.astype(f32)
        h_m = (h_m.reshape(B, S, M_WIDTH) * jax.nn.sigmoid(mo.astype(f32))).astype(x.dtype)
        br_m = h_m @ w_m_proj

        q_a = rmsnorm(aq.reshape(B, S, A_HEADS, A_DH), attn_q_norm_g) * (A_DH ** -0.5)
        k_a = rmsnorm(ak.reshape(B, S, A_KV_HEADS, A_DH), attn_k_norm_g)
        v_a = av.reshape(B, S, A_KV_HEADS, A_DH)
        h_a = banded_window_attention(q_a, k_a, v_a, attn_sink, rel_bias)
        br_a = h_a @ w_a_proj

        merged = (jax.nn.sigmoid(gm.astype(f32)) * br_m.astype(f32)
                  + jax.nn.sigmoid(ga.astype(f32)) * br_a.astype(f32)).astype(x.dtype)
        x = x + merged @ w_out

        xn2 = rmsnorm(x, norm2_g).reshape(B * S, D)
        x = x + peer_ffn(xn2, peer_wq, peer_keys, peer_u, peer_v).reshape(B, S, D)
    return x
```

```python
import contextlib
import math
import numpy as np
import concourse.bass as bass
import concourse.mybir as mybir
from concourse.bass_utils import run_bass_kernel_spmd

F32 = mybir.dt.float32
BF16 = mybir.dt.bfloat16
AF = mybir.ActivationFunctionType
ALU = mybir.AluOpType
AX = mybir.AxisListType

T = 2048
D = 2048
NT = 16
EPS = 1e-6
NEG = -1.0e30
O_MQ, O_MK, O_MV, O_MO, O_MG, O_AQ, O_AK, O_AV, O_GM, O_GA = (
    0, 2048, 4096, 6144, 8192, 8208, 10256, 10768, 11280, 13328)
NEB = 128


class Res:
    __slots__ = ("w", "r")

    def __init__(self):
        self.w = None
        self.r = []


class DSem:
    def __init__(self, sem):
        self.sem = sem
        self.val = 0


class _Rec:
    def __init__(self):
        self.call = None

    def __getattr__(self, name):
        def f(*a, **k):
            self.call = (name, a, k)
            return self
        return f


class Prog:
    ENGS = ("pe", "dve", "act", "pool", "sp")

    def __init__(self, nc, esems):
        self.nc = nc
        self.esem = esems
        self.cnt = {e: 0 for e in self.ENGS}
        self.ops = {e: [] for e in self.ENGS}
        self.seen = {e: {} for e in self.ENGS}
        self.nops = 0
        self.dsems = []
        self.fence = {e: [] for e in self.ENGS}

    def barrier(self):
        toks = [(self.esem[e], self.cnt[e], "x") for e in self.ENGS if self.cnt[e] > 0]
        toks += [(d.sem, d.val, "dma") for d in self.dsems if d.val > 0]
        for e in self.ENGS:
            self.fence[e] = list(toks)

    def op(self, eng, fn, reads=(), writes=(), dsem=None, inc=True):
        waits = {}

        def add(tok):
            if tok is None:
                return
            s, v, e = tok
            if e == "pe" and eng == "pe" and dsem is None:
                return
            k = id(s)
            if self.seen[eng].get(k, 0) >= v:
                return
            if k not in waits or waits[k][1] < v:
                waits[k] = (s, v)

        for R in reads:
            add(R.w)
        for R in writes:
            add(R.w)
            for t in R.r:
                add(t)
        if self.fence[eng]:
            for t in self.fence[eng]:
                add(t)
            self.fence[eng] = []
        for k, (s, v) in waits.items():
            self.seen[eng][k] = v
        if dsem is None:
            if inc:
                self.cnt[eng] += 1
                tok = (self.esem[eng], self.cnt[eng], eng)
                incs = (self.esem[eng], 1)
            else:
                tok = (self.esem[eng], self.cnt[eng] + 1, eng)
                incs = None
        else:
            dsem.val += 16
            tok = (dsem.sem, dsem.val, "dma")
            incs = (dsem.sem, 16)
        for R in reads:
            R.r.append(tok)
        for R in writes:
            R.w = tok
            R.r = []
        rec = _Rec()
        fn(rec)
        self.ops[eng].append((list(waits.values()), rec.call, incs))
        self.nops += 1
        return tok

    def finish(self, toks):
        waits = [(s, v) for (s, v, _) in toks if s is not None]
        self.ops["sp"].append((waits, ("nop", (), {}), (self.esem["sp"], 1)))

    def emit(self, block):
        engmap = {"pe": "tensor", "dve": "vector", "act": "scalar", "pool": "gpsimd", "sp": "sync"}
        for e in self.ENGS:
            ops = self.ops[e]

            def body(engine, ops=ops):
                for waits, fn, incs in ops:
                    for s, v in waits:
                        engine.wait_ge(s, v)
                    ins = getattr(engine, fn[0])(*fn[1], **fn[2])
                    if incs is not None:
                        ins.then_inc(incs[0], incs[1])

            getattr(block, engmap[e])(body)


def build_nc(debug=False, upto=None):
    nc = bass.Bass("TRN2", target_bir_lowering=False)

    LEVELS = ["A", "P1", "P", "G", "M", "T", "O", None]
    lvl = LEVELS.index(upto)
    in_names = []
    nc._in_names = in_names

    def din(name, shape, need=0):
        if lvl < need:
            return None
        in_names.append(name)
        return nc.dram_tensor(name, list(shape), F32, kind="ExternalInput").ap()

    dbgset = set(debug) if debug else set()

    def dscr(name, shape, dt):
        return nc.dram_tensor(name, list(shape), dt, kind="ExternalOutput" if name in dbgset else "Internal").ap()

    x_own = din("x_own", (T, D))
    x_halo = din("x_halo", (T, D))
    w_in = din("w_in", (D, 15376))
    w_gate = din("w_gate", (D, 16))
    gate_b = din("gate_b", (4, 4))
    n1g = din("n1g", (128, 16))
    n2g = din("n2g", (128, 16))
    mng = din("mng", (64, D), need=4)
    aqg = din("aqg", (128, 1))
    akg = din("akg", (128, 1))
    sinkb = din("sinkb", (128, 16), need=5)
    btab = din("btab", (128, 16 * 3 * 128), need=5)
    cst = din("cst", (128, 384 + 4096 + 256))
    w_mp = din("w_mp", (D, D), need=6)
    w_ap = din("w_ap", (D, D), need=6)
    w_o = din("w_o", (D, D), need=6)
    w_pq = din("w_pq", (D, D), need=6)
    keysT = din("keysT", (128, 16 * 128), need=6)
    uT = din("uT", (NEB, 128, 16 * 128), need=5)
    pv = din("pv", (NEB, 128, D), need=5)
    y = nc.dram_tensor("y", [T, D], F32, kind="ExternalOutput").ap()

    S_qT = dscr("S_qT", (D, T), BF16)
    S_kT = dscr("S_kT", (D, T), BF16)
    S_k = dscr("S_k", (T, D), BF16)
    S_v = dscr("S_v", (T, D), BF16)
    S_o = dscr("S_o", (T, D), BF16)
    S_kh = dscr("S_kh", (T, D), BF16)
    S_vh = dscr("S_vh", (T, D), BF16)
    S_aqT = dscr("S_aqT", (D, T), BF16)
    S_akT = dscr("S_akT", (512, T + 128), BF16)
    S_av = dscr("S_av", (T + 128, 512), BF16)
    S_gmT = dscr("S_gmT", (D, T), BF16)
    S_gaT = dscr("S_gaT", (D, T), BF16)
    S_g = dscr("S_g", (4, 4, 2 * T), F32)
    S_hf = dscr("S_hf", (T, D), F32)
    S_hm = dscr("S_hm", (T, D), BF16)
    S_haT = dscr("S_haT", (D, T), BF16)
    S_x1 = dscr("S_x1", (T, D), F32)
    S_sub = dscr("S_sub", (T, D), F32)
    S_UT = dscr("S_UT", (NEB, 128, D), BF16)
    S_V = dscr("S_V", (NEB, 128, D), BF16)

    with contextlib.ExitStack() as top:
        E = top.enter_context
        esems = {e: E(nc.semaphore("es_" + e)) for e in Prog.ENGS}
        P = Prog(nc, esems)
        block = E(nc.Block())
        nds = [0]

        def mkds(st=None):
            nds[0] += 1
            d_ = DSem(top.enter_context(nc.semaphore("ds%d" % nds[0])))
            P.dsems.append(d_)
            return d_

        def sb(st, name, shape, dt):
            return st.enter_context(nc.sbuf_tensor(name, list(shape), dt))

        cst_sb = sb(top, "cst_sb", (128, 128 + 128 + 64 + 64), F32)
        identb = sb(top, "identb", (128, 128), BF16)
        onesb = sb(top, "onesb", (128, 128), BF16)
        g1 = sb(top, "g1", (128, 16), F32)
        g2 = sb(top, "g2", (128, 16), F32)
        Rc = Res()
        dc = mkds()
        P.op("sp", lambda e: e.dma_start(out=cst_sb[:], in_=cst[:, 0:384]), writes=[Rc], dsem=dc)
        P.op("sp", lambda e: e.dma_start(out=g1[:], in_=n1g[:, :]), writes=[Rc], dsem=dc)
        P.op("sp", lambda e: e.dma_start(out=g2[:], in_=n2g[:, :]), writes=[Rc], dsem=dc)
        identf = cst_sb[:, 0:128]
        onesf = cst_sb[:, 128:256]
        maskf = cst_sb[0:64, 256:320]
        maskb = cst_sb[0:64, 320:384]
        Rcb = Res()
        P.op("dve", lambda e: e.tensor_copy(out=identb[:], in_=identf), reads=[Rc], writes=[Rcb])
        P.op("dve", lambda e: e.tensor_copy(out=onesb[:], in_=onesf), reads=[Rc], writes=[Rcb])
        CONST = [Rc, Rcb]

        ps = [E(nc.psum_tensor("ps%d" % i, [128, 512], F32)) for i in range(8)]
        Rps = [Res() for _ in range(8)]
        bank_ctr = [0]

        def nextbank(lo=0, hi=8):
            n = hi - lo
            b = lo + bank_ctr[0] % n
            bank_ctr[0] += 1
            return b

        def barrier_from(res_list):
            B = Res()
            toks = []
            for R in res_list:
                toks += R.r
                if R.w is not None:
                    toks.append(R.w)
            waits = [(s, v) for (s, v, _) in toks]
            P.cnt["sp"] += 1
            tok = (P.esem["sp"], P.cnt["sp"], "sp")
            P.ops["sp"].append((waits, ("nop", (), {}), (P.esem["sp"], 1)))
            B.w = tok
            return B

        def norm_T(st, get_tile, ntiles, xnT, RxnT, gt, tag):
            xt = [sb(st, "xt%s%d" % (tag, i), (128, D), F32) for i in range(2)]
            Rxt = [Res(), Res()]
            dxt = [mkds(st), mkds(st)]
            junk = sb(st, "junk" + tag, (128, D), BF16)
            xs = sb(st, "xs" + tag, (128, D), BF16)
            sm = sb(st, "sm" + tag, (128, 4), F32)
            Rj, Rxs, Rsm = Res(), Res(), Res()
            for t in range(ntiles):
                b = t % 2
                get_tile(t, xt[b], Rxt[b], dxt[b])
                P.op("act", lambda e, b=b: e.activation(out=junk[:], in_=xt[b][:], func=AF.Square, accum_out=sm[:, 0:1]),
                     reads=[Rxt[b]], writes=[Rj, Rsm])
                P.op("act", lambda e: e.activation(out=sm[:, 1:2], in_=sm[:, 0:1], func=AF.Ln, scale=1.0 / D, bias=EPS),
                     reads=[Rsm], writes=[Rsm])
                P.op("act", lambda e: e.activation(out=sm[:, 2:3], in_=sm[:, 1:2], func=AF.Exp, scale=-0.5),
                     reads=[Rsm], writes=[Rsm])
                P.op("dve", lambda e, b=b: e.tensor_scalar(out=xs[:], in0=xt[b][:], scalar1=sm[:, 2:3], scalar2=None, op0=ALU.mult),
                     reads=[Rxt[b], Rsm], writes=[Rxs])
                for g in range(4):
                    bk = nextbank()
                    ptv = ps[bk][:].bitcast(BF16)[:, 0:512].rearrange("p (j n) -> p j n", j=4)
                    for j in range(4):
                        c = g * 4 + j
                        P.op("pe", lambda e, c=c, j=j, ptv=ptv: e.transpose(out=ptv[:, j, :], in_=xs[:, c * 128:(c + 1) * 128], identity=identb[:]),
                             reads=[Rxs] + CONST, writes=[Rps[bk]], inc=(j == 3))
                    P.op("dve", lambda e, g=g, t=t, ptv=ptv: e.tensor_tensor(
                        out=xnT[:, g * 4:(g + 1) * 4, t * 128:(t + 1) * 128], in0=ptv,
                        in1=gt[:, g * 4:(g + 1) * 4].unsqueeze(2).broadcast_to([128, 4, 128]), op=ALU.mult),
                        reads=[Rps[bk]] + CONST, writes=[RxnT[t]])

        with contextlib.ExitStack() as st:
            xnT = sb(st, "xnT", (128, 16, T), BF16)
            RxnT = [Res() for _ in range(NT)]
            wr = [sb(st, "wr%d" % i, (128, 16, 512), BF16) for i in range(3)]
            Rwr = [Res() for _ in range(3)]
            dwr = [mkds(st) for _ in range(3)]
            wctr = [0]
            stg = [sb(st, "stg%d" % i, (128, 8192), BF16) for i in range(2)]
            Rstg = [Res(), Res()]
            dstg = [mkds(st), mkds(st)]
            sctr = [0]
            wg = sb(st, "wg", (128, 16, 16), BF16)
            gb = sb(st, "gb", (4, 4), F32)
            gqs = sb(st, "gqs", (128, 2), F32)
            gst = [sb(st, "gst%d" % i, (4, 512), F32) for i in range(2)]
            Rgst = [Res(), Res()]
            dgst = [mkds(st), mkds(st)]
            sqt = sb(st, "sqt", (128, 512), BF16)
            lnv = sb(st, "lnv", (128, 512), F32)
            Rsq, Rln = Res(), Res()
            Rw0 = Res()
            dw0 = mkds(st)
            dw0p = mkds(st)
            P.op("pool", lambda e: e.dma_start(out=wg[:], in_=w_gate.rearrange("(c p) n -> p c n", p=128)), writes=[Rw0], dsem=dw0p)
            P.op("sp", lambda e: e.dma_start(out=gb[:], in_=gate_b[:, :]), writes=[Rw0], dsem=dw0)
            P.op("sp", lambda e: e.dma_start(out=gqs[:, 0:1], in_=aqg[:, :]), writes=[Rw0], dsem=dw0)
            P.op("sp", lambda e: e.dma_start(out=gqs[:, 1:2], in_=akg[:, :]), writes=[Rw0], dsem=dw0)
            P.op("dve", lambda e: e.tensor_scalar(out=gqs[:, 0:1], in0=gqs[:, 0:1], scalar1=128.0 ** -0.5, scalar2=None, op0=ALU.mult),
                 reads=[Rw0], writes=[Rw0])

            def load_w(wsrc, c0, n):
                s = wctr[0] % 3
                wctr[0] += 1
                P.op("pool", lambda e: e.dma_start(out=wr[s][:, :, 0:n], in_=wsrc[:, c0:c0 + n].rearrange("(c p) n -> p c n", p=128)),
                     writes=[Rwr[s]], dsem=dwr[s])
                return s

            def new_stg():
                s = sctr[0] % 2
                sctr[0] += 1
                return s

            def fm_mm(s, j, tok0, ntok, bk):
                rd = [Rwr[s]] + RxnT[tok0 // 128:(tok0 + ntok + 127) // 128]
                for c in range(16):
                    P.op("pe", lambda e, c=c: e.matmul(ps[bk][:, 0:ntok], lhsT=wr[s][:, c, j * 128:(j + 1) * 128],
                                                         rhs=xnT[:, c, tok0:tok0 + ntok], start=(c == 0), stop=(c == 15)),
                         reads=rd, writes=[Rps[bk]], inc=(c == 15))

            def tm_mm(s, n, t, bk):
                rd = [Rwr[s], RxnT[t]]
                for c in range(16):
                    P.op("pe", lambda e, c=c: e.matmul(ps[bk][:, 0:n], lhsT=xnT[:, c, t * 128:(t + 1) * 128],
                                                         rhs=wr[s][:, c, 0:n], start=(c == 0), stop=(c == 15)),
                         reads=rd, writes=[Rps[bk]], inc=(c == 15))

            evctr = [0]

            def evac_copy(dst, bk, n, wres, func=None):
                if func is not None or evctr[0] % 2 == 0:
                    f = func if func is not None else AF.Copy
                    P.op("act", lambda e: e.activation(out=dst, in_=ps[bk][:, 0:n], func=f), reads=[Rps[bk]], writes=wres)
                else:
                    P.op("dve", lambda e: e.tensor_copy(out=dst, in_=ps[bk][:, 0:n]), reads=[Rps[bk]], writes=wres)
                evctr[0] += 1

            def evac_qknorm(dst, bk, n, gcol, wres):
                P.op("act", lambda e: e.activation(out=sqt[:, 0:n], in_=ps[bk][:, 0:n], func=AF.Square), reads=[Rps[bk]], writes=[Rsq])
                b2 = nextbank()
                P.op("pe", lambda e: e.matmul(ps[b2][:, 0:n], lhsT=onesb[:], rhs=sqt[:, 0:n], start=True, stop=True),
                     reads=[Rsq] + CONST, writes=[Rps[b2]])
                P.op("act", lambda e: e.activation(out=lnv[:, 0:n], in_=ps[b2][:, 0:n], func=AF.Ln, scale=1.0 / 128, bias=EPS),
                     reads=[Rps[b2]], writes=[Rln])
                P.op("act", lambda e: e.activation(out=lnv[:, 0:n], in_=lnv[:, 0:n], func=AF.Exp, scale=-0.5), reads=[Rln], writes=[Rln])
                P.op("dve", lambda e: e.scalar_tensor_tensor(out=dst, in0=ps[bk][:, 0:n], scalar=gqs[:, gcol:gcol + 1], in1=lnv[:, 0:n],
                                                              op0=ALU.mult, op1=ALU.mult), reads=[Rps[bk], Rln, Rw0], writes=wres)

            def fm_block(wsrc, c0, ncol, dstT, row0, kind, ntok=T, tokdst0=0):
                s = load_w(wsrc, c0, ncol)
                g = new_stg()
                nsub = ncol // 128
                sv = stg[g][:, 0:nsub * ntok].rearrange("p (j t) -> p j t", j=nsub)
                for j in range(nsub):
                    for tb in range(0, ntok, 512):
                        n = min(512, ntok - tb)
                        bk = nextbank()
                        fm_mm(s, j, tb, n, bk)
                        dst = sv[:, j, tb:tb + n]
                        if kind == "plain":
                            evac_copy(dst, bk, n, [Rstg[g]])
                        elif kind == "sig":
                            evac_copy(dst, bk, n, [Rstg[g]], func=AF.Sigmoid)
                        elif kind == "qn":
                            evac_qknorm(dst, bk, n, 0, [Rstg[g]])
                        elif kind == "kn":
                            evac_qknorm(dst, bk, n, 1, [Rstg[g]])
                P.op("sp", lambda e: e.dma_start(out=dstT[row0:row0 + ncol, tokdst0:tokdst0 + ntok].rearrange("(j p) t -> p j t", p=128), in_=sv),
                     reads=[Rstg[g]], dsem=dstg[g])

            def tm_block(wsrc, c0, ncol, dst, dcol0, kind, ntiles=NT, tokdst0=0):
                s = load_w(wsrc, c0, ncol)
                g = new_stg()
                sv = stg[g][:, 0:ntiles * ncol].rearrange("p (t n) -> p t n", t=ntiles)
                for t in range(ntiles):
                    bk = nextbank()
                    tm_mm(s, ncol, t, bk)
                    evac_copy(sv[:, t, :], bk, ncol, [Rstg[g]], func=(AF.Sigmoid if kind == "sig" else None))
                P.op("sp", lambda e: e.dma_start(out=dst[tokdst0:tokdst0 + ntiles * 128, dcol0:dcol0 + ncol].rearrange("(t p) n -> p t n", p=128), in_=sv),
                     reads=[Rstg[g]], dsem=dstg[g])

            def gate_block(ggs, tokdst0):
                for tb in range(4):
                    for gg in ggs:
                        bk = nextbank()
                        for c in range(16):
                            P.op("pe", lambda e, c=c: e.matmul(ps[bk][0:4, :], lhsT=wg[:, c, gg * 4:(gg + 1) * 4],
                                                                 rhs=xnT[:, c, tb * 512:(tb + 1) * 512], start=(c == 0), stop=(c == 15)),
                                 reads=[Rw0] + RxnT[tb * 4:tb * 4 + 4], writes=[Rps[bk]], inc=(c == 15))
                        q = (tb * 4 + gg) % 2
                        P.op("act", lambda e: e.activation(out=gst[q][:], in_=ps[bk][0:4, :], func=AF.Identity, bias=gb[:, gg:gg + 1]),
                             reads=[Rps[bk], Rw0], writes=[Rgst[q]])
                        P.op("sp", lambda e: e.dma_start(out=S_g[gg, :, tokdst0 + tb * 512:tokdst0 + (tb + 1) * 512], in_=gst[q][:]),
                             reads=[Rgst[q]], dsem=dgst[q])

            def x_tile_loader(xsrc):
                def get(t, dst, Rd, dsm):
                    P.op("sp", lambda e: e.dma_start(out=dst[:], in_=xsrc[t * 128:(t + 1) * 128, :]), writes=[Rd], dsem=dsm)
                return get

            with contextlib.ExitStack() as st2:
                norm_T(st2, x_tile_loader(x_halo), NT, xnT, RxnT, g1, "h")
                if upto == "A":
                    D_xnT = nc.dram_tensor("D_xnT", [128, 16 * T], BF16, kind="ExternalOutput").ap()
                    dsx = mkds()
                    tk_ = P.op("sp", lambda e: e.dma_start(out=D_xnT[:, :], in_=xnT[:].rearrange("p c t -> p (c t)")), reads=RxnT, dsem=dsx)
                    P.finish([tk_])
                    P.emit(block)
                    return nc
                gate_block([2, 3], T)
                for hb in range(4):
                    tm_block(w_in, O_MK + hb * 512, 512, S_kh, hb * 512, "plain")
                    tm_block(w_in, O_MV + hb * 512, 512, S_vh, hb * 512, "plain")
                fm_block(w_in, O_AK, 512, S_akT, 0, "kn", ntok=128, tokdst0=T)
                tm_block(w_in, O_AV, 512, S_av, 0, "plain", ntiles=1, tokdst0=T)
                if upto == "P1":
                    BP = barrier_from([Rstg[0], Rstg[1], Rgst[0], Rgst[1]])
                    P.finish([BP.w])
                    P.emit(block)
                    return nc
                norm_T(st2, x_tile_loader(x_own), NT, xnT, RxnT, g1, "o")
            P.barrier()
            gate_block([0, 1, 2, 3], 0)
            for hb in range(4):
                fm_block(w_in, O_MQ + hb * 512, 512, S_qT, hb * 512, "plain")
                fm_block(w_in, O_MK + hb * 512, 512, S_kT, hb * 512, "plain")
                tm_block(w_in, O_MK + hb * 512, 512, S_k, hb * 512, "plain")
                tm_block(w_in, O_MV + hb * 512, 512, S_v, hb * 512, "plain")
                tm_block(w_in, O_MO + hb * 512, 512, S_o, hb * 512, "sig")
                fm_block(w_in, O_AQ + hb * 512, 512, S_aqT, hb * 512, "qn")
                fm_block(w_in, O_GM + hb * 512, 512, S_gmT, hb * 512, "sig")
                fm_block(w_in, O_GA + hb * 512, 512, S_gaT, hb * 512, "sig")
            fm_block(w_in, O_AK, 512, S_akT, 0, "kn")
            tm_block(w_in, O_AV, 512, S_av, 0, "plain")
            PH_P_DONE = [Rstg[0], Rstg[1], Rgst[0], Rgst[1]]
        P.barrier()

        BP = barrier_from(PH_P_DONE)
        out_toks = [BP.w]
        UPTO = upto
        if upto != "P":
            build_rest(nc, P, top, sb, mkds, ps, Rps, nextbank, CONST, BP, locals(), out_toks)
        P.finish(out_toks)
        P.emit(block)
    return nc


def build_rest(nc, P, top, sb, mkds, ps, Rps, nextbank, CONST, BP, L, out_toks):
    identb, onesb, cst_sb = L["identb"], L["onesb"], L["cst_sb"]
    identf, onesf, maskf, maskb = L["identf"], L["onesf"], L["maskf"], L["maskb"]
    cst, g2 = L["cst"], L["g2"]
    S_g, S_qT, S_kT, S_k, S_v, S_o, S_kh, S_vh = (L[k] for k in "S_g S_qT S_kT S_k S_v S_o S_kh S_vh".split())
    S_hf, S_hm, mng = L["S_hf"], L["S_hm"], L["mng"]
    S_UT, S_V, uT, pv = L["S_UT"], L["S_V"], L["uT"], L["pv"]
    L["BU_holder"] = [None]
    barrier_from = L["barrier_from"]
    norm_T = L["norm_T"]

    TSf = sb(top, "TSf", (64, 2, 32, 4), F32)
    TSb = sb(top, "TSb", (64, 96, 4), F32)
    DBf = sb(top, "DBf", (128, 4, 32), F32)
    DBb = sb(top, "DBb", (128, 4, 64), F32)
    RTS = Res()
    with contextlib.ExitStack() as st:
        B = [sb(st, "gB%d" % i, (4, 4096), F32) for i in range(5)]
        RB = [Res() for _ in range(5)]
        dB = [mkds(st) for _ in range(2)]
        rmask = sb(st, "rmask", (4, 4096), F32)
        Rrm = Res()
        drm = mkds(st)
        P.op("sp", lambda e: e.dma_start(out=rmask[:], in_=cst[0:4, 384:384 + 4096]), writes=[Rrm], dsem=drm)
        amax = sb(st, "amax", (4, 64), F32)
        totc = sb(st, "totc", (4, 64), F32)
        Mtab = sb(st, "Mtab", (4, 65), F32)
        MC = sb(st, "MC", (4, 64), F32)
        dd = sb(st, "dd", (4, 64), F32)
        dexp = sb(st, "dexp", (4, 4, 64), F32)
        Rsm = Res()
        LN_S = math.log(512.0 ** -0.5)
        for d in range(2):
            Ltok = T if d == 0 else 2 * T
            nch = Ltok // 64
            li, zf, cum, cx, a = (B[i][:, 0:Ltok] for i in range(5))
            v3 = lambda ap: ap.rearrange("p (c l) -> p c l", l=64)
            P.op("sp", lambda e: e.dma_start(out=li, in_=S_g[d * 2, :, 0:Ltok]), reads=[BP], writes=[RB[0]], dsem=dB[0])
            P.op("sp", lambda e: e.dma_start(out=zf, in_=S_g[d * 2 + 1, :, 0:Ltok]), reads=[BP], writes=[RB[1]], dsem=dB[1])
            P.op("act", lambda e: e.activation(out=zf, in_=zf, func=AF.Exp, scale=-1.0), reads=[RB[1]], writes=[RB[1]])
            P.op("act", lambda e: e.activation(out=zf, in_=zf, func=AF.Ln, bias=1.0), reads=[RB[1]], writes=[RB[1]])
            P.op("dve", lambda e: e.tensor_tensor_scan(out=cum, data0=rmask[:, 0:Ltok], data1=zf, initial=0.0, op0=ALU.mult, op1=ALU.add),
                 reads=[RB[1], Rrm], writes=[RB[2]])
            tot3 = v3(cum)[:, :, 63:64]
            if d == 1:
                P.op("dve", lambda e: e.tensor_tensor(out=cx, in0=zf, in1=cum, op=ALU.subtract), reads=[RB[1], RB[2]], writes=[RB[3]])
                P.op("dve", lambda e: e.tensor_tensor(out=v3(cx), in0=v3(cx), in1=tot3.broadcast_to([4, nch, 64]), op=ALU.add),
                     reads=[RB[3], RB[2]], writes=[RB[3]])
                cxx, Rcx = cx, RB[3]
            else:
                cxx, Rcx = cum, RB[2]
            P.op("dve", lambda e: e.tensor_tensor(out=a, in0=li, in1=cxx, op=ALU.add), reads=[RB[0], Rcx], writes=[RB[4]])
            P.op("dve", lambda e: e.tensor_reduce(out=amax[:, 0:nch], in_=v3(a), axis=AX.X, op=ALU.max), reads=[RB[4]], writes=[Rsm])
            P.op("dve", lambda e: e.tensor_copy(out=totc[:, 0:nch], in_=tot3.rearrange("p c l -> p (c l)")), reads=[RB[2]], writes=[Rsm])
            if d == 0:
                P.op("dve", lambda e: e.memset(Mtab[:, 0:1], 0.0), writes=[Rsm])
                order = [(c, c, c + 1) for c in range(nch)]
            else:
                P.op("dve", lambda e: e.memset(Mtab[:, nch:nch + 1], 0.0), writes=[Rsm])
                order = [(c, c + 1, c) for c in range(nch - 1, -1, -1)]
            for (c, ip, inx) in order:
                P.op("dve", lambda e, c=c, ip=ip: e.tensor_tensor(out=MC[:, c:c + 1], in0=Mtab[:, ip:ip + 1], in1=amax[:, c:c + 1], op=ALU.max),
                     reads=[Rsm], writes=[Rsm])
                P.op("dve", lambda e, c=c, inx=inx: e.tensor_tensor(out=Mtab[:, inx:inx + 1], in0=MC[:, c:c + 1], in1=totc[:, c:c + 1], op=ALU.subtract),
                     reads=[Rsm], writes=[Rsm])
            mprev = Mtab[:, 0:nch] if d == 0 else Mtab[:, 1:nch + 1]
            P.op("dve", lambda e: e.tensor_tensor(out=dd[:, 0:nch], in0=mprev, in1=MC[:, 0:nch], op=ALU.subtract), reads=[Rsm], writes=[Rsm])
            P.op("act", lambda e: e.activation(out=dd[:, 0:nch], in_=dd[:, 0:nch], func=AF.Exp), reads=[Rsm], writes=[Rsm])
            mcb = MC[:, 0:nch].unsqueeze(2).broadcast_to([4, nch, 64])
            P.op("dve", lambda e: e.tensor_tensor(out=v3(zf), in0=v3(a), in1=mcb, op=ALU.subtract), reads=[RB[4], Rsm, RB[1]], writes=[RB[1]])
            P.op("act", lambda e: e.activation(out=zf, in_=zf, func=AF.Exp, bias=LN_S), reads=[RB[1]], writes=[RB[1]])
            P.op("dve", lambda e: e.tensor_tensor(out=v3(li), in0=v3(cxx), in1=mcb, op=ALU.subtract), reads=[Rcx, Rsm, RB[0]], writes=[RB[0]])
            P.op("act", lambda e: e.activation(out=li, in_=li, func=AF.Exp), reads=[RB[0]], writes=[RB[0]])
            bk = nextbank()
            ncols = 0
            for kind, src, Rsrc, nck in ((0, zf, RB[1], nch), (1, li, RB[0], 32)):
                for c in range(nck):
                    col = (kind * nch + c) * 4 if d == 1 else (kind * 32 + c) * 4
                    P.op("pe", lambda e, c=c, col=col, src=src: e.transpose(out=ps[bk][0:64, col:col + 4], in_=src[:, c * 64:(c + 1) * 64], identity=identf[0:4, 0:4]),
                         reads=[Rsrc] + CONST, writes=[Rps[bk]], inc=(c == nck - 1))
                    ncols = max(ncols, col + 4)
            dstTS = TSf[:].rearrange("p k c h -> p (k c h)") if d == 0 else TSb[:].rearrange("p c h -> p (c h)")
            P.op("dve", lambda e, dstTS=dstTS, ncols=ncols: e.tensor_copy(out=dstTS[:, 0:ncols], in_=ps[bk][0:64, 0:ncols]), reads=[Rps[bk]], writes=[RTS])
            P.op("dve", lambda e: e.tensor_tensor(out=dexp[:, :, 0:nch], in0=dd[:, 0:nch].unsqueeze(1).broadcast_to([4, 4, nch]),
                                                   in1=identf[0:4, 0:4].unsqueeze(2).broadcast_to([4, 4, nch]), op=ALU.mult),
                 reads=[Rsm] + CONST, writes=[Rsm])
            bk2 = nextbank()
            P.op("pe", lambda e: e.matmul(ps[bk2][:, 0:4 * nch].rearrange("p (h c) -> p h c", h=4), lhsT=onesf[0:4, :], rhs=dexp[:, :, 0:nch], start=True, stop=True),
                 reads=[Rsm] + CONST, writes=[Rps[bk2]])
            DBd = DBf if d == 0 else DBb
            P.op("dve", lambda e, DBd=DBd: e.tensor_copy(out=DBd[:], in_=ps[bk2][:, 0:4 * nch].rearrange("p (h c) -> p h c", h=4)),
                 reads=[Rps[bk2]], writes=[RTS])

    P.barrier()
    out_toks.append(RTS.w)
    if L["UPTO"] == "G":
        for nm, tl in (("D_TSf", TSf), ("D_TSb", TSb), ("D_DBf", DBf), ("D_DBb", DBb)):
            shp = list(tl[:].shape)
            flat = int(np.prod(shp[1:]))
            dd_ = nc.dram_tensor(nm, [shp[0], flat], F32, kind="ExternalOutput").ap()
            dsx = mkds()
            pat = {4: "p a b c -> p (a b c)", 3: "p a b -> p (a b)"}[len(shp)]
            out_toks.append(P.op("sp", lambda e: e.dma_start(out=dd_[:, :], in_=tl[:].rearrange(pat)), reads=[RTS], dsem=dsx))
        return
    with contextlib.ExitStack() as st:
        Cst = sb(st, "Cst", (128, 4, 512), F32)
        nst = sb(st, "nst", (128, 4), F32)
        Cb = sb(st, "Cb", (128, 4, 512), BF16)
        nb = sb(st, "nb", (128, 4), BF16)
        Cb_all = sb(st, "Cb_all", (128, 4, 4, 512), F32)
        nb_all = sb(st, "nb_all", (128, 4, 4), F32)
        RC, RCb, RCall = Res(), Res(), Res()
        qT = sb(st, "qT", (128, 4, T), BF16)
        kT = sb(st, "kT", (128, 4, T), BF16)
        kk = sb(st, "kk", (64, 32, 512), BF16)
        vv = sb(st, "vv", (64, 32, 512), BF16)
        Rq, Rk = Res(), Res()
        dq = [mkds(st) for _ in range(4)]
        kw = [sb(st, "kw%d" % i, (64, 512), BF16) for i in range(2)]
        Rkw = [Res(), Res()]
        St = [sb(st, "St%d" % i, (64, 64), BF16) for i in range(2)]
        RSt = [Res(), Res()]
        hst = [sb(st, "hst%d" % i, (64, 512), F32) for i in range(2)]
        Rhst = [Res(), Res()]
        dhst = [mkds(st), mkds(st)]
        hfl = [sb(st, "hfl%d" % i, (64, 512), F32) for i in range(2)]
        Rhfl = [Res(), Res()]
        dhfl = [mkds(st), mkds(st)]
        osl = [sb(st, "osl%d" % i, (64, 512), BF16) for i in range(2)]
        Rosl = [Res(), Res()]
        dosl = [mkds(st), mkds(st)]
        hmo = [sb(st, "hmo%d" % i, (64, 512), BF16) for i in range(2)]
        Rhmo = [Res(), Res()]
        dhmo = [mkds(st), mkds(st)]
        sm = sb(st, "msm", (64, 8), F32)
        Rms = Res()
        hjunk = sb(st, "hjunk", (64, 512), BF16)
        Rhj = Res()
        hsum = sb(st, "hsum", (64, 512), F32)
        Rhs = Res()
        mg = sb(st, "mgb", (64, D), F32)
        Rmg = Res()
        dmg = mkds(st)
        P.op("sp", lambda e: e.dma_start(out=mg[:], in_=mng[:, :]), writes=[Rmg], dsem=dmg)
        RShf = Res()
        RShm = Res()
        cc = [0]
        pcb = [sb(st, "pcb%d" % i, (128, D), BF16) for i in range(4)]
        Rpcb = [Res() for _ in range(4)]
        dpcb = [mkds(st) for _ in range(4)]
        dpcbo = [mkds(st) for _ in range(4)]
        RSU = Res()
        pc_state = [0]

        def precast_some(n):
            for _ in range(n):
                k = pc_state[0]
                if k >= 2 * NEB:
                    return
                pc_state[0] += 1
                eb, which = k // 2, k % 2
                src, dst = ((uT, S_UT), (pv, S_V))[which]
                i = k % 4
                for hh in range(2):
                    P.op("pool", lambda e: e.dma_start(out=pcb[i][:, hh * 1024:(hh + 1) * 1024], in_=src[eb, :, hh * 1024:(hh + 1) * 1024]),
                         writes=[Rpcb[i]], dsem=dpcb[i])
                P.op("sp", lambda e: e.dma_start(out=dst[eb, :, :], in_=pcb[i][:]), reads=[Rpcb[i]], writes=[RSU], dsem=dpcbo[i])

        def state_update(c, kchunk, vchunk, wk, decay, rd):
            i = cc[0] % 2
            cc[0] += 1
            P.op("act", lambda e: e.activation(out=kw[i][:], in_=kchunk, func=AF.Copy, scale=wk),
                 reads=rd + [RTS], writes=[Rkw[i]])
            bn = nextbank(7, 8)
            for j in range(4):
                P.op("pe", lambda e, j=j: e.matmul(ps[bn][:, j:j + 1], lhsT=kw[i][:, j * 128:(j + 1) * 128], rhs=onesb[0:64, 0:1], start=True, stop=True),
                     reads=[Rkw[i]] + CONST, writes=[Rps[bn]], inc=(j == 3))
            P.op("dve", lambda e: e.scalar_tensor_tensor(out=nst[:], in0=nst[:], scalar=decay, in1=ps[bn][:, 0:4], op0=ALU.mult, op1=ALU.add),
                 reads=[Rps[bn], RTS, RC], writes=[RC])
            for j in range(4):
                bu = nextbank(4, 7)
                P.op("pe", lambda e, j=j, bu=bu: e.matmul(ps[bu][:], lhsT=kw[i][:, j * 128:(j + 1) * 128], rhs=vchunk, start=True, stop=True),
                     reads=[Rkw[i]] + rd, writes=[Rps[bu]])
                P.op("dve", lambda e, j=j, bu=bu: e.scalar_tensor_tensor(out=Cst[:, j, :], in0=Cst[:, j, :], scalar=decay, in1=ps[bu][:], op0=ALU.mult, op1=ALU.add),
                     reads=[Rps[bu], RTS, RC], writes=[RC])

        for h in range(4):
            P.op("sp", lambda e: e.dma_start(out=kk[:], in_=S_kh[:, h * 512:(h + 1) * 512].rearrange("(c p) d -> p c d", p=64)),
                 reads=[BP], writes=[Rk], dsem=dq[2])
            P.op("sp", lambda e: e.dma_start(out=vv[:], in_=S_vh[:, h * 512:(h + 1) * 512].rearrange("(c p) d -> p c d", p=64)),
                 reads=[BP], writes=[Rk], dsem=dq[3])
            P.op("dve", lambda e: e.memset(Cst[:], 0.0), writes=[RC])
            P.op("dve", lambda e: e.memset(nst[:], 0.0), writes=[RC])
            for c in range(63, 31, -1):
                hc = c - 32
                state_update(c, kk[:, hc, :], vv[:, hc, :], TSb[:, c, h:h + 1], DBb[:, h, c:c + 1], [Rk])
                precast_some(1)
            P.op("act", lambda e: e.activation(out=Cb_all[:, h], in_=Cst[:], func=AF.Copy), reads=[RC], writes=[RCall])
            P.op("act", lambda e: e.activation(out=nb_all[:, h], in_=nst[:], func=AF.Copy), reads=[RC], writes=[RCall])
            P.op("sp", lambda e: e.dma_start(out=qT[:], in_=S_qT[h * 512:(h + 1) * 512, :].rearrange("(j p) t -> p j t", p=128)),
                 reads=[BP], writes=[Rq], dsem=dq[0])
            P.op("sp", lambda e: e.dma_start(out=kT[:], in_=S_kT[h * 512:(h + 1) * 512, :].rearrange("(j p) t -> p j t", p=128)),
                 reads=[BP], writes=[Rq], dsem=dq[1])
            P.op("sp", lambda e: e.dma_start(out=kk[:], in_=S_k[:, h * 512:(h + 1) * 512].rearrange("(c p) d -> p c d", p=64)),
                 reads=[BP], writes=[Rk], dsem=dq[2])
            P.op("sp", lambda e: e.dma_start(out=vv[:], in_=S_v[:, h * 512:(h + 1) * 512].rearrange("(c p) d -> p c d", p=64)),
                 reads=[BP], writes=[Rk], dsem=dq[3])
            for d in range(2):
                if d == 0:
                    P.op("dve", lambda e: e.memset(Cst[:], 0.0), writes=[RC])
                    P.op("dve", lambda e: e.memset(nst[:], 0.0), writes=[RC])
                    chunks = range(32)
                else:
                    P.op("act", lambda e: e.activation(out=Cst[:], in_=Cb_all[:, h], func=AF.Copy), reads=[RCall], writes=[RC])
                    P.op("act", lambda e: e.activation(out=nst[:], in_=nb_all[:, h], func=AF.Copy), reads=[RCall], writes=[RC])
                    chunks = range(31, -1, -1)
                for c in chunks:
                    tk = slice(c * 64, (c + 1) * 64)
                    if d == 0:
                        wk, e2, decay, mk = TSf[:, 0, c, h:h + 1], TSf[:, 1, c, h:h + 1], DBf[:, h, c:c + 1], maskf
                    else:
                        wk, e2, decay, mk = TSb[:, c, h:h + 1], TSb[:, 64 + c, h:h + 1], DBb[:, h, c:c + 1], maskb
                    i = cc[0] % 2
                    P.op("act", lambda e, decay=decay: e.activation(out=Cb[:], in_=Cst[:], func=AF.Copy, scale=decay), reads=[RC, RTS], writes=[RCb])
                    P.op("act", lambda e, decay=decay: e.activation(out=nb[:], in_=nst[:], func=AF.Copy, scale=decay), reads=[RC, RTS], writes=[RCb])
                    for j in range(4):
                        P.op("pe", lambda e, j=j, tk=tk: e.matmul(ps[0][0:64, 0:64], lhsT=kT[:, j, tk], rhs=qT[:, j, tk], start=(j == 0), stop=(j == 3)),
                             reads=[Rq], writes=[Rps[0]], inc=(j == 3))
                    P.op("dve", lambda e, wk=wk, mk=mk, i=i: e.scalar_tensor_tensor(out=St[i][:], in0=ps[0][0:64, 0:64], scalar=wk, in1=mk, op0=ALU.mult, op1=ALU.mult),
                         reads=[Rps[0], RTS] + CONST, writes=[RSt[i]])
                    for j in range(4):
                        P.op("pe", lambda e, j=j, tk=tk: e.matmul(ps[1][0:64, :], lhsT=qT[:, j, tk], rhs=Cb[:, j, :], start=(j == 0), stop=False),
                             reads=[Rq, RCb], writes=[Rps[1]], inc=False)
                    P.op("pe", lambda e, i=i, c=c: e.matmul(ps[1][0:64, :], lhsT=St[i][:], rhs=vv[:, c, :], start=False, stop=True),
                         reads=[RSt[i], Rk], writes=[Rps[1]])
                    for j in range(4):
                        P.op("pe", lambda e, j=j, tk=tk: e.matmul(ps[2][0:64, 0:1], lhsT=qT[:, j, tk], rhs=nb[:, j:j + 1], start=(j == 0), stop=False),
                             reads=[Rq, RCb], writes=[Rps[2]], inc=False)
                    P.op("pe", lambda e, i=i: e.matmul(ps[2][0:64, 0:1], lhsT=St[i][:], rhs=onesb[0:64, 0:1], start=False, stop=True),
                         reads=[RSt[i]] + CONST, writes=[Rps[2]])
                    P.op("act", lambda e: e.activation(out=sm[:, 5:6], in_=ps[2][0:64, 0:1], func=AF.Abs), reads=[Rps[2]], writes=[Rms])
                    P.op("dve", lambda e, e2=e2: e.tensor_scalar(out=sm[:, 0:1], in0=sm[:, 5:6], scalar1=e2, scalar2=None, op0=ALU.max),
                         reads=[Rms, RTS], writes=[Rms])
                    P.op("dve", lambda e: e.reciprocal(out=sm[:, 1:2], in_=sm[:, 0:1]), reads=[Rms], writes=[Rms])
                    tok_rows = slice(c * 64, (c + 1) * 64)
                    if d == 0:
                        q = c % 2
                        P.op("dve", lambda e, q=q: e.tensor_scalar(out=hst[q][:], in0=ps[1][0:64, :], scalar1=sm[:, 1:2], scalar2=None, op0=ALU.mult),
                             reads=[Rps[1], Rms], writes=[Rhst[q]])
                        P.op("sp", lambda e, q=q, tok_rows=tok_rows: e.dma_start(out=S_hf[tok_rows, h * 512:(h + 1) * 512], in_=hst[q][:]),
                             reads=[Rhst[q]], writes=[RShf], dsem=dhst[q])
                    else:
                        q = c % 2
                        P.op("sp", lambda e, q=q, tok_rows=tok_rows: e.dma_start(out=hfl[q][:], in_=S_hf[tok_rows, h * 512:(h + 1) * 512]),
                             reads=[RShf], writes=[Rhfl[q]], dsem=dhfl[q])
                        P.op("sp", lambda e, q=q, tok_rows=tok_rows: e.dma_start(out=osl[q][:], in_=S_o[tok_rows, h * 512:(h + 1) * 512]),
                             reads=[BP], writes=[Rosl[q]], dsem=dosl[q])
                        P.op("dve", lambda e, q=q: e.scalar_tensor_tensor(out=hsum[:], in0=ps[1][0:64, :], scalar=sm[:, 1:2], in1=hfl[q][:], op0=ALU.mult, op1=ALU.add),
                             reads=[Rps[1], Rms, Rhfl[q]], writes=[Rhs])
                        P.op("act", lambda e: e.activation(out=hjunk[:], in_=hsum[:], func=AF.Square, accum_out=sm[:, 2:3]), reads=[Rhs], writes=[Rhj, Rms])
                        P.op("act", lambda e: e.activation(out=sm[:, 3:4], in_=sm[:, 2:3], func=AF.Ln, scale=1.0 / 512, bias=EPS), reads=[Rms], writes=[Rms])
                        P.op("act", lambda e: e.activation(out=sm[:, 4:5], in_=sm[:, 3:4], func=AF.Exp, scale=-0.5), reads=[Rms], writes=[Rms])
                        P.op("dve", lambda e: e.scalar_tensor_tensor(out=hsum[:], in0=hsum[:], scalar=sm[:, 4:5], in1=mg[:, h * 512:(h + 1) * 512], op0=ALU.mult, op1=ALU.mult),
                             reads=[Rhs, Rms, Rmg], writes=[Rhs])
                        P.op("pool", lambda e, q=q: e.tensor_tensor(out=hmo[q][:], in0=hsum[:], in1=osl[q][:], op=ALU.mult),
                             reads=[Rhs, Rosl[q]], writes=[Rhmo[q]])
                        P.op("sp", lambda e, q=q, tok_rows=tok_rows: e.dma_start(out=S_hm[tok_rows, h * 512:(h + 1) * 512], in_=hmo[q][:]),
                             reads=[Rhmo[q]], writes=[RShm], dsem=dhmo[q])
                    state_update(c, kk[:, c, :], vv[:, c, :], wk, decay, [Rk])
                    precast_some(1)
        precast_some(2 * NEB)
        L["BU_holder"][0] = barrier_from([RSU] + Rpcb)
        BM = barrier_from([RShm, Rhmo[0], Rhmo[1]])
    P.barrier()
    out_toks.append(BM.w)
    if L["UPTO"] == "M":
        return
    build_tail(nc, P, top, sb, mkds, ps, Rps, nextbank, CONST, BP, BM, L, out_toks)


def build_tail(nc, P, top, sb, mkds, ps, Rps, nextbank, CONST, BP, BM, L, out_toks):
    identb, onesb = L["identb"], L["onesb"]
    g2 = L["g2"]
    S_aqT, S_akT, S_av, S_gmT, S_gaT, S_hm, S_haT, S_x1 = (L[k] for k in "S_aqT S_akT S_av S_gmT S_gaT S_hm S_haT S_x1".split())
    S_UT, S_V, uT, pv, keysT, S_sub = L["S_UT"], L["S_V"], L["uT"], L["pv"], L["keysT"], L["S_sub"]
    sinkb, btab, x_own, y = L["sinkb"], L["btab"], L["x_own"], L["y"]
    w_mp, w_ap, w_o, w_pq = L["w_mp"], L["w_ap"], L["w_o"], L["w_pq"]
    barrier_from, norm_T = L["barrier_from"], L["norm_T"]

    BU = L["BU_holder"][0]
    with contextlib.ExitStack() as st:
        EB = sb(st, "EB", (128, 16, 3, 128), F32)
        esk = sb(st, "esk", (128, 16), F32)
        REB = Res()
        dEB = mkds(st)
        P.op("sp", lambda e: e.dma_start(out=EB[:].rearrange("p h o q -> p (h o q)"), in_=btab[:, :]), writes=[REB], dsem=dEB)
        P.op("sp", lambda e: e.dma_start(out=esk[:], in_=sinkb[:, :]), writes=[REB], dsem=dEB)
        P.op("act", lambda e: e.activation(out=EB[:].rearrange("p h o q -> p (h o q)"), in_=EB[:].rearrange("p h o q -> p (h o q)"), func=AF.Exp),
             reads=[REB], writes=[REB])
        P.op("act", lambda e: e.activation(out=esk[:], in_=esk[:], func=AF.Exp), reads=[REB], writes=[REB])
        m128 = sb(st, "m128", (128, 2, 128), F32)
        dm = mkds(st)
        P.op("sp", lambda e: e.dma_start(out=m128[:].rearrange("p a q -> p (a q)"), in_=L["cst"][:, 4480:4736]), writes=[REB], dsem=dm)
        for o, a in ((0, 0), (2, 1)):
            P.op("dve", lambda e, o=o, a=a: e.tensor_tensor(out=EB[:, :, o, :], in0=EB[:, :, o, :], in1=m128[:, a, :].unsqueeze(1).broadcast_to([128, 16, 128]), op=ALU.mult),
                 reads=[REB], writes=[REB])
        aqT = sb(st, "aqT", (128, 16, T), BF16)
        akT = sb(st, "akT", (128, 4, T + 128), BF16)
        av = sb(st, "av", (128, 17, 512), BF16)
        Ra = Res()
        da = [mkds(st) for _ in range(3)]
        P.op("sp", lambda e: e.dma_start(out=aqT[:], in_=S_aqT.rearrange("(h p) t -> p h t", p=128)), reads=[BP], writes=[Ra], dsem=da[0])
        P.op("sp", lambda e: e.dma_start(out=akT[:], in_=S_akT.rearrange("(h p) t -> p h t", p=128)), reads=[BP], writes=[Ra], dsem=da[1])
        P.op("sp", lambda e: e.dma_start(out=av[:], in_=S_av.rearrange("(t p) n -> p t n", p=128)), reads=[BP], writes=[Ra], dsem=da[2])
        pex = [sb(st, "pex%d" % i, (128, 512), F32) for i in range(3)]
        Rpex = [Res() for _ in range(3)]
        pT = [sb(st, "pT%d" % i, (128, 512), BF16) for i in range(6)]
        RpT = [Res() for _ in range(6)]
        zt = sb(st, "zt", (128, 512), F32)
        Rz = Res()
        hst = [sb(st, "hag%d" % i, (128, 4, T), BF16) for i in range(2)]
        Rhst = [Res(), Res()]
        dhst = [mkds(st), mkds(st)]
        RSha = Res()
        kk = 0
        for g in range(4):
            for i in range(NT):
                os_ = [o for o in (-1, 0, 1) if i + o >= 0]
                pts = []
                for o in os_:
                    bk = nextbank(0, 4)
                    P.op("pe", lambda e, bk=bk, o=o, i=i: e.matmul(ps[bk][:].rearrange("p (h q) -> p h q", h=4), lhsT=akT[:, g, (i + o) * 128:(i + o + 1) * 128],
                                                                    rhs=aqT[:, 4 * g:4 * g + 4, i * 128:(i + 1) * 128], start=True, stop=True),
                         reads=[Ra], writes=[Rps[bk]])
                    a = kk % 3
                    b = kk % 6
                    kk += 1
                    P.op("act", lambda e, bk=bk, a=a: e.activation(out=pex[a][:], in_=ps[bk][:], func=AF.Exp), reads=[Rps[bk]], writes=[Rpex[a]])
                    P.op("dve", lambda e, a=a, b=b, o=o: e.tensor_tensor(out=pT[b][:].rearrange("p (h q) -> p h q", h=4), in0=pex[a][:].rearrange("p (h q) -> p h q", h=4),
                                                                          in1=EB[:, 4 * g:4 * g + 4, o + 1, :], op=ALU.mult),
                         reads=[Rpex[a], REB], writes=[RpT[b]])
                    pts.append((o, b))
                bo = nextbank(4, 6)
                bz = nextbank(6, 8)
                for n_, (o, b) in enumerate(pts):
                    P.op("pe", lambda e, o=o, b=b, n_=n_, i=i, bo=bo: e.matmul(ps[bo][:], lhsT=av[:, i + o, g * 128:(g + 1) * 128], rhs=pT[b][:], start=(n_ == 0), stop=(n_ == len(pts) - 1)),
                         reads=[Ra, RpT[b]], writes=[Rps[bo]], inc=(n_ == len(pts) - 1))
                for n_, (o, b) in enumerate(pts):
                    P.op("pe", lambda e, b=b, n_=n_, bz=bz: e.matmul(ps[bz][:], lhsT=onesb[:], rhs=pT[b][:], start=(n_ == 0), stop=(n_ == len(pts) - 1)),
                         reads=[RpT[b]] + CONST, writes=[Rps[bz]], inc=(n_ == len(pts) - 1))
                P.op("dve", lambda e, bz=bz: e.tensor_tensor(out=zt[:].rearrange("p (h q) -> p h q", h=4), in0=ps[bz][:].rearrange("p (h q) -> p h q", h=4),
                                                              in1=esk[:, 4 * g:4 * g + 4].unsqueeze(2).broadcast_to([128, 4, 128]), op=ALU.add),
                     reads=[Rps[bz], REB], writes=[Rz])
                P.op("dve", lambda e: e.reciprocal(out=zt[:], in_=zt[:]), reads=[Rz], writes=[Rz])
                P.op("dve", lambda e, bo=bo, i=i: e.tensor_tensor(out=hst[g % 2][:, :, i * 128:(i + 1) * 128], in0=ps[bo][:].rearrange("p (h q) -> p h q", h=4),
                                                                   in1=zt[:].rearrange("p (h q) -> p h q", h=4), op=ALU.mult),
                     reads=[Rps[bo], Rz], writes=[Rhst[g % 2]])
            P.op("sp", lambda e, g=g: e.dma_start(out=S_haT[g * 512:(g + 1) * 512, :].rearrange("(h p) t -> p h t", p=128), in_=hst[g % 2][:]),
                 reads=[Rhst[g % 2]], writes=[RSha], dsem=dhst[g % 2])
        BT = barrier_from([RSha, Rhst[0], Rhst[1]])
    P.barrier()
    out_toks.append(BT.w)
    if L["UPTO"] == "T":
        return

    with contextlib.ExitStack() as st:
        with contextlib.ExitStack() as st2:
            mT = sb(st2, "mT", (128, 16, T), BF16)
            RmT = Res()
            wr = [sb(st2, "owr%d" % i, (128, 16, 512), BF16) for i in range(2)]
            Rwr = [Res(), Res()]
            dwr = [mkds(st2), mkds(st2)]
            wc = [0]

            def load_w(wsrc, c0):
                s = wc[0] % 2
                wc[0] += 1
                P.op("pool", lambda e: e.dma_start(out=wr[s][:], in_=wsrc[:, c0:c0 + 512].rearrange("(c p) n -> p c n", p=128)), writes=[Rwr[s]], dsem=dwr[s])
                return s

            with contextlib.ExitStack() as st3:
                hT = sb(st3, "hT", (128, 16, T), BF16)
                RhT = Res()
                gl = [sb(st3, "gl%d" % i, (128, T), BF16) for i in range(2)]
                Rgl = [Res(), Res()]
                dgl = [mkds(st3), mkds(st3)]
                tmpf = sb(st3, "tmpf", (128, 512), F32)
                Rtf = Res()
                for br in range(2):
                    if br == 0:
                        ht = [sb(st3, "htl%d" % i, (128, D), BF16) for i in range(2)]
                        Rht = [Res(), Res()]
                        dht = [mkds(st3), mkds(st3)]
                        for t in range(NT):
                            b = t % 2
                            P.op("sp", lambda e, b=b, t=t: e.dma_start(out=ht[b][:], in_=S_hm[t * 128:(t + 1) * 128, :]), reads=[BM], writes=[Rht[b]], dsem=dht[b])
                            for gq in range(4):
                                bk = nextbank()
                                ptv = ps[bk][:].bitcast(BF16)[:, 0:512].rearrange("p (j n) -> p j n", j=4)
                                for j in range(4):
                                    c = gq * 4 + j
                                    P.op("pe", lambda e, c=c, j=j, ptv=ptv, b=b: e.transpose(out=ptv[:, j, :], in_=ht[b][:, c * 128:(c + 1) * 128], identity=identb[:]),
                                         reads=[Rht[b]] + CONST, writes=[Rps[bk]], inc=(j == 3))
                                if gq % 2 == 0:
                                    P.op("act", lambda e, gq=gq, t=t, ptv=ptv: e.activation(out=hT[:, gq * 4:(gq + 1) * 4, t * 128:(t + 1) * 128], in_=ptv, func=AF.Copy),
                                         reads=[Rps[bk]], writes=[RhT])
                                else:
                                    P.op("dve", lambda e, gq=gq, t=t, ptv=ptv: e.tensor_copy(out=hT[:, gq * 4:(gq + 1) * 4, t * 128:(t + 1) * 128], in_=ptv),
                                         reads=[Rps[bk]], writes=[RhT])
                        wsrc, gsrc = w_mp, S_gmT
                    else:
                        dhT = mkds(st3)
                        P.op("sp", lambda e: e.dma_start(out=hT[:], in_=S_haT.rearrange("(c p) t -> p c t", p=128)), reads=[BT], writes=[RhT], dsem=dhT)
                        wsrc, gsrc = w_ap, S_gaT
                    for cb_ in range(4):
                        s = load_w(wsrc, cb_ * 512)
                        for j in range(4):
                            jj = cb_ * 4 + j
                            q = jj % 2
                            P.op("sp", lambda e, q=q, jj=jj, gsrc=gsrc: e.dma_start(out=gl[q][:], in_=gsrc[jj * 128:(jj + 1) * 128, :]), reads=[BP], writes=[Rgl[q]], dsem=dgl[q])
                            for tb in range(4):
                                bk = nextbank()
                                for c in range(16):
                                    P.op("pe", lambda e, c=c, j=j, tb=tb, bk=bk, s=s: e.matmul(ps[bk][:], lhsT=wr[s][:, c, j * 128:(j + 1) * 128], rhs=hT[:, c, tb * 512:(tb + 1) * 512],
                                                                                                 start=(c == 0), stop=(c == 15)),
                                         reads=[Rwr[s], RhT], writes=[Rps[bk]], inc=(c == 15))
                                if br == 0:
                                    P.op("dve", lambda e, jj=jj, tb=tb, bk=bk, q=q: e.tensor_tensor(out=mT[:, jj, tb * 512:(tb + 1) * 512], in0=ps[bk][:], in1=gl[q][:, tb * 512:(tb + 1) * 512], op=ALU.mult),
                                         reads=[Rps[bk], Rgl[q]], writes=[RmT])
                                else:
                                    P.op("dve", lambda e, tb=tb, bk=bk, q=q: e.tensor_tensor(out=tmpf[:], in0=ps[bk][:], in1=gl[q][:, tb * 512:(tb + 1) * 512], op=ALU.mult),
                                         reads=[Rps[bk], Rgl[q]], writes=[Rtf])
                                    P.op("pool", lambda e, jj=jj, tb=tb: e.tensor_tensor(out=mT[:, jj, tb * 512:(tb + 1) * 512], in0=mT[:, jj, tb * 512:(tb + 1) * 512], in1=tmpf[:], op=ALU.add),
                                         reads=[Rtf, RmT], writes=[RmT])
            P.barrier()
            xl = [sb(st2, "xl%d" % i, (128, 512), F32) for i in range(2)]
            Rxl = [Res(), Res()]
            dxl = [mkds(st2), mkds(st2)]
            x1s = [sb(st2, "x1s%d" % i, (128, 512), F32) for i in range(2)]
            Rx1s = [Res(), Res()]
            dx1s = [mkds(st2), mkds(st2)]
            RSx1 = Res()
            kx = 0
            for cb_ in range(4):
                s = load_w(w_o, cb_ * 512)
                for t in range(NT):
                    b = kx % 2
                    kx += 1
                    P.op("sp", lambda e, b=b, t=t, cb_=cb_: e.dma_start(out=xl[b][:], in_=x_own[t * 128:(t + 1) * 128, cb_ * 512:(cb_ + 1) * 512]), writes=[Rxl[b]], dsem=dxl[b])
                    bk = nextbank()
                    for c in range(16):
                        P.op("pe", lambda e, c=c, t=t, bk=bk, s=s: e.matmul(ps[bk][:], lhsT=mT[:, c, t * 128:(t + 1) * 128], rhs=wr[s][:, c, :], start=(c == 0), stop=(c == 15)),
                             reads=[RmT, Rwr[s]], writes=[Rps[bk]], inc=(c == 15))
                    P.op("dve", lambda e, b=b, bk=bk: e.tensor_tensor(out=x1s[b][:], in0=ps[bk][:], in1=xl[b][:], op=ALU.add),
                         reads=[Rps[bk], Rxl[b]], writes=[Rx1s[b]])
                    P.op("sp", lambda e, b=b, t=t, cb_=cb_: e.dma_start(out=S_x1[t * 128:(t + 1) * 128, cb_ * 512:(cb_ + 1) * 512], in_=x1s[b][:]),
                         reads=[Rx1s[b]], writes=[RSx1], dsem=dx1s[b])
            BX = barrier_from([RSx1] + Rx1s)
        P.barrier()
        xn2T = sb(st, "xn2T", (128, 16, T), BF16)
        Rxn2 = [Res() for _ in range(NT)]
        with contextlib.ExitStack() as st2:
            def get_x1(t, dst, Rd, dsm):
                P.op("sp", lambda e: e.dma_start(out=dst[:], in_=S_x1[t * 128:(t + 1) * 128, :]), reads=[BX], writes=[Rd], dsem=dsm)
            norm_T(st2, get_x1, NT, xn2T, Rxn2, g2, "2")
        P.barrier()

        with contextlib.ExitStack() as stq:
            pqT = sb(stq, "pqT", (128, 16, T), BF16)
            RpqT = Res()
            with contextlib.ExitStack() as st2:
                wr = [sb(st2, "qwr%d" % i, (128, 16, 512), BF16) for i in range(2)]
                Rwr = [Res(), Res()]
                dwr = [mkds(st2), mkds(st2)]
                for cb_ in range(4):
                    s = cb_ % 2
                    P.op("pool", lambda e, s=s, cb_=cb_: e.dma_start(out=wr[s][:], in_=w_pq[:, cb_ * 512:(cb_ + 1) * 512].rearrange("(c p) n -> p c n", p=128)), writes=[Rwr[s]], dsem=dwr[s])
                    for j in range(4):
                        for tb in range(4):
                            bk = nextbank()
                            for c in range(16):
                                P.op("pe", lambda e, c=c, j=j, tb=tb, bk=bk, s=s: e.matmul(ps[bk][:], lhsT=wr[s][:, c, j * 128:(j + 1) * 128], rhs=xn2T[:, c, tb * 512:(tb + 1) * 512], start=(c == 0), stop=(c == 15)),
                                     reads=[Rwr[s]] + Rxn2[tb * 4:tb * 4 + 4], writes=[Rps[bk]], inc=(c == 15))
                            if (j + tb) % 2 == 0:
                                P.op("act", lambda e, j=j, tb=tb, bk=bk, cb_=cb_: e.activation(out=pqT[:, cb_ * 4 + j, tb * 512:(tb + 1) * 512], in_=ps[bk][:], func=AF.Copy), reads=[Rps[bk]], writes=[RpqT])
                            else:
                                P.op("dve", lambda e, j=j, tb=tb, bk=bk, cb_=cb_: e.tensor_copy(out=pqT[:, cb_ * 4 + j, tb * 512:(tb + 1) * 512], in_=ps[bk][:]), reads=[Rps[bk]], writes=[RpqT])
            P.barrier()
            kTb = sb(stq, "kTb", (128, 16, 128), BF16)
            RkT = Res()
            dkT = mkds(stq)
            P.op("pool", lambda e: e.dma_start(out=kTb[:].rearrange("p a n -> p (a n)"), in_=keysT[:, :]), writes=[RkT], dsem=dkT)
            subs = [sb(stq, "subs%d" % i, (128, 16, 128), F32) for i in range(2)]
            Rsubs = [Res(), Res()]
            dsubs = [mkds(stq), mkds(stq)]
            RSsub = Res()
            for t in range(NT):
                tk = slice(t * 128, (t + 1) * 128)
                b = t % 2
                for qd in range(4):
                    bk = nextbank()
                    for r in range(4):
                        hp = qd * 4 + r
                        P.op("pe", lambda e, hp=hp, r=r, bk=bk, tk=tk: e.matmul(ps[bk][:, r * 128:(r + 1) * 128], lhsT=pqT[:, hp, tk], rhs=kTb[:, hp, :], start=True, stop=True),
                             reads=[RpqT, RkT], writes=[Rps[bk]], inc=(r == 3))
                    P.op("act", lambda e, qd=qd, bk=bk, b=b: e.activation(out=subs[b][:, qd * 4:(qd + 1) * 4, :], in_=ps[bk][:].rearrange("p (r n) -> p r n", r=4), func=AF.Copy),
                         reads=[Rps[bk]], writes=[Rsubs[b]])
                P.op("sp", lambda e, b=b, tk=tk: e.dma_start(out=S_sub[tk, :], in_=subs[b][:].rearrange("p a n -> p (a n)")), reads=[Rsubs[b]], writes=[RSsub], dsem=dsubs[b])
            BS = barrier_from([RSsub] + Rsubs)
        P.barrier()
        out_toks.append(BS.w)
        out_toks.append(BX.w)
        if L["UPTO"] == "O":
            return
        sub = sb(st, "sub", (128, 16, 128), F32)
        tmp = sb(st, "ptmp", (128, 16, 128), F32)
        sv = sb(st, "psv", (128, 16, 16), F32)
        cand = sb(st, "cand", (128, 8, 256), F32)
        tmp2 = tmp[:].rearrange("p (h two) n -> p h (two n)", two=2)
        c1 = sb(st, "c1", (128, 8, 16), F32)
        dmat = sb(st, "pdm", (128, 8, 16), F32)
        sc = sb(st, "psc", (128, 8, 7), F32)
        dg = sb(st, "pdg", (128, 8, 128), BF16)
        Rdg = Res()
        b3 = sb(st, "b3", (128, 8, 128), F32)
        Rsub, Rsv, Rc1, Rsc, Rb3 = (Res() for _ in range(5))
        REg = [[Res(), Res(), Res()], [Res(), Res(), Res()]]
        RE = REg[0] + REg[1]
        RG = [Res() for _ in range(4)]
        Ebuf = [tmp[:, 8 * i:8 * (i + 1), :] for i in range(2)]
        Gp = [cand[:].rearrange("p h n -> p (h n)").bitcast(BF16)[:, 1024 * i:1024 * (i + 1)] for i in range(4)]
        dsub = mkds(st)
        hraw = [sb(st, "hraw%d" % i, (128, NEB, 128), BF16) for i in range(2)]
        Rh = [Res(), Res()]
        ub = [sb(st, "ub%d" % i, (128, 2, 16, 128), BF16) for i in range(2)]
        Rub = [Res() for _ in range(2)]
        dub = [mkds(st) for _ in range(2)]
        vb = [sb(st, "vb%d" % i, (128, 2, D), BF16) for i in range(2)]
        Rvb = [Res() for _ in range(2)]
        dvb = [mkds(st) for _ in range(2)]
        MARGIN = 2.0e-3
        Eb4 = sb(st, "Eb4", (128, 4, 128), F32)
        Ea4 = sb(st, "Ea4", (128, 4, 128), F32)
        REab = Res()
        hstg = [sb(st, "hstg%d" % i, (128, 256), BF16) for i in range(2)]
        Rhstg = [Res(), Res()]
        At = [sb(st, "At%d" % i, (128, 128), BF16) for i in range(3)]
        RAt = [Res() for _ in range(3)]
        x1l = [sb(st, "x1l%d" % i, (128, 256), F32) for i in range(1)] * 2
        Rx1l = [Res()] * 2
        dx1l = [mkds(st)] * 2
        yo = [sb(st, "yo%d" % i, (128, 256), F32) for i in range(1)] * 2
        Ryo = [Res()] * 2
        dyo = [mkds(st)] * 2
        sv4 = sv[:].rearrange("p (h two) k -> p h two k", two=2)
        sub4 = sub[:].rearrange("p (h two) n -> p h two n", two=2)
        ke = 0

        hb_bank = {}

        def h_mm(tt, eb):
            iu = (eb // 2) % 2
            P.op("sp", lambda e: e.dma_start(out=ub[iu][:].rearrange("p b c n -> p b (c n)"), in_=S_UT[eb:eb + 2, :, :].rearrange("b p n -> p b n")),
                 reads=[BU], writes=[Rub[iu]], dsem=dub[iu])
            bk = nextbank(4, 6)
            hb_bank[eb] = bk
            for c in range(16):
                P.op("pe", lambda e: e.matmul(ps[bk][:, 0:256].rearrange("p (b n) -> p b n", b=2), lhsT=xn2T[:, c, tt * 128:(tt + 1) * 128], rhs=ub[iu][:, :, c, :], start=(c == 0), stop=(c == 15)),
                     reads=[Rub[iu], Rxn2[tt]], writes=[Rps[bk]], inc=(c == 15))

        def h_cp1(tt, eb):
            bk = hb_bank[eb]
            k = (eb // 2) % 2
            P.op("dve", lambda e: e.tensor_copy(out=hstg[k][:], in_=ps[bk][:, 0:256]), reads=[Rps[bk]], writes=[Rhstg[k]])

        def h_tr(tt, eb):
            bk = hb_bank[eb]
            k = (eb // 2) % 2
            tv = ps[bk][:].bitcast(BF16)[:, 512:768]
            for j in range(2):
                P.op("pe", lambda e: e.transpose(out=tv[:, j * 128:(j + 1) * 128], in_=hstg[k][:, j * 128:(j + 1) * 128], identity=identb[:]),
                     reads=[Rhstg[k]] + CONST, writes=[Rps[bk]], inc=(j == 1))

        def h_cp2(tt, eb):
            bk = hb_bank[eb]
            tv = ps[bk][:].bitcast(BF16)[:, 512:768]
            P.op("act", lambda e: e.activation(out=hraw[tt % 2][:, eb:eb + 2, :], in_=tv.rearrange("p (b n) -> p b n", b=2), func=AF.Copy), reads=[Rps[bk]], writes=[Rh[tt % 2]])

        def h_gelu(tt):
            hv = hraw[tt % 2][:].rearrange("p a n -> p (a n)")
            for qq in range(4):
                P.op("act", lambda e: e.activation(out=hv[:, qq * 4096:(qq + 1) * 4096], in_=hv[:, qq * 4096:(qq + 1) * 4096], func=AF.Gelu),
                     reads=[Rh[tt % 2]], writes=[Rh[tt % 2]])

        for eb in range(0, NEB, 2):
            h_mm(0, eb)
            h_cp1(0, eb)
            h_tr(0, eb)
            h_cp2(0, eb)
        h_gelu(0)
        for t in range(NT):
            tk = slice(t * 128, (t + 1) * 128)
            P.op("sp", lambda e, tk=tk: e.dma_start(out=sub[:].rearrange("p a n -> p (a n)"), in_=S_sub[tk, :]), reads=[BS], writes=[Rsub], dsem=dsub)
            for hp in range(16):
                P.op("dve", lambda e, hp=hp: e.max(out=sv[:, hp, 0:8], in_=sub[:, hp, :]), reads=[Rsub], writes=[Rsv])
                P.op("dve", lambda e, hp=hp: e.match_replace(out=tmp[:, hp, :], in_to_replace=sv[:, hp, 0:8], in_values=sub[:, hp, :], imm_value=NEG), reads=[Rsub, Rsv], writes=RE)
                P.op("dve", lambda e, hp=hp: e.max(out=sv[:, hp, 8:16], in_=tmp[:, hp, :]), reads=RE, writes=[Rsv])
            P.op("dve", lambda e: e.tensor_tensor(out=cand[:].rearrange("p h (a b) -> p h a b", a=16), in0=sv4[:, :, 0, :].unsqueeze(3).broadcast_to([128, 8, 16, 16]),
                                                   in1=sv4[:, :, 1, :].unsqueeze(2).broadcast_to([128, 8, 16, 16]), op=ALU.add), reads=[Rsv], writes=RG)
            for h in range(8):
                P.op("dve", lambda e, h=h: e.max(out=c1[:, h, 0:8], in_=cand[:, h, :]), reads=RG, writes=[Rc1])
                P.op("dve", lambda e, h=h: e.match_replace(out=tmp2[:, h, :], in_to_replace=c1[:, h, 0:8], in_values=cand[:, h, :], imm_value=NEG), reads=RG + [Rc1], writes=RE)
                P.op("dve", lambda e, h=h: e.max(out=c1[:, h, 8:16], in_=tmp2[:, h, :]), reads=RE, writes=[Rc1])
            P.op("dve", lambda e: e.tensor_tensor(out=dmat[:], in0=c1[:], in1=c1[:, :, 0:1].broadcast_to([128, 8, 16]), op=ALU.subtract), reads=[Rc1], writes=[Rsc])
            P.op("act", lambda e: e.activation(out=dmat[:], in_=dmat[:], func=AF.Exp), reads=[Rsc], writes=[Rsc])
            P.op("dve", lambda e: e.tensor_reduce(out=sc[:, :, 0], in_=dmat[:], axis=AX.X, op=ALU.add), reads=[Rsc], writes=[Rsc])
            P.op("act", lambda e: e.activation(out=sc[:, :, 1], in_=sc[:, :, 0], func=AF.Ln), reads=[Rsc], writes=[Rsc])
            P.op("dve", lambda e: e.scalar_tensor_tensor(out=sc[:, :, 2], in0=c1[:, :, 0], scalar=-1.0, in1=sc[:, :, 1], op0=ALU.mult, op1=ALU.subtract), reads=[Rsc, Rc1], writes=[Rsc])
            P.op("dve", lambda e: e.tensor_tensor(out=sc[:, :, 3], in0=c1[:, :, 15], in1=sc[:, :, 2], op=ALU.add), reads=[Rsc, Rc1], writes=[Rsc])
            P.op("act", lambda e: e.activation(out=sc[:, :, 3], in_=sc[:, :, 3], func=AF.Exp, bias=-MARGIN), reads=[Rsc], writes=[Rsc])
            P.op("dve", lambda e: e.tensor_scalar(out=sc[:, :, 4], in0=c1[:, :, 15], scalar1=-1.0, scalar2=MARGIN, op0=ALU.mult, op1=ALU.add), reads=[Rc1, Rsc], writes=[Rsc])
            P.op("dve", lambda e: e.tensor_tensor(out=b3[:], in0=sub4[:, :, 0, :], in1=sc[:, :, 4:5].broadcast_to([128, 8, 128]), op=ALU.add), reads=[Rsub, Rsc], writes=[Rb3])
            for h in range(8):
                P.op("pool", lambda e: e.tensor_scalar(out=dg[:, h, :], in0=identb[:], scalar1=sc[:, h, 3:4], scalar2=1.0, op0=ALU.mult, op1=ALU.mult),
                     reads=[Rsc] + CONST, writes=[Rdg])
            P.op("dve", lambda e: e.tensor_scalar(out=sc[:, :, 5], in0=sv4[:, :, 1, 0], scalar1=-1.0, scalar2=None, op0=ALU.mult), reads=[Rsv, Rsc], writes=[Rsc])
            P.op("dve", lambda e: e.tensor_tensor(out=sc[:, :, 6], in0=sc[:, :, 4], in1=sv4[:, :, 1, 0], op=ALU.add), reads=[Rsv, Rsc], writes=[Rsc])
            for h in range(4, 8):
                P.op("act", lambda e: e.activation(out=Eb4[:, h - 4, :], in_=sub4[:, h, 1, :], func=AF.Exp, bias=sc[:, h, 5:6]), reads=[Rsub, Rsc], writes=[REab])
                P.op("act", lambda e: e.activation(out=Ea4[:, h - 4, :], in_=sub4[:, h, 0, :], func=AF.Exp, bias=sc[:, h, 6:7]), reads=[Rsub, Rsc], writes=[REab])
            gel = hraw[t % 2]
            Rgel = Rh[t % 2]
            pend_out = None
            for eb in range(NEB):
                if eb % 2 == 0:
                    iv = (eb // 2) % 2
                    P.op("sp", lambda e: e.dma_start(out=vb[iv][:], in_=S_V[eb:eb + 2, :, :].rearrange("b p n -> p b n")), reads=[BU], writes=[Rvb[iv]], dsem=dvb[iv])
                es = eb % 2
                gs = eb % 4
                for h in range(4):
                    P.op("act", lambda e: e.activation(out=Ebuf[es][:, h, :], in_=sub4[:, h, 1, :], func=AF.Exp, bias=b3[:, h, eb:eb + 1]),
                         reads=[Rsub, Rb3], writes=[REg[es][0]])
                for h in (4, 5):
                    P.op("pool", lambda e: e.tensor_scalar(out=Ebuf[es][:, h, :], in0=Eb4[:, h - 4, :], scalar1=Ea4[:, h - 4, eb:eb + 1], scalar2=1.0, op0=ALU.mult, op1=ALU.mult),
                         reads=[REab], writes=[REg[es][1]])
                for h in (6, 7):
                    P.op("dve", lambda e: e.tensor_scalar(out=Ebuf[es][:, h, :], in0=Eb4[:, h - 4, :], scalar1=Ea4[:, h - 4, eb:eb + 1], scalar2=None, op0=ALU.mult),
                         reads=[REab], writes=[REg[es][2]])
                P.op("dve", lambda e: e.scalar_tensor_tensor(out=Gp[gs], in0=Ebuf[es].rearrange("p h n -> p (h n)"), scalar=1.0, in1=Ebuf[es].rearrange("p h n -> p (h n)"),
                                                              op0=ALU.is_ge, op1=ALU.mult),
                     reads=REg[es], writes=[RG[gs]])
                if t + 1 < NT:
                    if eb % 2 == 0:
                        h_mm(t + 1, eb)
                    else:
                        h_tr(t + 1, eb - 1)
                bg = nextbank(6, 8)
                for h in range(8):
                    P.op("pe", lambda e: e.matmul(ps[bg][:, 0:128], lhsT=Gp[gs][:, h * 128:(h + 1) * 128], rhs=dg[:, h, :], start=(h == 0), stop=(h == 7)),
                         reads=[RG[gs], Rdg], writes=[Rps[bg]], inc=(h == 7))
                if pend_out is not None:
                    pend_out()
                ia = eb % 3
                P.op("dve", lambda e: e.tensor_tensor(out=At[ia][:], in0=ps[bg][:, 0:128], in1=gel[:, eb, :], op=ALU.mult),
                     reads=[Rps[bg], Rgel], writes=[RAt[ia]])
                if t + 1 < NT:
                    if eb % 2 == 0:
                        h_cp1(t + 1, eb)
                    else:
                        h_cp2(t + 1, eb - 1)

                def mk_out(eb=eb, ia=ia):
                    iv2 = (eb // 2) % 2
                    for db in range(4):
                        P.op("pe", lambda e: e.matmul(ps[db][:], lhsT=At[ia][:], rhs=vb[iv2][:, eb % 2, db * 512:(db + 1) * 512], start=(eb == 0), stop=(eb == NEB - 1)),
                             reads=[RAt[ia], Rvb[iv2]], writes=[Rps[db]], inc=(db == 3))
                pend_out = mk_out
            pend_out()
            if t + 1 < NT:
                h_gelu(t + 1)
            for d8 in range(8):
                q = 0
                db, hf_ = d8 // 2, d8 % 2
                cs = slice(d8 * 256, (d8 + 1) * 256)
                P.op("sp", lambda e: e.dma_start(out=x1l[q][:], in_=S_x1[tk, cs]), reads=[BX], writes=[Rx1l[q]], dsem=dx1l[q])
                P.op("dve", lambda e: e.tensor_tensor(out=yo[q][:], in0=ps[db][:, hf_ * 256:(hf_ + 1) * 256], in1=x1l[q][:], op=ALU.add),
                     reads=[Rps[db], Rx1l[q]], writes=[Ryo[q]])
                tok = P.op("sp", lambda e: e.dma_start(out=y[tk, cs], in_=yo[q][:]), reads=[Ryo[q]], dsem=dyo[q])
                out_toks.append(tok)


def _t5_bucket_static(rel):
    half, max_exact = 16, 8
    ret = np.where(rel > 0, half, 0)
    n = np.abs(rel)
    nf = np.maximum(n, 1).astype(np.float32)
    large = max_exact + (np.log(nf / max_exact) / math.log(128 / max_exact) * (half - max_exact)).astype(np.int32)
    large = np.minimum(large, half - 1)
    return ret + np.where(n < max_exact, n, large)


def _consts():
    c = np.zeros((128, 384 + 4096 + 256), np.float32)
    c[:, 0:128] = np.eye(128, dtype=np.float32)
    c[:, 128:256] = 1.0
    s = np.arange(64)
    c[0:64, 256:320] = (s[:, None] <= s[None, :]).astype(np.float32)
    c[0:64, 320:384] = (s[:, None] >= s[None, :]).astype(np.float32)
    big = np.ones((128, 4096), np.float32)
    k = np.arange(128)
    m_prev = (k[:, None] >= k[None, :]).astype(np.float32)
    m_next = (k[:, None] <= k[None, :]).astype(np.float32)
    c[:, 384:4480] = big
    return c, m_prev, m_next


_NC_CACHE = {}


def kernel(x, norm1_g, w_in, mlstm_gate_b, mlstm_norm_g, w_m_proj, attn_q_norm_g, attn_k_norm_g,
           attn_sink, rel_bias, w_a_proj, w_out, norm2_g, peer_wq, peer_keys, peer_u, peer_v, _debug=False, _upto=None):
    f = lambda a: np.ascontiguousarray(np.asarray(a, dtype=np.float32))
    x, w_in = f(x), f(w_in)
    cst, m_prev, m_next = _consts()
    rm = np.ones((4, 4096), np.float32)
    rm[:, ::64] = 0.0
    shared = {}
    shared["n1g"] = f(np.asarray(norm1_g).reshape(16, 128).T)
    shared["n2g"] = f(np.asarray(norm2_g).reshape(16, 128).T)
    shared["mng"] = f(np.broadcast_to(np.asarray(mlstm_norm_g).reshape(1, D), (64, D)))
    shared["aqg"] = f(np.asarray(attn_q_norm_g).reshape(128, 1))
    shared["akg"] = f(np.asarray(attn_k_norm_g).reshape(128, 1))
    shared["sinkb"] = f(np.broadcast_to(np.asarray(attn_sink).reshape(1, 16), (128, 16)))
    shared["w_in"] = w_in
    shared["w_mp"] = f(w_m_proj)
    shared["w_ap"] = f(w_a_proj)
    shared["w_o"] = f(w_out)
    shared["w_pq"] = f(peer_wq)
    shared["keysT"] = f(np.asarray(peer_keys).reshape(16, 128, 128).transpose(2, 0, 1).reshape(128, 16 * 128))
    shared["uT"] = f(np.asarray(peer_u).reshape(NEB, 128, 16, 128).transpose(0, 3, 2, 1).reshape(NEB, 128, 16 * 128))
    shared["pv"] = f(np.asarray(peer_v).reshape(NEB, 128, D))
    rb = np.asarray(rel_bias, dtype=np.float32)
    gbias = np.asarray(mlstm_gate_b, dtype=np.float32)
    wg_full = w_in[:, O_MG:O_MG + 16]
    kq = np.arange(128)
    in_maps = []
    for core in range(8):
        b, half = core // 2, core % 2
        xs = x[b]
        if half == 1:
            xs = xs[::-1]
        m = dict(shared)
        m["x_own"] = f(xs[:T])
        m["x_halo"] = f(xs[T:])
        cols = []
        gb = np.zeros((4, 4), np.float32)
        for d in range(2):
            td = d ^ half
            for kind in range(2):
                cols.append(wg_full[:, td * 8 + kind * 4: td * 8 + kind * 4 + 4])
                gb[:, d * 2 + kind] = gbias[td, kind, :]
        m["w_gate"] = f(np.concatenate(cols, axis=1))
        m["gate_b"] = gb
        bt = np.zeros((128, 16, 3, 128), np.float32)
        for o in range(3):
            rel = (kq[:, None] + (o - 1) * 128) - kq[None, :]
            if half == 1:
                rel = -rel
            bk = _t5_bucket_static(rel)
            bt[:, :, o, :] = rb[bk].transpose(0, 2, 1)
        m["btab"] = f(bt.reshape(128, -1))
        c2 = cst.copy()
        c2[:, 4480:4480 + 128] = m_prev
        c2[:, 4480 + 128:4480 + 256] = m_next
        c2[0:4, 384:4480] = rm
        m["cst"] = c2
        in_maps.append(m)
    key = (tuple(_debug) if _debug else None, _upto)
    if key not in _NC_CACHE:
        _NC_CACHE[key] = build_nc(debug=_debug, upto=_upto)
    nc = _NC_CACHE[key]
    in_maps = [{k: m[k] for k in nc._in_names} for m in in_maps]
    res = run_bass_kernel_spmd(nc, in_maps, core_ids=list(range(8)))
    out = np.zeros((4, 4096, D), np.float32)
    for core in range(8):
        b, half = core // 2, core % 2
        yc = res.results[core].get("y", np.zeros((T, D), np.float32)) if _debug else res.results[core]["y"]
        if half == 0:
            out[b, :T] = yc
        else:
            out[b, T:] = yc[::-1]
    if _debug:
        return out, res
    return out
```

```python
import contextlib
import math
import numpy as np
import concourse.bass as bass
import concourse.mybir as mybir
from concourse.bass_utils import run_bass_kernel_spmd

F32 = mybir.dt.float32
BF16 = mybir.dt.bfloat16
AF = mybir.ActivationFunctionType
ALU = mybir.AluOpType
AX = mybir.AxisListType

T = 2048
D = 2048
NT = 16
EPS = 1e-6
NEG = -1.0e30
O_MQ, O_MK, O_MV, O_MO, O_MG, O_AQ, O_AK, O_AV, O_GM, O_GA = (
    0, 2048, 4096, 6144, 8192, 8208, 10256, 10768, 11280, 13328)
NEB = 128


class Res:
    __slots__ = ("w", "r")

    def __init__(self):
        self.w = None
        self.r = []


class DSem:
    def __init__(self, sem):
        self.sem = sem
        self.val = 0


class _Rec:
    def __init__(self):
        self.call = None

    def __getattr__(self, name):
        def f(*a, **k):
            self.call = (name, a, k)
            return self
        return f


class Prog:
    ENGS = ("pe", "dve", "act", "pool", "sp")

    def __init__(self, nc, esems):
        self.nc = nc
        self.esem = esems
        self.cnt = {e: 0 for e in self.ENGS}
        self.ops = {e: [] for e in self.ENGS}
        self.seen = {e: {} for e in self.ENGS}
        self.nops = 0
        self.dsems = []
        self.fence = {e: [] for e in self.ENGS}

    def barrier(self):
        toks = [(self.esem[e], self.cnt[e], "x") for e in self.ENGS if self.cnt[e] > 0]
        toks += [(d.sem, d.val, "dma") for d in self.dsems if d.val > 0]
        for e in self.ENGS:
            self.fence[e] = list(toks)

    def op(self, eng, fn, reads=(), writes=(), dsem=None, inc=True):
        waits = {}

        def add(tok):
            if tok is None:
                return
            s, v, e = tok
            if e == "pe" and eng == "pe" and dsem is None:
                return
            k = id(s)
            if self.seen[eng].get(k, 0) >= v:
                return
            if k not in waits or waits[k][1] < v:
                waits[k] = (s, v)

        for R in reads:
            add(R.w)
        for R in writes:
            add(R.w)
            for t in R.r:
                add(t)
        if self.fence[eng]:
            for t in self.fence[eng]:
                add(t)
            self.fence[eng] = []
        for k, (s, v) in waits.items():
            self.seen[eng][k] = v
        if dsem is None:
            if inc:
                self.cnt[eng] += 1
                tok = (self.esem[eng], self.cnt[eng], eng)
                incs = (self.esem[eng], 1)
            else:
                tok = (self.esem[eng], self.cnt[eng] + 1, eng)
                incs = None
        else:
            dsem.val += 16
            tok = (dsem.sem, dsem.val, "dma")
            incs = (dsem.sem, 16)
        for R in reads:
            R.r.append(tok)
        for R in writes:
            R.w = tok
            R.r = []
        rec = _Rec()
        fn(rec)
        self.ops[eng].append((list(waits.values()), rec.call, incs))
        self.nops += 1
        return tok

    def finish(self, toks):
        waits = [(s, v) for (s, v, _) in toks if s is not None]
        self.ops["sp"].append((waits, ("nop", (), {}), (self.esem["sp"], 1)))

    def emit(self, block):
        engmap = {"pe": "tensor", "dve": "vector", "act": "scalar", "pool": "gpsimd", "sp": "sync"}
        for e in self.ENGS:
            ops = self.ops[e]

            def body(engine, ops=ops):
                for waits, fn, incs in ops:
                    for s, v in waits:
                        engine.wait_ge(s, v)
                    ins = getattr(engine, fn[0])(*fn[1], **fn[2])
                    if incs is not None:
                        ins.then_inc(incs[0], incs[1])

            getattr(block, engmap[e])(body)


def build_nc(debug=False, upto=None):
    nc = bass.Bass("TRN2", target_bir_lowering=False)

    LEVELS = ["A", "P1", "P", "G", "M", "T", "O", None]
    lvl = LEVELS.index(upto)
    in_names = []
    nc._in_names = in_names

    def din(name, shape, need=0):
        if lvl < need:
            return None
        in_names.append(name)
        return nc.dram_tensor(name, list(shape), F32, kind="ExternalInput").ap()

    dbgset = set(debug) if debug else set()

    def dscr(name, shape, dt):
        return nc.dram_tensor(name, list(shape), dt, kind="ExternalOutput" if name in dbgset else "Internal").ap()

    x_own = din("x_own", (T, D))
    x_halo = din("x_halo", (T, D))
    w_in = din("w_in", (D, 15376))
    w_gate = din("w_gate", (D, 16))
    gate_b = din("gate_b", (4, 4))
    n1g = din("n1g", (128, 16))
    n2g = din("n2g", (128, 16))
    mng = din("mng", (64, D), need=4)
    aqg = din("aqg", (128, 1))
    akg = din("akg", (128, 1))
    sinkb = din("sinkb", (128, 16), need=5)
    btab = din("btab", (128, 16 * 3 * 128), need=5)
    cst = din("cst", (128, 384 + 4096 + 256))
    w_mp = din("w_mp", (D, D), need=6)
    w_ap = din("w_ap", (D, D), need=6)
    w_o = din("w_o", (D, D), need=6)
    w_pq = din("w_pq", (D, D), need=6)
    keysT = din("keysT", (128, 16 * 128), need=6)
    uT = din("uT", (NEB, 128, 16 * 128), need=5)
    pv = din("pv", (NEB, 128, D), need=5)
    y = nc.dram_tensor("y", [T, D], F32, kind="ExternalOutput").ap()

    S_qT = dscr("S_qT", (D, T), BF16)
    S_kT = dscr("S_kT", (D, T), BF16)
    S_k = dscr("S_k", (T, D), BF16)
    S_v = dscr("S_v", (T, D), BF16)
    S_o = dscr("S_o", (T, D), BF16)
    S_kh = dscr("S_kh", (T, D), BF16)
    S_vh = dscr("S_vh", (T, D), BF16)
    S_aqT = dscr("S_aqT", (D, T), BF16)
    S_akT = dscr("S_akT", (512, T + 128), BF16)
    S_av = dscr("S_av", (T + 128, 512), BF16)
    S_gmT = dscr("S_gmT", (D, T), BF16)
    S_gaT = dscr("S_gaT", (D, T), BF16)
    S_g = dscr("S_g", (4, 4, 2 * T), F32)
    S_hf = dscr("S_hf", (T, D), F32)
    S_hm = dscr("S_hm", (T, D), BF16)
    S_haT = dscr("S_haT", (D, T), BF16)
    S_x1 = dscr("S_x1", (T, D), F32)
    S_sub = dscr("S_sub", (T, D), F32)
    S_xn2 = dscr("S_xn2", (NT, 128, 16 * 128), BF16)
    S_UT = dscr("S_UT", (NEB, 128, D), BF16)
    S_V = dscr("S_V", (NEB, 128, D), BF16)

    with contextlib.ExitStack() as top:
        E = top.enter_context
        esems = {e: E(nc.semaphore("es_" + e)) for e in Prog.ENGS}
        P = Prog(nc, esems)
        block = E(nc.Block())
        nds = [0]

        def mkds(st=None):
            nds[0] += 1
            d_ = DSem(top.enter_context(nc.semaphore("ds%d" % nds[0])))
            P.dsems.append(d_)
            return d_

        def sb(st, name, shape, dt):
            return st.enter_context(nc.sbuf_tensor(name, list(shape), dt))

        cst_sb = sb(top, "cst_sb", (128, 128 + 128 + 64 + 64), F32)
        identb = sb(top, "identb", (128, 128), BF16)
        onesb = sb(top, "onesb", (128, 128), BF16)
        g1 = sb(top, "g1", (128, 16), F32)
        g2 = sb(top, "g2", (128, 16), F32)
        Rc = Res()
        dc = mkds()
        P.op("sp", lambda e: e.dma_start(out=cst_sb[:], in_=cst[:, 0:384]), writes=[Rc], dsem=dc)
        P.op("sp", lambda e: e.dma_start(out=g1[:], in_=n1g[:, :]), writes=[Rc], dsem=dc)
        P.op("sp", lambda e: e.dma_start(out=g2[:], in_=n2g[:, :]), writes=[Rc], dsem=dc)
        identf = cst_sb[:, 0:128]
        onesf = cst_sb[:, 128:256]
        maskf = cst_sb[0:64, 256:320]
        maskb = cst_sb[0:64, 320:384]
        Rcb = Res()
        P.op("dve", lambda e: e.tensor_copy(out=identb[:], in_=identf), reads=[Rc], writes=[Rcb])
        P.op("dve", lambda e: e.tensor_copy(out=onesb[:], in_=onesf), reads=[Rc], writes=[Rcb])
        CONST = [Rc, Rcb]

        ps = [E(nc.psum_tensor("ps%d" % i, [128, 512], F32)) for i in range(8)]
        Rps = [Res() for _ in range(8)]
        bank_ctr = [0]

        def nextbank(lo=0, hi=8):
            n = hi - lo
            b = lo + bank_ctr[0] % n
            bank_ctr[0] += 1
            return b

        def barrier_from(res_list):
            B = Res()
            toks = []
            for R in res_list:
                toks += R.r
                if R.w is not None:
                    toks.append(R.w)
            waits = [(s, v) for (s, v, _) in toks]
            P.cnt["sp"] += 1
            tok = (P.esem["sp"], P.cnt["sp"], "sp")
            P.ops["sp"].append((waits, ("nop", (), {}), (P.esem["sp"], 1)))
            B.w = tok
            return B

        def norm_T(st, get_tile, ntiles, xnT, RxnT, gt, tag):
            xt = [sb(st, "xt%s%d" % (tag, i), (128, D), F32) for i in range(2)]
            Rxt = [Res(), Res()]
            dxt = [mkds(st), mkds(st)]
            junk = sb(st, "junk" + tag, (128, D), BF16)
            xs = sb(st, "xs" + tag, (128, D), BF16)
            sm = sb(st, "sm" + tag, (128, 4), F32)
            Rj, Rxs, Rsm = Res(), Res(), Res()
            for t in range(ntiles):
                b = t % 2
                get_tile(t, xt[b], Rxt[b], dxt[b])
                P.op("act", lambda e, b=b: e.activation(out=junk[:], in_=xt[b][:], func=AF.Square, accum_out=sm[:, 0:1]),
                     reads=[Rxt[b]], writes=[Rj, Rsm])
                P.op("act", lambda e: e.activation(out=sm[:, 1:2], in_=sm[:, 0:1], func=AF.Ln, scale=1.0 / D, bias=EPS),
                     reads=[Rsm], writes=[Rsm])
                P.op("act", lambda e: e.activation(out=sm[:, 2:3], in_=sm[:, 1:2], func=AF.Exp, scale=-0.5),
                     reads=[Rsm], writes=[Rsm])
                P.op("dve", lambda e, b=b: e.tensor_scalar(out=xs[:], in0=xt[b][:], scalar1=sm[:, 2:3], scalar2=None, op0=ALU.mult),
                     reads=[Rxt[b], Rsm], writes=[Rxs])
                for g in range(4):
                    bk = nextbank()
                    ptv = ps[bk][:].bitcast(BF16)[:, 0:512].rearrange("p (j n) -> p j n", j=4)
                    for j in range(4):
                        c = g * 4 + j
                        P.op("pe", lambda e, c=c, j=j, ptv=ptv: e.transpose(out=ptv[:, j, :], in_=xs[:, c * 128:(c + 1) * 128], identity=identb[:]),
                             reads=[Rxs] + CONST, writes=[Rps[bk]], inc=(j == 3))
                    P.op("dve", lambda e, g=g, t=t, ptv=ptv: e.tensor_tensor(
                        out=xnT[:, g * 4:(g + 1) * 4, t * 128:(t + 1) * 128], in0=ptv,
                        in1=gt[:, g * 4:(g + 1) * 4].unsqueeze(2).broadcast_to([128, 4, 128]), op=ALU.mult),
                        reads=[Rps[bk]] + CONST, writes=[RxnT[t]])

        with contextlib.ExitStack() as st:
            xnT = sb(st, "xnT", (128, 16, T), BF16)
            RxnT = [Res() for _ in range(NT)]
            wr = [sb(st, "wr%d" % i, (128, 16, 512), BF16) for i in range(3)]
            Rwr = [Res() for _ in range(3)]
            dwr = [mkds(st) for _ in range(3)]
            wctr = [0]
            stg = [sb(st, "stg%d" % i, (128, 8192), BF16) for i in range(2)]
            Rstg = [Res(), Res()]
            dstg = [mkds(st), mkds(st)]
            sctr = [0]
            wg = sb(st, "wg", (128, 16, 16), BF16)
            gb = sb(st, "gb", (4, 4), F32)
            gqs = sb(st, "gqs", (128, 2), F32)
            gst = [sb(st, "gst%d" % i, (4, 512), F32) for i in range(2)]
            Rgst = [Res(), Res()]
            dgst = [mkds(st), mkds(st)]
            sqt = sb(st, "sqt", (128, 512), BF16)
            lnv = sb(st, "lnv", (128, 512), F32)
            Rsq, Rln = Res(), Res()
            Rw0 = Res()
            dw0 = mkds(st)
            dw0p = mkds(st)
            P.op("pool", lambda e: e.dma_start(out=wg[:], in_=w_gate.rearrange("(c p) n -> p c n", p=128)), writes=[Rw0], dsem=dw0p)
            P.op("sp", lambda e: e.dma_start(out=gb[:], in_=gate_b[:, :]), writes=[Rw0], dsem=dw0)
            P.op("sp", lambda e: e.dma_start(out=gqs[:, 0:1], in_=aqg[:, :]), writes=[Rw0], dsem=dw0)
            P.op("sp", lambda e: e.dma_start(out=gqs[:, 1:2], in_=akg[:, :]), writes=[Rw0], dsem=dw0)
            P.op("dve", lambda e: e.tensor_scalar(out=gqs[:, 0:1], in0=gqs[:, 0:1], scalar1=128.0 ** -0.5, scalar2=None, op0=ALU.mult),
                 reads=[Rw0], writes=[Rw0])

            def load_w(wsrc, c0, n):
                s = wctr[0] % 3
                wctr[0] += 1
                P.op("pool", lambda e: e.dma_start(out=wr[s][:, :, 0:n], in_=wsrc[:, c0:c0 + n].rearrange("(c p) n -> p c n", p=128)),
                     writes=[Rwr[s]], dsem=dwr[s])
                return s

            def new_stg():
                s = sctr[0] % 2
                sctr[0] += 1
                return s

            def fm_mm(s, j, tok0, ntok, bk):
                rd = [Rwr[s]] + RxnT[tok0 // 128:(tok0 + ntok + 127) // 128]
                for c in range(16):
                    P.op("pe", lambda e, c=c: e.matmul(ps[bk][:, 0:ntok], lhsT=wr[s][:, c, j * 128:(j + 1) * 128],
                                                         rhs=xnT[:, c, tok0:tok0 + ntok], start=(c == 0), stop=(c == 15)),
                         reads=rd, writes=[Rps[bk]], inc=(c == 15))

            def tm_mm(s, n, t, bk):
                rd = [Rwr[s], RxnT[t]]
                for c in range(16):
                    P.op("pe", lambda e, c=c: e.matmul(ps[bk][:, 0:n], lhsT=xnT[:, c, t * 128:(t + 1) * 128],
                                                         rhs=wr[s][:, c, 0:n], start=(c == 0), stop=(c == 15)),
                         reads=rd, writes=[Rps[bk]], inc=(c == 15))

            evctr = [0]

            def evac_copy(dst, bk, n, wres, func=None):
                if func is not None or evctr[0] % 2 == 0:
                    f = func if func is not None else AF.Copy
                    P.op("act", lambda e: e.activation(out=dst, in_=ps[bk][:, 0:n], func=f), reads=[Rps[bk]], writes=wres)
                else:
                    P.op("dve", lambda e: e.tensor_copy(out=dst, in_=ps[bk][:, 0:n]), reads=[Rps[bk]], writes=wres)
                evctr[0] += 1

            def evac_qknorm(dst, bk, n, gcol, wres):
                P.op("act", lambda e: e.activation(out=sqt[:, 0:n], in_=ps[bk][:, 0:n], func=AF.Square), reads=[Rps[bk]], writes=[Rsq])
                b2 = nextbank()
                P.op("pe", lambda e: e.matmul(ps[b2][:, 0:n], lhsT=onesb[:], rhs=sqt[:, 0:n], start=True, stop=True),
                     reads=[Rsq] + CONST, writes=[Rps[b2]])
                P.op("act", lambda e: e.activation(out=lnv[:, 0:n], in_=ps[b2][:, 0:n], func=AF.Ln, scale=1.0 / 128, bias=EPS),
                     reads=[Rps[b2]], writes=[Rln])
                P.op("act", lambda e: e.activation(out=lnv[:, 0:n], in_=lnv[:, 0:n], func=AF.Exp, scale=-0.5), reads=[Rln], writes=[Rln])
                P.op("dve", lambda e: e.scalar_tensor_tensor(out=dst, in0=ps[bk][:, 0:n], scalar=gqs[:, gcol:gcol + 1], in1=lnv[:, 0:n],
                                                              op0=ALU.mult, op1=ALU.mult), reads=[Rps[bk], Rln, Rw0], writes=wres)

            def fm_block(wsrc, c0, ncol, dstT, row0, kind, ntok=T, tokdst0=0):
                s = load_w(wsrc, c0, ncol)
                g = new_stg()
                nsub = ncol // 128
                sv = stg[g][:, 0:nsub * ntok].rearrange("p (j t) -> p j t", j=nsub)
                for j in range(nsub):
                    for tb in range(0, ntok, 512):
                        n = min(512, ntok - tb)
                        bk = nextbank()
                        fm_mm(s, j, tb, n, bk)
                        dst = sv[:, j, tb:tb + n]
                        if kind == "plain":
                            evac_copy(dst, bk, n, [Rstg[g]])
                        elif kind == "sig":
                            evac_copy(dst, bk, n, [Rstg[g]], func=AF.Sigmoid)
                        elif kind == "qn":
                            evac_qknorm(dst, bk, n, 0, [Rstg[g]])
                        elif kind == "kn":
                            evac_qknorm(dst, bk, n, 1, [Rstg[g]])
                P.op("sp", lambda e: e.dma_start(out=dstT[row0:row0 + ncol, tokdst0:tokdst0 + ntok].rearrange("(j p) t -> p j t", p=128), in_=sv),
                     reads=[Rstg[g]], dsem=dstg[g])

            def tm_block(wsrc, c0, ncol, dst, dcol0, kind, ntiles=NT, tokdst0=0):
                s = load_w(wsrc, c0, ncol)
                g = new_stg()
                sv = stg[g][:, 0:ntiles * ncol].rearrange("p (t n) -> p t n", t=ntiles)
                for t in range(ntiles):
                    bk = nextbank()
                    tm_mm(s, ncol, t, bk)
                    evac_copy(sv[:, t, :], bk, ncol, [Rstg[g]], func=(AF.Sigmoid if kind == "sig" else None))
                P.op("sp", lambda e: e.dma_start(out=dst[tokdst0:tokdst0 + ntiles * 128, dcol0:dcol0 + ncol].rearrange("(t p) n -> p t n", p=128), in_=sv),
                     reads=[Rstg[g]], dsem=dstg[g])

            def gate_block(ggs, tokdst0):
                for tb in range(4):
                    for gg in ggs:
                        bk = nextbank()
                        for c in range(16):
                            P.op("pe", lambda e, c=c: e.matmul(ps[bk][0:4, :], lhsT=wg[:, c, gg * 4:(gg + 1) * 4],
                                                                 rhs=xnT[:, c, tb * 512:(tb + 1) * 512], start=(c == 0), stop=(c == 15)),
                                 reads=[Rw0] + RxnT[tb * 4:tb * 4 + 4], writes=[Rps[bk]], inc=(c == 15))
                        q = (tb * 4 + gg) % 2
                        P.op("act", lambda e: e.activation(out=gst[q][:], in_=ps[bk][0:4, :], func=AF.Identity, bias=gb[:, gg:gg + 1]),
                             reads=[Rps[bk], Rw0], writes=[Rgst[q]])
                        P.op("sp", lambda e: e.dma_start(out=S_g[gg, :, tokdst0 + tb * 512:tokdst0 + (tb + 1) * 512], in_=gst[q][:]),
                             reads=[Rgst[q]], dsem=dgst[q])

            def x_tile_loader(xsrc):
                def get(t, dst, Rd, dsm):
                    P.op("sp", lambda e: e.dma_start(out=dst[:], in_=xsrc[t * 128:(t + 1) * 128, :]), writes=[Rd], dsem=dsm)
                return get

            with contextlib.ExitStack() as st2:
                norm_T(st2, x_tile_loader(x_halo), NT, xnT, RxnT, g1, "h")
                if upto == "A":
                    D_xnT = nc.dram_tensor("D_xnT", [128, 16 * T], BF16, kind="ExternalOutput").ap()
                    dsx = mkds()
                    tk_ = P.op("sp", lambda e: e.dma_start(out=D_xnT[:, :], in_=xnT[:].rearrange("p c t -> p (c t)")), reads=RxnT, dsem=dsx)
                    P.finish([tk_])
                    P.emit(block)
                    return nc
                gate_block([2, 3], T)
                for hb in range(4):
                    tm_block(w_in, O_MK + hb * 512, 512, S_kh, hb * 512, "plain")
                    tm_block(w_in, O_MV + hb * 512, 512, S_vh, hb * 512, "plain")
                fm_block(w_in, O_AK, 512, S_akT, 0, "kn", ntok=128, tokdst0=T)
                tm_block(w_in, O_AV, 512, S_av, 0, "plain", ntiles=1, tokdst0=T)
                if upto == "P1":
                    BP = barrier_from([Rstg[0], Rstg[1], Rgst[0], Rgst[1]])
                    P.finish([BP.w])
                    P.emit(block)
                    return nc
                norm_T(st2, x_tile_loader(x_own), NT, xnT, RxnT, g1, "o")
            P.barrier()
            gate_block([0, 1, 2, 3], 0)
            for hb in range(4):
                fm_block(w_in, O_MQ + hb * 512, 512, S_qT, hb * 512, "plain")
                fm_block(w_in, O_MK + hb * 512, 512, S_kT, hb * 512, "plain")
                tm_block(w_in, O_MK + hb * 512, 512, S_k, hb * 512, "plain")
                tm_block(w_in, O_MV + hb * 512, 512, S_v, hb * 512, "plain")
                tm_block(w_in, O_MO + hb * 512, 512, S_o, hb * 512, "sig")
                fm_block(w_in, O_AQ + hb * 512, 512, S_aqT, hb * 512, "qn")
                fm_block(w_in, O_GM + hb * 512, 512, S_gmT, hb * 512, "sig")
                fm_block(w_in, O_GA + hb * 512, 512, S_gaT, hb * 512, "sig")
            fm_block(w_in, O_AK, 512, S_akT, 0, "kn")
            tm_block(w_in, O_AV, 512, S_av, 0, "plain")
            PH_P_DONE = [Rstg[0], Rstg[1], Rgst[0], Rgst[1]]
        P.barrier()

        BP = barrier_from(PH_P_DONE)
        out_toks = [BP.w]
        UPTO = upto
        if upto != "P":
            build_rest(nc, P, top, sb, mkds, ps, Rps, nextbank, CONST, BP, locals(), out_toks)
        P.finish(out_toks)
        P.emit(block)
    return nc


def build_rest(nc, P, top, sb, mkds, ps, Rps, nextbank, CONST, BP, L, out_toks):
    identb, onesb, cst_sb = L["identb"], L["onesb"], L["cst_sb"]
    identf, onesf, maskf, maskb = L["identf"], L["onesf"], L["maskf"], L["maskb"]
    cst, g2 = L["cst"], L["g2"]
    S_g, S_qT, S_kT, S_k, S_v, S_o, S_kh, S_vh = (L[k] for k in "S_g S_qT S_kT S_k S_v S_o S_kh S_vh".split())
    S_hf, S_hm, mng = L["S_hf"], L["S_hm"], L["mng"]
    S_UT, S_V, uT, pv = L["S_UT"], L["S_V"], L["uT"], L["pv"]
    L["BU_holder"] = [None]
    barrier_from = L["barrier_from"]
    norm_T = L["norm_T"]

    TSf = sb(top, "TSf", (64, 2, 32, 4), F32)
    TSb = sb(top, "TSb", (64, 96, 4), F32)
    DBf = sb(top, "DBf", (128, 4, 32), F32)
    DBb = sb(top, "DBb", (128, 4, 64), F32)
    RTS = Res()
    with contextlib.ExitStack() as st:
        B = [sb(st, "gB%d" % i, (4, 4096), F32) for i in range(5)]
        RB = [Res() for _ in range(5)]
        dB = [mkds(st) for _ in range(2)]
        rmask = sb(st, "rmask", (4, 4096), F32)
        Rrm = Res()
        drm = mkds(st)
        P.op("sp", lambda e: e.dma_start(out=rmask[:], in_=cst[0:4, 384:384 + 4096]), writes=[Rrm], dsem=drm)
        amax = sb(st, "amax", (4, 64), F32)
        totc = sb(st, "totc", (4, 64), F32)
        Mtab = sb(st, "Mtab", (4, 65), F32)
        MC = sb(st, "MC", (4, 64), F32)
        dd = sb(st, "dd", (4, 64), F32)
        dexp = sb(st, "dexp", (4, 4, 64), F32)
        Rsm = Res()
        LN_S = math.log(512.0 ** -0.5)
        for d in range(2):
            Ltok = T if d == 0 else 2 * T
            nch = Ltok // 64
            li, zf, cum, cx, a = (B[i][:, 0:Ltok] for i in range(5))
            v3 = lambda ap: ap.rearrange("p (c l) -> p c l", l=64)
            P.op("sp", lambda e: e.dma_start(out=li, in_=S_g[d * 2, :, 0:Ltok]), reads=[BP], writes=[RB[0]], dsem=dB[0])
            P.op("sp", lambda e: e.dma_start(out=zf, in_=S_g[d * 2 + 1, :, 0:Ltok]), reads=[BP], writes=[RB[1]], dsem=dB[1])
            P.op("act", lambda e: e.activation(out=zf, in_=zf, func=AF.Exp, scale=-1.0), reads=[RB[1]], writes=[RB[1]])
            P.op("act", lambda e: e.activation(out=zf, in_=zf, func=AF.Ln, bias=1.0), reads=[RB[1]], writes=[RB[1]])
            P.op("dve", lambda e: e.tensor_tensor_scan(out=cum, data0=rmask[:, 0:Ltok], data1=zf, initial=0.0, op0=ALU.mult, op1=ALU.add),
                 reads=[RB[1], Rrm], writes=[RB[2]])
            tot3 = v3(cum)[:, :, 63:64]
            if d == 1:
                P.op("dve", lambda e: e.tensor_tensor(out=cx, in0=zf, in1=cum, op=ALU.subtract), reads=[RB[1], RB[2]], writes=[RB[3]])
                P.op("dve", lambda e: e.tensor_tensor(out=v3(cx), in0=v3(cx), in1=tot3.broadcast_to([4, nch, 64]), op=ALU.add),
                     reads=[RB[3], RB[2]], writes=[RB[3]])
                cxx, Rcx = cx, RB[3]
            else:
                cxx, Rcx = cum, RB[2]
            P.op("dve", lambda e: e.tensor_tensor(out=a, in0=li, in1=cxx, op=ALU.add), reads=[RB[0], Rcx], writes=[RB[4]])
            P.op("dve", lambda e: e.tensor_reduce(out=amax[:, 0:nch], in_=v3(a), axis=AX.X, op=ALU.max), reads=[RB[4]], writes=[Rsm])
            P.op("dve", lambda e: e.tensor_copy(out=totc[:, 0:nch], in_=tot3.rearrange("p c l -> p (c l)")), reads=[RB[2]], writes=[Rsm])
            if d == 0:
                P.op("dve", lambda e: e.memset(Mtab[:, 0:1], 0.0), writes=[Rsm])
                order = [(c, c, c + 1) for c in range(nch)]
            else:
                P.op("dve", lambda e: e.memset(Mtab[:, nch:nch + 1], 0.0), writes=[Rsm])
                order = [(c, c + 1, c) for c in range(nch - 1, -1, -1)]
            for (c, ip, inx) in order:
                P.op("dve", lambda e, c=c, ip=ip: e.tensor_tensor(out=MC[:, c:c + 1], in0=Mtab[:, ip:ip + 1], in1=amax[:, c:c + 1], op=ALU.max),
                     reads=[Rsm], writes=[Rsm])
                P.op("dve", lambda e, c=c, inx=inx: e.tensor_tensor(out=Mtab[:, inx:inx + 1], in0=MC[:, c:c + 1], in1=totc[:, c:c + 1], op=ALU.subtract),
                     reads=[Rsm], writes=[Rsm])
            mprev = Mtab[:, 0:nch] if d == 0 else Mtab[:, 1:nch + 1]
            P.op("dve", lambda e: e.tensor_tensor(out=dd[:, 0:nch], in0=mprev, in1=MC[:, 0:nch], op=ALU.subtract), reads=[Rsm], writes=[Rsm])
            P.op("act", lambda e: e.activation(out=dd[:, 0:nch], in_=dd[:, 0:nch], func=AF.Exp), reads=[Rsm], writes=[Rsm])
            mcb = MC[:, 0:nch].unsqueeze(2).broadcast_to([4, nch, 64])
            P.op("dve", lambda e: e.tensor_tensor(out=v3(zf), in0=v3(a), in1=mcb, op=ALU.subtract), reads=[RB[4], Rsm, RB[1]], writes=[RB[1]])
            P.op("act", lambda e: e.activation(out=zf, in_=zf, func=AF.Exp, bias=LN_S), reads=[RB[1]], writes=[RB[1]])
            P.op("dve", lambda e: e.tensor_tensor(out=v3(li), in0=v3(cxx), in1=mcb, op=ALU.subtract), reads=[Rcx, Rsm, RB[0]], writes=[RB[0]])
            P.op("act", lambda e: e.activation(out=li, in_=li, func=AF.Exp), reads=[RB[0]], writes=[RB[0]])
            bk = nextbank()
            ncols = 0
            for kind, src, Rsrc, nck in ((0, zf, RB[1], nch), (1, li, RB[0], 32)):
                for c in range(nck):
                    col = (kind * nch + c) * 4 if d == 1 else (kind * 32 + c) * 4
                    P.op("pe", lambda e, c=c, col=col, src=src: e.transpose(out=ps[bk][0:64, col:col + 4], in_=src[:, c * 64:(c + 1) * 64], identity=identf[0:4, 0:4]),
                         reads=[Rsrc] + CONST, writes=[Rps[bk]], inc=(c == nck - 1))
                    ncols = max(ncols, col + 4)
            dstTS = TSf[:].rearrange("p k c h -> p (k c h)") if d == 0 else TSb[:].rearrange("p c h -> p (c h)")
            P.op("dve", lambda e, dstTS=dstTS, ncols=ncols: e.tensor_copy(out=dstTS[:, 0:ncols], in_=ps[bk][0:64, 0:ncols]), reads=[Rps[bk]], writes=[RTS])
            P.op("dve", lambda e: e.tensor_tensor(out=dexp[:, :, 0:nch], in0=dd[:, 0:nch].unsqueeze(1).broadcast_to([4, 4, nch]),
                                                   in1=identf[0:4, 0:4].unsqueeze(2).broadcast_to([4, 4, nch]), op=ALU.mult),
                 reads=[Rsm] + CONST, writes=[Rsm])
            bk2 = nextbank()
            P.op("pe", lambda e: e.matmul(ps[bk2][:, 0:4 * nch].rearrange("p (h c) -> p h c", h=4), lhsT=onesf[0:4, :], rhs=dexp[:, :, 0:nch], start=True, stop=True),
                 reads=[Rsm] + CONST, writes=[Rps[bk2]])
            DBd = DBf if d == 0 else DBb
            P.op("dve", lambda e, DBd=DBd: e.tensor_copy(out=DBd[:], in_=ps[bk2][:, 0:4 * nch].rearrange("p (h c) -> p h c", h=4)),
                 reads=[Rps[bk2]], writes=[RTS])

    P.barrier()
    out_toks.append(RTS.w)
    if L["UPTO"] == "G":
        for nm, tl in (("D_TSf", TSf), ("D_TSb", TSb), ("D_DBf", DBf), ("D_DBb", DBb)):
            shp = list(tl[:].shape)
            flat = int(np.prod(shp[1:]))
            dd_ = nc.dram_tensor(nm, [shp[0], flat], F32, kind="ExternalOutput").ap()
            dsx = mkds()
            pat = {4: "p a b c -> p (a b c)", 3: "p a b -> p (a b)"}[len(shp)]
            out_toks.append(P.op("sp", lambda e: e.dma_start(out=dd_[:, :], in_=tl[:].rearrange(pat)), reads=[RTS], dsem=dsx))
        return
    with contextlib.ExitStack() as st:
        Cst = sb(st, "Cst", (128, 4, 512), F32)
        nst = sb(st, "nst", (128, 4), F32)
        Cb = sb(st, "Cb", (128, 4, 512), BF16)
        nb = sb(st, "nb", (128, 4), BF16)
        Cb_all = sb(st, "Cb_all", (128, 4, 4, 512), F32)
        nb_all = sb(st, "nb_all", (128, 4, 4), F32)
        RC, RCb, RCall = Res(), Res(), Res()
        qT = sb(st, "qT", (128, 4, T), BF16)
        kT = sb(st, "kT", (128, 4, T), BF16)
        kk = sb(st, "kk", (64, 32, 512), BF16)
        vv = sb(st, "vv", (64, 32, 512), BF16)
        Rq, Rk = Res(), Res()
        dq = [mkds(st) for _ in range(4)]
        kw = [sb(st, "kw%d" % i, (64, 512), BF16) for i in range(2)]
        Rkw = [Res(), Res()]
        St = [sb(st, "St%d" % i, (64, 64), BF16) for i in range(2)]
        RSt = [Res(), Res()]
        hst = [sb(st, "hst%d" % i, (64, 512), F32) for i in range(2)]
        Rhst = [Res(), Res()]
        dhst = [mkds(st), mkds(st)]
        hfl = [sb(st, "hfl%d" % i, (64, 512), F32) for i in range(2)]
        Rhfl = [Res(), Res()]
        dhfl = [mkds(st), mkds(st)]
        osl = [sb(st, "osl%d" % i, (64, 512), BF16) for i in range(2)]
        Rosl = [Res(), Res()]
        dosl = [mkds(st), mkds(st)]
        hmo = [sb(st, "hmo%d" % i, (64, 512), BF16) for i in range(2)]
        Rhmo = [Res(), Res()]
        dhmo = [mkds(st), mkds(st)]
        sm = sb(st, "msm", (64, 8), F32)
        Rms = Res()
        hjunk = sb(st, "hjunk", (64, 512), BF16)
        Rhj = Res()
        hsum = sb(st, "hsum", (64, 512), F32)
        Rhs = Res()
        mg = sb(st, "mgb", (64, D), F32)
        Rmg = Res()
        dmg = mkds(st)
        P.op("sp", lambda e: e.dma_start(out=mg[:], in_=mng[:, :]), writes=[Rmg], dsem=dmg)
        RShf = Res()
        RShm = Res()
        cc = [0]
        pcb = [sb(st, "pcb%d" % i, (128, D), BF16) for i in range(4)]
        Rpcb = [Res() for _ in range(4)]
        dpcb = [mkds(st) for _ in range(4)]
        dpcbo = [mkds(st) for _ in range(4)]
        RSU = Res()
        pc_state = [0]

        def precast_some(n):
            for _ in range(n):
                k = pc_state[0]
                if k >= 2 * NEB:
                    return
                pc_state[0] += 1
                eb, which = k // 2, k % 2
                src, dst = ((uT, S_UT), (pv, S_V))[which]
                i = k % 4
                for hh in range(2):
                    P.op("pool", lambda e: e.dma_start(out=pcb[i][:, hh * 1024:(hh + 1) * 1024], in_=src[eb, :, hh * 1024:(hh + 1) * 1024]),
                         writes=[Rpcb[i]], dsem=dpcb[i])
                P.op("sp", lambda e: e.dma_start(out=dst[eb, :, :], in_=pcb[i][:]), reads=[Rpcb[i]], writes=[RSU], dsem=dpcbo[i])

        def state_update(c, kchunk, vchunk, wk, decay, rd):
            i = cc[0] % 2
            cc[0] += 1
            P.op("act", lambda e: e.activation(out=kw[i][:], in_=kchunk, func=AF.Copy, scale=wk),
                 reads=rd + [RTS], writes=[Rkw[i]])
            bn = nextbank(7, 8)
            for j in range(4):
                P.op("pe", lambda e, j=j: e.matmul(ps[bn][:, j:j + 1], lhsT=kw[i][:, j * 128:(j + 1) * 128], rhs=onesb[0:64, 0:1], start=True, stop=True),
                     reads=[Rkw[i]] + CONST, writes=[Rps[bn]], inc=(j == 3))
            P.op("dve", lambda e: e.scalar_tensor_tensor(out=nst[:], in0=nst[:], scalar=decay, in1=ps[bn][:, 0:4], op0=ALU.mult, op1=ALU.add),
                 reads=[Rps[bn], RTS, RC], writes=[RC])
            for j in range(4):
                bu = nextbank(4, 7)
                P.op("pe", lambda e, j=j, bu=bu: e.matmul(ps[bu][:], lhsT=kw[i][:, j * 128:(j + 1) * 128], rhs=vchunk, start=True, stop=True),
                     reads=[Rkw[i]] + rd, writes=[Rps[bu]])
                P.op("dve", lambda e, j=j, bu=bu: e.scalar_tensor_tensor(out=Cst[:, j, :], in0=Cst[:, j, :], scalar=decay, in1=ps[bu][:], op0=ALU.mult, op1=ALU.add),
                     reads=[Rps[bu], RTS, RC], writes=[RC])

        for h in range(4):
            P.op("sp", lambda e: e.dma_start(out=kk[:], in_=S_kh[:, h * 512:(h + 1) * 512].rearrange("(c p) d -> p c d", p=64)),
                 reads=[BP], writes=[Rk], dsem=dq[2])
            P.op("sp", lambda e: e.dma_start(out=vv[:], in_=S_vh[:, h * 512:(h + 1) * 512].rearrange("(c p) d -> p c d", p=64)),
                 reads=[BP], writes=[Rk], dsem=dq[3])
            P.op("dve", lambda e: e.memset(Cst[:], 0.0), writes=[RC])
            P.op("dve", lambda e: e.memset(nst[:], 0.0), writes=[RC])
            for c in range(63, 31, -1):
                hc = c - 32
                state_update(c, kk[:, hc, :], vv[:, hc, :], TSb[:, c, h:h + 1], DBb[:, h, c:c + 1], [Rk])
                precast_some(1)
            P.op("act", lambda e: e.activation(out=Cb_all[:, h], in_=Cst[:], func=AF.Copy), reads=[RC], writes=[RCall])
            P.op("act", lambda e: e.activation(out=nb_all[:, h], in_=nst[:], func=AF.Copy), reads=[RC], writes=[RCall])
            P.op("sp", lambda e: e.dma_start(out=qT[:], in_=S_qT[h * 512:(h + 1) * 512, :].rearrange("(j p) t -> p j t", p=128)),
                 reads=[BP], writes=[Rq], dsem=dq[0])
            P.op("sp", lambda e: e.dma_start(out=kT[:], in_=S_kT[h * 512:(h + 1) * 512, :].rearrange("(j p) t -> p j t", p=128)),
                 reads=[BP], writes=[Rq], dsem=dq[1])
            P.op("sp", lambda e: e.dma_start(out=kk[:], in_=S_k[:, h * 512:(h + 1) * 512].rearrange("(c p) d -> p c d", p=64)),
                 reads=[BP], writes=[Rk], dsem=dq[2])
            P.op("sp", lambda e: e.dma_start(out=vv[:], in_=S_v[:, h * 512:(h + 1) * 512].rearrange("(c p) d -> p c d", p=64)),
                 reads=[BP], writes=[Rk], dsem=dq[3])
            for d in range(2):
                if d == 0:
                    P.op("dve", lambda e: e.memset(Cst[:], 0.0), writes=[RC])
                    P.op("dve", lambda e: e.memset(nst[:], 0.0), writes=[RC])
                    chunks = range(32)
                else:
                    P.op("act", lambda e: e.activation(out=Cst[:], in_=Cb_all[:, h], func=AF.Copy), reads=[RCall], writes=[RC])
                    P.op("act", lambda e: e.activation(out=nst[:], in_=nb_all[:, h], func=AF.Copy), reads=[RCall], writes=[RC])
                    chunks = range(31, -1, -1)
                for c in chunks:
                    tk = slice(c * 64, (c + 1) * 64)
                    if d == 0:
                        wk, e2, decay, mk = TSf[:, 0, c, h:h + 1], TSf[:, 1, c, h:h + 1], DBf[:, h, c:c + 1], maskf
                    else:
                        wk, e2, decay, mk = TSb[:, c, h:h + 1], TSb[:, 64 + c, h:h + 1], DBb[:, h, c:c + 1], maskb
                    i = cc[0] % 2
                    P.op("act", lambda e, decay=decay: e.activation(out=Cb[:], in_=Cst[:], func=AF.Copy, scale=decay), reads=[RC, RTS], writes=[RCb])
                    P.op("act", lambda e, decay=decay: e.activation(out=nb[:], in_=nst[:], func=AF.Copy, scale=decay), reads=[RC, RTS], writes=[RCb])
                    for j in range(4):
                        P.op("pe", lambda e, j=j, tk=tk: e.matmul(ps[0][0:64, 0:64], lhsT=kT[:, j, tk], rhs=qT[:, j, tk], start=(j == 0), stop=(j == 3)),
                             reads=[Rq], writes=[Rps[0]], inc=(j == 3))
                    P.op("dve", lambda e, wk=wk, mk=mk, i=i: e.scalar_tensor_tensor(out=St[i][:], in0=ps[0][0:64, 0:64], scalar=wk, in1=mk, op0=ALU.mult, op1=ALU.mult),
                         reads=[Rps[0], RTS] + CONST, writes=[RSt[i]])
                    for j in range(4):
                        P.op("pe", lambda e, j=j, tk=tk: e.matmul(ps[1][0:64, :], lhsT=qT[:, j, tk], rhs=Cb[:, j, :], start=(j == 0), stop=False),
                             reads=[Rq, RCb], writes=[Rps[1]], inc=False)
                    P.op("pe", lambda e, i=i, c=c: e.matmul(ps[1][0:64, :], lhsT=St[i][:], rhs=vv[:, c, :], start=False, stop=True),
                         reads=[RSt[i], Rk], writes=[Rps[1]])
                    for j in range(4):
                        P.op("pe", lambda e, j=j, tk=tk: e.matmul(ps[2][0:64, 0:1], lhsT=qT[:, j, tk], rhs=nb[:, j:j + 1], start=(j == 0), stop=False),
                             reads=[Rq, RCb], writes=[Rps[2]], inc=False)
                    P.op("pe", lambda e, i=i: e.matmul(ps[2][0:64, 0:1], lhsT=St[i][:], rhs=onesb[0:64, 0:1], start=False, stop=True),
                         reads=[RSt[i]] + CONST, writes=[Rps[2]])
                    P.op("act", lambda e: e.activation(out=sm[:, 5:6], in_=ps[2][0:64, 0:1], func=AF.Abs), reads=[Rps[2]], writes=[Rms])
                    P.op("dve", lambda e, e2=e2: e.tensor_scalar(out=sm[:, 0:1], in0=sm[:, 5:6], scalar1=e2, scalar2=None, op0=ALU.max),
                         reads=[Rms, RTS], writes=[Rms])
                    P.op("dve", lambda e: e.reciprocal(out=sm[:, 1:2], in_=sm[:, 0:1]), reads=[Rms], writes=[Rms])
                    tok_rows = slice(c * 64, (c + 1) * 64)
                    if d == 0:
                        q = c % 2
                        P.op("dve", lambda e, q=q: e.tensor_scalar(out=hst[q][:], in0=ps[1][0:64, :], scalar1=sm[:, 1:2], scalar2=None, op0=ALU.mult),
                             reads=[Rps[1], Rms], writes=[Rhst[q]])
                        P.op("sp", lambda e, q=q, tok_rows=tok_rows: e.dma_start(out=S_hf[tok_rows, h * 512:(h + 1) * 512], in_=hst[q][:]),
                             reads=[Rhst[q]], writes=[RShf], dsem=dhst[q])
                    else:
                        q = c % 2
                        P.op("sp", lambda e, q=q, tok_rows=tok_rows: e.dma_start(out=hfl[q][:], in_=S_hf[tok_rows, h * 512:(h + 1) * 512]),
                             reads=[RShf], writes=[Rhfl[q]], dsem=dhfl[q])
                        P.op("sp", lambda e, q=q, tok_rows=tok_rows: e.dma_start(out=osl[q][:], in_=S_o[tok_rows, h * 512:(h + 1) * 512]),
                             reads=[BP], writes=[Rosl[q]], dsem=dosl[q])
                        P.op("dve", lambda e, q=q: e.scalar_tensor_tensor(out=hsum[:], in0=ps[1][0:64, :], scalar=sm[:, 1:2], in1=hfl[q][:], op0=ALU.mult, op1=ALU.add),
                             reads=[Rps[1], Rms, Rhfl[q]], writes=[Rhs])
                        P.op("act", lambda e: e.activation(out=hjunk[:], in_=hsum[:], func=AF.Square, accum_out=sm[:, 2:3]), reads=[Rhs], writes=[Rhj, Rms])
                        P.op("act", lambda e: e.activation(out=sm[:, 3:4], in_=sm[:, 2:3], func=AF.Ln, scale=1.0 / 512, bias=EPS), reads=[Rms], writes=[Rms])
                        P.op("act", lambda e: e.activation(out=sm[:, 4:5], in_=sm[:, 3:4], func=AF.Exp, scale=-0.5), reads=[Rms], writes=[Rms])
                        P.op("dve", lambda e: e.scalar_tensor_tensor(out=hsum[:], in0=hsum[:], scalar=sm[:, 4:5], in1=mg[:, h * 512:(h + 1) * 512], op0=ALU.mult, op1=ALU.mult),
                             reads=[Rhs, Rms, Rmg], writes=[Rhs])
                        P.op("pool", lambda e, q=q: e.tensor_tensor(out=hmo[q][:], in0=hsum[:], in1=osl[q][:], op=ALU.mult),
                             reads=[Rhs, Rosl[q]], writes=[Rhmo[q]])
                        P.op("sp", lambda e, q=q, tok_rows=tok_rows: e.dma_start(out=S_hm[tok_rows, h * 512:(h + 1) * 512], in_=hmo[q][:]),
                             reads=[Rhmo[q]], writes=[RShm], dsem=dhmo[q])
                    state_update(c, kk[:, c, :], vv[:, c, :], wk, decay, [Rk])
                    precast_some(1)
        precast_some(2 * NEB)
        L["BU_holder"][0] = barrier_from([RSU] + Rpcb)
        BM = barrier_from([RShm, Rhmo[0], Rhmo[1]])
    P.barrier()
    out_toks.append(BM.w)
    if L["UPTO"] == "M":
        return
    build_tail(nc, P, top, sb, mkds, ps, Rps, nextbank, CONST, BP, BM, L, out_toks)


def build_tail(nc, P, top, sb, mkds, ps, Rps, nextbank, CONST, BP, BM, L, out_toks):
    identb, onesb = L["identb"], L["onesb"]
    g2 = L["g2"]
    S_aqT, S_akT, S_av, S_gmT, S_gaT, S_hm, S_haT, S_x1 = (L[k] for k in "S_aqT S_akT S_av S_gmT S_gaT S_hm S_haT S_x1".split())
    S_UT, S_V, uT, pv, keysT, S_sub, S_xn2 = L["S_UT"], L["S_V"], L["uT"], L["pv"], L["keysT"], L["S_sub"], L["S_xn2"]
    sinkb, btab, x_own, y = L["sinkb"], L["btab"], L["x_own"], L["y"]
    w_mp, w_ap, w_o, w_pq = L["w_mp"], L["w_ap"], L["w_o"], L["w_pq"]
    barrier_from, norm_T = L["barrier_from"], L["norm_T"]

    BU = L["BU_holder"][0]
    with contextlib.ExitStack() as st:
        EB = sb(st, "EB", (128, 16, 3, 128), F32)
        esk = sb(st, "esk", (128, 16), F32)
        REB = Res()
        dEB = mkds(st)
        P.op("sp", lambda e: e.dma_start(out=EB[:].rearrange("p h o q -> p (h o q)"), in_=btab[:, :]), writes=[REB], dsem=dEB)
        P.op("sp", lambda e: e.dma_start(out=esk[:], in_=sinkb[:, :]), writes=[REB], dsem=dEB)
        P.op("act", lambda e: e.activation(out=EB[:].rearrange("p h o q -> p (h o q)"), in_=EB[:].rearrange("p h o q -> p (h o q)"), func=AF.Exp),
             reads=[REB], writes=[REB])
        P.op("act", lambda e: e.activation(out=esk[:], in_=esk[:], func=AF.Exp), reads=[REB], writes=[REB])
        m128 = sb(st, "m128", (128, 2, 128), F32)
        dm = mkds(st)
        P.op("sp", lambda e: e.dma_start(out=m128[:].rearrange("p a q -> p (a q)"), in_=L["cst"][:, 4480:4736]), writes=[REB], dsem=dm)
        for o, a in ((0, 0), (2, 1)):
            P.op("dve", lambda e, o=o, a=a: e.tensor_tensor(out=EB[:, :, o, :], in0=EB[:, :, o, :], in1=m128[:, a, :].unsqueeze(1).broadcast_to([128, 16, 128]), op=ALU.mult),
                 reads=[REB], writes=[REB])
        aqT = sb(st, "aqT", (128, 16, T), BF16)
        akT = sb(st, "akT", (128, 4, T + 128), BF16)
        av = sb(st, "av", (128, 17, 512), BF16)
        Ra = Res()
        da = [mkds(st) for _ in range(3)]
        P.op("sp", lambda e: e.dma_start(out=aqT[:], in_=S_aqT.rearrange("(h p) t -> p h t", p=128)), reads=[BP], writes=[Ra], dsem=da[0])
        P.op("sp", lambda e: e.dma_start(out=akT[:], in_=S_akT.rearrange("(h p) t -> p h t", p=128)), reads=[BP], writes=[Ra], dsem=da[1])
        P.op("sp", lambda e: e.dma_start(out=av[:], in_=S_av.rearrange("(t p) n -> p t n", p=128)), reads=[BP], writes=[Ra], dsem=da[2])
        pex = [sb(st, "pex%d" % i, (128, 512), F32) for i in range(3)]
        Rpex = [Res() for _ in range(3)]
        pT = [sb(st, "pT%d" % i, (128, 512), BF16) for i in range(6)]
        RpT = [Res() for _ in range(6)]
        zt = sb(st, "zt", (128, 512), F32)
        Rz = Res()
        hst = [sb(st, "hag%d" % i, (128, 4, T), BF16) for i in range(2)]
        Rhst = [Res(), Res()]
        dhst = [mkds(st), mkds(st)]
        RSha = Res()
        kk = 0
        for g in range(4):
            for i in range(NT):
                os_ = [o for o in (-1, 0, 1) if i + o >= 0]
                pts = []
                for o in os_:
                    bk = nextbank(0, 4)
                    P.op("pe", lambda e, bk=bk, o=o, i=i: e.matmul(ps[bk][:].rearrange("p (h q) -> p h q", h=4), lhsT=akT[:, g, (i + o) * 128:(i + o + 1) * 128],
                                                                    rhs=aqT[:, 4 * g:4 * g + 4, i * 128:(i + 1) * 128], start=True, stop=True),
                         reads=[Ra], writes=[Rps[bk]])
                    a = kk % 3
                    b = kk % 6
                    kk += 1
                    P.op("act", lambda e, bk=bk, a=a: e.activation(out=pex[a][:], in_=ps[bk][:], func=AF.Exp), reads=[Rps[bk]], writes=[Rpex[a]])
                    P.op("dve", lambda e, a=a, b=b, o=o: e.tensor_tensor(out=pT[b][:].rearrange("p (h q) -> p h q", h=4), in0=pex[a][:].rearrange("p (h q) -> p h q", h=4),
                                                                          in1=EB[:, 4 * g:4 * g + 4, o + 1, :], op=ALU.mult),
                         reads=[Rpex[a], REB], writes=[RpT[b]])
                    pts.append((o, b))
                bo = nextbank(4, 6)
                bz = nextbank(6, 8)
                for n_, (o, b) in enumerate(pts):
                    P.op("pe", lambda e, o=o, b=b, n_=n_, i=i, bo=bo: e.matmul(ps[bo][:], lhsT=av[:, i + o, g * 128:(g + 1) * 128], rhs=pT[b][:], start=(n_ == 0), stop=(n_ == len(pts) - 1)),
                         reads=[Ra, RpT[b]], writes=[Rps[bo]], inc=(n_ == len(pts) - 1))
                for n_, (o, b) in enumerate(pts):
                    P.op("pe", lambda e, b=b, n_=n_, bz=bz: e.matmul(ps[bz][:], lhsT=onesb[:], rhs=pT[b][:], start=(n_ == 0), stop=(n_ == len(pts) - 1)),
                         reads=[RpT[b]] + CONST, writes=[Rps[bz]], inc=(n_ == len(pts) - 1))
                P.op("dve", lambda e, bz=bz: e.tensor_tensor(out=zt[:].rearrange("p (h q) -> p h q", h=4), in0=ps[bz][:].rearrange("p (h q) -> p h q", h=4),
                                                              in1=esk[:, 4 * g:4 * g + 4].unsqueeze(2).broadcast_to([128, 4, 128]), op=ALU.add),
                     reads=[Rps[bz], REB], writes=[Rz])
                P.op("dve", lambda e: e.reciprocal(out=zt[:], in_=zt[:]), reads=[Rz], writes=[Rz])
                P.op("dve", lambda e, bo=bo, i=i: e.tensor_tensor(out=hst[g % 2][:, :, i * 128:(i + 1) * 128], in0=ps[bo][:].rearrange("p (h q) -> p h q", h=4),
                                                                   in1=zt[:].rearrange("p (h q) -> p h q", h=4), op=ALU.mult),
                     reads=[Rps[bo], Rz], writes=[Rhst[g % 2]])
            P.op("sp", lambda e, g=g: e.dma_start(out=S_haT[g * 512:(g + 1) * 512, :].rearrange("(h p) t -> p h t", p=128), in_=hst[g % 2][:]),
                 reads=[Rhst[g % 2]], writes=[RSha], dsem=dhst[g % 2])
        BT = barrier_from([RSha, Rhst[0], Rhst[1]])
    P.barrier()
    out_toks.append(BT.w)
    if L["UPTO"] == "T":
        return

    with contextlib.ExitStack() as st:
        with contextlib.ExitStack() as st2:
            mT = sb(st2, "mT", (128, 16, T), BF16)
            RmT = Res()
            wr = [sb(st2, "owr%d" % i, (128, 16, 512), BF16) for i in range(2)]
            Rwr = [Res(), Res()]
            dwr = [mkds(st2), mkds(st2)]
            wc = [0]

            def load_w(wsrc, c0):
                s = wc[0] % 2
                wc[0] += 1
                P.op("pool", lambda e: e.dma_start(out=wr[s][:], in_=wsrc[:, c0:c0 + 512].rearrange("(c p) n -> p c n", p=128)), writes=[Rwr[s]], dsem=dwr[s])
                return s

            with contextlib.ExitStack() as st3:
                hT = sb(st3, "hT", (128, 16, T), BF16)
                RhT = Res()
                gl = [sb(st3, "gl%d" % i, (128, T), BF16) for i in range(2)]
                Rgl = [Res(), Res()]
                dgl = [mkds(st3), mkds(st3)]
                tmpf = sb(st3, "tmpf", (128, 512), F32)
                Rtf = Res()
                for br in range(2):
                    if br == 0:
                        ht = [sb(st3, "htl%d" % i, (128, D), BF16) for i in range(2)]
                        Rht = [Res(), Res()]
                        dht = [mkds(st3), mkds(st3)]
                        for t in range(NT):
                            b = t % 2
                            P.op("sp", lambda e, b=b, t=t: e.dma_start(out=ht[b][:], in_=S_hm[t * 128:(t + 1) * 128, :]), reads=[BM], writes=[Rht[b]], dsem=dht[b])
                            for gq in range(4):
                                bk = nextbank()
                                ptv = ps[bk][:].bitcast(BF16)[:, 0:512].rearrange("p (j n) -> p j n", j=4)
                                for j in range(4):
                                    c = gq * 4 + j
                                    P.op("pe", lambda e, c=c, j=j, ptv=ptv, b=b: e.transpose(out=ptv[:, j, :], in_=ht[b][:, c * 128:(c + 1) * 128], identity=identb[:]),
                                         reads=[Rht[b]] + CONST, writes=[Rps[bk]], inc=(j == 3))
                                if gq % 2 == 0:
                                    P.op("act", lambda e, gq=gq, t=t, ptv=ptv: e.activation(out=hT[:, gq * 4:(gq + 1) * 4, t * 128:(t + 1) * 128], in_=ptv, func=AF.Copy),
                                         reads=[Rps[bk]], writes=[RhT])
                                else:
                                    P.op("dve", lambda e, gq=gq, t=t, ptv=ptv: e.tensor_copy(out=hT[:, gq * 4:(gq + 1) * 4, t * 128:(t + 1) * 128], in_=ptv),
                                         reads=[Rps[bk]], writes=[RhT])
                        wsrc, gsrc = w_mp, S_gmT
                    else:
                        dhT = mkds(st3)
                        P.op("sp", lambda e: e.dma_start(out=hT[:], in_=S_haT.rearrange("(c p) t -> p c t", p=128)), reads=[BT], writes=[RhT], dsem=dhT)
                        wsrc, gsrc = w_ap, S_gaT
                    for cb_ in range(4):
                        s = load_w(wsrc, cb_ * 512)
                        for j in range(4):
                            jj = cb_ * 4 + j
                            q = jj % 2
                            P.op("sp", lambda e, q=q, jj=jj, gsrc=gsrc: e.dma_start(out=gl[q][:], in_=gsrc[jj * 128:(jj + 1) * 128, :]), reads=[BP], writes=[Rgl[q]], dsem=dgl[q])
                            for tb in range(4):
                                bk = nextbank()
                                for c in range(16):
                                    P.op("pe", lambda e, c=c, j=j, tb=tb, bk=bk, s=s: e.matmul(ps[bk][:], lhsT=wr[s][:, c, j * 128:(j + 1) * 128], rhs=hT[:, c, tb * 512:(tb + 1) * 512],
                                                                                                 start=(c == 0), stop=(c == 15)),
                                         reads=[Rwr[s], RhT], writes=[Rps[bk]], inc=(c == 15))
                                if br == 0:
                                    P.op("dve", lambda e, jj=jj, tb=tb, bk=bk, q=q: e.tensor_tensor(out=mT[:, jj, tb * 512:(tb + 1) * 512], in0=ps[bk][:], in1=gl[q][:, tb * 512:(tb + 1) * 512], op=ALU.mult),
                                         reads=[Rps[bk], Rgl[q]], writes=[RmT])
                                else:
                                    P.op("dve", lambda e, tb=tb, bk=bk, q=q: e.tensor_tensor(out=tmpf[:], in0=ps[bk][:], in1=gl[q][:, tb * 512:(tb + 1) * 512], op=ALU.mult),
                                         reads=[Rps[bk], Rgl[q]], writes=[Rtf])
                                    P.op("pool", lambda e, jj=jj, tb=tb: e.tensor_tensor(out=mT[:, jj, tb * 512:(tb + 1) * 512], in0=mT[:, jj, tb * 512:(tb + 1) * 512], in1=tmpf[:], op=ALU.add),
                                         reads=[Rtf, RmT], writes=[RmT])
            P.barrier()
            xl = [sb(st2, "xl%d" % i, (128, 512), F32) for i in range(2)]
            Rxl = [Res(), Res()]
            dxl = [mkds(st2), mkds(st2)]
            x1s = [sb(st2, "x1s%d" % i, (128, 512), F32) for i in range(2)]
            Rx1s = [Res(), Res()]
            dx1s = [mkds(st2), mkds(st2)]
            RSx1 = Res()
            kx = 0
            for cb_ in range(4):
                s = load_w(w_o, cb_ * 512)
                for t in range(NT):
                    b = kx % 2
                    kx += 1
                    P.op("sp", lambda e, b=b, t=t, cb_=cb_: e.dma_start(out=xl[b][:], in_=x_own[t * 128:(t + 1) * 128, cb_ * 512:(cb_ + 1) * 512]), writes=[Rxl[b]], dsem=dxl[b])
                    bk = nextbank()
                    for c in range(16):
                        P.op("pe", lambda e, c=c, t=t, bk=bk, s=s: e.matmul(ps[bk][:], lhsT=mT[:, c, t * 128:(t + 1) * 128], rhs=wr[s][:, c, :], start=(c == 0), stop=(c == 15)),
                             reads=[RmT, Rwr[s]], writes=[Rps[bk]], inc=(c == 15))
                    P.op("dve", lambda e, b=b, bk=bk: e.tensor_tensor(out=x1s[b][:], in0=ps[bk][:], in1=xl[b][:], op=ALU.add),
                         reads=[Rps[bk], Rxl[b]], writes=[Rx1s[b]])
                    P.op("sp", lambda e, b=b, t=t, cb_=cb_: e.dma_start(out=S_x1[t * 128:(t + 1) * 128, cb_ * 512:(cb_ + 1) * 512], in_=x1s[b][:]),
                         reads=[Rx1s[b]], writes=[RSx1], dsem=dx1s[b])
            BX = barrier_from([RSx1] + Rx1s)
        P.barrier()
        stx = contextlib.ExitStack()
        xn2T = sb(stx, "xn2T", (128, 16, T), BF16)
        Rxn2 = [Res() for _ in range(NT)]
        with contextlib.ExitStack() as st2:
            def get_x1(t, dst, Rd, dsm):
                P.op("sp", lambda e: e.dma_start(out=dst[:], in_=S_x1[t * 128:(t + 1) * 128, :]), reads=[BX], writes=[Rd], dsem=dsm)
            norm_T(st2, get_x1, NT, xn2T, Rxn2, g2, "2")
        P.barrier()

        with contextlib.ExitStack() as stq:
            pqT = sb(stq, "pqT", (128, 16, T), BF16)
            RpqT = Res()
            with contextlib.ExitStack() as st2:
                wr = [sb(st2, "qwr%d" % i, (128, 16, 512), BF16) for i in range(2)]
                Rwr = [Res(), Res()]
                dwr = [mkds(st2), mkds(st2)]
                for cb_ in range(4):
                    s = cb_ % 2
                    P.op("pool", lambda e, s=s, cb_=cb_: e.dma_start(out=wr[s][:], in_=w_pq[:, cb_ * 512:(cb_ + 1) * 512].rearrange("(c p) n -> p c n", p=128)), writes=[Rwr[s]], dsem=dwr[s])
                    for j in range(4):
                        for tb in range(4):
                            bk = nextbank()
                            for c in range(16):
                                P.op("pe", lambda e, c=c, j=j, tb=tb, bk=bk, s=s: e.matmul(ps[bk][:], lhsT=wr[s][:, c, j * 128:(j + 1) * 128], rhs=xn2T[:, c, tb * 512:(tb + 1) * 512], start=(c == 0), stop=(c == 15)),
                                     reads=[Rwr[s]] + Rxn2[tb * 4:tb * 4 + 4], writes=[Rps[bk]], inc=(c == 15))
                            if (j + tb) % 2 == 0:
                                P.op("act", lambda e, j=j, tb=tb, bk=bk, cb_=cb_: e.activation(out=pqT[:, cb_ * 4 + j, tb * 512:(tb + 1) * 512], in_=ps[bk][:], func=AF.Copy), reads=[Rps[bk]], writes=[RpqT])
                            else:
                                P.op("dve", lambda e, j=j, tb=tb, bk=bk, cb_=cb_: e.tensor_copy(out=pqT[:, cb_ * 4 + j, tb * 512:(tb + 1) * 512], in_=ps[bk][:]), reads=[Rps[bk]], writes=[RpqT])
            P.barrier()
            kTb = sb(stq, "kTb", (128, 16, 128), BF16)
            RkT = Res()
            dkT = mkds(stq)
            P.op("pool", lambda e: e.dma_start(out=kTb[:].rearrange("p a n -> p (a n)"), in_=keysT[:, :]), writes=[RkT], dsem=dkT)
            subs = [sb(stq, "subs%d" % i, (128, 16, 128), F32) for i in range(2)]
            Rsubs = [Res(), Res()]
            dsubs = [mkds(stq), mkds(stq)]
            RSsub = Res()
            for t in range(NT):
                tk = slice(t * 128, (t + 1) * 128)
                b = t % 2
                for qd in range(4):
                    bk = nextbank()
                    for r in range(4):
                        hp = qd * 4 + r
                        P.op("pe", lambda e, hp=hp, r=r, bk=bk, tk=tk: e.matmul(ps[bk][:, r * 128:(r + 1) * 128], lhsT=pqT[:, hp, tk], rhs=kTb[:, hp, :], start=True, stop=True),
                             reads=[RpqT, RkT], writes=[Rps[bk]], inc=(r == 3))
                    P.op("act", lambda e, qd=qd, bk=bk, b=b: e.activation(out=subs[b][:, qd * 4:(qd + 1) * 4, :], in_=ps[bk][:].rearrange("p (r n) -> p r n", r=4), func=AF.Copy),
                         reads=[Rps[bk]], writes=[Rsubs[b]])
                P.op("sp", lambda e, b=b, tk=tk: e.dma_start(out=S_sub[tk, :], in_=subs[b][:].rearrange("p a n -> p (a n)")), reads=[Rsubs[b]], writes=[RSsub], dsem=dsubs[b])
            BS = barrier_from([RSsub] + Rsubs)
        RSxn2 = Res()
        dxd = mkds(st)
        for t in range(NT):
            P.op("sp", lambda e: e.dma_start(out=S_xn2[t, :, :].rearrange("p (c n) -> p c n", c=16), in_=xn2T[:, :, t * 128:(t + 1) * 128]), reads=[Rxn2[t]], writes=[RSxn2], dsem=dxd)
        BXN = barrier_from([RSxn2])
        stx.close()
        P.barrier()
        out_toks.append(BS.w)
        out_toks.append(BX.w)
        if L["UPTO"] == "O":
            return
        sub = sb(st, "sub", (128, 16, 128), F32)
        tmp = sb(st, "ptmp", (128, 16, 128), F32)
        sv = sb(st, "psv", (128, 16, 16), F32)
        cand = sb(st, "cand", (128, 8, 256), F32)
        tmp2 = tmp[:].rearrange("p (h two) n -> p h (two n)", two=2)
        c1 = sb(st, "c1", (128, 8, 16), F32)
        dmat = sb(st, "pdm", (128, 8, 16), F32)
        sc = sb(st, "psc", (128, 8, 7), F32)
        dg = sb(st, "pdg", (128, 8, 128), BF16)
        Rdg = Res()
        b3 = sb(st, "b3", (128, 8, 128), F32)
        Rsub, Rsv, Rc1, Rsc, Rb3 = (Res() for _ in range(5))
        REg = [[Res(), Res(), Res()], [Res(), Res(), Res()]]
        RE = REg[0] + REg[1]
        RG = [Res() for _ in range(4)]
        Ebuf = [tmp[:, 8 * i:8 * (i + 1), :] for i in range(2)]
        Gp = [cand[:].rearrange("p h n -> p (h n)").bitcast(BF16)[:, 1024 * i:1024 * (i + 1)] for i in range(4)]
        dsub = mkds(st)
        hraw = [sb(st, "hraw%d" % i, (128, NEB, 128), BF16) for i in range(2)]
        Rh = [Res(), Res()]
        NRING = 4
        ub = [sb(st, "ub%d" % i, (128, 2, 16, 128), BF16) for i in range(NRING)]
        Rub = [Res() for _ in range(NRING)]
        dub = [mkds(st) for _ in range(NRING)]
        vb = [sb(st, "vb%d" % i, (128, 2, D), BF16) for i in range(NRING)]
        Rvb = [Res() for _ in range(NRING)]
        dvb = [mkds(st) for _ in range(NRING)]
        xt2 = [sb(st, "xt2_%d" % i, (128, 16, 128), BF16) for i in range(2)]
        Rxt2 = [Res(), Res()]
        dxt2 = [mkds(st), mkds(st)]
        ut_next = [0]
        v_next = [0]

        def issue_ut(upto):
            while ut_next[0] < min(upto, NT * 64):
                g = ut_next[0]
                ut_next[0] += 1
                p_ = g % 64
                i = g % NRING
                P.op("sp", lambda e: e.dma_start(out=ub[i][:].rearrange("p b c n -> p b (c n)"), in_=S_UT[2 * p_:2 * p_ + 2, :, :].rearrange("b p n -> p b n")),
                     reads=[BU], writes=[Rub[i]], dsem=dub[i])

        def issue_v(upto):
            while v_next[0] < min(upto, NT * 64):
                g = v_next[0]
                v_next[0] += 1
                p_ = g % 64
                i = g % NRING
                P.op("sp", lambda e: e.dma_start(out=vb[i][:], in_=S_V[2 * p_:2 * p_ + 2, :, :].rearrange("b p n -> p b n")), reads=[BU], writes=[Rvb[i]], dsem=dvb[i])

        def load_xt2(tt):
            P.op("sp", lambda e: e.dma_start(out=xt2[tt % 2][:].rearrange("p c n -> p (c n)"), in_=S_xn2[tt, :, :]), reads=[BXN], writes=[Rxt2[tt % 2]], dsem=dxt2[tt % 2])

        MARGIN = 2.0e-3
        Eb4 = sb(st, "Eb4", (128, 4, 128), F32)
        Ea4 = sb(st, "Ea4", (128, 4, 128), F32)
        REab = Res()
        hstg = [sb(st, "hstg%d" % i, (128, 256), BF16) for i in range(2)]
        Rhstg = [Res(), Res()]
        At = [sb(st, "At%d" % i, (128, 128), BF16) for i in range(3)]
        RAt = [Res() for _ in range(3)]
        x1l = [sb(st, "x1l%d" % i, (128, 256), F32) for i in range(1)] * 2
        Rx1l = [Res()] * 2
        dx1l = [mkds(st)] * 2
        yo = [sb(st, "yo%d" % i, (128, 256), F32) for i in range(1)] * 2
        Ryo = [Res()] * 2
        dyo = [mkds(st)] * 2
        sv4 = sv[:].rearrange("p (h two) k -> p h two k", two=2)
        sub4 = sub[:].rearrange("p (h two) n -> p h two n", two=2)
        ke = 0

        hb_bank = {}

        def h_mm(tt, eb):
            g = tt * 64 + eb // 2
            issue_ut(g + NRING)
            iu = g % NRING
            bk = nextbank(4, 6)
            hb_bank[eb] = bk
            for c in range(16):
                P.op("pe", lambda e: e.matmul(ps[bk][:, 0:256].rearrange("p (b n) -> p b n", b=2), lhsT=xt2[tt % 2][:, c, :], rhs=ub[iu][:, :, c, :], start=(c == 0), stop=(c == 15)),
                     reads=[Rub[iu], Rxt2[tt % 2]], writes=[Rps[bk]], inc=(c == 15))

        def h_cp1(tt, eb):
            bk = hb_bank[eb]
            k = (eb // 2) % 2
            P.op("dve", lambda e: e.tensor_copy(out=hstg[k][:], in_=ps[bk][:, 0:256]), reads=[Rps[bk]], writes=[Rhstg[k]])

        def h_tr(tt, eb):
            bk = hb_bank[eb]
            k = (eb // 2) % 2
            tv = ps[bk][:].bitcast(BF16)[:, 512:768]
            for j in range(2):
                P.op("pe", lambda e: e.transpose(out=tv[:, j * 128:(j + 1) * 128], in_=hstg[k][:, j * 128:(j + 1) * 128], identity=identb[:]),
                     reads=[Rhstg[k]] + CONST, writes=[Rps[bk]], inc=(j == 1))

        def h_cp2(tt, eb):
            bk = hb_bank[eb]
            tv = ps[bk][:].bitcast(BF16)[:, 512:768]
            P.op("act", lambda e: e.activation(out=hraw[tt % 2][:, eb:eb + 2, :], in_=tv.rearrange("p (b n) -> p b n", b=2), func=AF.Copy), reads=[Rps[bk]], writes=[Rh[tt % 2]])

        def h_gelu(tt):
            hv = hraw[tt % 2][:].rearrange("p a n -> p (a n)")
            for qq in range(4):
                P.op("act", lambda e: e.activation(out=hv[:, qq * 4096:(qq + 1) * 4096], in_=hv[:, qq * 4096:(qq + 1) * 4096], func=AF.Gelu),
                     reads=[Rh[tt % 2]], writes=[Rh[tt % 2]])

        load_xt2(0)
        for eb in range(0, NEB, 2):
            h_mm(0, eb)
            h_cp1(0, eb)
            h_tr(0, eb)
            h_cp2(0, eb)
        h_gelu(0)
        for t in range(NT):
            tk = slice(t * 128, (t + 1) * 128)
            P.op("sp", lambda e, tk=tk: e.dma_start(out=sub[:].rearrange("p a n -> p (a n)"), in_=S_sub[tk, :]), reads=[BS], writes=[Rsub], dsem=dsub)
            for hp in range(16):
                P.op("dve", lambda e, hp=hp: e.max(out=sv[:, hp, 0:8], in_=sub[:, hp, :]), reads=[Rsub], writes=[Rsv])
                P.op("dve", lambda e, hp=hp: e.match_replace(out=tmp[:, hp, :], in_to_replace=sv[:, hp, 0:8], in_values=sub[:, hp, :], imm_value=NEG), reads=[Rsub, Rsv], writes=RE)
                P.op("dve", lambda e, hp=hp: e.max(out=sv[:, hp, 8:16], in_=tmp[:, hp, :]), reads=RE, writes=[Rsv])
            P.op("dve", lambda e: e.tensor_tensor(out=cand[:].rearrange("p h (a b) -> p h a b", a=16), in0=sv4[:, :, 0, :].unsqueeze(3).broadcast_to([128, 8, 16, 16]),
                                                   in1=sv4[:, :, 1, :].unsqueeze(2).broadcast_to([128, 8, 16, 16]), op=ALU.add), reads=[Rsv], writes=RG)
            for h in range(8):
                P.op("dve", lambda e, h=h: e.max(out=c1[:, h, 0:8], in_=cand[:, h, :]), reads=RG, writes=[Rc1])
                P.op("dve", lambda e, h=h: e.match_replace(out=tmp2[:, h, :], in_to_replace=c1[:, h, 0:8], in_values=cand[:, h, :], imm_value=NEG), reads=RG + [Rc1], writes=RE)
                P.op("dve", lambda e, h=h: e.max(out=c1[:, h, 8:16], in_=tmp2[:, h, :]), reads=RE, writes=[Rc1])
            P.op("dve", lambda e: e.tensor_tensor(out=dmat[:], in0=c1[:], in1=c1[:, :, 0:1].broadcast_to([128, 8, 16]), op=ALU.subtract), reads=[Rc1], writes=[Rsc])
            P.op("act", lambda e: e.activation(out=dmat[:], in_=dmat[:], func=AF.Exp), reads=[Rsc], writes=[Rsc])
            P.op("dve", lambda e: e.tensor_reduce(out=sc[:, :, 0], in_=dmat[:], axis=AX.X, op=ALU.add), reads=[Rsc], writes=[Rsc])
            P.op("act", lambda e: e.activation(out=sc[:, :, 1], in_=sc[:, :, 0], func=AF.Ln), reads=[Rsc], writes=[Rsc])
            P.op("dve", lambda e: e.scalar_tensor_tensor(out=sc[:, :, 2], in0=c1[:, :, 0], scalar=-1.0, in1=sc[:, :, 1], op0=ALU.mult, op1=ALU.subtract), reads=[Rsc, Rc1], writes=[Rsc])
            P.op("dve", lambda e: e.tensor_tensor(out=sc[:, :, 3], in0=c1[:, :, 15], in1=sc[:, :, 2], op=ALU.add), reads=[Rsc, Rc1], writes=[Rsc])
            P.op("act", lambda e: e.activation(out=sc[:, :, 3], in_=sc[:, :, 3], func=AF.Exp, bias=-MARGIN), reads=[Rsc], writes=[Rsc])
            P.op("dve", lambda e: e.tensor_scalar(out=sc[:, :, 4], in0=c1[:, :, 15], scalar1=-1.0, scalar2=MARGIN, op0=ALU.mult, op1=ALU.add), reads=[Rc1, Rsc], writes=[Rsc])
            P.op("dve", lambda e: e.tensor_tensor(out=b3[:], in0=sub4[:, :, 0, :], in1=sc[:, :, 4:5].broadcast_to([128, 8, 128]), op=ALU.add), reads=[Rsub, Rsc], writes=[Rb3])
            for h in range(8):
                P.op("pool", lambda e: e.tensor_scalar(out=dg[:, h, :], in0=identb[:], scalar1=sc[:, h, 3:4], scalar2=1.0, op0=ALU.mult, op1=ALU.mult),
                     reads=[Rsc] + CONST, writes=[Rdg])
            P.op("dve", lambda e: e.tensor_scalar(out=sc[:, :, 5], in0=sv4[:, :, 1, 0], scalar1=-1.0, scalar2=None, op0=ALU.mult), reads=[Rsv, Rsc], writes=[Rsc])
            P.op("dve", lambda e: e.tensor_tensor(out=sc[:, :, 6], in0=sc[:, :, 4], in1=sv4[:, :, 1, 0], op=ALU.add), reads=[Rsv, Rsc], writes=[Rsc])
            for h in range(4, 8):
                P.op("act", lambda e: e.activation(out=Eb4[:, h - 4, :], in_=sub4[:, h, 1, :], func=AF.Exp, bias=sc[:, h, 5:6]), reads=[Rsub, Rsc], writes=[REab])
                P.op("act", lambda e: e.activation(out=Ea4[:, h - 4, :], in_=sub4[:, h, 0, :], func=AF.Exp, bias=sc[:, h, 6:7]), reads=[Rsub, Rsc], writes=[REab])
            gel = hraw[t % 2]
            Rgel = Rh[t % 2]
            pend_out = None
            for eb in range(NEB):
                if eb == 0 and t + 1 < NT:
                    load_xt2(t + 1)
                if eb % 2 == 1 or eb == 0:
                    issue_v(t * 64 + eb // 2 + NRING)
                es = eb % 2
                gs = eb % 4
                for h in range(4):
                    P.op("act", lambda e: e.activation(out=Ebuf[es][:, h, :], in_=sub4[:, h, 1, :], func=AF.Exp, bias=b3[:, h, eb:eb + 1]),
                         reads=[Rsub, Rb3], writes=[REg[es][0]])
                for h in (4, 5):
                    P.op("pool", lambda e: e.tensor_scalar(out=Ebuf[es][:, h, :], in0=Eb4[:, h - 4, :], scalar1=Ea4[:, h - 4, eb:eb + 1], scalar2=1.0, op0=ALU.mult, op1=ALU.mult),
                         reads=[REab], writes=[REg[es][1]])
                for h in (6, 7):
                    P.op("dve", lambda e: e.tensor_scalar(out=Ebuf[es][:, h, :], in0=Eb4[:, h - 4, :], scalar1=Ea4[:, h - 4, eb:eb + 1], scalar2=None, op0=ALU.mult),
                         reads=[REab], writes=[REg[es][2]])
                P.op("dve", lambda e: e.scalar_tensor_tensor(out=Gp[gs], in0=Ebuf[es].rearrange("p h n -> p (h n)"), scalar=1.0, in1=Ebuf[es].rearrange("p h n -> p (h n)"),
                                                              op0=ALU.is_ge, op1=ALU.mult),
                     reads=REg[es], writes=[RG[gs]])
                if t + 1 < NT:
                    if eb % 2 == 0:
                        h_mm(t + 1, eb)
                    else:
                        h_tr(t + 1, eb - 1)
                bg = nextbank(6, 8)
                for h in range(8):
                    P.op("pe", lambda e: e.matmul(ps[bg][:, 0:128], lhsT=Gp[gs][:, h * 128:(h + 1) * 128], rhs=dg[:, h, :], start=(h == 0), stop=(h == 7)),
                         reads=[RG[gs], Rdg], writes=[Rps[bg]], inc=(h == 7))
                if pend_out is not None:
                    pend_out()
                ia = eb % 3
                P.op("dve", lambda e: e.tensor_tensor(out=At[ia][:], in0=ps[bg][:, 0:128], in1=gel[:, eb, :], op=ALU.mult),
                     reads=[Rps[bg], Rgel], writes=[RAt[ia]])
                if t + 1 < NT:
                    if eb % 2 == 0:
                        h_cp1(t + 1, eb)
                    else:
                        h_cp2(t + 1, eb - 1)

                def mk_out(eb=eb, ia=ia, t=t):
                    iv2 = (t * 64 + eb // 2) % NRING
                    for db in range(4):
                        P.op("pe", lambda e: e.matmul(ps[db][:], lhsT=At[ia][:], rhs=vb[iv2][:, eb % 2, db * 512:(db + 1) * 512], start=(eb == 0), stop=(eb == NEB - 1)),
                             reads=[RAt[ia], Rvb[iv2]], writes=[Rps[db]], inc=(db == 3))
                pend_out = mk_out
            pend_out()
            if t + 1 < NT:
                h_gelu(t + 1)
            for d8 in range(8):
                q = 0
                db, hf_ = d8 // 2, d8 % 2
                cs = slice(d8 * 256, (d8 + 1) * 256)
                P.op("sp", lambda e: e.dma_start(out=x1l[q][:], in_=S_x1[tk, cs]), reads=[BX], writes=[Rx1l[q]], dsem=dx1l[q])
                P.op("dve", lambda e: e.tensor_tensor(out=yo[q][:], in0=ps[db][:, hf_ * 256:(hf_ + 1) * 256], in1=x1l[q][:], op=ALU.add),
                     reads=[Rps[db], Rx1l[q]], writes=[Ryo[q]])
                tok = P.op("sp", lambda e: e.dma_start(out=y[tk, cs], in_=yo[q][:]), reads=[Ryo[q]], dsem=dyo[q])
                out_toks.append(tok)


def _t5_bucket_static(rel):
    half, max_exact = 16, 8
    ret = np.where(rel > 0, half, 0)
    n = np.abs(rel)
    nf = np.maximum(n, 1).astype(np.float32)
    large = max_exact + (np.log(nf / max_exact) / math.log(128 / max_exact) * (half - max_exact)).astype(np.int32)
    large = np.minimum(large, half - 1)
    return ret + np.where(n < max_exact, n, large)


def _consts():
    c = np.zeros((128, 384 + 4096 + 256), np.float32)
    c[:, 0:128] = np.eye(128, dtype=np.float32)
    c[:, 128:256] = 1.0
    s = np.arange(64)
    c[0:64, 256:320] = (s[:, None] <= s[None, :]).astype(np.float32)
    c[0:64, 320:384] = (s[:, None] >= s[None, :]).astype(np.float32)
    big = np.ones((128, 4096), np.float32)
    k = np.arange(128)
    m_prev = (k[:, None] >= k[None, :]).astype(np.float32)
    m_next = (k[:, None] <= k[None, :]).astype(np.float32)
    c[:, 384:4480] = big
    return c, m_prev, m_next


_NC_CACHE = {}


def kernel(x, norm1_g, w_in, mlstm_gate_b, mlstm_norm_g, w_m_proj, attn_q_norm_g, attn_k_norm_g,
           attn_sink, rel_bias, w_a_proj, w_out, norm2_g, peer_wq, peer_keys, peer_u, peer_v, _debug=False, _upto=None):
    f = lambda a: np.ascontiguousarray(np.asarray(a, dtype=np.float32))
    x, w_in = f(x), f(w_in)
    cst, m_prev, m_next = _consts()
    rm = np.ones((4, 4096), np.float32)
    rm[:, ::64] = 0.0
    shared = {}
    shared["n1g"] = f(np.asarray(norm1_g).reshape(16, 128).T)
    shared["n2g"] = f(np.asarray(norm2_g).reshape(16, 128).T)
    shared["mng"] = f(np.broadcast_to(np.asarray(mlstm_norm_g).reshape(1, D), (64, D)))
    shared["aqg"] = f(np.asarray(attn_q_norm_g).reshape(128, 1))
    shared["akg"] = f(np.asarray(attn_k_norm_g).reshape(128, 1))
    shared["sinkb"] = f(np.broadcast_to(np.asarray(attn_sink).reshape(1, 16), (128, 16)))
    shared["w_in"] = w_in
    shared["w_mp"] = f(w_m_proj)
    shared["w_ap"] = f(w_a_proj)
    shared["w_o"] = f(w_out)
    shared["w_pq"] = f(peer_wq)
    shared["keysT"] = f(np.asarray(peer_keys).reshape(16, 128, 128).transpose(2, 0, 1).reshape(128, 16 * 128))
    shared["uT"] = f(np.asarray(peer_u).reshape(NEB, 128, 16, 128).transpose(0, 3, 2, 1).reshape(NEB, 128, 16 * 128))
    shared["pv"] = f(np.asarray(peer_v).reshape(NEB, 128, D))
    rb = np.asarray(rel_bias, dtype=np.float32)
    gbias = np.asarray(mlstm_gate_b, dtype=np.float32)
    wg_full = w_in[:, O_MG:O_MG + 16]
    kq = np.arange(128)
    in_maps = []
    for core in range(8):
        b, half = core // 2, core % 2
        xs = x[b]
        if half == 1:
            xs = xs[::-1]
        m = dict(shared)
        m["x_own"] = f(xs[:T])
        m["x_halo"] = f(xs[T:])
        cols = []
        gb = np.zeros((4, 4), np.float32)
        for d in range(2):
            td = d ^ half
            for kind in range(2):
                cols.append(wg_full[:, td * 8 + kind * 4: td * 8 + kind * 4 + 4])
                gb[:, d * 2 + kind] = gbias[td, kind, :]
        m["w_gate"] = f(np.concatenate(cols, axis=1))
        m["gate_b"] = gb
        bt = np.zeros((128, 16, 3, 128), np.float32)
        for o in range(3):
            rel = (kq[:, None] + (o - 1) * 128) - kq[None, :]
            if half == 1:
                rel = -rel
            bk = _t5_bucket_static(rel)
            bt[:, :, o, :] = rb[bk].transpose(0, 2, 1)
        m["btab"] = f(bt.reshape(128, -1))
        c2 = cst.copy()
        c2[:, 4480:4480 + 128] = m_prev
        c2[:, 4480 + 128:4480 + 256] = m_next
        c2[0:4, 384:4480] = rm
        m["cst"] = c2
        in_maps.append(m)
    key = (tuple(_debug) if _debug else None, _upto)
    if key not in _NC_CACHE:
        _NC_CACHE[key] = build_nc(debug=_debug, upto=_upto)
    nc = _NC_CACHE[key]
    in_maps = [{k: m[k] for k in nc._in_names} for m in in_maps]
    res = run_bass_kernel_spmd(nc, in_maps, core_ids=list(range(8)))
    out = np.zeros((4, 4096, D), np.float32)
    for core in range(8):
        b, half = core // 2, core % 2
        yc = res.results[core].get("y", np.zeros((T, D), np.float32)) if _debug else res.results[core]["y"]
        if half == 0:
            out[b, :T] = yc
        else:
            out[b, T:] = yc[::-1]
    if _debug:
        return out, res
    return out
```

```python
import contextlib
import math
import numpy as np
import concourse.bass as bass
import concourse.mybir as mybir
from concourse.bass_utils import run_bass_kernel_spmd

F32 = mybir.dt.float32
BF16 = mybir.dt.bfloat16
AF = mybir.ActivationFunctionType
ALU = mybir.AluOpType
AX = mybir.AxisListType

T = 2048
D = 2048
NT = 16
EPS = 1e-6
NEG = -1.0e30
O_MQ, O_MK, O_MV, O_MO, O_MG, O_AQ, O_AK, O_AV, O_GM, O_GA = (
    0, 2048, 4096, 6144, 8192, 8208, 10256, 10768, 11280, 13328)
NEB = 128


class Res:
    __slots__ = ("w", "r")

    def __init__(self):
        self.w = None
        self.r = []


class DSem:
    def __init__(self, sem):
        self.sem = sem
        self.val = 0


class _Rec:
    def __init__(self):
        self.call = None

    def __getattr__(self, name):
        def f(*a, **k):
            self.call = (name, a, k)
            return self
        return f


class Prog:
    ENGS = ("pe", "dve", "act", "pool", "sp")

    def __init__(self, nc, esems):
        self.nc = nc
        self.esem = esems
        self.cnt = {e: 0 for e in self.ENGS}
        self.ops = {e: [] for e in self.ENGS}
        self.seen = {e: {} for e in self.ENGS}
        self.nops = 0
        self.dsems = []
        self.fence = {e: [] for e in self.ENGS}

    def barrier(self):
        toks = [(self.esem[e], self.cnt[e], "x") for e in self.ENGS if self.cnt[e] > 0]
        toks += [(d.sem, d.val, "dma") for d in self.dsems if d.val > 0]
        for e in self.ENGS:
            self.fence[e] = list(toks)

    def op(self, eng, fn, reads=(), writes=(), dsem=None, inc=True):
        waits = {}

        def add(tok):
            if tok is None:
                return
            s, v, e = tok
            if e == "pe" and eng == "pe" and dsem is None:
                return
            k = id(s)
            if self.seen[eng].get(k, 0) >= v:
                return
            if k not in waits or waits[k][1] < v:
                waits[k] = (s, v)

        for R in reads:
            add(R.w)
        for R in writes:
            add(R.w)
            for t in R.r:
                add(t)
        if self.fence[eng]:
            for t in self.fence[eng]:
                add(t)
            self.fence[eng] = []
        for k, (s, v) in waits.items():
            self.seen[eng][k] = v
        if dsem is None:
            if inc:
                self.cnt[eng] += 1
                tok = (self.esem[eng], self.cnt[eng], eng)
                incs = (self.esem[eng], 1)
            else:
                tok = (self.esem[eng], self.cnt[eng] + 1, eng)
                incs = None
        else:
            dsem.val += 16
            tok = (dsem.sem, dsem.val, "dma")
            incs = (dsem.sem, 16)
        for R in reads:
            R.r.append(tok)
        for R in writes:
            R.w = tok
            R.r = []
        rec = _Rec()
        fn(rec)
        self.ops[eng].append((list(waits.values()), rec.call, incs))
        self.nops += 1
        return tok

    def finish(self, toks):
        waits = [(s, v) for (s, v, _) in toks if s is not None]
        self.ops["sp"].append((waits, ("nop", (), {}), (self.esem["sp"], 1)))

    def emit(self, block):
        engmap = {"pe": "tensor", "dve": "vector", "act": "scalar", "pool": "gpsimd", "sp": "sync"}
        for e in self.ENGS:
            ops = self.ops[e]

            def body(engine, ops=ops):
                for waits, fn, incs in ops:
                    for s, v in waits:
                        engine.wait_ge(s, v)
                    ins = getattr(engine, fn[0])(*fn[1], **fn[2])
                    if incs is not None:
                        ins.then_inc(incs[0], incs[1])

            getattr(block, engmap[e])(body)


def build_nc(debug=False, upto=None):
    nc = bass.Bass("TRN2", target_bir_lowering=False)

    LEVELS = ["A", "P1", "P", "G", "M", "T", "O", None]
    lvl = LEVELS.index(upto)
    in_names = []
    nc._in_names = in_names

    def din(name, shape, need=0):
        if lvl < need:
            return None
        in_names.append(name)
        return nc.dram_tensor(name, list(shape), F32, kind="ExternalInput").ap()

    dbgset = set(debug) if debug else set()

    def dscr(name, shape, dt):
        return nc.dram_tensor(name, list(shape), dt, kind="ExternalOutput" if name in dbgset else "Internal").ap()

    x_own = din("x_own", (T, D))
    x_halo = din("x_halo", (T, D))
    w_in = din("w_in", (D, 15376))
    w_gate = din("w_gate", (D, 16))
    gate_b = din("gate_b", (4, 4))
    n1g = din("n1g", (128, 16))
    n2g = din("n2g", (128, 16))
    mng = din("mng", (64, D), need=4)
    aqg = din("aqg", (128, 1))
    akg = din("akg", (128, 1))
    sinkb = din("sinkb", (128, 16), need=5)
    btab = din("btab", (128, 16 * 3 * 128), need=5)
    cst = din("cst", (128, 384 + 4096 + 256))
    w_mp = din("w_mp", (D, D), need=6)
    w_ap = din("w_ap", (D, D), need=6)
    w_o = din("w_o", (D, D), need=6)
    w_pq = din("w_pq", (D, D), need=6)
    keysT = din("keysT", (128, 16 * 128), need=6)
    uT = din("uT", (NEB, 128, 16 * 128), need=5)
    pv = din("pv", (NEB, 128, D), need=5)
    y = nc.dram_tensor("y", [T, D], F32, kind="ExternalOutput").ap()

    S_qT = dscr("S_qT", (D, T), BF16)
    S_kT = dscr("S_kT", (D, T), BF16)
    S_k = dscr("S_k", (T, D), BF16)
    S_v = dscr("S_v", (T, D), BF16)
    S_o = dscr("S_o", (T, D), BF16)
    S_kh = dscr("S_kh", (T, D), BF16)
    S_vh = dscr("S_vh", (T, D), BF16)
    S_aqT = dscr("S_aqT", (D, T), BF16)
    S_akT = dscr("S_akT", (512, T + 128), BF16)
    S_av = dscr("S_av", (T + 128, 512), BF16)
    S_gmT = dscr("S_gmT", (D, T), BF16)
    S_gaT = dscr("S_gaT", (D, T), BF16)
    S_g = dscr("S_g", (4, 4, 2 * T), F32)
    S_hf = dscr("S_hf", (T, D), F32)
    S_hm = dscr("S_hm", (T, D), BF16)
    S_haT = dscr("S_haT", (D, T), BF16)
    S_x1 = dscr("S_x1", (T, D), F32)
    S_sub = dscr("S_sub", (T, D), F32)
    S_xn2 = dscr("S_xn2", (NT, 128, 16 * 128), BF16)
    S_UT = dscr("S_UT", (NEB, 128, D), BF16)
    S_V = dscr("S_V", (NEB, 128, D), BF16)

    with contextlib.ExitStack() as top:
        E = top.enter_context
        esems = {e: E(nc.semaphore("es_" + e)) for e in Prog.ENGS}
        P = Prog(nc, esems)
        block = E(nc.Block())
        nds = [0]

        def mkds(st=None):
            nds[0] += 1
            d_ = DSem(top.enter_context(nc.semaphore("ds%d" % nds[0])))
            P.dsems.append(d_)
            return d_

        def sb(st, name, shape, dt):
            return st.enter_context(nc.sbuf_tensor(name, list(shape), dt))

        cst_sb = sb(top, "cst_sb", (128, 128 + 128 + 64 + 64), F32)
        identb = sb(top, "identb", (128, 128), BF16)
        onesb = sb(top, "onesb", (128, 128), BF16)
        g1 = sb(top, "g1", (128, 16), F32)
        g2 = sb(top, "g2", (128, 16), F32)
        Rc = Res()
        dc = mkds()
        P.op("sp", lambda e: e.dma_start(out=cst_sb[:], in_=cst[:, 0:384]), writes=[Rc], dsem=dc)
        P.op("sp", lambda e: e.dma_start(out=g1[:], in_=n1g[:, :]), writes=[Rc], dsem=dc)
        P.op("sp", lambda e: e.dma_start(out=g2[:], in_=n2g[:, :]), writes=[Rc], dsem=dc)
        identf = cst_sb[:, 0:128]
        onesf = cst_sb[:, 128:256]
        maskf = cst_sb[0:64, 256:320]
        maskb = cst_sb[0:64, 320:384]
        Rcb = Res()
        P.op("dve", lambda e: e.tensor_copy(out=identb[:], in_=identf), reads=[Rc], writes=[Rcb])
        P.op("dve", lambda e: e.tensor_copy(out=onesb[:], in_=onesf), reads=[Rc], writes=[Rcb])
        CONST = [Rc, Rcb]

        ps = [E(nc.psum_tensor("ps%d" % i, [128, 512], F32)) for i in range(8)]
        Rps = [Res() for _ in range(8)]
        bank_ctr = [0]

        def nextbank(lo=0, hi=8):
            n = hi - lo
            b = lo + bank_ctr[0] % n
            bank_ctr[0] += 1
            return b

        def barrier_from(res_list):
            B = Res()
            toks = []
            for R in res_list:
                toks += R.r
                if R.w is not None:
                    toks.append(R.w)
            waits = [(s, v) for (s, v, _) in toks]
            P.cnt["sp"] += 1
            tok = (P.esem["sp"], P.cnt["sp"], "sp")
            P.ops["sp"].append((waits, ("nop", (), {}), (P.esem["sp"], 1)))
            B.w = tok
            return B

        def norm_T(st, get_tile, ntiles, xnT, RxnT, gt, tag):
            xt = [sb(st, "xt%s%d" % (tag, i), (128, D), F32) for i in range(2)]
            Rxt = [Res(), Res()]
            dxt = [mkds(st), mkds(st)]
            junk = sb(st, "junk" + tag, (128, D), BF16)
            xs = sb(st, "xs" + tag, (128, D), BF16)
            sm = sb(st, "sm" + tag, (128, 4), F32)
            Rj, Rxs, Rsm = Res(), Res(), Res()
            for t in range(ntiles):
                b = t % 2
                get_tile(t, xt[b], Rxt[b], dxt[b])
                P.op("act", lambda e, b=b: e.activation(out=junk[:], in_=xt[b][:], func=AF.Square, accum_out=sm[:, 0:1]),
                     reads=[Rxt[b]], writes=[Rj, Rsm])
                P.op("act", lambda e: e.activation(out=sm[:, 1:2], in_=sm[:, 0:1], func=AF.Ln, scale=1.0 / D, bias=EPS),
                     reads=[Rsm], writes=[Rsm])
                P.op("act", lambda e: e.activation(out=sm[:, 2:3], in_=sm[:, 1:2], func=AF.Exp, scale=-0.5),
                     reads=[Rsm], writes=[Rsm])
                P.op("dve", lambda e, b=b: e.tensor_scalar(out=xs[:], in0=xt[b][:], scalar1=sm[:, 2:3], scalar2=None, op0=ALU.mult),
                     reads=[Rxt[b], Rsm], writes=[Rxs])
                for g in range(4):
                    bk = nextbank()
                    ptv = ps[bk][:].bitcast(BF16)[:, 0:512].rearrange("p (j n) -> p j n", j=4)
                    for j in range(4):
                        c = g * 4 + j
                        P.op("pe", lambda e, c=c, j=j, ptv=ptv: e.transpose(out=ptv[:, j, :], in_=xs[:, c * 128:(c + 1) * 128], identity=identb[:]),
                             reads=[Rxs] + CONST, writes=[Rps[bk]], inc=(j == 3))
                    P.op("dve", lambda e, g=g, t=t, ptv=ptv: e.tensor_tensor(
                        out=xnT[:, g * 4:(g + 1) * 4, t * 128:(t + 1) * 128], in0=ptv,
                        in1=gt[:, g * 4:(g + 1) * 4].unsqueeze(2).broadcast_to([128, 4, 128]), op=ALU.mult),
                        reads=[Rps[bk]] + CONST, writes=[RxnT[t]])

        with contextlib.ExitStack() as st:
            xnT = sb(st, "xnT", (128, 16, T), BF16)
            RxnT = [Res() for _ in range(NT)]
            wr = [sb(st, "wr%d" % i, (128, 16, 512), BF16) for i in range(3)]
            Rwr = [Res() for _ in range(3)]
            dwr = [mkds(st) for _ in range(3)]
            wctr = [0]
            stg = [sb(st, "stg%d" % i, (128, 8192), BF16) for i in range(2)]
            Rstg = [Res(), Res()]
            dstg = [mkds(st), mkds(st)]
            sctr = [0]
            wg = sb(st, "wg", (128, 16, 16), BF16)
            gb = sb(st, "gb", (4, 4), F32)
            gqs = sb(st, "gqs", (128, 2), F32)
            gst = [sb(st, "gst%d" % i, (4, 512), F32) for i in range(2)]
            Rgst = [Res(), Res()]
            dgst = [mkds(st), mkds(st)]
            sqt = sb(st, "sqt", (128, 512), BF16)
            lnv = sb(st, "lnv", (128, 512), F32)
            Rsq, Rln = Res(), Res()
            Rw0 = Res()
            dw0 = mkds(st)
            dw0p = mkds(st)
            P.op("pool", lambda e: e.dma_start(out=wg[:], in_=w_gate.rearrange("(c p) n -> p c n", p=128)), writes=[Rw0], dsem=dw0p)
            P.op("sp", lambda e: e.dma_start(out=gb[:], in_=gate_b[:, :]), writes=[Rw0], dsem=dw0)
            P.op("sp", lambda e: e.dma_start(out=gqs[:, 0:1], in_=aqg[:, :]), writes=[Rw0], dsem=dw0)
            P.op("sp", lambda e: e.dma_start(out=gqs[:, 1:2], in_=akg[:, :]), writes=[Rw0], dsem=dw0)
            P.op("dve", lambda e: e.tensor_scalar(out=gqs[:, 0:1], in0=gqs[:, 0:1], scalar1=128.0 ** -0.5, scalar2=None, op0=ALU.mult),
                 reads=[Rw0], writes=[Rw0])

            def load_w(wsrc, c0, n):
                s = wctr[0] % 3
                wctr[0] += 1
                P.op("pool", lambda e: e.dma_start(out=wr[s][:, :, 0:n], in_=wsrc[:, c0:c0 + n].rearrange("(c p) n -> p c n", p=128)),
                     writes=[Rwr[s]], dsem=dwr[s])
                return s

            def new_stg():
                s = sctr[0] % 2
                sctr[0] += 1
                return s

            def fm_mm(s, j, tok0, ntok, bk):
                rd = [Rwr[s]] + RxnT[tok0 // 128:(tok0 + ntok + 127) // 128]
                for c in range(16):
                    P.op("pe", lambda e, c=c: e.matmul(ps[bk][:, 0:ntok], lhsT=wr[s][:, c, j * 128:(j + 1) * 128],
                                                         rhs=xnT[:, c, tok0:tok0 + ntok], start=(c == 0), stop=(c == 15)),
                         reads=rd, writes=[Rps[bk]], inc=(c == 15))

            def tm_mm(s, n, t, bk):
                rd = [Rwr[s], RxnT[t]]
                for c in range(16):
                    P.op("pe", lambda e, c=c: e.matmul(ps[bk][:, 0:n], lhsT=xnT[:, c, t * 128:(t + 1) * 128],
                                                         rhs=wr[s][:, c, 0:n], start=(c == 0), stop=(c == 15)),
                         reads=rd, writes=[Rps[bk]], inc=(c == 15))

            evctr = [0]

            def evac_copy(dst, bk, n, wres, func=None):
                if func is not None or evctr[0] % 2 == 0:
                    f = func if func is not None else AF.Copy
                    P.op("act", lambda e: e.activation(out=dst, in_=ps[bk][:, 0:n], func=f), reads=[Rps[bk]], writes=wres)
                else:
                    P.op("dve", lambda e: e.tensor_copy(out=dst, in_=ps[bk][:, 0:n]), reads=[Rps[bk]], writes=wres)
                evctr[0] += 1

            def evac_qknorm(dst, bk, n, gcol, wres):
                P.op("act", lambda e: e.activation(out=sqt[:, 0:n], in_=ps[bk][:, 0:n], func=AF.Square), reads=[Rps[bk]], writes=[Rsq])
                b2 = nextbank()
                P.op("pe", lambda e: e.matmul(ps[b2][:, 0:n], lhsT=onesb[:], rhs=sqt[:, 0:n], start=True, stop=True),
                     reads=[Rsq] + CONST, writes=[Rps[b2]])
                P.op("act", lambda e: e.activation(out=lnv[:, 0:n], in_=ps[b2][:, 0:n], func=AF.Ln, scale=1.0 / 128, bias=EPS),
                     reads=[Rps[b2]], writes=[Rln])
                P.op("act", lambda e: e.activation(out=lnv[:, 0:n], in_=lnv[:, 0:n], func=AF.Exp, scale=-0.5), reads=[Rln], writes=[Rln])
                P.op("dve", lambda e: e.scalar_tensor_tensor(out=dst, in0=ps[bk][:, 0:n], scalar=gqs[:, gcol:gcol + 1], in1=lnv[:, 0:n],
                                                              op0=ALU.mult, op1=ALU.mult), reads=[Rps[bk], Rln, Rw0], writes=wres)

            def fm_block(wsrc, c0, ncol, dstT, row0, kind, ntok=T, tokdst0=0):
                s = load_w(wsrc, c0, ncol)
                g = new_stg()
                nsub = ncol // 128
                sv = stg[g][:, 0:nsub * ntok].rearrange("p (j t) -> p j t", j=nsub)
                for j in range(nsub):
                    for tb in range(0, ntok, 512):
                        n = min(512, ntok - tb)
                        bk = nextbank()
                        fm_mm(s, j, tb, n, bk)
                        dst = sv[:, j, tb:tb + n]
                        if kind == "plain":
                            evac_copy(dst, bk, n, [Rstg[g]])
                        elif kind == "sig":
                            evac_copy(dst, bk, n, [Rstg[g]], func=AF.Sigmoid)
                        elif kind == "qn":
                            evac_qknorm(dst, bk, n, 0, [Rstg[g]])
                        elif kind == "kn":
                            evac_qknorm(dst, bk, n, 1, [Rstg[g]])
                P.op("sp", lambda e: e.dma_start(out=dstT[row0:row0 + ncol, tokdst0:tokdst0 + ntok].rearrange("(j p) t -> p j t", p=128), in_=sv),
                     reads=[Rstg[g]], dsem=dstg[g])

            def tm_block(wsrc, c0, ncol, dst, dcol0, kind, ntiles=NT, tokdst0=0):
                s = load_w(wsrc, c0, ncol)
                g = new_stg()
                sv = stg[g][:, 0:ntiles * ncol].rearrange("p (t n) -> p t n", t=ntiles)
                for t in range(ntiles):
                    bk = nextbank()
                    tm_mm(s, ncol, t, bk)
                    evac_copy(sv[:, t, :], bk, ncol, [Rstg[g]], func=(AF.Sigmoid if kind == "sig" else None))
                P.op("sp", lambda e: e.dma_start(out=dst[tokdst0:tokdst0 + ntiles * 128, dcol0:dcol0 + ncol].rearrange("(t p) n -> p t n", p=128), in_=sv),
                     reads=[Rstg[g]], dsem=dstg[g])

            def gate_block(ggs, tokdst0):
                for tb in range(4):
                    for gg in ggs:
                        bk = nextbank()
                        for c in range(16):
                            P.op("pe", lambda e, c=c: e.matmul(ps[bk][0:4, :], lhsT=wg[:, c, gg * 4:(gg + 1) * 4],
                                                                 rhs=xnT[:, c, tb * 512:(tb + 1) * 512], start=(c == 0), stop=(c == 15)),
                                 reads=[Rw0] + RxnT[tb * 4:tb * 4 + 4], writes=[Rps[bk]], inc=(c == 15))
                        q = (tb * 4 + gg) % 2
                        P.op("act", lambda e: e.activation(out=gst[q][:], in_=ps[bk][0:4, :], func=AF.Identity, bias=gb[:, gg:gg + 1]),
                             reads=[Rps[bk], Rw0], writes=[Rgst[q]])
                        P.op("sp", lambda e: e.dma_start(out=S_g[gg, :, tokdst0 + tb * 512:tokdst0 + (tb + 1) * 512], in_=gst[q][:]),
                             reads=[Rgst[q]], dsem=dgst[q])

            def x_tile_loader(xsrc):
                def get(t, dst, Rd, dsm):
                    P.op("sp", lambda e: e.dma_start(out=dst[:], in_=xsrc[t * 128:(t + 1) * 128, :]), writes=[Rd], dsem=dsm)
                return get

            with contextlib.ExitStack() as st2:
                norm_T(st2, x_tile_loader(x_halo), NT, xnT, RxnT, g1, "h")
                if upto == "A":
                    D_xnT = nc.dram_tensor("D_xnT", [128, 16 * T], BF16, kind="ExternalOutput").ap()
                    dsx = mkds()
                    tk_ = P.op("sp", lambda e: e.dma_start(out=D_xnT[:, :], in_=xnT[:].rearrange("p c t -> p (c t)")), reads=RxnT, dsem=dsx)
                    P.finish([tk_])
                    P.emit(block)
                    return nc
                gate_block([2, 3], T)
                for hb in range(4):
                    tm_block(w_in, O_MK + hb * 512, 512, S_kh, hb * 512, "plain")
                    tm_block(w_in, O_MV + hb * 512, 512, S_vh, hb * 512, "plain")
                fm_block(w_in, O_AK, 512, S_akT, 0, "kn", ntok=128, tokdst0=T)
                tm_block(w_in, O_AV, 512, S_av, 0, "plain", ntiles=1, tokdst0=T)
                if upto == "P1":
                    BP = barrier_from([Rstg[0], Rstg[1], Rgst[0], Rgst[1]])
                    P.finish([BP.w])
                    P.emit(block)
                    return nc
                norm_T(st2, x_tile_loader(x_own), NT, xnT, RxnT, g1, "o")
            P.barrier()
            gate_block([0, 1, 2, 3], 0)
            for hb in range(4):
                fm_block(w_in, O_MQ + hb * 512, 512, S_qT, hb * 512, "plain")
                fm_block(w_in, O_MK + hb * 512, 512, S_kT, hb * 512, "plain")
                tm_block(w_in, O_MK + hb * 512, 512, S_k, hb * 512, "plain")
                tm_block(w_in, O_MV + hb * 512, 512, S_v, hb * 512, "plain")
                tm_block(w_in, O_MO + hb * 512, 512, S_o, hb * 512, "sig")
                fm_block(w_in, O_AQ + hb * 512, 512, S_aqT, hb * 512, "qn")
                fm_block(w_in, O_GM + hb * 512, 512, S_gmT, hb * 512, "sig")
                fm_block(w_in, O_GA + hb * 512, 512, S_gaT, hb * 512, "sig")
            fm_block(w_in, O_AK, 512, S_akT, 0, "kn")
            tm_block(w_in, O_AV, 512, S_av, 0, "plain")
            PH_P_DONE = [Rstg[0], Rstg[1], Rgst[0], Rgst[1]]
        P.barrier()

        BP = barrier_from(PH_P_DONE)
        out_toks = [BP.w]
        UPTO = upto
        if upto != "P":
            build_rest(nc, P, top, sb, mkds, ps, Rps, nextbank, CONST, BP, locals(), out_toks)
        P.finish(out_toks)
        P.emit(block)
    return nc


def build_rest(nc, P, top, sb, mkds, ps, Rps, nextbank, CONST, BP, L, out_toks):
    identb, onesb, cst_sb = L["identb"], L["onesb"], L["cst_sb"]
    identf, onesf, maskf, maskb = L["identf"], L["onesf"], L["maskf"], L["maskb"]
    cst, g2 = L["cst"], L["g2"]
    S_g, S_qT, S_kT, S_k, S_v, S_o, S_kh, S_vh = (L[k] for k in "S_g S_qT S_kT S_k S_v S_o S_kh S_vh".split())
    S_hf, S_hm, mng = L["S_hf"], L["S_hm"], L["mng"]
    S_UT, S_V, uT, pv = L["S_UT"], L["S_V"], L["uT"], L["pv"]
    L["BU_holder"] = [None]
    barrier_from = L["barrier_from"]
    norm_T = L["norm_T"]

    TSf = sb(top, "TSf", (64, 2, 32, 4), F32)
    TSb = sb(top, "TSb", (64, 96, 4), F32)
    DBf = sb(top, "DBf", (128, 4, 32), F32)
    DBb = sb(top, "DBb", (128, 4, 64), F32)
    RTS = Res()
    with contextlib.ExitStack() as st:
        B = [sb(st, "gB%d" % i, (4, 4096), F32) for i in range(5)]
        RB = [Res() for _ in range(5)]
        dB = [mkds(st) for _ in range(2)]
        rmask = sb(st, "rmask", (4, 4096), F32)
        Rrm = Res()
        drm = mkds(st)
        P.op("sp", lambda e: e.dma_start(out=rmask[:], in_=cst[0:4, 384:384 + 4096]), writes=[Rrm], dsem=drm)
        amax = sb(st, "amax", (4, 64), F32)
        totc = sb(st, "totc", (4, 64), F32)
        Mtab = sb(st, "Mtab", (4, 65), F32)
        MC = sb(st, "MC", (4, 64), F32)
        dd = sb(st, "dd", (4, 64), F32)
        dexp = sb(st, "dexp", (4, 4, 64), F32)
        Rsm = Res()
        LN_S = math.log(512.0 ** -0.5)
        for d in range(2):
            Ltok = T if d == 0 else 2 * T
            nch = Ltok // 64
            li, zf, cum, cx, a = (B[i][:, 0:Ltok] for i in range(5))
            v3 = lambda ap: ap.rearrange("p (c l) -> p c l", l=64)
            P.op("sp", lambda e: e.dma_start(out=li, in_=S_g[d * 2, :, 0:Ltok]), reads=[BP], writes=[RB[0]], dsem=dB[0])
            P.op("sp", lambda e: e.dma_start(out=zf, in_=S_g[d * 2 + 1, :, 0:Ltok]), reads=[BP], writes=[RB[1]], dsem=dB[1])
            P.op("act", lambda e: e.activation(out=zf, in_=zf, func=AF.Exp, scale=-1.0), reads=[RB[1]], writes=[RB[1]])
            P.op("act", lambda e: e.activation(out=zf, in_=zf, func=AF.Ln, bias=1.0), reads=[RB[1]], writes=[RB[1]])
            P.op("dve", lambda e: e.tensor_tensor_scan(out=cum, data0=rmask[:, 0:Ltok], data1=zf, initial=0.0, op0=ALU.mult, op1=ALU.add),
                 reads=[RB[1], Rrm], writes=[RB[2]])
            tot3 = v3(cum)[:, :, 63:64]
            if d == 1:
                P.op("dve", lambda e: e.tensor_tensor(out=cx, in0=zf, in1=cum, op=ALU.subtract), reads=[RB[1], RB[2]], writes=[RB[3]])
                P.op("dve", lambda e: e.tensor_tensor(out=v3(cx), in0=v3(cx), in1=tot3.broadcast_to([4, nch, 64]), op=ALU.add),
                     reads=[RB[3], RB[2]], writes=[RB[3]])
                cxx, Rcx = cx, RB[3]
            else:
                cxx, Rcx = cum, RB[2]
            P.op("dve", lambda e: e.tensor_tensor(out=a, in0=li, in1=cxx, op=ALU.add), reads=[RB[0], Rcx], writes=[RB[4]])
            P.op("dve", lambda e: e.tensor_reduce(out=amax[:, 0:nch], in_=v3(a), axis=AX.X, op=ALU.max), reads=[RB[4]], writes=[Rsm])
            P.op("dve", lambda e: e.tensor_copy(out=totc[:, 0:nch], in_=tot3.rearrange("p c l -> p (c l)")), reads=[RB[2]], writes=[Rsm])
            if d == 0:
                P.op("dve", lambda e: e.memset(Mtab[:, 0:1], 0.0), writes=[Rsm])
                order = [(c, c, c + 1) for c in range(nch)]
            else:
                P.op("dve", lambda e: e.memset(Mtab[:, nch:nch + 1], 0.0), writes=[Rsm])
                order = [(c, c + 1, c) for c in range(nch - 1, -1, -1)]
            for (c, ip, inx) in order:
                P.op("dve", lambda e, c=c, ip=ip: e.tensor_tensor(out=MC[:, c:c + 1], in0=Mtab[:, ip:ip + 1], in1=amax[:, c:c + 1], op=ALU.max),
                     reads=[Rsm], writes=[Rsm])
                P.op("dve", lambda e, c=c, inx=inx: e.tensor_tensor(out=Mtab[:, inx:inx + 1], in0=MC[:, c:c + 1], in1=totc[:, c:c + 1], op=ALU.subtract),
                     reads=[Rsm], writes=[Rsm])
            mprev = Mtab[:, 0:nch] if d == 0 else Mtab[:, 1:nch + 1]
            P.op("dve", lambda e: e.tensor_tensor(out=dd[:, 0:nch], in0=mprev, in1=MC[:, 0:nch], op=ALU.subtract), reads=[Rsm], writes=[Rsm])
            P.op("act", lambda e: e.activation(out=dd[:, 0:nch], in_=dd[:, 0:nch], func=AF.Exp), reads=[Rsm], writes=[Rsm])
            mcb = MC[:, 0:nch].unsqueeze(2).broadcast_to([4, nch, 64])
            P.op("dve", lambda e: e.tensor_tensor(out=v3(zf), in0=v3(a), in1=mcb, op=ALU.subtract), reads=[RB[4], Rsm, RB[1]], writes=[RB[1]])
            P.op("act", lambda e: e.activation(out=zf, in_=zf, func=AF.Exp, bias=LN_S), reads=[RB[1]], writes=[RB[1]])
            P.op("dve", lambda e: e.tensor_tensor(out=v3(li), in0=v3(cxx), in1=mcb, op=ALU.subtract), reads=[Rcx, Rsm, RB[0]], writes=[RB[0]])
            P.op("act", lambda e: e.activation(out=li, in_=li, func=AF.Exp), reads=[RB[0]], writes=[RB[0]])
            bk = nextbank()
            ncols = 0
            for kind, src, Rsrc, nck in ((0, zf, RB[1], nch), (1, li, RB[0], 32)):
                for c in range(nck):
                    col = (kind * nch + c) * 4 if d == 1 else (kind * 32 + c) * 4
                    P.op("pe", lambda e, c=c, col=col, src=src: e.transpose(out=ps[bk][0:64, col:col + 4], in_=src[:, c * 64:(c + 1) * 64], identity=identf[0:4, 0:4]),
                         reads=[Rsrc] + CONST, writes=[Rps[bk]], inc=(c == nck - 1))
                    ncols = max(ncols, col + 4)
            dstTS = TSf[:].rearrange("p k c h -> p (k c h)") if d == 0 else TSb[:].rearrange("p c h -> p (c h)")
            P.op("dve", lambda e, dstTS=dstTS, ncols=ncols: e.tensor_copy(out=dstTS[:, 0:ncols], in_=ps[bk][0:64, 0:ncols]), reads=[Rps[bk]], writes=[RTS])
            P.op("dve", lambda e: e.tensor_tensor(out=dexp[:, :, 0:nch], in0=dd[:, 0:nch].unsqueeze(1).broadcast_to([4, 4, nch]),
                                                   in1=identf[0:4, 0:4].unsqueeze(2).broadcast_to([4, 4, nch]), op=ALU.mult),
                 reads=[Rsm] + CONST, writes=[Rsm])
            bk2 = nextbank()
            P.op("pe", lambda e: e.matmul(ps[bk2][:, 0:4 * nch].rearrange("p (h c) -> p h c", h=4), lhsT=onesf[0:4, :], rhs=dexp[:, :, 0:nch], start=True, stop=True),
                 reads=[Rsm] + CONST, writes=[Rps[bk2]])
            DBd = DBf if d == 0 else DBb
            P.op("dve", lambda e, DBd=DBd: e.tensor_copy(out=DBd[:], in_=ps[bk2][:, 0:4 * nch].rearrange("p (h c) -> p h c", h=4)),
                 reads=[Rps[bk2]], writes=[RTS])

    P.barrier()
    out_toks.append(RTS.w)
    if L["UPTO"] == "G":
        for nm, tl in (("D_TSf", TSf), ("D_TSb", TSb), ("D_DBf", DBf), ("D_DBb", DBb)):
            shp = list(tl[:].shape)
            flat = int(np.prod(shp[1:]))
            dd_ = nc.dram_tensor(nm, [shp[0], flat], F32, kind="ExternalOutput").ap()
            dsx = mkds()
            pat = {4: "p a b c -> p (a b c)", 3: "p a b -> p (a b)"}[len(shp)]
            out_toks.append(P.op("sp", lambda e: e.dma_start(out=dd_[:, :], in_=tl[:].rearrange(pat)), reads=[RTS], dsem=dsx))
        return
    sthm = contextlib.ExitStack()
    Cb_all = sb(sthm, "Cb_all", (128, 4, 4, 512), F32)
    nb_all = sb(sthm, "nb_all", (128, 4, 4), F32)
    RCall = Res()
    pcb = [sb(sthm, "pcb%d" % i, (128, D), BF16) for i in range(4)]
    Rpcb = [Res() for _ in range(4)]
    dpcb = [mkds() for _ in range(4)]
    dpcbo = [mkds() for _ in range(4)]
    RSU = Res()
    pc_state = [0]

    def precast_some(n):
        for _ in range(n):
            k = pc_state[0]
            if k >= 2 * NEB:
                return
            pc_state[0] += 1
            eb, which = k // 2, k % 2
            src, dst = ((uT, S_UT), (pv, S_V))[which]
            i = k % 4
            for hh in range(2):
                P.op("pool", lambda e: e.dma_start(out=pcb[i][:, hh * 1024:(hh + 1) * 1024], in_=src[eb, :, hh * 1024:(hh + 1) * 1024]),
                     writes=[Rpcb[i]], dsem=dpcb[i])
            P.op("sp", lambda e: e.dma_start(out=dst[eb, :, :], in_=pcb[i][:]), reads=[Rpcb[i]], writes=[RSU], dsem=dpcbo[i])

    def make_chain(st, tag, sbank, nbank, light=False):
        ch = dict(
            Cst=sb(st, "Cst" + tag, (128, 4, 512), F32), nst=sb(st, "nst" + tag, (128, 4), F32),
            RC=Res(), RCb=Res(),
            kw=[sb(st, "kw%s%d" % (tag, i), (64, 512), BF16) for i in range(2)], Rkw=[Res(), Res()],
            RSt=[Res(), Res()], Rms=Res(), cc=[0], sbank=sbank, nbank=nbank)
        if not light:
            ch.update(Cb=sb(st, "Cb" + tag, (128, 4, 512), BF16), nb=sb(st, "nb" + tag, (128, 4), BF16),
                      St=[sb(st, "St%s%d" % (tag, i), (64, 64), BF16) for i in range(2)],
                      sm=sb(st, "msm" + tag, (64, 8), F32))
        return ch

    def state_update(ch, kchunk, vchunk, wk, decay, rd):
        i = ch["cc"][0] % 2
        ch["cc"][0] += 1
        kw, Rkw, Cst, nst, RC = ch["kw"], ch["Rkw"], ch["Cst"], ch["nst"], ch["RC"]
        P.op("act", lambda e: e.activation(out=kw[i][:], in_=kchunk, func=AF.Copy, scale=wk), reads=rd + [RTS], writes=[Rkw[i]])
        bn = 7
        for j in range(4):
            P.op("pe", lambda e: e.matmul(ps[bn][:, j:j + 1], lhsT=kw[i][:, j * 128:(j + 1) * 128], rhs=onesb[0:64, 0:1], start=True, stop=True),
                 reads=[Rkw[i]] + CONST, writes=[Rps[bn]], inc=(j == 3))
        P.op("dve", lambda e: e.scalar_tensor_tensor(out=nst[:], in0=nst[:], scalar=decay, in1=ps[bn][:, 0:4], op0=ALU.mult, op1=ALU.add),
             reads=[Rps[bn], RTS, RC], writes=[RC])
        for j in range(4):
            bu = nextbank(4, 7)
            P.op("pe", lambda e: e.matmul(ps[bu][:], lhsT=kw[i][:, j * 128:(j + 1) * 128], rhs=vchunk, start=True, stop=True),
                 reads=[Rkw[i]] + rd, writes=[Rps[bu]])
            P.op("dve", lambda e: e.scalar_tensor_tensor(out=Cst[:, j, :], in0=Cst[:, j, :], scalar=decay, in1=ps[bu][:], op0=ALU.mult, op1=ALU.add),
                 reads=[Rps[bu], RTS, RC], writes=[RC])

    with contextlib.ExitStack() as st:
        chs = [make_chain(st, "h%d" % i, 0, 0, light=True) for i in range(2)]
        kkh = [sb(st, "kkh%d" % i, (64, 32, 512), BF16) for i in range(2)]
        vvh = [sb(st, "vvh%d" % i, (64, 32, 512), BF16) for i in range(2)]
        Rkh = [Res(), Res()]
        dkh = [mkds() for _ in range(4)]
        for hp in range(2):
            for i in range(2):
                h = hp * 2 + i
                P.op("sp", lambda e: e.dma_start(out=kkh[i][:], in_=S_kh[:, h * 512:(h + 1) * 512].rearrange("(c p) d -> p c d", p=64)),
                     reads=[BP], writes=[Rkh[i]], dsem=dkh[2 * i])
                P.op("sp", lambda e: e.dma_start(out=vvh[i][:], in_=S_vh[:, h * 512:(h + 1) * 512].rearrange("(c p) d -> p c d", p=64)),
                     reads=[BP], writes=[Rkh[i]], dsem=dkh[2 * i + 1])
                P.op("dve", lambda e: e.memset(chs[i]["Cst"][:], 0.0), writes=[chs[i]["RC"]])
                P.op("dve", lambda e: e.memset(chs[i]["nst"][:], 0.0), writes=[chs[i]["RC"]])
            for c in range(63, 31, -1):
                hc = c - 32
                for i in range(2):
                    h = hp * 2 + i
                    state_update(chs[i], kkh[i][:, hc, :], vvh[i][:, hc, :], TSb[:, c, h:h + 1], DBb[:, h, c:c + 1], [Rkh[i]])
                precast_some(2)
            for i in range(2):
                h = hp * 2 + i
                P.op("act", lambda e: e.activation(out=Cb_all[:, h], in_=chs[i]["Cst"][:], func=AF.Copy), reads=[chs[i]["RC"]], writes=[RCall])
                P.op("act", lambda e: e.activation(out=nb_all[:, h], in_=chs[i]["nst"][:], func=AF.Copy), reads=[chs[i]["RC"]], writes=[RCall])
    P.barrier()

    with contextlib.ExitStack() as st:
        chs = [make_chain(st, "m0", 0, 1), make_chain(st, "m1", 2, 3)]
        qT = sb(st, "qT", (128, 4, T), BF16)
        kT = sb(st, "kT", (128, 4, T), BF16)
        kk = sb(st, "kk", (64, 32, 512), BF16)
        vv = sb(st, "vv", (64, 32, 512), BF16)
        Rq, Rk = Res(), Res()
        dq = [mkds() for _ in range(4)]
        hst = [sb(st, "hst%d" % i, (64, 512), F32) for i in range(2)]
        Rhst = [Res(), Res()]
        dhst = [mkds(), mkds()]
        hfl = [sb(st, "hfl%d" % i, (64, 512), F32) for i in range(4)]
        Rhfl = [Res() for _ in range(4)]
        dhfl = [mkds() for _ in range(4)]
        osl = [sb(st, "osl%d" % i, (64, 512), BF16) for i in range(4)]
        Rosl = [Res() for _ in range(4)]
        dosl = [mkds() for _ in range(4)]

        def prefetch_fin(h, d, c, slot):
            q = d * 2 + slot
            tk = slice(c * 64, (c + 1) * 64)
            P.op("sp", lambda e: e.dma_start(out=hfl[q][:], in_=S_hf[tk, h * 512:(h + 1) * 512]), reads=[RShf[c]], writes=[Rhfl[q]], dsem=dhfl[q])
            P.op("sp", lambda e: e.dma_start(out=osl[q][:], in_=S_o[tk, h * 512:(h + 1) * 512]), reads=[BP], writes=[Rosl[q]], dsem=dosl[q])
        hmo = [sb(st, "hmo%d" % i, (64, 512), BF16) for i in range(2)]
        Rhmo = [Res(), Res()]
        dhmo = [mkds(), mkds()]
        hjunk = sb(st, "hjunk", (64, 512), BF16)
        Rhj = Res()
        hsum = [sb(st, "hsum%d" % i, (64, 512), F32) for i in range(2)]
        Rhs = [Res(), Res()]
        mg = sb(st, "mgb", (64, 512), F32)
        Rmg = Res()
        dmg = mkds()
        RShf = [Res() for _ in range(32)]
        RShm = Res()

        def chunk_step(h, d, c, second, slot=0):
            ch = chs[d]
            Cst, nst, Cb, nb, RC, RCb, St, RSt, sm, Rms = (ch[k] for k in "Cst nst Cb nb RC RCb St RSt sm Rms".split())
            b_s, b_n = ch["sbank"], ch["nbank"]
            tk = slice(c * 64, (c + 1) * 64)
            if d == 0:
                wk, e2, decay, mk = TSf[:, 0, c, h:h + 1], TSf[:, 1, c, h:h + 1], DBf[:, h, c:c + 1], maskf
            else:
                wk, e2, decay, mk = TSb[:, c, h:h + 1], TSb[:, 64 + c, h:h + 1], DBb[:, h, c:c + 1], maskb
            i = ch["cc"][0] % 2
            P.op("act", lambda e: e.activation(out=Cb[:], in_=Cst[:], func=AF.Copy, scale=decay), reads=[RC, RTS], writes=[RCb])
            P.op("act", lambda e: e.activation(out=nb[:], in_=nst[:], func=AF.Copy, scale=decay), reads=[RC, RTS], writes=[RCb])
            for j in range(4):
                P.op("pe", lambda e: e.matmul(ps[b_s][0:64, 0:64], lhsT=kT[:, j, tk], rhs=qT[:, j, tk], start=(j == 0), stop=(j == 3)),
                     reads=[Rq], writes=[Rps[b_s]], inc=(j == 3))
            P.op("dve", lambda e: e.scalar_tensor_tensor(out=St[i][:], in0=ps[b_s][0:64, 0:64], scalar=wk, in1=mk, op0=ALU.mult, op1=ALU.mult),
                 reads=[Rps[b_s], RTS] + CONST, writes=[RSt[i]])
            for j in range(4):
                P.op("pe", lambda e: e.matmul(ps[b_n][0:64, :], lhsT=qT[:, j, tk], rhs=Cb[:, j, :], start=(j == 0), stop=False),
                     reads=[Rq, RCb], writes=[Rps[b_n]], inc=False)
            P.op("pe", lambda e: e.matmul(ps[b_n][0:64, :], lhsT=St[i][:], rhs=vv[:, c, :], start=False, stop=True),
                 reads=[RSt[i], Rk], writes=[Rps[b_n]])
            for j in range(4):
                P.op("pe", lambda e: e.matmul(ps[b_s][0:64, 64:65], lhsT=qT[:, j, tk], rhs=nb[:, j:j + 1], start=(j == 0), stop=False),
                     reads=[Rq, RCb], writes=[Rps[b_s]], inc=False)
            P.op("pe", lambda e: e.matmul(ps[b_s][0:64, 64:65], lhsT=St[i][:], rhs=onesb[0:64, 0:1], start=False, stop=True),
                 reads=[RSt[i]] + CONST, writes=[Rps[b_s]])
            P.op("act", lambda e: e.activation(out=sm[:, 5:6], in_=ps[b_s][0:64, 64:65], func=AF.Abs), reads=[Rps[b_s]], writes=[Rms])
            P.op("dve", lambda e: e.tensor_scalar(out=sm[:, 0:1], in0=sm[:, 5:6], scalar1=e2, scalar2=None, op0=ALU.max), reads=[Rms, RTS], writes=[Rms])
            P.op("dve", lambda e: e.reciprocal(out=sm[:, 1:2], in_=sm[:, 0:1]), reads=[Rms], writes=[Rms])
            q = d
            if not second:
                P.op("dve", lambda e: e.tensor_scalar(out=hst[q][:], in0=ps[b_n][0:64, :], scalar1=sm[:, 1:2], scalar2=None, op0=ALU.mult),
                     reads=[Rps[b_n], Rms], writes=[Rhst[q]])
                P.op("sp", lambda e: e.dma_start(out=S_hf[tk, h * 512:(h + 1) * 512], in_=hst[q][:]), reads=[Rhst[q]], writes=[RShf[c]], dsem=dhst[q])
            else:
                q4 = d * 2 + slot
                P.op("dve", lambda e: e.scalar_tensor_tensor(out=hsum[q][:], in0=ps[b_n][0:64, :], scalar=sm[:, 1:2], in1=hfl[q4][:], op0=ALU.mult, op1=ALU.add),
                     reads=[Rps[b_n], Rms, Rhfl[q4]], writes=[Rhs[q]])
                P.op("act", lambda e: e.activation(out=hjunk[:], in_=hsum[q][:], func=AF.Square, accum_out=sm[:, 2:3]), reads=[Rhs[q]], writes=[Rhj, Rms])
                P.op("act", lambda e: e.activation(out=sm[:, 3:4], in_=sm[:, 2:3], func=AF.Ln, scale=1.0 / 512, bias=EPS), reads=[Rms], writes=[Rms])
                P.op("act", lambda e: e.activation(out=sm[:, 4:5], in_=sm[:, 3:4], func=AF.Exp, scale=-0.5), reads=[Rms], writes=[Rms])
                P.op("dve", lambda e: e.scalar_tensor_tensor(out=hsum[q][:], in0=hsum[q][:], scalar=sm[:, 4:5], in1=mg[:], op0=ALU.mult, op1=ALU.mult),
                     reads=[Rhs[q], Rms, Rmg], writes=[Rhs[q]])
                P.op("pool", lambda e: e.tensor_tensor(out=hmo[q][:], in0=hsum[q][:], in1=osl[q4][:], op=ALU.mult),
                     reads=[Rhs[q], Rosl[q4]], writes=[Rhmo[q]])
                P.op("sp", lambda e: e.dma_start(out=S_hm[tk, h * 512:(h + 1) * 512], in_=hmo[q][:]), reads=[Rhmo[q]], writes=[RShm], dsem=dhmo[q])
            state_update(ch, kk[:, c, :], vv[:, c, :], wk, decay, [Rk])

        for h in range(4):
            P.op("sp", lambda e: e.dma_start(out=qT[:], in_=S_qT[h * 512:(h + 1) * 512, :].rearrange("(j p) t -> p j t", p=128)),
                 reads=[BP], writes=[Rq], dsem=dq[0])
            P.op("sp", lambda e: e.dma_start(out=kT[:], in_=S_kT[h * 512:(h + 1) * 512, :].rearrange("(j p) t -> p j t", p=128)),
                 reads=[BP], writes=[Rq], dsem=dq[1])
            P.op("sp", lambda e: e.dma_start(out=kk[:], in_=S_k[:, h * 512:(h + 1) * 512].rearrange("(c p) d -> p c d", p=64)),
                 reads=[BP], writes=[Rk], dsem=dq[2])
            P.op("sp", lambda e: e.dma_start(out=vv[:], in_=S_v[:, h * 512:(h + 1) * 512].rearrange("(c p) d -> p c d", p=64)),
                 reads=[BP], writes=[Rk], dsem=dq[3])
            P.op("sp", lambda e: e.dma_start(out=mg[:], in_=mng[:, h * 512:(h + 1) * 512]), writes=[Rmg], dsem=dmg)
            P.op("dve", lambda e: e.memset(chs[0]["Cst"][:], 0.0), writes=[chs[0]["RC"]])
            P.op("dve", lambda e: e.memset(chs[0]["nst"][:], 0.0), writes=[chs[0]["RC"]])
            P.op("act", lambda e: e.activation(out=chs[1]["Cst"][:], in_=Cb_all[:, h], func=AF.Copy), reads=[RCall], writes=[chs[1]["RC"]])
            P.op("act", lambda e: e.activation(out=chs[1]["nst"][:], in_=nb_all[:, h], func=AF.Copy), reads=[RCall], writes=[chs[1]["RC"]])
            for s_ in range(32):
                chunk_step(h, 0, s_, s_ >= 16, s_ % 2)
                chunk_step(h, 1, 31 - s_, s_ >= 16, s_ % 2)
                if 15 <= s_ < 31:
                    prefetch_fin(h, 0, s_ + 1, (s_ + 1) % 2)
                    prefetch_fin(h, 1, 31 - (s_ + 1), (s_ + 1) % 2)
                precast_some(1)
        precast_some(2 * NEB)
        L["BU_holder"][0] = barrier_from([RSU] + Rpcb)
        BM = barrier_from([RShm, Rhmo[0], Rhmo[1]])
    sthm.close()
    P.barrier()
    out_toks.append(BM.w)
    if L["UPTO"] == "M":
        return
    build_tail(nc, P, top, sb, mkds, ps, Rps, nextbank, CONST, BP, BM, L, out_toks)


def build_tail(nc, P, top, sb, mkds, ps, Rps, nextbank, CONST, BP, BM, L, out_toks):
    identb, onesb = L["identb"], L["onesb"]
    g2 = L["g2"]
    S_aqT, S_akT, S_av, S_gmT, S_gaT, S_hm, S_haT, S_x1 = (L[k] for k in "S_aqT S_akT S_av S_gmT S_gaT S_hm S_haT S_x1".split())
    S_UT, S_V, uT, pv, keysT, S_sub, S_xn2 = L["S_UT"], L["S_V"], L["uT"], L["pv"], L["keysT"], L["S_sub"], L["S_xn2"]
    sinkb, btab, x_own, y = L["sinkb"], L["btab"], L["x_own"], L["y"]
    w_mp, w_ap, w_o, w_pq = L["w_mp"], L["w_ap"], L["w_o"], L["w_pq"]
    barrier_from, norm_T = L["barrier_from"], L["norm_T"]

    BU = L["BU_holder"][0]
    with contextlib.ExitStack() as st:
        EB = sb(st, "EB", (128, 16, 3, 128), F32)
        esk = sb(st, "esk", (128, 16), F32)
        REB = Res()
        dEB = mkds(st)
        P.op("sp", lambda e: e.dma_start(out=EB[:].rearrange("p h o q -> p (h o q)"), in_=btab[:, :]), writes=[REB], dsem=dEB)
        P.op("sp", lambda e: e.dma_start(out=esk[:], in_=sinkb[:, :]), writes=[REB], dsem=dEB)
        P.op("act", lambda e: e.activation(out=EB[:].rearrange("p h o q -> p (h o q)"), in_=EB[:].rearrange("p h o q -> p (h o q)"), func=AF.Exp),
             reads=[REB], writes=[REB])
        P.op("act", lambda e: e.activation(out=esk[:], in_=esk[:], func=AF.Exp), reads=[REB], writes=[REB])
        m128 = sb(st, "m128", (128, 2, 128), F32)
        dm = mkds(st)
        P.op("sp", lambda e: e.dma_start(out=m128[:].rearrange("p a q -> p (a q)"), in_=L["cst"][:, 4480:4736]), writes=[REB], dsem=dm)
        for o, a in ((0, 0), (2, 1)):
            P.op("dve", lambda e, o=o, a=a: e.tensor_tensor(out=EB[:, :, o, :], in0=EB[:, :, o, :], in1=m128[:, a, :].unsqueeze(1).broadcast_to([128, 16, 128]), op=ALU.mult),
                 reads=[REB], writes=[REB])
        aqT = sb(st, "aqT", (128, 16, T), BF16)
        akT = sb(st, "akT", (128, 4, T + 128), BF16)
        av = sb(st, "av", (128, 17, 512), BF16)
        Ra = Res()
        da = [mkds(st) for _ in range(3)]
        P.op("sp", lambda e: e.dma_start(out=aqT[:], in_=S_aqT.rearrange("(h p) t -> p h t", p=128)), reads=[BP], writes=[Ra], dsem=da[0])
        P.op("sp", lambda e: e.dma_start(out=akT[:], in_=S_akT.rearrange("(h p) t -> p h t", p=128)), reads=[BP], writes=[Ra], dsem=da[1])
        P.op("sp", lambda e: e.dma_start(out=av[:], in_=S_av.rearrange("(t p) n -> p t n", p=128)), reads=[BP], writes=[Ra], dsem=da[2])
        pex = [sb(st, "pex%d" % i, (128, 512), F32) for i in range(3)]
        Rpex = [Res() for _ in range(3)]
        pT = [sb(st, "pT%d" % i, (128, 512), BF16) for i in range(6)]
        RpT = [Res() for _ in range(6)]
        zt = sb(st, "zt", (128, 512), F32)
        Rz = Res()
        hst = [sb(st, "hag%d" % i, (128, 4, T), BF16) for i in range(2)]
        Rhst = [Res(), Res()]
        dhst = [mkds(st), mkds(st)]
        RSha = Res()
        kk = 0
        for g in range(4):
            for i in range(NT):
                os_ = [o for o in (-1, 0, 1) if i + o >= 0]
                pts = []
                for o in os_:
                    bk = nextbank(0, 4)
                    P.op("pe", lambda e, bk=bk, o=o, i=i: e.matmul(ps[bk][:].rearrange("p (h q) -> p h q", h=4), lhsT=akT[:, g, (i + o) * 128:(i + o + 1) * 128],
                                                                    rhs=aqT[:, 4 * g:4 * g + 4, i * 128:(i + 1) * 128], start=True, stop=True),
                         reads=[Ra], writes=[Rps[bk]])
                    a = kk % 3
                    b = kk % 6
                    kk += 1
                    P.op("act", lambda e, bk=bk, a=a: e.activation(out=pex[a][:], in_=ps[bk][:], func=AF.Exp), reads=[Rps[bk]], writes=[Rpex[a]])
                    P.op("dve", lambda e, a=a, b=b, o=o: e.tensor_tensor(out=pT[b][:].rearrange("p (h q) -> p h q", h=4), in0=pex[a][:].rearrange("p (h q) -> p h q", h=4),
                                                                          in1=EB[:, 4 * g:4 * g + 4, o + 1, :], op=ALU.mult),
                         reads=[Rpex[a], REB], writes=[RpT[b]])
                    pts.append((o, b))
                bo = nextbank(4, 6)
                bz = nextbank(6, 8)
                for n_, (o, b) in enumerate(pts):
                    P.op("pe", lambda e, o=o, b=b, n_=n_, i=i, bo=bo: e.matmul(ps[bo][:], lhsT=av[:, i + o, g * 128:(g + 1) * 128], rhs=pT[b][:], start=(n_ == 0), stop=(n_ == len(pts) - 1)),
                         reads=[Ra, RpT[b]], writes=[Rps[bo]], inc=(n_ == len(pts) - 1))
                for n_, (o, b) in enumerate(pts):
                    P.op("pe", lambda e, b=b, n_=n_, bz=bz: e.matmul(ps[bz][:], lhsT=onesb[:], rhs=pT[b][:], start=(n_ == 0), stop=(n_ == len(pts) - 1)),
                         reads=[RpT[b]] + CONST, writes=[Rps[bz]], inc=(n_ == len(pts) - 1))
                P.op("dve", lambda e, bz=bz: e.tensor_tensor(out=zt[:].rearrange("p (h q) -> p h q", h=4), in0=ps[bz][:].rearrange("p (h q) -> p h q", h=4),
                                                              in1=esk[:, 4 * g:4 * g + 4].unsqueeze(2).broadcast_to([128, 4, 128]), op=ALU.add),
                     reads=[Rps[bz], REB], writes=[Rz])
                P.op("dve", lambda e: e.reciprocal(out=zt[:], in_=zt[:]), reads=[Rz], writes=[Rz])
                P.op("dve", lambda e, bo=bo, i=i: e.tensor_tensor(out=hst[g % 2][:, :, i * 128:(i + 1) * 128], in0=ps[bo][:].rearrange("p (h q) -> p h q", h=4),
                                                                   in1=zt[:].rearrange("p (h q) -> p h q", h=4), op=ALU.mult),
                     reads=[Rps[bo], Rz], writes=[Rhst[g % 2]])
            P.op("sp", lambda e, g=g: e.dma_start(out=S_haT[g * 512:(g + 1) * 512, :].rearrange("(h p) t -> p h t", p=128), in_=hst[g % 2][:]),
                 reads=[Rhst[g % 2]], writes=[RSha], dsem=dhst[g % 2])
        BT = barrier_from([RSha, Rhst[0], Rhst[1]])
    P.barrier()
    out_toks.append(BT.w)
    if L["UPTO"] == "T":
        return

    with contextlib.ExitStack() as st:
        with contextlib.ExitStack() as st2:
            mT = sb(st2, "mT", (128, 16, T), BF16)
            RmT = Res()
            wr = [sb(st2, "owr%d" % i, (128, 16, 512), BF16) for i in range(2)]
            Rwr = [Res(), Res()]
            dwr = [mkds(st2), mkds(st2)]
            wc = [0]

            def load_w(wsrc, c0):
                s = wc[0] % 2
                wc[0] += 1
                P.op("pool", lambda e: e.dma_start(out=wr[s][:], in_=wsrc[:, c0:c0 + 512].rearrange("(c p) n -> p c n", p=128)), writes=[Rwr[s]], dsem=dwr[s])
                return s

            with contextlib.ExitStack() as st3:
                hT = sb(st3, "hT", (128, 16, T), BF16)
                RhT = Res()
                gl = [sb(st3, "gl%d" % i, (128, T), BF16) for i in range(2)]
                Rgl = [Res(), Res()]
                dgl = [mkds(st3), mkds(st3)]
                tmpf = sb(st3, "tmpf", (128, 512), F32)
                Rtf = Res()
                for br in range(2):
                    if br == 0:
                        ht = [sb(st3, "htl%d" % i, (128, D), BF16) for i in range(2)]
                        Rht = [Res(), Res()]
                        dht = [mkds(st3), mkds(st3)]
                        for t in range(NT):
                            b = t % 2
                            P.op("sp", lambda e, b=b, t=t: e.dma_start(out=ht[b][:], in_=S_hm[t * 128:(t + 1) * 128, :]), reads=[BM], writes=[Rht[b]], dsem=dht[b])
                            for gq in range(4):
                                bk = nextbank()
                                ptv = ps[bk][:].bitcast(BF16)[:, 0:512].rearrange("p (j n) -> p j n", j=4)
                                for j in range(4):
                                    c = gq * 4 + j
                                    P.op("pe", lambda e, c=c, j=j, ptv=ptv, b=b: e.transpose(out=ptv[:, j, :], in_=ht[b][:, c * 128:(c + 1) * 128], identity=identb[:]),
                                         reads=[Rht[b]] + CONST, writes=[Rps[bk]], inc=(j == 3))
                                if gq % 2 == 0:
                                    P.op("act", lambda e, gq=gq, t=t, ptv=ptv: e.activation(out=hT[:, gq * 4:(gq + 1) * 4, t * 128:(t + 1) * 128], in_=ptv, func=AF.Copy),
                                         reads=[Rps[bk]], writes=[RhT])
                                else:
                                    P.op("dve", lambda e, gq=gq, t=t, ptv=ptv: e.tensor_copy(out=hT[:, gq * 4:(gq + 1) * 4, t * 128:(t + 1) * 128], in_=ptv),
                                         reads=[Rps[bk]], writes=[RhT])
                        wsrc, gsrc = w_mp, S_gmT
                    else:
                        dhT = mkds(st3)
                        P.op("sp", lambda e: e.dma_start(out=hT[:], in_=S_haT.rearrange("(c p) t -> p c t", p=128)), reads=[BT], writes=[RhT], dsem=dhT)
                        wsrc, gsrc = w_ap, S_gaT
                    for cb_ in range(4):
                        s = load_w(wsrc, cb_ * 512)
                        for j in range(4):
                            jj = cb_ * 4 + j
                            q = jj % 2
                            P.op("sp", lambda e, q=q, jj=jj, gsrc=gsrc: e.dma_start(out=gl[q][:], in_=gsrc[jj * 128:(jj + 1) * 128, :]), reads=[BP], writes=[Rgl[q]], dsem=dgl[q])
                            for tb in range(4):
                                bk = nextbank()
                                for c in range(16):
                                    P.op("pe", lambda e, c=c, j=j, tb=tb, bk=bk, s=s: e.matmul(ps[bk][:], lhsT=wr[s][:, c, j * 128:(j + 1) * 128], rhs=hT[:, c, tb * 512:(tb + 1) * 512],
                                                                                                 start=(c == 0), stop=(c == 15)),
                                         reads=[Rwr[s], RhT], writes=[Rps[bk]], inc=(c == 15))
                                if br == 0:
                                    P.op("dve", lambda e, jj=jj, tb=tb, bk=bk, q=q: e.tensor_tensor(out=mT[:, jj, tb * 512:(tb + 1) * 512], in0=ps[bk][:], in1=gl[q][:, tb * 512:(tb + 1) * 512], op=ALU.mult),
                                         reads=[Rps[bk], Rgl[q]], writes=[RmT])
                                else:
                                    P.op("dve", lambda e, tb=tb, bk=bk, q=q: e.tensor_tensor(out=tmpf[:], in0=ps[bk][:], in1=gl[q][:, tb * 512:(tb + 1) * 512], op=ALU.mult),
                                         reads=[Rps[bk], Rgl[q]], writes=[Rtf])
                                    P.op("pool", lambda e, jj=jj, tb=tb: e.tensor_tensor(out=mT[:, jj, tb * 512:(tb + 1) * 512], in0=mT[:, jj, tb * 512:(tb + 1) * 512], in1=tmpf[:], op=ALU.add),
                                         reads=[Rtf, RmT], writes=[RmT])
            P.barrier()
            xl = [sb(st2, "xl%d" % i, (128, 512), F32) for i in range(2)]
            Rxl = [Res(), Res()]
            dxl = [mkds(st2), mkds(st2)]
            x1s = [sb(st2, "x1s%d" % i, (128, 512), F32) for i in range(2)]
            Rx1s = [Res(), Res()]
            dx1s = [mkds(st2), mkds(st2)]
            RSx1 = Res()
            kx = 0
            for cb_ in range(4):
                s = load_w(w_o, cb_ * 512)
                for t in range(NT):
                    b = kx % 2
                    kx += 1
                    P.op("sp", lambda e, b=b, t=t, cb_=cb_: e.dma_start(out=xl[b][:], in_=x_own[t * 128:(t + 1) * 128, cb_ * 512:(cb_ + 1) * 512]), writes=[Rxl[b]], dsem=dxl[b])
                    bk = nextbank()
                    for c in range(16):
                        P.op("pe", lambda e, c=c, t=t, bk=bk, s=s: e.matmul(ps[bk][:], lhsT=mT[:, c, t * 128:(t + 1) * 128], rhs=wr[s][:, c, :], start=(c == 0), stop=(c == 15)),
                             reads=[RmT, Rwr[s]], writes=[Rps[bk]], inc=(c == 15))
                    P.op("dve", lambda e, b=b, bk=bk: e.tensor_tensor(out=x1s[b][:], in0=ps[bk][:], in1=xl[b][:], op=ALU.add),
                         reads=[Rps[bk], Rxl[b]], writes=[Rx1s[b]])
                    P.op("sp", lambda e, b=b, t=t, cb_=cb_: e.dma_start(out=S_x1[t * 128:(t + 1) * 128, cb_ * 512:(cb_ + 1) * 512], in_=x1s[b][:]),
                         reads=[Rx1s[b]], writes=[RSx1], dsem=dx1s[b])
            BX = barrier_from([RSx1] + Rx1s)
        P.barrier()
        stx = contextlib.ExitStack()
        xn2T = sb(stx, "xn2T", (128, 16, T), BF16)
        Rxn2 = [Res() for _ in range(NT)]
        with contextlib.ExitStack() as st2:
            def get_x1(t, dst, Rd, dsm):
                P.op("sp", lambda e: e.dma_start(out=dst[:], in_=S_x1[t * 128:(t + 1) * 128, :]), reads=[BX], writes=[Rd], dsem=dsm)
            norm_T(st2, get_x1, NT, xn2T, Rxn2, g2, "2")
        P.barrier()

        with contextlib.ExitStack() as stq:
            pqT = sb(stq, "pqT", (128, 16, T), BF16)
            RpqT = Res()
            with contextlib.ExitStack() as st2:
                wr = [sb(st2, "qwr%d" % i, (128, 16, 512), BF16) for i in range(2)]
                Rwr = [Res(), Res()]
                dwr = [mkds(st2), mkds(st2)]
                for cb_ in range(4):
                    s = cb_ % 2
                    P.op("pool", lambda e, s=s, cb_=cb_: e.dma_start(out=wr[s][:], in_=w_pq[:, cb_ * 512:(cb_ + 1) * 512].rearrange("(c p) n -> p c n", p=128)), writes=[Rwr[s]], dsem=dwr[s])
                    for j in range(4):
                        for tb in range(4):
                            bk = nextbank()
                            for c in range(16):
                                P.op("pe", lambda e, c=c, j=j, tb=tb, bk=bk, s=s: e.matmul(ps[bk][:], lhsT=wr[s][:, c, j * 128:(j + 1) * 128], rhs=xn2T[:, c, tb * 512:(tb + 1) * 512], start=(c == 0), stop=(c == 15)),
                                     reads=[Rwr[s]] + Rxn2[tb * 4:tb * 4 + 4], writes=[Rps[bk]], inc=(c == 15))
                            if (j + tb) % 2 == 0:
                                P.op("act", lambda e, j=j, tb=tb, bk=bk, cb_=cb_: e.activation(out=pqT[:, cb_ * 4 + j, tb * 512:(tb + 1) * 512], in_=ps[bk][:], func=AF.Copy), reads=[Rps[bk]], writes=[RpqT])
                            else:
                                P.op("dve", lambda e, j=j, tb=tb, bk=bk, cb_=cb_: e.tensor_copy(out=pqT[:, cb_ * 4 + j, tb * 512:(tb + 1) * 512], in_=ps[bk][:]), reads=[Rps[bk]], writes=[RpqT])
            P.barrier()
            kTb = sb(stq, "kTb", (128, 16, 128), BF16)
            RkT = Res()
            dkT = mkds(stq)
            P.op("pool", lambda e: e.dma_start(out=kTb[:].rearrange("p a n -> p (a n)"), in_=keysT[:, :]), writes=[RkT], dsem=dkT)
            subs = [sb(stq, "subs%d" % i, (128, 16, 128), F32) for i in range(2)]
            Rsubs = [Res(), Res()]
            dsubs = [mkds(stq), mkds(stq)]
            RSsub = Res()
            for t in range(NT):
                tk = slice(t * 128, (t + 1) * 128)
                b = t % 2
                for qd in range(4):
                    bk = nextbank()
                    for r in range(4):
                        hp = qd * 4 + r
                        P.op("pe", lambda e, hp=hp, r=r, bk=bk, tk=tk: e.matmul(ps[bk][:, r * 128:(r + 1) * 128], lhsT=pqT[:, hp, tk], rhs=kTb[:, hp, :], start=True, stop=True),
                             reads=[RpqT, RkT], writes=[Rps[bk]], inc=(r == 3))
                    P.op("act", lambda e, qd=qd, bk=bk, b=b: e.activation(out=subs[b][:, qd * 4:(qd + 1) * 4, :], in_=ps[bk][:].rearrange("p (r n) -> p r n", r=4), func=AF.Copy),
                         reads=[Rps[bk]], writes=[Rsubs[b]])
                P.op("sp", lambda e, b=b, tk=tk: e.dma_start(out=S_sub[tk, :], in_=subs[b][:].rearrange("p a n -> p (a n)")), reads=[Rsubs[b]], writes=[RSsub], dsem=dsubs[b])
            BS = barrier_from([RSsub] + Rsubs)
        RSxn2 = Res()
        dxd = mkds(st)
        for t in range(NT):
            P.op("sp", lambda e: e.dma_start(out=S_xn2[t, :, :].rearrange("p (c n) -> p c n", c=16), in_=xn2T[:, :, t * 128:(t + 1) * 128]), reads=[Rxn2[t]], writes=[RSxn2], dsem=dxd)
        BXN = barrier_from([RSxn2])
        stx.close()
        P.barrier()
        out_toks.append(BS.w)
        out_toks.append(BX.w)
        if L["UPTO"] == "O":
            return
        sub = sb(st, "sub", (128, 16, 128), F32)
        tmp = sb(st, "ptmp", (128, 16, 128), F32)
        sv = sb(st, "psv", (128, 16, 16), F32)
        cand = sb(st, "cand", (128, 8, 256), F32)
        tmp2 = tmp[:].rearrange("p (h two) n -> p h (two n)", two=2)
        c1 = sb(st, "c1", (128, 8, 16), F32)
        dmat = sb(st, "pdm", (128, 8, 16), F32)
        sc = sb(st, "psc", (128, 8, 7), F32)
        dg = sb(st, "pdg", (128, 8, 128), BF16)
        Rdg = Res()
        b3 = sb(st, "b3", (128, 8, 128), F32)
        Rsub, Rsv, Rc1, Rsc, Rb3 = (Res() for _ in range(5))
        REg = [[Res(), Res(), Res()], [Res(), Res(), Res()]]
        RE = REg[0] + REg[1]
        RG = [Res() for _ in range(4)]
        Ebuf = [tmp[:, 8 * i:8 * (i + 1), :] for i in range(2)]
        Gp = [cand[:].rearrange("p h n -> p (h n)").bitcast(BF16)[:, 1024 * i:1024 * (i + 1)] for i in range(4)]
        dsub = mkds(st)
        hraw = [sb(st, "hraw%d" % i, (128, NEB, 128), BF16) for i in range(2)]
        Rh = [Res(), Res()]
        NRING = 4
        ub = [sb(st, "ub%d" % i, (128, 2, 16, 128), BF16) for i in range(NRING)]
        Rub = [Res() for _ in range(NRING)]
        dub = [mkds(st) for _ in range(NRING)]
        vb = [sb(st, "vb%d" % i, (128, 2, D), BF16) for i in range(NRING)]
        Rvb = [Res() for _ in range(NRING)]
        dvb = [mkds(st) for _ in range(NRING)]
        xt2 = [sb(st, "xt2_%d" % i, (128, 16, 128), BF16) for i in range(2)]
        Rxt2 = [Res(), Res()]
        dxt2 = [mkds(st), mkds(st)]
        ut_next = [0]
        v_next = [0]

        def issue_ut(upto):
            while ut_next[0] < min(upto, NT * 64):
                g = ut_next[0]
                ut_next[0] += 1
                p_ = g % 64
                i = g % NRING
                P.op("sp", lambda e: e.dma_start(out=ub[i][:].rearrange("p b c n -> p b (c n)"), in_=S_UT[2 * p_:2 * p_ + 2, :, :].rearrange("b p n -> p b n")),
                     reads=[BU], writes=[Rub[i]], dsem=dub[i])

        def issue_v(upto):
            while v_next[0] < min(upto, NT * 64):
                g = v_next[0]
                v_next[0] += 1
                p_ = g % 64
                i = g % NRING
                P.op("sp", lambda e: e.dma_start(out=vb[i][:], in_=S_V[2 * p_:2 * p_ + 2, :, :].rearrange("b p n -> p b n")), reads=[BU], writes=[Rvb[i]], dsem=dvb[i])

        def load_xt2(tt):
            P.op("sp", lambda e: e.dma_start(out=xt2[tt % 2][:].rearrange("p c n -> p (c n)"), in_=S_xn2[tt, :, :]), reads=[BXN], writes=[Rxt2[tt % 2]], dsem=dxt2[tt % 2])

        MARGIN = 2.0e-3
        Eb4 = sb(st, "Eb4", (128, 4, 128), F32)
        Ea4 = sb(st, "Ea4", (128, 4, 128), F32)
        REab = Res()
        hstg = [sb(st, "hstg%d" % i, (128, 256), BF16) for i in range(2)]
        Rhstg = [Res(), Res()]
        At = [sb(st, "At%d" % i, (128, 128), BF16) for i in range(3)]
        RAt = [Res() for _ in range(3)]
        x1l = [sb(st, "x1l%d" % i, (128, 256), F32) for i in range(1)] * 2
        Rx1l = [Res()] * 2
        dx1l = [mkds(st)] * 2
        yo = [sb(st, "yo%d" % i, (128, 256), F32) for i in range(1)] * 2
        Ryo = [Res()] * 2
        dyo = [mkds(st)] * 2
        sv4 = sv[:].rearrange("p (h two) k -> p h two k", two=2)
        sub4 = sub[:].rearrange("p (h two) n -> p h two n", two=2)
        ke = 0

        hb_bank = {}

        def h_mm(tt, eb):
            g = tt * 64 + eb // 2
            issue_ut(g + NRING)
            iu = g % NRING
            bk = nextbank(4, 6)
            hb_bank[eb] = bk
            for c in range(16):
                P.op("pe", lambda e: e.matmul(ps[bk][:, 0:256].rearrange("p (b n) -> p b n", b=2), lhsT=xt2[tt % 2][:, c, :], rhs=ub[iu][:, :, c, :], start=(c == 0), stop=(c == 15)),
                     reads=[Rub[iu], Rxt2[tt % 2]], writes=[Rps[bk]], inc=(c == 15))

        def h_cp1(tt, eb):
            bk = hb_bank[eb]
            k = (eb // 2) % 2
            P.op("dve", lambda e: e.tensor_copy(out=hstg[k][:], in_=ps[bk][:, 0:256]), reads=[Rps[bk]], writes=[Rhstg[k]])

        def h_tr(tt, eb):
            bk = hb_bank[eb]
            k = (eb // 2) % 2
            tv = ps[bk][:].bitcast(BF16)[:, 512:768]
            for j in range(2):
                P.op("pe", lambda e: e.transpose(out=tv[:, j * 128:(j + 1) * 128], in_=hstg[k][:, j * 128:(j + 1) * 128], identity=identb[:]),
                     reads=[Rhstg[k]] + CONST, writes=[Rps[bk]], inc=(j == 1))

        def h_cp2(tt, eb):
            bk = hb_bank[eb]
            tv = ps[bk][:].bitcast(BF16)[:, 512:768]
            P.op("act", lambda e: e.activation(out=hraw[tt % 2][:, eb:eb + 2, :], in_=tv.rearrange("p (b n) -> p b n", b=2), func=AF.Copy), reads=[Rps[bk]], writes=[Rh[tt % 2]])

        def h_gelu(tt):
            hv = hraw[tt % 2][:].rearrange("p a n -> p (a n)")
            for qq in range(4):
                P.op("act", lambda e: e.activation(out=hv[:, qq * 4096:(qq + 1) * 4096], in_=hv[:, qq * 4096:(qq + 1) * 4096], func=AF.Gelu),
                     reads=[Rh[tt % 2]], writes=[Rh[tt % 2]])

        load_xt2(0)
        for eb in range(0, NEB, 2):
            h_mm(0, eb)
            h_cp1(0, eb)
            h_tr(0, eb)
            h_cp2(0, eb)
        h_gelu(0)
        for t in range(NT):
            tk = slice(t * 128, (t + 1) * 128)
            P.op("sp", lambda e, tk=tk: e.dma_start(out=sub[:].rearrange("p a n -> p (a n)"), in_=S_sub[tk, :]), reads=[BS], writes=[Rsub], dsem=dsub)
            for hp in range(16):
                P.op("dve", lambda e, hp=hp: e.max(out=sv[:, hp, 0:8], in_=sub[:, hp, :]), reads=[Rsub], writes=[Rsv])
                P.op("dve", lambda e, hp=hp: e.match_replace(out=tmp[:, hp, :], in_to_replace=sv[:, hp, 0:8], in_values=sub[:, hp, :], imm_value=NEG), reads=[Rsub, Rsv], writes=RE)
                P.op("dve", lambda e, hp=hp: e.max(out=sv[:, hp, 8:16], in_=tmp[:, hp, :]), reads=RE, writes=[Rsv])
            P.op("dve", lambda e: e.tensor_tensor(out=cand[:].rearrange("p h (a b) -> p h a b", a=16), in0=sv4[:, :, 0, :].unsqueeze(3).broadcast_to([128, 8, 16, 16]),
                                                   in1=sv4[:, :, 1, :].unsqueeze(2).broadcast_to([128, 8, 16, 16]), op=ALU.add), reads=[Rsv], writes=RG)
            for h in range(8):
                P.op("dve", lambda e, h=h: e.max(out=c1[:, h, 0:8], in_=cand[:, h, :]), reads=RG, writes=[Rc1])
                P.op("dve", lambda e, h=h: e.match_replace(out=tmp2[:, h, :], in_to_replace=c1[:, h, 0:8], in_values=cand[:, h, :], imm_value=NEG), reads=RG + [Rc1], writes=RE)
                P.op("dve", lambda e, h=h: e.max(out=c1[:, h, 8:16], in_=tmp2[:, h, :]), reads=RE, writes=[Rc1])
            P.op("dve", lambda e: e.tensor_tensor(out=dmat[:], in0=c1[:], in1=c1[:, :, 0:1].broadcast_to([128, 8, 16]), op=ALU.subtract), reads=[Rc1], writes=[Rsc])
            P.op("act", lambda e: e.activation(out=dmat[:], in_=dmat[:], func=AF.Exp), reads=[Rsc], writes=[Rsc])
            P.op("dve", lambda e: e.tensor_reduce(out=sc[:, :, 0], in_=dmat[:], axis=AX.X, op=ALU.add), reads=[Rsc], writes=[Rsc])
            P.op("act", lambda e: e.activation(out=sc[:, :, 1], in_=sc[:, :, 0], func=AF.Ln), reads=[Rsc], writes=[Rsc])
            P.op("dve", lambda e: e.scalar_tensor_tensor(out=sc[:, :, 2], in0=c1[:, :, 0], scalar=-1.0, in1=sc[:, :, 1], op0=ALU.mult, op1=ALU.subtract), reads=[Rsc, Rc1], writes=[Rsc])
            P.op("dve", lambda e: e.tensor_tensor(out=sc[:, :, 3], in0=c1[:, :, 15], in1=sc[:, :, 2], op=ALU.add), reads=[Rsc, Rc1], writes=[Rsc])
            P.op("act", lambda e: e.activation(out=sc[:, :, 3], in_=sc[:, :, 3], func=AF.Exp, bias=-MARGIN), reads=[Rsc], writes=[Rsc])
            P.op("dve", lambda e: e.tensor_scalar(out=sc[:, :, 4], in0=c1[:, :, 15], scalar1=-1.0, scalar2=MARGIN, op0=ALU.mult, op1=ALU.add), reads=[Rc1, Rsc], writes=[Rsc])
            P.op("dve", lambda e: e.tensor_tensor(out=b3[:], in0=sub4[:, :, 0, :], in1=sc[:, :, 4:5].broadcast_to([128, 8, 128]), op=ALU.add), reads=[Rsub, Rsc], writes=[Rb3])
            for h in range(8):
                P.op("pool", lambda e: e.tensor_scalar(out=dg[:, h, :], in0=identb[:], scalar1=sc[:, h, 3:4], scalar2=1.0, op0=ALU.mult, op1=ALU.mult),
                     reads=[Rsc] + CONST, writes=[Rdg])
            P.op("dve", lambda e: e.tensor_scalar(out=sc[:, :, 5], in0=sv4[:, :, 1, 0], scalar1=-1.0, scalar2=None, op0=ALU.mult), reads=[Rsv, Rsc], writes=[Rsc])
            P.op("dve", lambda e: e.tensor_tensor(out=sc[:, :, 6], in0=sc[:, :, 4], in1=sv4[:, :, 1, 0], op=ALU.add), reads=[Rsv, Rsc], writes=[Rsc])
            for h in range(4, 8):
                P.op("act", lambda e: e.activation(out=Eb4[:, h - 4, :], in_=sub4[:, h, 1, :], func=AF.Exp, bias=sc[:, h, 5:6]), reads=[Rsub, Rsc], writes=[REab])
                P.op("act", lambda e: e.activation(out=Ea4[:, h - 4, :], in_=sub4[:, h, 0, :], func=AF.Exp, bias=sc[:, h, 6:7]), reads=[Rsub, Rsc], writes=[REab])
            gel = hraw[t % 2]
            Rgel = Rh[t % 2]
            pend_out = None
            for eb in range(NEB):
                if eb == 0 and t + 1 < NT:
                    load_xt2(t + 1)
                if eb % 2 == 1 or eb == 0:
                    issue_v(t * 64 + eb // 2 + NRING)
                es = eb % 2
                gs = eb % 4
                for h in range(4):
                    P.op("act", lambda e: e.activation(out=Ebuf[es][:, h, :], in_=sub4[:, h, 1, :], func=AF.Exp, bias=b3[:, h, eb:eb + 1]),
                         reads=[Rsub, Rb3], writes=[REg[es][0]])
                for h in (4, 5):
                    P.op("pool", lambda e: e.tensor_scalar(out=Ebuf[es][:, h, :], in0=Eb4[:, h - 4, :], scalar1=Ea4[:, h - 4, eb:eb + 1], scalar2=1.0, op0=ALU.mult, op1=ALU.mult),
                         reads=[REab], writes=[REg[es][1]])
                for h in (6, 7):
                    P.op("dve", lambda e: e.tensor_scalar(out=Ebuf[es][:, h, :], in0=Eb4[:, h - 4, :], scalar1=Ea4[:, h - 4, eb:eb + 1], scalar2=None, op0=ALU.mult),
                         reads=[REab], writes=[REg[es][2]])
                P.op("dve", lambda e: e.scalar_tensor_tensor(out=Gp[gs], in0=Ebuf[es].rearrange("p h n -> p (h n)"), scalar=1.0, in1=Ebuf[es].rearrange("p h n -> p (h n)"),
                                                              op0=ALU.is_ge, op1=ALU.mult),
                     reads=REg[es], writes=[RG[gs]])
                if t + 1 < NT:
                    if eb % 2 == 0:
                        h_mm(t + 1, eb)
                    else:
                        h_tr(t + 1, eb - 1)
                bg = nextbank(6, 8)
                for h in range(8):
                    P.op("pe", lambda e: e.matmul(ps[bg][:, 0:128], lhsT=Gp[gs][:, h * 128:(h + 1) * 128], rhs=dg[:, h, :], start=(h == 0), stop=(h == 7)),
                         reads=[RG[gs], Rdg], writes=[Rps[bg]], inc=(h == 7))
                if pend_out is not None:
                    pend_out()
                ia = eb % 3
                P.op("dve", lambda e: e.tensor_tensor(out=At[ia][:], in0=ps[bg][:, 0:128], in1=gel[:, eb, :], op=ALU.mult),
                     reads=[Rps[bg], Rgel], writes=[RAt[ia]])
                if t + 1 < NT:
                    if eb % 2 == 0:
                        h_cp1(t + 1, eb)
                    else:
                        h_cp2(t + 1, eb - 1)

                def mk_out(eb=eb, ia=ia, t=t):
                    iv2 = (t * 64 + eb // 2) % NRING
                    for db in range(4):
                        P.op("pe", lambda e: e.matmul(ps[db][:], lhsT=At[ia][:], rhs=vb[iv2][:, eb % 2, db * 512:(db + 1) * 512], start=(eb == 0), stop=(eb == NEB - 1)),
                             reads=[RAt[ia], Rvb[iv2]], writes=[Rps[db]], inc=(db == 3))
                pend_out = mk_out
            pend_out()
            if t + 1 < NT:
                h_gelu(t + 1)
            for d8 in range(8):
                q = 0
                db, hf_ = d8 // 2, d8 % 2
                cs = slice(d8 * 256, (d8 + 1) * 256)
                P.op("sp", lambda e: e.dma_start(out=x1l[q][:], in_=S_x1[tk, cs]), reads=[BX], writes=[Rx1l[q]], dsem=dx1l[q])
                P.op("dve", lambda e: e.tensor_tensor(out=yo[q][:], in0=ps[db][:, hf_ * 256:(hf_ + 1) * 256], in1=x1l[q][:], op=ALU.add),
                     reads=[Rps[db], Rx1l[q]], writes=[Ryo[q]])
                tok = P.op("sp", lambda e: e.dma_start(out=y[tk, cs], in_=yo[q][:]), reads=[Ryo[q]], dsem=dyo[q])
                out_toks.append(tok)


def _t5_bucket_static(rel):
    half, max_exact = 16, 8
    ret = np.where(rel > 0, half, 0)
    n = np.abs(rel)
    nf = np.maximum(n, 1).astype(np.float32)
    large = max_exact + (np.log(nf / max_exact) / math.log(128 / max_exact) * (half - max_exact)).astype(np.int32)
    large = np.minimum(large, half - 1)
    return ret + np.where(n < max_exact, n, large)


def _consts():
    c = np.zeros((128, 384 + 4096 + 256), np.float32)
    c[:, 0:128] = np.eye(128, dtype=np.float32)
    c[:, 128:256] = 1.0
    s = np.arange(64)
    c[0:64, 256:320] = (s[:, None] <= s[None, :]).astype(np.float32)
    c[0:64, 320:384] = (s[:, None] >= s[None, :]).astype(np.float32)
    big = np.ones((128, 4096), np.float32)
    k = np.arange(128)
    m_prev = (k[:, None] >= k[None, :]).astype(np.float32)
    m_next = (k[:, None] <= k[None, :]).astype(np.float32)
    c[:, 384:4480] = big
    return c, m_prev, m_next


_NC_CACHE = {}


def kernel(x, norm1_g, w_in, mlstm_gate_b, mlstm_norm_g, w_m_proj, attn_q_norm_g, attn_k_norm_g,
           attn_sink, rel_bias, w_a_proj, w_out, norm2_g, peer_wq, peer_keys, peer_u, peer_v, _debug=False, _upto=None):
    f = lambda a: np.ascontiguousarray(np.asarray(a, dtype=np.float32))
    x, w_in = f(x), f(w_in)
    cst, m_prev, m_next = _consts()
    rm = np.ones((4, 4096), np.float32)
    rm[:, ::64] = 0.0
    shared = {}
    shared["n1g"] = f(np.asarray(norm1_g).reshape(16, 128).T)
    shared["n2g"] = f(np.asarray(norm2_g).reshape(16, 128).T)
    shared["mng"] = f(np.broadcast_to(np.asarray(mlstm_norm_g).reshape(1, D), (64, D)))
    shared["aqg"] = f(np.asarray(attn_q_norm_g).reshape(128, 1))
    shared["akg"] = f(np.asarray(attn_k_norm_g).reshape(128, 1))
    shared["sinkb"] = f(np.broadcast_to(np.asarray(attn_sink).reshape(1, 16), (128, 16)))
    shared["w_in"] = w_in
    shared["w_mp"] = f(w_m_proj)
    shared["w_ap"] = f(w_a_proj)
    shared["w_o"] = f(w_out)
    shared["w_pq"] = f(peer_wq)
    shared["keysT"] = f(np.asarray(peer_keys).reshape(16, 128, 128).transpose(2, 0, 1).reshape(128, 16 * 128))
    shared["uT"] = f(np.asarray(peer_u).reshape(NEB, 128, 16, 128).transpose(0, 3, 2, 1).reshape(NEB, 128, 16 * 128))
    shared["pv"] = f(np.asarray(peer_v).reshape(NEB, 128, D))
    rb = np.asarray(rel_bias, dtype=np.float32)
    gbias = np.asarray(mlstm_gate_b, dtype=np.float32)
    wg_full = w_in[:, O_MG:O_MG + 16]
    kq = np.arange(128)
    in_maps = []
    for core in range(8):
        b, half = core // 2, core % 2
        xs = x[b]
        if half == 1:
            xs = xs[::-1]
        m = dict(shared)
        m["x_own"] = f(xs[:T])
        m["x_halo"] = f(xs[T:])
        cols = []
        gb = np.zeros((4, 4), np.float32)
        for d in range(2):
            td = d ^ half
            for kind in range(2):
                cols.append(wg_full[:, td * 8 + kind * 4: td * 8 + kind * 4 + 4])
                gb[:, d * 2 + kind] = gbias[td, kind, :]
        m["w_gate"] = f(np.concatenate(cols, axis=1))
        m["gate_b"] = gb
        bt = np.zeros((128, 16, 3, 128), np.float32)
        for o in range(3):
            rel = (kq[:, None] + (o - 1) * 128) - kq[None, :]
            if half == 1:
                rel = -rel
            bk = _t5_bucket_static(rel)
            bt[:, :, o, :] = rb[bk].transpose(0, 2, 1)
        m["btab"] = f(bt.reshape(128, -1))
        c2 = cst.copy()
        c2[:, 4480:4480 + 128] = m_prev
        c2[:, 4480 + 128:4480 + 256] = m_next
        c2[0:4, 384:4480] = rm
        m["cst"] = c2
        in_maps.append(m)
    key = (tuple(_debug) if _debug else None, _upto)
    if key not in _NC_CACHE:
        _NC_CACHE[key] = build_nc(debug=_debug, upto=_upto)
    nc = _NC_CACHE[key]
    in_maps = [{k: m[k] for k in nc._in_names} for m in in_maps]
    res = run_bass_kernel_spmd(nc, in_maps, core_ids=list(range(8)))
    out = np.zeros((4, 4096, D), np.float32)
    for core in range(8):
        b, half = core // 2, core % 2
        yc = res.results[core].get("y", np.zeros((T, D), np.float32)) if _debug else res.results[core]["y"]
        if half == 0:
            out[b, :T] = yc
        else:
            out[b, T:] = yc[::-1]
    if _debug:
        return out, res
    return out
```

```python
import contextlib
import math
import numpy as np
import concourse.bass as bass
import concourse.mybir as mybir
from concourse.bass_utils import run_bass_kernel_spmd

F32 = mybir.dt.float32
BF16 = mybir.dt.bfloat16
AF = mybir.ActivationFunctionType
ALU = mybir.AluOpType
AX = mybir.AxisListType

T = 2048
D = 2048
NT = 16
EPS = 1e-6
NEG = -1.0e30
O_MQ, O_MK, O_MV, O_MO, O_MG, O_AQ, O_AK, O_AV, O_GM, O_GA = (
    0, 2048, 4096, 6144, 8192, 8208, 10256, 10768, 11280, 13328)
NEB = 128


class Res:
    __slots__ = ("w", "r")

    def __init__(self):
        self.w = None
        self.r = []


class DSem:
    def __init__(self, sem):
        self.sem = sem
        self.val = 0


class _Rec:
    def __init__(self):
        self.call = None

    def __getattr__(self, name):
        def f(*a, **k):
            self.call = (name, a, k)
            return self
        return f


class Prog:
    ENGS = ("pe", "dve", "act", "pool", "sp")

    def __init__(self, nc, esems):
        self.nc = nc
        self.esem = esems
        self.cnt = {e: 0 for e in self.ENGS}
        self.ops = {e: [] for e in self.ENGS}
        self.seen = {e: {} for e in self.ENGS}
        self.nops = 0
        self.dsems = []
        self.fence = {e: [] for e in self.ENGS}

    def barrier(self):
        toks = [(self.esem[e], self.cnt[e], "x") for e in self.ENGS if self.cnt[e] > 0]
        toks += [(d.sem, d.val, "dma") for d in self.dsems if d.val > 0]
        for e in self.ENGS:
            self.fence[e] = list(toks)

    def op(self, eng, fn, reads=(), writes=(), dsem=None, inc=True):
        waits = {}

        def add(tok):
            if tok is None:
                return
            s, v, e = tok
            if e == "pe" and eng == "pe" and dsem is None:
                return
            k = id(s)
            if self.seen[eng].get(k, 0) >= v:
                return
            if k not in waits or waits[k][1] < v:
                waits[k] = (s, v)

        for R in reads:
            add(R.w)
        for R in writes:
            add(R.w)
            for t in R.r:
                add(t)
        if self.fence[eng]:
            for t in self.fence[eng]:
                add(t)
            self.fence[eng] = []
        for k, (s, v) in waits.items():
            self.seen[eng][k] = v
        if dsem is None:
            if inc:
                self.cnt[eng] += 1
                tok = (self.esem[eng], self.cnt[eng], eng)
                incs = (self.esem[eng], 1)
            else:
                tok = (self.esem[eng], self.cnt[eng] + 1, eng)
                incs = None
        else:
            dsem.val += 16
            tok = (dsem.sem, dsem.val, "dma")
            incs = (dsem.sem, 16)
        for R in reads:
            R.r.append(tok)
        for R in writes:
            R.w = tok
            R.r = []
        rec = _Rec()
        fn(rec)
        self.ops[eng].append((list(waits.values()), rec.call, incs))
        self.nops += 1
        return tok

    def finish(self, toks):
        waits = [(s, v) for (s, v, _) in toks if s is not None]
        self.ops["sp"].append((waits, ("nop", (), {}), (self.esem["sp"], 1)))

    def emit(self, block):
        engmap = {"pe": "tensor", "dve": "vector", "act": "scalar", "pool": "gpsimd", "sp": "sync"}
        for e in self.ENGS:
            ops = self.ops[e]

            def body(engine, ops=ops):
                for waits, fn, incs in ops:
                    for s, v in waits:
                        engine.wait_ge(s, v)
                    ins = getattr(engine, fn[0])(*fn[1], **fn[2])
                    if incs is not None:
                        ins.then_inc(incs[0], incs[1])

            getattr(block, engmap[e])(body)


def build_nc(debug=False, upto=None):
    nc = bass.Bass("TRN2", target_bir_lowering=False)

    LEVELS = ["A", "P1", "P", "G", "M", "T", "O", None]
    lvl = LEVELS.index(upto)
    in_names = []
    nc._in_names = in_names

    def din(name, shape, need=0):
        if lvl < need:
            return None
        in_names.append(name)
        return nc.dram_tensor(name, list(shape), F32, kind="ExternalInput").ap()

    dbgset = set(debug) if debug else set()

    def dscr(name, shape, dt):
        return nc.dram_tensor(name, list(shape), dt, kind="ExternalOutput" if name in dbgset else "Internal").ap()

    x_own = din("x_own", (T, D))
    x_halo = din("x_halo", (T, D))
    w_in = din("w_in", (D, 15376))
    w_gate = din("w_gate", (D, 16))
    gate_b = din("gate_b", (4, 4))
    n1g = din("n1g", (128, 16))
    n2g = din("n2g", (128, 16))
    mng = din("mng", (64, D), need=4)
    aqg = din("aqg", (128, 1))
    akg = din("akg", (128, 1))
    sinkb = din("sinkb", (128, 16), need=5)
    btab = din("btab", (128, 16 * 3 * 128), need=5)
    cst = din("cst", (128, 384 + 4096 + 256))
    w_mp = din("w_mp", (D, D), need=6)
    w_ap = din("w_ap", (D, D), need=6)
    w_o = din("w_o", (D, D), need=6)
    w_pq = din("w_pq", (D, D), need=6)
    keysT = din("keysT", (128, 16 * 128), need=6)
    uT = din("uT", (NEB, 128, 16 * 128), need=5)
    pv = din("pv", (NEB, 128, D), need=5)
    y = nc.dram_tensor("y", [T, D], F32, kind="ExternalOutput").ap()

    S_qT = dscr("S_qT", (D, T), BF16)
    S_kT = dscr("S_kT", (D, T), BF16)
    S_k = dscr("S_k", (T, D), BF16)
    S_v = dscr("S_v", (T, D), BF16)
    S_o = dscr("S_o", (T, D), BF16)
    S_kh = dscr("S_kh", (T, D), BF16)
    S_vh = dscr("S_vh", (T, D), BF16)
    S_aqT = dscr("S_aqT", (D, T), BF16)
    S_akT = dscr("S_akT", (512, T + 128), BF16)
    S_av = dscr("S_av", (T + 128, 512), BF16)
    S_gmT = dscr("S_gmT", (D, T), BF16)
    S_gaT = dscr("S_gaT", (D, T), BF16)
    S_g = dscr("S_g", (4, 4, 2 * T), F32)
    S_hf = dscr("S_hf", (T, D), F32)
    S_hm = dscr("S_hm", (T, D), BF16)
    S_haT = dscr("S_haT", (D, T), BF16)
    S_x1 = dscr("S_x1", (T, D), F32)
    S_sub = dscr("S_sub", (T, D), F32)
    S_xn2 = dscr("S_xn2", (NT, 128, 16 * 128), BF16)
    S_UT = dscr("S_UT", (NEB, 128, D), BF16)
    S_V = dscr("S_V", (NEB, 128, D), BF16)

    with contextlib.ExitStack() as top:
        E = top.enter_context
        esems = {e: E(nc.semaphore("es_" + e)) for e in Prog.ENGS}
        P = Prog(nc, esems)
        block = E(nc.Block())
        nds = [0]

        def mkds(st=None):
            nds[0] += 1
            d_ = DSem(top.enter_context(nc.semaphore("ds%d" % nds[0])))
            P.dsems.append(d_)
            return d_

        def sb(st, name, shape, dt):
            return st.enter_context(nc.sbuf_tensor(name, list(shape), dt))

        cst_sb = sb(top, "cst_sb", (128, 128 + 128 + 64 + 64), F32)
        identb = sb(top, "identb", (128, 128), BF16)
        onesb = sb(top, "onesb", (128, 128), BF16)
        g1 = sb(top, "g1", (128, 16), F32)
        g2 = sb(top, "g2", (128, 16), F32)
        Rc = Res()
        dc = mkds()
        P.op("sp", lambda e: e.dma_start(out=cst_sb[:], in_=cst[:, 0:384]), writes=[Rc], dsem=dc)
        P.op("sp", lambda e: e.dma_start(out=g1[:], in_=n1g[:, :]), writes=[Rc], dsem=dc)
        P.op("sp", lambda e: e.dma_start(out=g2[:], in_=n2g[:, :]), writes=[Rc], dsem=dc)
        identf = cst_sb[:, 0:128]
        onesf = cst_sb[:, 128:256]
        maskf = cst_sb[0:64, 256:320]
        maskb = cst_sb[0:64, 320:384]
        Rcb = Res()
        P.op("dve", lambda e: e.tensor_copy(out=identb[:], in_=identf), reads=[Rc], writes=[Rcb])
        P.op("dve", lambda e: e.tensor_copy(out=onesb[:], in_=onesf), reads=[Rc], writes=[Rcb])
        CONST = [Rc, Rcb]

        ps = [E(nc.psum_tensor("ps%d" % i, [128, 512], F32)) for i in range(8)]
        Rps = [Res() for _ in range(8)]
        bank_ctr = [0]

        def nextbank(lo=0, hi=8):
            n = hi - lo
            b = lo + bank_ctr[0] % n
            bank_ctr[0] += 1
            return b

        def barrier_from(res_list):
            B = Res()
            toks = []
            for R in res_list:
                toks += R.r
                if R.w is not None:
                    toks.append(R.w)
            waits = [(s, v) for (s, v, _) in toks]
            P.cnt["sp"] += 1
            tok = (P.esem["sp"], P.cnt["sp"], "sp")
            P.ops["sp"].append((waits, ("nop", (), {}), (P.esem["sp"], 1)))
            B.w = tok
            return B

        def norm_T(st, get_tile, ntiles, xnT, RxnT, gt, tag):
            xt = [sb(st, "xt%s%d" % (tag, i), (128, D), F32) for i in range(2)]
            Rxt = [Res(), Res()]
            dxt = [mkds(st), mkds(st)]
            junk = sb(st, "junk" + tag, (128, D), BF16)
            xs = sb(st, "xs" + tag, (128, D), BF16)
            sm = sb(st, "sm" + tag, (128, 4), F32)
            Rj, Rxs, Rsm = Res(), Res(), Res()
            for t in range(ntiles):
                b = t % 2
                get_tile(t, xt[b], Rxt[b], dxt[b])
                P.op("act", lambda e, b=b: e.activation(out=junk[:], in_=xt[b][:], func=AF.Square, accum_out=sm[:, 0:1]),
                     reads=[Rxt[b]], writes=[Rj, Rsm])
                P.op("act", lambda e: e.activation(out=sm[:, 1:2], in_=sm[:, 0:1], func=AF.Ln, scale=1.0 / D, bias=EPS),
                     reads=[Rsm], writes=[Rsm])
                P.op("act", lambda e: e.activation(out=sm[:, 2:3], in_=sm[:, 1:2], func=AF.Exp, scale=-0.5),
                     reads=[Rsm], writes=[Rsm])
                P.op("dve", lambda e, b=b: e.tensor_scalar(out=xs[:], in0=xt[b][:], scalar1=sm[:, 2:3], scalar2=None, op0=ALU.mult),
                     reads=[Rxt[b], Rsm], writes=[Rxs])
                for g in range(4):
                    bk = nextbank()
                    ptv = ps[bk][:].bitcast(BF16)[:, 0:512].rearrange("p (j n) -> p j n", j=4)
                    for j in range(4):
                        c = g * 4 + j
                        P.op("pe", lambda e, c=c, j=j, ptv=ptv: e.transpose(out=ptv[:, j, :], in_=xs[:, c * 128:(c + 1) * 128], identity=identb[:]),
                             reads=[Rxs] + CONST, writes=[Rps[bk]], inc=(j == 3))
                    P.op("dve", lambda e, g=g, t=t, ptv=ptv: e.tensor_tensor(
                        out=xnT[:, g * 4:(g + 1) * 4, t * 128:(t + 1) * 128], in0=ptv,
                        in1=gt[:, g * 4:(g + 1) * 4].unsqueeze(2).broadcast_to([128, 4, 128]), op=ALU.mult),
                        reads=[Rps[bk]] + CONST, writes=[RxnT[t]])

        with contextlib.ExitStack() as st:
            xnT = sb(st, "xnT", (128, 16, T), BF16)
            RxnT = [Res() for _ in range(NT)]
            wr = [sb(st, "wr%d" % i, (128, 16, 512), BF16) for i in range(3)]
            Rwr = [Res() for _ in range(3)]
            dwr = [mkds(st) for _ in range(3)]
            wctr = [0]
            stg = [sb(st, "stg%d" % i, (128, 8192), BF16) for i in range(2)]
            Rstg = [Res(), Res()]
            dstg = [mkds(st), mkds(st)]
            sctr = [0]
            wg = sb(st, "wg", (128, 16, 16), BF16)
            gb = sb(st, "gb", (4, 4), F32)
            gqs = sb(st, "gqs", (128, 2), F32)
            gst = [sb(st, "gst%d" % i, (4, 512), F32) for i in range(2)]
            Rgst = [Res(), Res()]
            dgst = [mkds(st), mkds(st)]
            sqt = sb(st, "sqt", (128, 512), BF16)
            lnv = sb(st, "lnv", (128, 512), F32)
            Rsq, Rln = Res(), Res()
            Rw0 = Res()
            dw0 = mkds(st)
            dw0p = mkds(st)
            P.op("pool", lambda e: e.dma_start(out=wg[:], in_=w_gate.rearrange("(c p) n -> p c n", p=128)), writes=[Rw0], dsem=dw0p)
            P.op("sp", lambda e: e.dma_start(out=gb[:], in_=gate_b[:, :]), writes=[Rw0], dsem=dw0)
            P.op("sp", lambda e: e.dma_start(out=gqs[:, 0:1], in_=aqg[:, :]), writes=[Rw0], dsem=dw0)
            P.op("sp", lambda e: e.dma_start(out=gqs[:, 1:2], in_=akg[:, :]), writes=[Rw0], dsem=dw0)
            P.op("dve", lambda e: e.tensor_scalar(out=gqs[:, 0:1], in0=gqs[:, 0:1], scalar1=128.0 ** -0.5, scalar2=None, op0=ALU.mult),
                 reads=[Rw0], writes=[Rw0])

            def load_w(wsrc, c0, n):
                s = wctr[0] % 3
                wctr[0] += 1
                P.op("pool", lambda e: e.dma_start(out=wr[s][:, :, 0:n], in_=wsrc[:, c0:c0 + n].rearrange("(c p) n -> p c n", p=128)),
                     writes=[Rwr[s]], dsem=dwr[s])
                return s

            def new_stg():
                s = sctr[0] % 2
                sctr[0] += 1
                return s

            def fm_mm(s, j, tok0, ntok, bk):
                rd = [Rwr[s]] + RxnT[tok0 // 128:(tok0 + ntok + 127) // 128]
                for c in range(16):
                    P.op("pe", lambda e, c=c: e.matmul(ps[bk][:, 0:ntok], lhsT=wr[s][:, c, j * 128:(j + 1) * 128],
                                                         rhs=xnT[:, c, tok0:tok0 + ntok], start=(c == 0), stop=(c == 15)),
                         reads=rd, writes=[Rps[bk]], inc=(c == 15))

            def tm_mm(s, n, t, bk):
                rd = [Rwr[s], RxnT[t]]
                for c in range(16):
                    P.op("pe", lambda e, c=c: e.matmul(ps[bk][:, 0:n], lhsT=xnT[:, c, t * 128:(t + 1) * 128],
                                                         rhs=wr[s][:, c, 0:n], start=(c == 0), stop=(c == 15)),
                         reads=rd, writes=[Rps[bk]], inc=(c == 15))

            evctr = [0]

            def evac_copy(dst, bk, n, wres, func=None):
                if func is not None or evctr[0] % 2 == 0:
                    f = func if func is not None else AF.Copy
                    P.op("act", lambda e: e.activation(out=dst, in_=ps[bk][:, 0:n], func=f), reads=[Rps[bk]], writes=wres)
                else:
                    P.op("dve", lambda e: e.tensor_copy(out=dst, in_=ps[bk][:, 0:n]), reads=[Rps[bk]], writes=wres)
                evctr[0] += 1

            def evac_qknorm(dst, bk, n, gcol, wres):
                P.op("act", lambda e: e.activation(out=sqt[:, 0:n], in_=ps[bk][:, 0:n], func=AF.Square), reads=[Rps[bk]], writes=[Rsq])
                b2 = nextbank()
                P.op("pe", lambda e: e.matmul(ps[b2][:, 0:n], lhsT=onesb[:], rhs=sqt[:, 0:n], start=True, stop=True),
                     reads=[Rsq] + CONST, writes=[Rps[b2]])
                P.op("act", lambda e: e.activation(out=lnv[:, 0:n], in_=ps[b2][:, 0:n], func=AF.Ln, scale=1.0 / 128, bias=EPS),
                     reads=[Rps[b2]], writes=[Rln])
                P.op("act", lambda e: e.activation(out=lnv[:, 0:n], in_=lnv[:, 0:n], func=AF.Exp, scale=-0.5), reads=[Rln], writes=[Rln])
                P.op("dve", lambda e: e.scalar_tensor_tensor(out=dst, in0=ps[bk][:, 0:n], scalar=gqs[:, gcol:gcol + 1], in1=lnv[:, 0:n],
                                                              op0=ALU.mult, op1=ALU.mult), reads=[Rps[bk], Rln, Rw0], writes=wres)

            def fm_block(wsrc, c0, ncol, dstT, row0, kind, ntok=T, tokdst0=0):
                s = load_w(wsrc, c0, ncol)
                g = new_stg()
                nsub = ncol // 128
                sv = stg[g][:, 0:nsub * ntok].rearrange("p (j t) -> p j t", j=nsub)
                for j in range(nsub):
                    for tb in range(0, ntok, 512):
                        n = min(512, ntok - tb)
                        bk = nextbank()
                        fm_mm(s, j, tb, n, bk)
                        dst = sv[:, j, tb:tb + n]
                        if kind == "plain":
                            evac_copy(dst, bk, n, [Rstg[g]])
                        elif kind == "sig":
                            evac_copy(dst, bk, n, [Rstg[g]], func=AF.Sigmoid)
                        elif kind == "qn":
                            evac_qknorm(dst, bk, n, 0, [Rstg[g]])
                        elif kind == "kn":
                            evac_qknorm(dst, bk, n, 1, [Rstg[g]])
                P.op("sp", lambda e: e.dma_start(out=dstT[row0:row0 + ncol, tokdst0:tokdst0 + ntok].rearrange("(j p) t -> p j t", p=128), in_=sv),
                     reads=[Rstg[g]], dsem=dstg[g])

            def tm_block(wsrc, c0, ncol, dst, dcol0, kind, ntiles=NT, tokdst0=0):
                s = load_w(wsrc, c0, ncol)
                g = new_stg()
                sv = stg[g][:, 0:ntiles * ncol].rearrange("p (t n) -> p t n", t=ntiles)
                for t in range(ntiles):
                    bk = nextbank()
                    tm_mm(s, ncol, t, bk)
                    evac_copy(sv[:, t, :], bk, ncol, [Rstg[g]], func=(AF.Sigmoid if kind == "sig" else None))
                P.op("sp", lambda e: e.dma_start(out=dst[tokdst0:tokdst0 + ntiles * 128, dcol0:dcol0 + ncol].rearrange("(t p) n -> p t n", p=128), in_=sv),
                     reads=[Rstg[g]], dsem=dstg[g])

            def gate_block(ggs, tokdst0):
                for tb in range(4):
                    for gg in ggs:
                        bk = nextbank()
                        for c in range(16):
                            P.op("pe", lambda e, c=c: e.matmul(ps[bk][0:4, :], lhsT=wg[:, c, gg * 4:(gg + 1) * 4],
                                                                 rhs=xnT[:, c, tb * 512:(tb + 1) * 512], start=(c == 0), stop=(c == 15)),
                                 reads=[Rw0] + RxnT[tb * 4:tb * 4 + 4], writes=[Rps[bk]], inc=(c == 15))
                        q = (tb * 4 + gg) % 2
                        P.op("act", lambda e: e.activation(out=gst[q][:], in_=ps[bk][0:4, :], func=AF.Identity, bias=gb[:, gg:gg + 1]),
                             reads=[Rps[bk], Rw0], writes=[Rgst[q]])
                        P.op("sp", lambda e: e.dma_start(out=S_g[gg, :, tokdst0 + tb * 512:tokdst0 + (tb + 1) * 512], in_=gst[q][:]),
                             reads=[Rgst[q]], dsem=dgst[q])

            def x_tile_loader(xsrc):
                def get(t, dst, Rd, dsm):
                    P.op("sp", lambda e: e.dma_start(out=dst[:], in_=xsrc[t * 128:(t + 1) * 128, :]), writes=[Rd], dsem=dsm)
                return get

            with contextlib.ExitStack() as st2:
                norm_T(st2, x_tile_loader(x_halo), NT, xnT, RxnT, g1, "h")
                if upto == "A":
                    D_xnT = nc.dram_tensor("D_xnT", [128, 16 * T], BF16, kind="ExternalOutput").ap()
                    dsx = mkds()
                    tk_ = P.op("sp", lambda e: e.dma_start(out=D_xnT[:, :], in_=xnT[:].rearrange("p c t -> p (c t)")), reads=RxnT, dsem=dsx)
                    P.finish([tk_])
                    P.emit(block)
                    return nc
                gate_block([2, 3], T)
                for hb in range(4):
                    tm_block(w_in, O_MK + hb * 512, 512, S_kh, hb * 512, "plain")
                    tm_block(w_in, O_MV + hb * 512, 512, S_vh, hb * 512, "plain")
                fm_block(w_in, O_AK, 512, S_akT, 0, "kn", ntok=128, tokdst0=T)
                tm_block(w_in, O_AV, 512, S_av, 0, "plain", ntiles=1, tokdst0=T)
                if upto == "P1":
                    BP = barrier_from([Rstg[0], Rstg[1], Rgst[0], Rgst[1]])
                    P.finish([BP.w])
                    P.emit(block)
                    return nc
                norm_T(st2, x_tile_loader(x_own), NT, xnT, RxnT, g1, "o")
            P.barrier()
            gate_block([0, 1, 2, 3], 0)
            for hb in range(4):
                fm_block(w_in, O_MQ + hb * 512, 512, S_qT, hb * 512, "plain")
                fm_block(w_in, O_MK + hb * 512, 512, S_kT, hb * 512, "plain")
                tm_block(w_in, O_MK + hb * 512, 512, S_k, hb * 512, "plain")
                tm_block(w_in, O_MV + hb * 512, 512, S_v, hb * 512, "plain")
                tm_block(w_in, O_MO + hb * 512, 512, S_o, hb * 512, "sig")
                fm_block(w_in, O_AQ + hb * 512, 512, S_aqT, hb * 512, "qn")
                fm_block(w_in, O_GM + hb * 512, 512, S_gmT, hb * 512, "sig")
                fm_block(w_in, O_GA + hb * 512, 512, S_gaT, hb * 512, "sig")
            fm_block(w_in, O_AK, 512, S_akT, 0, "kn")
            tm_block(w_in, O_AV, 512, S_av, 0, "plain")
            PH_P_DONE = [Rstg[0], Rstg[1], Rgst[0], Rgst[1]]
        P.barrier()

        BP = barrier_from(PH_P_DONE)
        out_toks = [BP.w]
        UPTO = upto
        if upto != "P":
            build_rest(nc, P, top, sb, mkds, ps, Rps, nextbank, CONST, BP, locals(), out_toks)
        P.finish(out_toks)
        P.emit(block)
    return nc


def build_rest(nc, P, top, sb, mkds, ps, Rps, nextbank, CONST, BP, L, out_toks):
    identb, onesb, cst_sb = L["identb"], L["onesb"], L["cst_sb"]
    identf, onesf, maskf, maskb = L["identf"], L["onesf"], L["maskf"], L["maskb"]
    cst, g2 = L["cst"], L["g2"]
    S_g, S_qT, S_kT, S_k, S_v, S_o, S_kh, S_vh = (L[k] for k in "S_g S_qT S_kT S_k S_v S_o S_kh S_vh".split())
    S_hf, S_hm, mng = L["S_hf"], L["S_hm"], L["mng"]
    S_UT, S_V, uT, pv = L["S_UT"], L["S_V"], L["uT"], L["pv"]
    L["BU_holder"] = [None]
    barrier_from = L["barrier_from"]
    norm_T = L["norm_T"]

    TSf = sb(top, "TSf", (64, 2, 32, 4), F32)
    TSb = sb(top, "TSb", (64, 96, 4), F32)
    DBf = sb(top, "DBf", (128, 4, 32), F32)
    DBb = sb(top, "DBb", (128, 4, 64), F32)
    RTS = Res()
    with contextlib.ExitStack() as st:
        B = [sb(st, "gB%d" % i, (4, 4096), F32) for i in range(5)]
        RB = [Res() for _ in range(5)]
        dB = [mkds(st) for _ in range(2)]
        rmask = sb(st, "rmask", (4, 4096), F32)
        Rrm = Res()
        drm = mkds(st)
        P.op("sp", lambda e: e.dma_start(out=rmask[:], in_=cst[0:4, 384:384 + 4096]), writes=[Rrm], dsem=drm)
        amax = sb(st, "amax", (4, 64), F32)
        totc = sb(st, "totc", (4, 64), F32)
        Mtab = sb(st, "Mtab", (4, 65), F32)
        MC = sb(st, "MC", (4, 64), F32)
        dd = sb(st, "dd", (4, 64), F32)
        dexp = sb(st, "dexp", (4, 4, 64), F32)
        Rsm = Res()
        LN_S = math.log(512.0 ** -0.5)
        for d in range(2):
            Ltok = T if d == 0 else 2 * T
            nch = Ltok // 64
            li, zf, cum, cx, a = (B[i][:, 0:Ltok] for i in range(5))
            v3 = lambda ap: ap.rearrange("p (c l) -> p c l", l=64)
            P.op("sp", lambda e: e.dma_start(out=li, in_=S_g[d * 2, :, 0:Ltok]), reads=[BP], writes=[RB[0]], dsem=dB[0])
            P.op("sp", lambda e: e.dma_start(out=zf, in_=S_g[d * 2 + 1, :, 0:Ltok]), reads=[BP], writes=[RB[1]], dsem=dB[1])
            P.op("act", lambda e: e.activation(out=zf, in_=zf, func=AF.Exp, scale=-1.0), reads=[RB[1]], writes=[RB[1]])
            P.op("act", lambda e: e.activation(out=zf, in_=zf, func=AF.Ln, bias=1.0), reads=[RB[1]], writes=[RB[1]])
            P.op("dve", lambda e: e.tensor_tensor_scan(out=cum, data0=rmask[:, 0:Ltok], data1=zf, initial=0.0, op0=ALU.mult, op1=ALU.add),
                 reads=[RB[1], Rrm], writes=[RB[2]])
            tot3 = v3(cum)[:, :, 63:64]
            if d == 1:
                P.op("dve", lambda e: e.tensor_tensor(out=cx, in0=zf, in1=cum, op=ALU.subtract), reads=[RB[1], RB[2]], writes=[RB[3]])
                P.op("dve", lambda e: e.tensor_tensor(out=v3(cx), in0=v3(cx), in1=tot3.broadcast_to([4, nch, 64]), op=ALU.add),
                     reads=[RB[3], RB[2]], writes=[RB[3]])
                cxx, Rcx = cx, RB[3]
            else:
                cxx, Rcx = cum, RB[2]
            P.op("dve", lambda e: e.tensor_tensor(out=a, in0=li, in1=cxx, op=ALU.add), reads=[RB[0], Rcx], writes=[RB[4]])
            P.op("dve", lambda e: e.tensor_reduce(out=amax[:, 0:nch], in_=v3(a), axis=AX.X, op=ALU.max), reads=[RB[4]], writes=[Rsm])
            P.op("dve", lambda e: e.tensor_copy(out=totc[:, 0:nch], in_=tot3.rearrange("p c l -> p (c l)")), reads=[RB[2]], writes=[Rsm])
            if d == 0:
                P.op("dve", lambda e: e.memset(Mtab[:, 0:1], 0.0), writes=[Rsm])
                order = [(c, c, c + 1) for c in range(nch)]
            else:
                P.op("dve", lambda e: e.memset(Mtab[:, nch:nch + 1], 0.0), writes=[Rsm])
                order = [(c, c + 1, c) for c in range(nch - 1, -1, -1)]
            for (c, ip, inx) in order:
                P.op("dve", lambda e, c=c, ip=ip: e.tensor_tensor(out=MC[:, c:c + 1], in0=Mtab[:, ip:ip + 1], in1=amax[:, c:c + 1], op=ALU.max),
                     reads=[Rsm], writes=[Rsm])
                P.op("dve", lambda e, c=c, inx=inx: e.tensor_tensor(out=Mtab[:, inx:inx + 1], in0=MC[:, c:c + 1], in1=totc[:, c:c + 1], op=ALU.subtract),
                     reads=[Rsm], writes=[Rsm])
            mprev = Mtab[:, 0:nch] if d == 0 else Mtab[:, 1:nch + 1]
            P.op("dve", lambda e: e.tensor_tensor(out=dd[:, 0:nch], in0=mprev, in1=MC[:, 0:nch], op=ALU.subtract), reads=[Rsm], writes=[Rsm])
            P.op("act", lambda e: e.activation(out=dd[:, 0:nch], in_=dd[:, 0:nch], func=AF.Exp), reads=[Rsm], writes=[Rsm])
            mcb = MC[:, 0:nch].unsqueeze(2).broadcast_to([4, nch, 64])
            P.op("dve", lambda e: e.tensor_tensor(out=v3(zf), in0=v3(a), in1=mcb, op=ALU.subtract), reads=[RB[4], Rsm, RB[1]], writes=[RB[1]])
            P.op("act", lambda e: e.activation(out=zf, in_=zf, func=AF.Exp, bias=LN_S), reads=[RB[1]], writes=[RB[1]])
            P.op("dve", lambda e: e.tensor_tensor(out=v3(li), in0=v3(cxx), in1=mcb, op=ALU.subtract), reads=[Rcx, Rsm, RB[0]], writes=[RB[0]])
            P.op("act", lambda e: e.activation(out=li, in_=li, func=AF.Exp), reads=[RB[0]], writes=[RB[0]])
            bk = nextbank()
            ncols = 0
            for kind, src, Rsrc, nck in ((0, zf, RB[1], nch), (1, li, RB[0], 32)):
                for c in range(nck):
                    col = (kind * nch + c) * 4 if d == 1 else (kind * 32 + c) * 4
                    P.op("pe", lambda e, c=c, col=col, src=src: e.transpose(out=ps[bk][0:64, col:col + 4], in_=src[:, c * 64:(c + 1) * 64], identity=identf[0:4, 0:4]),
                         reads=[Rsrc] + CONST, writes=[Rps[bk]], inc=(c == nck - 1))
                    ncols = max(ncols, col + 4)
            dstTS = TSf[:].rearrange("p k c h -> p (k c h)") if d == 0 else TSb[:].rearrange("p c h -> p (c h)")
            P.op("dve", lambda e, dstTS=dstTS, ncols=ncols: e.tensor_copy(out=dstTS[:, 0:ncols], in_=ps[bk][0:64, 0:ncols]), reads=[Rps[bk]], writes=[RTS])
            P.op("dve", lambda e: e.tensor_tensor(out=dexp[:, :, 0:nch], in0=dd[:, 0:nch].unsqueeze(1).broadcast_to([4, 4, nch]),
                                                   in1=identf[0:4, 0:4].unsqueeze(2).broadcast_to([4, 4, nch]), op=ALU.mult),
                 reads=[Rsm] + CONST, writes=[Rsm])
            bk2 = nextbank()
            P.op("pe", lambda e: e.matmul(ps[bk2][:, 0:4 * nch].rearrange("p (h c) -> p h c", h=4), lhsT=onesf[0:4, :], rhs=dexp[:, :, 0:nch], start=True, stop=True),
                 reads=[Rsm] + CONST, writes=[Rps[bk2]])
            DBd = DBf if d == 0 else DBb
            P.op("dve", lambda e, DBd=DBd: e.tensor_copy(out=DBd[:], in_=ps[bk2][:, 0:4 * nch].rearrange("p (h c) -> p h c", h=4)),
                 reads=[Rps[bk2]], writes=[RTS])

    P.barrier()
    out_toks.append(RTS.w)
    if L["UPTO"] == "G":
        for nm, tl in (("D_TSf", TSf), ("D_TSb", TSb), ("D_DBf", DBf), ("D_DBb", DBb)):
            shp = list(tl[:].shape)
            flat = int(np.prod(shp[1:]))
            dd_ = nc.dram_tensor(nm, [shp[0], flat], F32, kind="ExternalOutput").ap()
            dsx = mkds()
            pat = {4: "p a b c -> p (a b c)", 3: "p a b -> p (a b)"}[len(shp)]
            out_toks.append(P.op("sp", lambda e: e.dma_start(out=dd_[:, :], in_=tl[:].rearrange(pat)), reads=[RTS], dsem=dsx))
        return
    sthm = contextlib.ExitStack()
    Cb_all = sb(sthm, "Cb_all", (128, 4, 4, 512), F32)
    nb_all = sb(sthm, "nb_all", (128, 4, 4), F32)
    RCall = Res()
    pcb = [sb(sthm, "pcb%d" % i, (128, D), BF16) for i in range(4)]
    Rpcb = [Res() for _ in range(4)]
    dpcb = [mkds() for _ in range(4)]
    dpcbo = [mkds() for _ in range(4)]
    RSU = Res()
    pc_state = [0]

    def precast_some(n):
        for _ in range(n):
            k = pc_state[0]
            if k >= 2 * NEB:
                return
            pc_state[0] += 1
            eb, which = k // 2, k % 2
            src, dst = ((uT, S_UT), (pv, S_V))[which]
            i = k % 4
            for hh in range(2):
                P.op("pool", lambda e: e.dma_start(out=pcb[i][:, hh * 1024:(hh + 1) * 1024], in_=src[eb, :, hh * 1024:(hh + 1) * 1024]),
                     writes=[Rpcb[i]], dsem=dpcb[i])
            P.op("sp", lambda e: e.dma_start(out=dst[eb, :, :], in_=pcb[i][:]), reads=[Rpcb[i]], writes=[RSU], dsem=dpcbo[i])

    def make_chain(st, tag, sbank, nbank, light=False):
        ch = dict(
            Cst=sb(st, "Cst" + tag, (128, 4, 512), F32), nst=sb(st, "nst" + tag, (128, 4), F32),
            RC=Res(), RCb=Res(),
            kw=[sb(st, "kw%s%d" % (tag, i), (64, 512), BF16) for i in range(2)], Rkw=[Res(), Res()],
            RSt=[Res(), Res()], Rms=Res(), cc=[0], sbank=sbank, nbank=nbank)
        if not light:
            ch.update(Cb=sb(st, "Cb" + tag, (128, 4, 512), BF16), nb=sb(st, "nb" + tag, (128, 4), BF16),
                      St=[sb(st, "St%s%d" % (tag, i), (64, 64), BF16) for i in range(2)],
                      sm=sb(st, "msm" + tag, (64, 8), F32))
        return ch

    def state_update(ch, kchunk, vchunk, wk, decay, rd):
        i = ch["cc"][0] % 2
        ch["cc"][0] += 1
        kw, Rkw, Cst, nst, RC = ch["kw"], ch["Rkw"], ch["Cst"], ch["nst"], ch["RC"]
        P.op("act", lambda e: e.activation(out=kw[i][:], in_=kchunk, func=AF.Copy, scale=wk), reads=rd + [RTS], writes=[Rkw[i]])
        bn = 7
        for j in range(4):
            P.op("pe", lambda e: e.matmul(ps[bn][:, j:j + 1], lhsT=kw[i][:, j * 128:(j + 1) * 128], rhs=onesb[0:64, 0:1], start=True, stop=True),
                 reads=[Rkw[i]] + CONST, writes=[Rps[bn]], inc=(j == 3))
        P.op("dve", lambda e: e.scalar_tensor_tensor(out=nst[:], in0=nst[:], scalar=decay, in1=ps[bn][:, 0:4], op0=ALU.mult, op1=ALU.add),
             reads=[Rps[bn], RTS, RC], writes=[RC])
        for j in range(4):
            bu = nextbank(4, 7)
            P.op("pe", lambda e: e.matmul(ps[bu][:], lhsT=kw[i][:, j * 128:(j + 1) * 128], rhs=vchunk, start=True, stop=True),
                 reads=[Rkw[i]] + rd, writes=[Rps[bu]])
            P.op("dve", lambda e: e.scalar_tensor_tensor(out=Cst[:, j, :], in0=Cst[:, j, :], scalar=decay, in1=ps[bu][:], op0=ALU.mult, op1=ALU.add),
                 reads=[Rps[bu], RTS, RC], writes=[RC])

    with contextlib.ExitStack() as st:
        chs = [make_chain(st, "h%d" % i, 0, 0, light=True) for i in range(2)]
        kkh = [sb(st, "kkh%d" % i, (64, 32, 512), BF16) for i in range(2)]
        vvh = [sb(st, "vvh%d" % i, (64, 32, 512), BF16) for i in range(2)]
        Rkh = [Res(), Res()]
        dkh = [mkds() for _ in range(4)]
        for hp in range(2):
            for i in range(2):
                h = hp * 2 + i
                P.op("sp", lambda e: e.dma_start(out=kkh[i][:], in_=S_kh[:, h * 512:(h + 1) * 512].rearrange("(c p) d -> p c d", p=64)),
                     reads=[BP], writes=[Rkh[i]], dsem=dkh[2 * i])
                P.op("sp", lambda e: e.dma_start(out=vvh[i][:], in_=S_vh[:, h * 512:(h + 1) * 512].rearrange("(c p) d -> p c d", p=64)),
                     reads=[BP], writes=[Rkh[i]], dsem=dkh[2 * i + 1])
                P.op("dve", lambda e: e.memset(chs[i]["Cst"][:], 0.0), writes=[chs[i]["RC"]])
                P.op("dve", lambda e: e.memset(chs[i]["nst"][:], 0.0), writes=[chs[i]["RC"]])
            for c in range(63, 31, -1):
                hc = c - 32
                for i in range(2):
                    h = hp * 2 + i
                    state_update(chs[i], kkh[i][:, hc, :], vvh[i][:, hc, :], TSb[:, c, h:h + 1], DBb[:, h, c:c + 1], [Rkh[i]])
                precast_some(2)
            for i in range(2):
                h = hp * 2 + i
                P.op("act", lambda e: e.activation(out=Cb_all[:, h], in_=chs[i]["Cst"][:], func=AF.Copy), reads=[chs[i]["RC"]], writes=[RCall])
                P.op("act", lambda e: e.activation(out=nb_all[:, h], in_=chs[i]["nst"][:], func=AF.Copy), reads=[chs[i]["RC"]], writes=[RCall])
    P.barrier()

    with contextlib.ExitStack() as st:
        chs = [make_chain(st, "m0", 0, 1), make_chain(st, "m1", 2, 3)]
        qT = sb(st, "qT", (128, 4, T), BF16)
        kT = sb(st, "kT", (128, 4, T), BF16)
        kk = sb(st, "kk", (64, 32, 512), BF16)
        vv = sb(st, "vv", (64, 32, 512), BF16)
        Rq, Rk = Res(), Res()
        dq = [mkds() for _ in range(4)]
        hst = [sb(st, "hst%d" % i, (64, 512), F32) for i in range(2)]
        Rhst = [Res(), Res()]
        dhst = [mkds(), mkds()]
        hfl = [sb(st, "hfl%d" % i, (64, 512), F32) for i in range(4)]
        Rhfl = [Res() for _ in range(4)]
        dhfl = [mkds() for _ in range(4)]
        osl = [sb(st, "osl%d" % i, (64, 512), BF16) for i in range(4)]
        Rosl = [Res() for _ in range(4)]
        dosl = [mkds() for _ in range(4)]

        def prefetch_fin(h, d, c, slot):
            q = d * 2 + slot
            tk = slice(c * 64, (c + 1) * 64)
            P.op("sp", lambda e: e.dma_start(out=hfl[q][:], in_=S_hf[tk, h * 512:(h + 1) * 512]), reads=[RShf[c]], writes=[Rhfl[q]], dsem=dhfl[q])
            P.op("sp", lambda e: e.dma_start(out=osl[q][:], in_=S_o[tk, h * 512:(h + 1) * 512]), reads=[BP], writes=[Rosl[q]], dsem=dosl[q])
        hmo = [sb(st, "hmo%d" % i, (64, 512), BF16) for i in range(2)]
        Rhmo = [Res(), Res()]
        dhmo = [mkds(), mkds()]
        hjunk = sb(st, "hjunk", (64, 512), BF16)
        Rhj = Res()
        hsum = [sb(st, "hsum%d" % i, (64, 512), F32) for i in range(2)]
        Rhs = [Res(), Res()]
        mg = sb(st, "mgb", (64, 512), F32)
        Rmg = Res()
        dmg = mkds()
        RShf = [Res() for _ in range(32)]
        RShm = Res()

        def chunk_step(h, d, c, second, slot=0):
            ch = chs[d]
            Cst, nst, Cb, nb, RC, RCb, St, RSt, sm, Rms = (ch[k] for k in "Cst nst Cb nb RC RCb St RSt sm Rms".split())
            b_s, b_n = ch["sbank"], ch["nbank"]
            tk = slice(c * 64, (c + 1) * 64)
            if d == 0:
                wk, e2, decay, mk = TSf[:, 0, c, h:h + 1], TSf[:, 1, c, h:h + 1], DBf[:, h, c:c + 1], maskf
            else:
                wk, e2, decay, mk = TSb[:, c, h:h + 1], TSb[:, 64 + c, h:h + 1], DBb[:, h, c:c + 1], maskb
            i = ch["cc"][0] % 2
            P.op("act", lambda e: e.activation(out=Cb[:], in_=Cst[:], func=AF.Copy, scale=decay), reads=[RC, RTS], writes=[RCb])
            P.op("act", lambda e: e.activation(out=nb[:], in_=nst[:], func=AF.Copy, scale=decay), reads=[RC, RTS], writes=[RCb])
            for j in range(4):
                P.op("pe", lambda e: e.matmul(ps[b_s][0:64, 0:64], lhsT=kT[:, j, tk], rhs=qT[:, j, tk], start=(j == 0), stop=(j == 3)),
                     reads=[Rq], writes=[Rps[b_s]], inc=(j == 3))
            P.op("dve", lambda e: e.scalar_tensor_tensor(out=St[i][:], in0=ps[b_s][0:64, 0:64], scalar=wk, in1=mk, op0=ALU.mult, op1=ALU.mult),
                 reads=[Rps[b_s], RTS] + CONST, writes=[RSt[i]])
            for j in range(4):
                P.op("pe", lambda e: e.matmul(ps[b_n][0:64, :], lhsT=qT[:, j, tk], rhs=Cb[:, j, :], start=(j == 0), stop=False),
                     reads=[Rq, RCb], writes=[Rps[b_n]], inc=False)
            P.op("pe", lambda e: e.matmul(ps[b_n][0:64, :], lhsT=St[i][:], rhs=vv[:, c, :], start=False, stop=True),
                 reads=[RSt[i], Rk], writes=[Rps[b_n]])
            for j in range(4):
                P.op("pe", lambda e: e.matmul(ps[b_s][0:64, 64:65], lhsT=qT[:, j, tk], rhs=nb[:, j:j + 1], start=(j == 0), stop=False),
                     reads=[Rq, RCb], writes=[Rps[b_s]], inc=False)
            P.op("pe", lambda e: e.matmul(ps[b_s][0:64, 64:65], lhsT=St[i][:], rhs=onesb[0:64, 0:1], start=False, stop=True),
                 reads=[RSt[i]] + CONST, writes=[Rps[b_s]])
            P.op("act", lambda e: e.activation(out=sm[:, 5:6], in_=ps[b_s][0:64, 64:65], func=AF.Abs), reads=[Rps[b_s]], writes=[Rms])
            P.op("dve", lambda e: e.tensor_scalar(out=sm[:, 0:1], in0=sm[:, 5:6], scalar1=e2, scalar2=None, op0=ALU.max), reads=[Rms, RTS], writes=[Rms])
            P.op("dve", lambda e: e.reciprocal(out=sm[:, 1:2], in_=sm[:, 0:1]), reads=[Rms], writes=[Rms])
            q = d
            if not second:
                P.op("dve", lambda e: e.tensor_scalar(out=hst[q][:], in0=ps[b_n][0:64, :], scalar1=sm[:, 1:2], scalar2=None, op0=ALU.mult),
                     reads=[Rps[b_n], Rms], writes=[Rhst[q]])
                P.op("sp", lambda e: e.dma_start(out=S_hf[tk, h * 512:(h + 1) * 512], in_=hst[q][:]), reads=[Rhst[q]], writes=[RShf[c]], dsem=dhst[q])
            else:
                q4 = d * 2 + slot
                P.op("dve", lambda e: e.scalar_tensor_tensor(out=hsum[q][:], in0=ps[b_n][0:64, :], scalar=sm[:, 1:2], in1=hfl[q4][:], op0=ALU.mult, op1=ALU.add),
                     reads=[Rps[b_n], Rms, Rhfl[q4]], writes=[Rhs[q]])
                P.op("act", lambda e: e.activation(out=hjunk[:], in_=hsum[q][:], func=AF.Square, accum_out=sm[:, 2:3]), reads=[Rhs[q]], writes=[Rhj, Rms])
                P.op("act", lambda e: e.activation(out=sm[:, 3:4], in_=sm[:, 2:3], func=AF.Ln, scale=1.0 / 512, bias=EPS), reads=[Rms], writes=[Rms])
                P.op("act", lambda e: e.activation(out=sm[:, 4:5], in_=sm[:, 3:4], func=AF.Exp, scale=-0.5), reads=[Rms], writes=[Rms])
                P.op("dve", lambda e: e.scalar_tensor_tensor(out=hsum[q][:], in0=hsum[q][:], scalar=sm[:, 4:5], in1=mg[:], op0=ALU.mult, op1=ALU.mult),
                     reads=[Rhs[q], Rms, Rmg], writes=[Rhs[q]])
                P.op("pool", lambda e: e.tensor_tensor(out=hmo[q][:], in0=hsum[q][:], in1=osl[q4][:], op=ALU.mult),
                     reads=[Rhs[q], Rosl[q4]], writes=[Rhmo[q]])
                P.op("sp", lambda e: e.dma_start(out=S_hm[tk, h * 512:(h + 1) * 512], in_=hmo[q][:]), reads=[Rhmo[q]], writes=[RShm], dsem=dhmo[q])
            state_update(ch, kk[:, c, :], vv[:, c, :], wk, decay, [Rk])

        for h in range(4):
            P.op("sp", lambda e: e.dma_start(out=qT[:], in_=S_qT[h * 512:(h + 1) * 512, :].rearrange("(j p) t -> p j t", p=128)),
                 reads=[BP], writes=[Rq], dsem=dq[0])
            P.op("sp", lambda e: e.dma_start(out=kT[:], in_=S_kT[h * 512:(h + 1) * 512, :].rearrange("(j p) t -> p j t", p=128)),
                 reads=[BP], writes=[Rq], dsem=dq[1])
            P.op("sp", lambda e: e.dma_start(out=kk[:], in_=S_k[:, h * 512:(h + 1) * 512].rearrange("(c p) d -> p c d", p=64)),
                 reads=[BP], writes=[Rk], dsem=dq[2])
            P.op("sp", lambda e: e.dma_start(out=vv[:], in_=S_v[:, h * 512:(h + 1) * 512].rearrange("(c p) d -> p c d", p=64)),
                 reads=[BP], writes=[Rk], dsem=dq[3])
            P.op("sp", lambda e: e.dma_start(out=mg[:], in_=mng[:, h * 512:(h + 1) * 512]), writes=[Rmg], dsem=dmg)
            P.op("dve", lambda e: e.memset(chs[0]["Cst"][:], 0.0), writes=[chs[0]["RC"]])
            P.op("dve", lambda e: e.memset(chs[0]["nst"][:], 0.0), writes=[chs[0]["RC"]])
            P.op("act", lambda e: e.activation(out=chs[1]["Cst"][:], in_=Cb_all[:, h], func=AF.Copy), reads=[RCall], writes=[chs[1]["RC"]])
            P.op("act", lambda e: e.activation(out=chs[1]["nst"][:], in_=nb_all[:, h], func=AF.Copy), reads=[RCall], writes=[chs[1]["RC"]])
            for s_ in range(32):
                chunk_step(h, 0, s_, s_ >= 16, s_ % 2)
                chunk_step(h, 1, 31 - s_, s_ >= 16, s_ % 2)
                if 15 <= s_ < 31:
                    prefetch_fin(h, 0, s_ + 1, (s_ + 1) % 2)
                    prefetch_fin(h, 1, 31 - (s_ + 1), (s_ + 1) % 2)
                precast_some(1)
        precast_some(2 * NEB)
        L["BU_holder"][0] = barrier_from([RSU] + Rpcb)
        BM = barrier_from([RShm, Rhmo[0], Rhmo[1]])
    sthm.close()
    P.barrier()
    out_toks.append(BM.w)
    if L["UPTO"] == "M":
        return
    build_tail(nc, P, top, sb, mkds, ps, Rps, nextbank, CONST, BP, BM, L, out_toks)


def build_tail(nc, P, top, sb, mkds, ps, Rps, nextbank, CONST, BP, BM, L, out_toks):
    identb, onesb = L["identb"], L["onesb"]
    g2 = L["g2"]
    S_aqT, S_akT, S_av, S_gmT, S_gaT, S_hm, S_haT, S_x1 = (L[k] for k in "S_aqT S_akT S_av S_gmT S_gaT S_hm S_haT S_x1".split())
    S_UT, S_V, uT, pv, keysT, S_sub, S_xn2 = L["S_UT"], L["S_V"], L["uT"], L["pv"], L["keysT"], L["S_sub"], L["S_xn2"]
    sinkb, btab, x_own, y = L["sinkb"], L["btab"], L["x_own"], L["y"]
    w_mp, w_ap, w_o, w_pq = L["w_mp"], L["w_ap"], L["w_o"], L["w_pq"]
    barrier_from, norm_T = L["barrier_from"], L["norm_T"]

    BU = L["BU_holder"][0]
    with contextlib.ExitStack() as st:
        EB = sb(st, "EB", (128, 16, 3, 128), F32)
        esk = sb(st, "esk", (128, 16), F32)
        REB = Res()
        dEB = mkds(st)
        P.op("sp", lambda e: e.dma_start(out=EB[:].rearrange("p h o q -> p (h o q)"), in_=btab[:, :]), writes=[REB], dsem=dEB)
        P.op("sp", lambda e: e.dma_start(out=esk[:], in_=sinkb[:, :]), writes=[REB], dsem=dEB)
        P.op("act", lambda e: e.activation(out=EB[:].rearrange("p h o q -> p (h o q)"), in_=EB[:].rearrange("p h o q -> p (h o q)"), func=AF.Exp),
             reads=[REB], writes=[REB])
        P.op("act", lambda e: e.activation(out=esk[:], in_=esk[:], func=AF.Exp), reads=[REB], writes=[REB])
        m128 = sb(st, "m128", (128, 2, 128), F32)
        dm = mkds(st)
        P.op("sp", lambda e: e.dma_start(out=m128[:].rearrange("p a q -> p (a q)"), in_=L["cst"][:, 4480:4736]), writes=[REB], dsem=dm)
        for o, a in ((0, 0), (2, 1)):
            P.op("dve", lambda e, o=o, a=a: e.tensor_tensor(out=EB[:, :, o, :], in0=EB[:, :, o, :], in1=m128[:, a, :].unsqueeze(1).broadcast_to([128, 16, 128]), op=ALU.mult),
                 reads=[REB], writes=[REB])
        aqT = sb(st, "aqT", (128, 16, T), BF16)
        akT = sb(st, "akT", (128, 4, T + 128), BF16)
        av = sb(st, "av", (128, 17, 512), BF16)
        Ra = Res()
        da = [mkds(st) for _ in range(3)]
        P.op("sp", lambda e: e.dma_start(out=aqT[:], in_=S_aqT.rearrange("(h p) t -> p h t", p=128)), reads=[BP], writes=[Ra], dsem=da[0])
        P.op("sp", lambda e: e.dma_start(out=akT[:], in_=S_akT.rearrange("(h p) t -> p h t", p=128)), reads=[BP], writes=[Ra], dsem=da[1])
        P.op("sp", lambda e: e.dma_start(out=av[:], in_=S_av.rearrange("(t p) n -> p t n", p=128)), reads=[BP], writes=[Ra], dsem=da[2])
        pex = [sb(st, "pex%d" % i, (128, 512), F32) for i in range(3)]
        Rpex = [Res() for _ in range(3)]
        pT = [sb(st, "pT%d" % i, (128, 512), BF16) for i in range(6)]
        RpT = [Res() for _ in range(6)]
        zt = sb(st, "zt", (128, 512), F32)
        Rz = Res()
        hst = [sb(st, "hag%d" % i, (128, 4, T), BF16) for i in range(2)]
        Rhst = [Res(), Res()]
        dhst = [mkds(st), mkds(st)]
        RSha = Res()
        kk = 0
        for g in range(4):
            for i in range(NT):
                os_ = [o for o in (-1, 0, 1) if i + o >= 0]
                pts = []
                for o in os_:
                    bk = nextbank(0, 4)
                    P.op("pe", lambda e, bk=bk, o=o, i=i: e.matmul(ps[bk][:].rearrange("p (h q) -> p h q", h=4), lhsT=akT[:, g, (i + o) * 128:(i + o + 1) * 128],
                                                                    rhs=aqT[:, 4 * g:4 * g + 4, i * 128:(i + 1) * 128], start=True, stop=True),
                         reads=[Ra], writes=[Rps[bk]])
                    a = kk % 3
                    b = kk % 6
                    kk += 1
                    P.op("act", lambda e, bk=bk, a=a: e.activation(out=pex[a][:], in_=ps[bk][:], func=AF.Exp), reads=[Rps[bk]], writes=[Rpex[a]])
                    P.op("dve", lambda e, a=a, b=b, o=o: e.tensor_tensor(out=pT[b][:].rearrange("p (h q) -> p h q", h=4), in0=pex[a][:].rearrange("p (h q) -> p h q", h=4),
                                                                          in1=EB[:, 4 * g:4 * g + 4, o + 1, :], op=ALU.mult),
                         reads=[Rpex[a], REB], writes=[RpT[b]])
                    pts.append((o, b))
                bo = nextbank(4, 6)
                bz = nextbank(6, 8)
                for n_, (o, b) in enumerate(pts):
                    P.op("pe", lambda e, o=o, b=b, n_=n_, i=i, bo=bo: e.matmul(ps[bo][:], lhsT=av[:, i + o, g * 128:(g + 1) * 128], rhs=pT[b][:], start=(n_ == 0), stop=(n_ == len(pts) - 1)),
                         reads=[Ra, RpT[b]], writes=[Rps[bo]], inc=(n_ == len(pts) - 1))
                for n_, (o, b) in enumerate(pts):
                    P.op("pe", lambda e, b=b, n_=n_, bz=bz: e.matmul(ps[bz][:], lhsT=onesb[:], rhs=pT[b][:], start=(n_ == 0), stop=(n_ == len(pts) - 1)),
                         reads=[RpT[b]] + CONST, writes=[Rps[bz]], inc=(n_ == len(pts) - 1))
                P.op("dve", lambda e, bz=bz: e.tensor_tensor(out=zt[:].rearrange("p (h q) -> p h q", h=4), in0=ps[bz][:].rearrange("p (h q) -> p h q", h=4),
                                                              in1=esk[:, 4 * g:4 * g + 4].unsqueeze(2).broadcast_to([128, 4, 128]), op=ALU.add),
                     reads=[Rps[bz], REB], writes=[Rz])
                P.op("dve", lambda e: e.reciprocal(out=zt[:], in_=zt[:]), reads=[Rz], writes=[Rz])
                P.op("dve", lambda e, bo=bo, i=i: e.tensor_tensor(out=hst[g % 2][:, :, i * 128:(i + 1) * 128], in0=ps[bo][:].rearrange("p (h q) -> p h q", h=4),
                                                                   in1=zt[:].rearrange("p (h q) -> p h q", h=4), op=ALU.mult),
                     reads=[Rps[bo], Rz], writes=[Rhst[g % 2]])
            P.op("sp", lambda e, g=g: e.dma_start(out=S_haT[g * 512:(g + 1) * 512, :].rearrange("(h p) t -> p h t", p=128), in_=hst[g % 2][:]),
                 reads=[Rhst[g % 2]], writes=[RSha], dsem=dhst[g % 2])
        BT = barrier_from([RSha, Rhst[0], Rhst[1]])
    P.barrier()
    out_toks.append(BT.w)
    if L["UPTO"] == "T":
        return

    with contextlib.ExitStack() as st:
        with contextlib.ExitStack() as st2:
            mT = sb(st2, "mT", (128, 16, T), BF16)
            RmT = Res()
            wr = [sb(st2, "owr%d" % i, (128, 16, 512), BF16) for i in range(2)]
            Rwr = [Res(), Res()]
            dwr = [mkds(st2), mkds(st2)]
            wc = [0]

            def load_w(wsrc, c0):
                s = wc[0] % 2
                wc[0] += 1
                P.op("pool", lambda e: e.dma_start(out=wr[s][:], in_=wsrc[:, c0:c0 + 512].rearrange("(c p) n -> p c n", p=128)), writes=[Rwr[s]], dsem=dwr[s])
                return s

            with contextlib.ExitStack() as st3:
                hT = sb(st3, "hT", (128, 16, T), BF16)
                RhT = Res()
                gl = [sb(st3, "gl%d" % i, (128, T), BF16) for i in range(2)]
                Rgl = [Res(), Res()]
                dgl = [mkds(st3), mkds(st3)]
                tmpf = sb(st3, "tmpf", (128, 512), F32)
                Rtf = Res()
                for br in range(2):
                    if br == 0:
                        ht = [sb(st3, "htl%d" % i, (128, D), BF16) for i in range(2)]
                        Rht = [Res(), Res()]
                        dht = [mkds(st3), mkds(st3)]
                        for t in range(NT):
                            b = t % 2
                            P.op("sp", lambda e, b=b, t=t: e.dma_start(out=ht[b][:], in_=S_hm[t * 128:(t + 1) * 128, :]), reads=[BM], writes=[Rht[b]], dsem=dht[b])
                            for gq in range(4):
                                bk = nextbank()
                                ptv = ps[bk][:].bitcast(BF16)[:, 0:512].rearrange("p (j n) -> p j n", j=4)
                                for j in range(4):
                                    c = gq * 4 + j
                                    P.op("pe", lambda e, c=c, j=j, ptv=ptv, b=b: e.transpose(out=ptv[:, j, :], in_=ht[b][:, c * 128:(c + 1) * 128], identity=identb[:]),
                                         reads=[Rht[b]] + CONST, writes=[Rps[bk]], inc=(j == 3))
                                if gq % 2 == 0:
                                    P.op("act", lambda e, gq=gq, t=t, ptv=ptv: e.activation(out=hT[:, gq * 4:(gq + 1) * 4, t * 128:(t + 1) * 128], in_=ptv, func=AF.Copy),
                                         reads=[Rps[bk]], writes=[RhT])
                                else:
                                    P.op("dve", lambda e, gq=gq, t=t, ptv=ptv: e.tensor_copy(out=hT[:, gq * 4:(gq + 1) * 4, t * 128:(t + 1) * 128], in_=ptv),
                                         reads=[Rps[bk]], writes=[RhT])
                        wsrc, gsrc = w_mp, S_gmT
                    else:
                        dhT = mkds(st3)
                        P.op("sp", lambda e: e.dma_start(out=hT[:], in_=S_haT.rearrange("(c p) t -> p c t", p=128)), reads=[BT], writes=[RhT], dsem=dhT)
                        wsrc, gsrc = w_ap, S_gaT
                    for cb_ in range(4):
                        s = load_w(wsrc, cb_ * 512)
                        for j in range(4):
                            jj = cb_ * 4 + j
                            q = jj % 2
                            P.op("sp", lambda e, q=q, jj=jj, gsrc=gsrc: e.dma_start(out=gl[q][:], in_=gsrc[jj * 128:(jj + 1) * 128, :]), reads=[BP], writes=[Rgl[q]], dsem=dgl[q])
                            for tb in range(4):
                                bk = nextbank()
                                for c in range(16):
                                    P.op("pe", lambda e, c=c, j=j, tb=tb, bk=bk, s=s: e.matmul(ps[bk][:], lhsT=wr[s][:, c, j * 128:(j + 1) * 128], rhs=hT[:, c, tb * 512:(tb + 1) * 512],
                                                                                                 start=(c == 0), stop=(c == 15)),
                                         reads=[Rwr[s], RhT], writes=[Rps[bk]], inc=(c == 15))
                                if br == 0:
                                    P.op("dve", lambda e, jj=jj, tb=tb, bk=bk, q=q: e.tensor_tensor(out=mT[:, jj, tb * 512:(tb + 1) * 512], in0=ps[bk][:], in1=gl[q][:, tb * 512:(tb + 1) * 512], op=ALU.mult),
                                         reads=[Rps[bk], Rgl[q]], writes=[RmT])
                                else:
                                    P.op("dve", lambda e, tb=tb, bk=bk, q=q: e.tensor_tensor(out=tmpf[:], in0=ps[bk][:], in1=gl[q][:, tb * 512:(tb + 1) * 512], op=ALU.mult),
                                         reads=[Rps[bk], Rgl[q]], writes=[Rtf])
                                    P.op("pool", lambda e, jj=jj, tb=tb: e.tensor_tensor(out=mT[:, jj, tb * 512:(tb + 1) * 512], in0=mT[:, jj, tb * 512:(tb + 1) * 512], in1=tmpf[:], op=ALU.add),
                                         reads=[Rtf, RmT], writes=[RmT])
            P.barrier()
            xl = [sb(st2, "xl%d" % i, (128, 512), F32) for i in range(2)]
            Rxl = [Res(), Res()]
            dxl = [mkds(st2), mkds(st2)]
            x1s = [sb(st2, "x1s%d" % i, (128, 512), F32) for i in range(2)]
            Rx1s = [Res(), Res()]
            dx1s = [mkds(st2), mkds(st2)]
            RSx1 = Res()
            kx = 0
            for cb_ in range(4):
                s = load_w(w_o, cb_ * 512)
                for t in range(NT):
                    b = kx % 2
                    kx += 1
                    P.op("sp", lambda e, b=b, t=t, cb_=cb_: e.dma_start(out=xl[b][:], in_=x_own[t * 128:(t + 1) * 128, cb_ * 512:(cb_ + 1) * 512]), writes=[Rxl[b]], dsem=dxl[b])
                    bk = nextbank()
                    for c in range(16):
                        P.op("pe", lambda e, c=c, t=t, bk=bk, s=s: e.matmul(ps[bk][:], lhsT=mT[:, c, t * 128:(t + 1) * 128], rhs=wr[s][:, c, :], start=(c == 0), stop=(c == 15)),
                             reads=[RmT, Rwr[s]], writes=[Rps[bk]], inc=(c == 15))
                    P.op("dve", lambda e, b=b, bk=bk: e.tensor_tensor(out=x1s[b][:], in0=ps[bk][:], in1=xl[b][:], op=ALU.add),
                         reads=[Rps[bk], Rxl[b]], writes=[Rx1s[b]])
                    P.op("sp", lambda e, b=b, t=t, cb_=cb_: e.dma_start(out=S_x1[t * 128:(t + 1) * 128, cb_ * 512:(cb_ + 1) * 512], in_=x1s[b][:]),
                         reads=[Rx1s[b]], writes=[RSx1], dsem=dx1s[b])
            BX = barrier_from([RSx1] + Rx1s)
        P.barrier()
        stx = contextlib.ExitStack()
        xn2T = sb(stx, "xn2T", (128, 16, T), BF16)
        Rxn2 = [Res() for _ in range(NT)]
        with contextlib.ExitStack() as st2:
            def get_x1(t, dst, Rd, dsm):
                P.op("sp", lambda e: e.dma_start(out=dst[:], in_=S_x1[t * 128:(t + 1) * 128, :]), reads=[BX], writes=[Rd], dsem=dsm)
            norm_T(st2, get_x1, NT, xn2T, Rxn2, g2, "2")
        P.barrier()

        with contextlib.ExitStack() as stq:
            pqT = sb(stq, "pqT", (128, 16, T), BF16)
            RpqT = Res()
            with contextlib.ExitStack() as st2:
                wr = [sb(st2, "qwr%d" % i, (128, 16, 512), BF16) for i in range(2)]
                Rwr = [Res(), Res()]
                dwr = [mkds(st2), mkds(st2)]
                for cb_ in range(4):
                    s = cb_ % 2
                    P.op("pool", lambda e, s=s, cb_=cb_: e.dma_start(out=wr[s][:], in_=w_pq[:, cb_ * 512:(cb_ + 1) * 512].rearrange("(c p) n -> p c n", p=128)), writes=[Rwr[s]], dsem=dwr[s])
                    for j in range(4):
                        for tb in range(4):
                            bk = nextbank()
                            for c in range(16):
                                P.op("pe", lambda e, c=c, j=j, tb=tb, bk=bk, s=s: e.matmul(ps[bk][:], lhsT=wr[s][:, c, j * 128:(j + 1) * 128], rhs=xn2T[:, c, tb * 512:(tb + 1) * 512], start=(c == 0), stop=(c == 15)),
                                     reads=[Rwr[s]] + Rxn2[tb * 4:tb * 4 + 4], writes=[Rps[bk]], inc=(c == 15))
                            if (j + tb) % 2 == 0:
                                P.op("act", lambda e, j=j, tb=tb, bk=bk, cb_=cb_: e.activation(out=pqT[:, cb_ * 4 + j, tb * 512:(tb + 1) * 512], in_=ps[bk][:], func=AF.Copy), reads=[Rps[bk]], writes=[RpqT])
                            else:
                                P.op("dve", lambda e, j=j, tb=tb, bk=bk, cb_=cb_: e.tensor_copy(out=pqT[:, cb_ * 4 + j, tb * 512:(tb + 1) * 512], in_=ps[bk][:]), reads=[Rps[bk]], writes=[RpqT])
            P.barrier()
            kTb = sb(stq, "kTb", (128, 16, 128), BF16)
            RkT = Res()
            dkT = mkds(stq)
            P.op("pool", lambda e: e.dma_start(out=kTb[:].rearrange("p a n -> p (a n)"), in_=keysT[:, :]), writes=[RkT], dsem=dkT)
            subs = [sb(stq, "subs%d" % i, (128, 16, 128), F32) for i in range(2)]
            Rsubs = [Res(), Res()]
            dsubs = [mkds(stq), mkds(stq)]
            RSsub = Res()
            for t in range(NT):
                tk = slice(t * 128, (t + 1) * 128)
                b = t % 2
                for qd in range(4):
                    bk = nextbank()
                    for r in range(4):
                        hp = qd * 4 + r
                        P.op("pe", lambda e, hp=hp, r=r, bk=bk, tk=tk: e.matmul(ps[bk][:, r * 128:(r + 1) * 128], lhsT=pqT[:, hp, tk], rhs=kTb[:, hp, :], start=True, stop=True),
                             reads=[RpqT, RkT], writes=[Rps[bk]], inc=(r == 3))
                    P.op("act", lambda e, qd=qd, bk=bk, b=b: e.activation(out=subs[b][:, qd * 4:(qd + 1) * 4, :], in_=ps[bk][:].rearrange("p (r n) -> p r n", r=4), func=AF.Copy),
                         reads=[Rps[bk]], writes=[Rsubs[b]])
                P.op("sp", lambda e, b=b, tk=tk: e.dma_start(out=S_sub[tk, :], in_=subs[b][:].rearrange("p a n -> p (a n)")), reads=[Rsubs[b]], writes=[RSsub], dsem=dsubs[b])
            BS = barrier_from([RSsub] + Rsubs)
        RSxn2 = Res()
        dxd = mkds(st)
        for t in range(NT):
            P.op("sp", lambda e: e.dma_start(out=S_xn2[t, :, :].rearrange("p (c n) -> p c n", c=16), in_=xn2T[:, :, t * 128:(t + 1) * 128]), reads=[Rxn2[t]], writes=[RSxn2], dsem=dxd)
        BXN = barrier_from([RSxn2])
        stx.close()
        P.barrier()
        out_toks.append(BS.w)
        out_toks.append(BX.w)
        if L["UPTO"] == "O":
            return
        sub = sb(st, "sub", (128, 16, 128), F32)
        tmp = sb(st, "ptmp", (128, 16, 128), F32)
        sv = sb(st, "psv", (128, 16, 16), F32)
        cand = sb(st, "cand", (128, 8, 256), F32)
        tmp2 = tmp[:].rearrange("p (h two) n -> p h (two n)", two=2)
        c1 = sb(st, "c1", (128, 8, 16), F32)
        dmat = sb(st, "pdm", (128, 8, 16), F32)
        sc = sb(st, "psc", (128, 8, 7), F32)
        dg = sb(st, "pdg", (128, 8, 128), BF16)
        Rdg = Res()
        b3 = sb(st, "b3", (128, 8, 128), F32)
        Rsub, Rsv, Rc1, Rsc, Rb3 = (Res() for _ in range(5))
        REg = [[Res(), Res(), Res()], [Res(), Res(), Res()]]
        RE = REg[0] + REg[1]
        RG = [Res() for _ in range(4)]
        Ebuf = [tmp[:, 8 * i:8 * (i + 1), :] for i in range(2)]
        Gp = [cand[:].rearrange("p h n -> p (h n)").bitcast(BF16)[:, 1024 * i:1024 * (i + 1)] for i in range(4)]
        dsub = mkds(st)
        hraw = [sb(st, "hraw%d" % i, (128, NEB, 128), BF16) for i in range(2)]
        Rh = [Res(), Res()]
        NRING = 4
        ub = [sb(st, "ub%d" % i, (128, 2, 16, 128), BF16) for i in range(NRING)]
        Rub = [Res() for _ in range(NRING)]
        dub = [mkds(st) for _ in range(NRING)]
        vb = [sb(st, "vb%d" % i, (128, 2, D), BF16) for i in range(NRING)]
        Rvb = [Res() for _ in range(NRING)]
        dvb = [mkds(st) for _ in range(NRING)]
        xt2 = [sb(st, "xt2_%d" % i, (128, 16, 128), BF16) for i in range(2)]
        Rxt2 = [Res(), Res()]
        dxt2 = [mkds(st), mkds(st)]
        ut_next = [0]
        v_next = [0]

        def issue_ut(upto):
            while ut_next[0] < min(upto, NT * 64):
                g = ut_next[0]
                ut_next[0] += 1
                p_ = g % 64
                i = g % NRING
                P.op("sp", lambda e: e.dma_start(out=ub[i][:].rearrange("p b c n -> p b (c n)"), in_=S_UT[2 * p_:2 * p_ + 2, :, :].rearrange("b p n -> p b n")),
                     reads=[BU], writes=[Rub[i]], dsem=dub[i])

        def issue_v(upto):
            while v_next[0] < min(upto, NT * 64):
                g = v_next[0]
                v_next[0] += 1
                p_ = g % 64
                i = g % NRING
                P.op("sp", lambda e: e.dma_start(out=vb[i][:], in_=S_V[2 * p_:2 * p_ + 2, :, :].rearrange("b p n -> p b n")), reads=[BU], writes=[Rvb[i]], dsem=dvb[i])

        def load_xt2(tt):
            P.op("sp", lambda e: e.dma_start(out=xt2[tt % 2][:].rearrange("p c n -> p (c n)"), in_=S_xn2[tt, :, :]), reads=[BXN], writes=[Rxt2[tt % 2]], dsem=dxt2[tt % 2])

        MARGIN = 2.0e-3
        Eb4 = sb(st, "Eb4", (128, 4, 128), F32)
        Ea4 = sb(st, "Ea4", (128, 4, 128), F32)
        REab = Res()
        hstg = [sb(st, "hstg%d" % i, (128, 256), BF16) for i in range(2)]
        Rhstg = [Res(), Res()]
        At = [sb(st, "At%d" % i, (128, 128), BF16) for i in range(3)]
        RAt = [Res() for _ in range(3)]
        x1l = [sb(st, "x1l%d" % i, (128, 256), F32) for i in range(1)] * 2
        Rx1l = [Res()] * 2
        dx1l = [mkds(st)] * 2
        yo = [sb(st, "yo%d" % i, (128, 256), F32) for i in range(1)] * 2
        Ryo = [Res()] * 2
        dyo = [mkds(st)] * 2
        sv4 = sv[:].rearrange("p (h two) k -> p h two k", two=2)
        sub4 = sub[:].rearrange("p (h two) n -> p h two n", two=2)
        ke = 0

        hb_bank = {}

        def h_mm(tt, eb):
            g = tt * 64 + eb // 2
            issue_ut(g + NRING)
            iu = g % NRING
            bk = nextbank(4, 6)
            hb_bank[eb] = bk
            for c in range(16):
                P.op("pe", lambda e: e.matmul(ps[bk][:, 0:256].rearrange("p (b n) -> p b n", b=2), lhsT=xt2[tt % 2][:, c, :], rhs=ub[iu][:, :, c, :], start=(c == 0), stop=(c == 15)),
                     reads=[Rub[iu], Rxt2[tt % 2]], writes=[Rps[bk]], inc=(c == 15))

        def h_cp1(tt, eb):
            bk = hb_bank[eb]
            k = (eb // 2) % 2
            P.op("dve", lambda e: e.tensor_copy(out=hstg[k][:], in_=ps[bk][:, 0:256]), reads=[Rps[bk]], writes=[Rhstg[k]])

        def h_tr(tt, eb):
            bk = hb_bank[eb]
            k = (eb // 2) % 2
            tv = ps[bk][:].bitcast(BF16)[:, 512:768]
            for j in range(2):
                P.op("pe", lambda e: e.transpose(out=tv[:, j * 128:(j + 1) * 128], in_=hstg[k][:, j * 128:(j + 1) * 128], identity=identb[:]),
                     reads=[Rhstg[k]] + CONST, writes=[Rps[bk]], inc=(j == 1))

        def h_cp2(tt, eb):
            bk = hb_bank[eb]
            tv = ps[bk][:].bitcast(BF16)[:, 512:768]
            P.op("act", lambda e: e.activation(out=hraw[tt % 2][:, eb:eb + 2, :], in_=tv.rearrange("p (b n) -> p b n", b=2), func=AF.Copy), reads=[Rps[bk]], writes=[Rh[tt % 2]])

        def h_gelu(tt):
            hv = hraw[tt % 2][:].rearrange("p a n -> p (a n)")
            for qq in range(4):
                P.op("act", lambda e: e.activation(out=hv[:, qq * 4096:(qq + 1) * 4096], in_=hv[:, qq * 4096:(qq + 1) * 4096], func=AF.Gelu),
                     reads=[Rh[tt % 2]], writes=[Rh[tt % 2]])

        load_xt2(0)
        for eb in range(0, NEB, 2):
            h_mm(0, eb)
            h_cp1(0, eb)
            h_tr(0, eb)
            h_cp2(0, eb)
        h_gelu(0)
        for t in range(NT):
            tk = slice(t * 128, (t + 1) * 128)
            P.op("sp", lambda e, tk=tk: e.dma_start(out=sub[:].rearrange("p a n -> p (a n)"), in_=S_sub[tk, :]), reads=[BS], writes=[Rsub], dsem=dsub)
            for hp in range(16):
                P.op("dve", lambda e, hp=hp: e.max(out=sv[:, hp, 0:8], in_=sub[:, hp, :]), reads=[Rsub], writes=[Rsv])
                P.op("dve", lambda e, hp=hp: e.match_replace(out=tmp[:, hp, :], in_to_replace=sv[:, hp, 0:8], in_values=sub[:, hp, :], imm_value=NEG), reads=[Rsub, Rsv], writes=RE)
                P.op("dve", lambda e, hp=hp: e.max(out=sv[:, hp, 8:16], in_=tmp[:, hp, :]), reads=RE, writes=[Rsv])
            P.op("dve", lambda e: e.tensor_tensor(out=cand[:].rearrange("p h (a b) -> p h a b", a=16), in0=sv4[:, :, 0, :].unsqueeze(3).broadcast_to([128, 8, 16, 16]),
                                                   in1=sv4[:, :, 1, :].unsqueeze(2).broadcast_to([128, 8, 16, 16]), op=ALU.add), reads=[Rsv], writes=RG)
            for h in range(8):
                P.op("dve", lambda e, h=h: e.max(out=c1[:, h, 0:8], in_=cand[:, h, :]), reads=RG, writes=[Rc1])
                P.op("dve", lambda e, h=h: e.match_replace(out=tmp2[:, h, :], in_to_replace=c1[:, h, 0:8], in_values=cand[:, h, :], imm_value=NEG), reads=RG + [Rc1], writes=RE)
                P.op("dve", lambda e, h=h: e.max(out=c1[:, h, 8:16], in_=tmp2[:, h, :]), reads=RE, writes=[Rc1])
            P.op("dve", lambda e: e.tensor_tensor(out=dmat[:], in0=c1[:], in1=c1[:, :, 0:1].broadcast_to([128, 8, 16]), op=ALU.subtract), reads=[Rc1], writes=[Rsc])
            P.op("act", lambda e: e.activation(out=dmat[:], in_=dmat[:], func=AF.Exp), reads=[Rsc], writes=[Rsc])
            P.op("dve", lambda e: e.tensor_reduce(out=sc[:, :, 0], in_=dmat[:], axis=AX.X, op=ALU.add), reads=[Rsc], writes=[Rsc])
            P.op("act", lambda e: e.activation(out=sc[:, :, 1], in_=sc[:, :, 0], func=AF.Ln), reads=[Rsc], writes=[Rsc])
            P.op("dve", lambda e: e.scalar_tensor_tensor(out=sc[:, :, 2], in0=c1[:, :, 0], scalar=-1.0, in1=sc[:, :, 1], op0=ALU.mult, op1=ALU.subtract), reads=[Rsc, Rc1], writes=[Rsc])
            P.op("dve", lambda e: e.tensor_tensor(out=sc[:, :, 3], in0=c1[:, :, 15], in1=sc[:, :, 2], op=ALU.add), reads=[Rsc, Rc1], writes=[Rsc])
            P.op("act", lambda e: e.activation(out=sc[:, :, 3], in_=sc[:, :, 3], func=AF.Exp, bias=-MARGIN), reads=[Rsc], writes=[Rsc])
            P.op("dve", lambda e: e.tensor_scalar(out=sc[:, :, 4], in0=c1[:, :, 15], scalar1=-1.0, scalar2=MARGIN, op0=ALU.mult, op1=ALU.add), reads=[Rc1, Rsc], writes=[Rsc])
            P.op("dve", lambda e: e.tensor_tensor(out=b3[:], in0=sub4[:, :, 0, :], in1=sc[:, :, 4:5].broadcast_to([128, 8, 128]), op=ALU.add), reads=[Rsub, Rsc], writes=[Rb3])
            for h in range(8):
                P.op("pool", lambda e: e.tensor_scalar(out=dg[:, h, :], in0=identb[:], scalar1=sc[:, h, 3:4], scalar2=1.0, op0=ALU.mult, op1=ALU.mult),
                     reads=[Rsc] + CONST, writes=[Rdg])
            P.op("dve", lambda e: e.tensor_scalar(out=sc[:, :, 5], in0=sv4[:, :, 1, 0], scalar1=-1.0, scalar2=None, op0=ALU.mult), reads=[Rsv, Rsc], writes=[Rsc])
            P.op("dve", lambda e: e.tensor_tensor(out=sc[:, :, 6], in0=sc[:, :, 4], in1=sv4[:, :, 1, 0], op=ALU.add), reads=[Rsv, Rsc], writes=[Rsc])
            for h in range(4, 8):
                P.op("act", lambda e: e.activation(out=Eb4[:, h - 4, :], in_=sub4[:, h, 1, :], func=AF.Exp, bias=sc[:, h, 5:6]), reads=[Rsub, Rsc], writes=[REab])
                P.op("act", lambda e: e.activation(out=Ea4[:, h - 4, :], in_=sub4[:, h, 0, :], func=AF.Exp, bias=sc[:, h, 6:7]), reads=[Rsub, Rsc], writes=[REab])
            gel = hraw[t % 2]
            Rgel = Rh[t % 2]
            pend_out = None
            for eb in range(NEB):
                if eb == 0 and t + 1 < NT:
                    load_xt2(t + 1)
                if eb % 2 == 1 or eb == 0:
                    issue_v(t * 64 + eb // 2 + NRING)
                es = eb % 2
                gs = eb % 4
                for h in range(4):
                    P.op("act", lambda e: e.activation(out=Ebuf[es][:, h, :], in_=sub4[:, h, 1, :], func=AF.Exp, bias=b3[:, h, eb:eb + 1]),
                         reads=[Rsub, Rb3], writes=[REg[es][0]])
                for h in (4, 5):
                    P.op("pool", lambda e: e.tensor_scalar(out=Ebuf[es][:, h, :], in0=Eb4[:, h - 4, :], scalar1=Ea4[:, h - 4, eb:eb + 1], scalar2=1.0, op0=ALU.mult, op1=ALU.mult),
                         reads=[REab], writes=[REg[es][1]])
                for h in (6, 7):
                    P.op("dve", lambda e: e.tensor_scalar(out=Ebuf[es][:, h, :], in0=Eb4[:, h - 4, :], scalar1=Ea4[:, h - 4, eb:eb + 1], scalar2=None, op0=ALU.mult),
                         reads=[REab], writes=[REg[es][2]])
                P.op("dve", lambda e: e.scalar_tensor_tensor(out=Gp[gs], in0=Ebuf[es].rearrange("p h n -> p (h n)"), scalar=1.0, in1=Ebuf[es].rearrange("p h n -> p (h n)"),
                                                              op0=ALU.is_ge, op1=ALU.mult),
                     reads=REg[es], writes=[RG[gs]])
                if t + 1 < NT:
                    if eb % 2 == 0:
                        h_mm(t + 1, eb)
                    else:
                        h_tr(t + 1, eb - 1)
                bg = nextbank(6, 8)
                for h in range(8):
                    P.op("pe", lambda e: e.matmul(ps[bg][:, 0:128], lhsT=Gp[gs][:, h * 128:(h + 1) * 128], rhs=dg[:, h, :], start=(h == 0), stop=(h == 7)),
                         reads=[RG[gs], Rdg], writes=[Rps[bg]], inc=(h == 7))
                if pend_out is not None:
                    pend_out()
                ia = eb % 3
                P.op("dve", lambda e: e.tensor_tensor(out=At[ia][:], in0=ps[bg][:, 0:128], in1=gel[:, eb, :], op=ALU.mult),
                     reads=[Rps[bg], Rgel], writes=[RAt[ia]])
                if t + 1 < NT:
                    if eb % 2 == 0:
                        h_cp1(t + 1, eb)
                    else:
                        h_cp2(t + 1, eb - 1)

                def mk_out(eb=eb, ia=ia, t=t):
                    iv2 = (t * 64 + eb // 2) % NRING
                    for db in range(4):
                        P.op("pe", lambda e: e.matmul(ps[db][:], lhsT=At[ia][:], rhs=vb[iv2][:, eb % 2, db * 512:(db + 1) * 512], start=(eb == 0), stop=(eb == NEB - 1)),
                             reads=[RAt[ia], Rvb[iv2]], writes=[Rps[db]], inc=(db == 3))
                pend_out = mk_out
            pend_out()
            if t + 1 < NT:
                h_gelu(t + 1)
            x1big = tmp[:].rearrange("p a n -> p (a n)")
            ybig = cand[:].rearrange("p h n -> p (h n)")
            P.op("sp", lambda e: e.dma_start(out=x1big, in_=S_x1[tk, :]), reads=[BX], writes=RE, dsem=dx1l[0])
            for db in range(4):
                P.op("dve", lambda e: e.tensor_tensor(out=ybig[:, db * 512:(db + 1) * 512], in0=ps[db][:], in1=x1big[:, db * 512:(db + 1) * 512], op=ALU.add),
                     reads=[Rps[db]] + RE, writes=RG)
            tok = P.op("sp", lambda e: e.dma_start(out=y[tk, :], in_=ybig), reads=RG, dsem=dyo[0])
            out_toks.append(tok)


def _t5_bucket_static(rel):
    half, max_exact = 16, 8
    ret = np.where(rel > 0, half, 0)
    n = np.abs(rel)
    nf = np.maximum(n, 1).astype(np.float32)
    large = max_exact + (np.log(nf / max_exact) / math.log(128 / max_exact) * (half - max_exact)).astype(np.int32)
    large = np.minimum(large, half - 1)
    return ret + np.where(n < max_exact, n, large)


def _consts():
    c = np.zeros((128, 384 + 4096 + 256), np.float32)
    c[:, 0:128] = np.eye(128, dtype=np.float32)
    c[:, 128:256] = 1.0
    s = np.arange(64)
    c[0:64, 256:320] = (s[:, None] <= s[None, :]).astype(np.float32)
    c[0:64, 320:384] = (s[:, None] >= s[None, :]).astype(np.float32)
    big = np.ones((128, 4096), np.float32)
    k = np.arange(128)
    m_prev = (k[:, None] >= k[None, :]).astype(np.float32)
    m_next = (k[:, None] <= k[None, :]).astype(np.float32)
    c[:, 384:4480] = big
    return c, m_prev, m_next


_NC_CACHE = {}


def kernel(x, norm1_g, w_in, mlstm_gate_b, mlstm_norm_g, w_m_proj, attn_q_norm_g, attn_k_norm_g,
           attn_sink, rel_bias, w_a_proj, w_out, norm2_g, peer_wq, peer_keys, peer_u, peer_v, _debug=False, _upto=None):
    f = lambda a: np.ascontiguousarray(np.asarray(a, dtype=np.float32))
    x, w_in = f(x), f(w_in)
    cst, m_prev, m_next = _consts()
    rm = np.ones((4, 4096), np.float32)
    rm[:, ::64] = 0.0
    shared = {}
    shared["n1g"] = f(np.asarray(norm1_g).reshape(16, 128).T)
    shared["n2g"] = f(np.asarray(norm2_g).reshape(16, 128).T)
    shared["mng"] = f(np.broadcast_to(np.asarray(mlstm_norm_g).reshape(1, D), (64, D)))
    shared["aqg"] = f(np.asarray(attn_q_norm_g).reshape(128, 1))
    shared["akg"] = f(np.asarray(attn_k_norm_g).reshape(128, 1))
    shared["sinkb"] = f(np.broadcast_to(np.asarray(attn_sink).reshape(1, 16), (128, 16)))
    shared["w_in"] = w_in
    shared["w_mp"] = f(w_m_proj)
    shared["w_ap"] = f(w_a_proj)
    shared["w_o"] = f(w_out)
    shared["w_pq"] = f(peer_wq)
    shared["keysT"] = f(np.asarray(peer_keys).reshape(16, 128, 128).transpose(2, 0, 1).reshape(128, 16 * 128))
    shared["uT"] = f(np.asarray(peer_u).reshape(NEB, 128, 16, 128).transpose(0, 3, 2, 1).reshape(NEB, 128, 16 * 128))
    shared["pv"] = f(np.asarray(peer_v).reshape(NEB, 128, D))
    rb = np.asarray(rel_bias, dtype=np.float32)
    gbias = np.asarray(mlstm_gate_b, dtype=np.float32)
    wg_full = w_in[:, O_MG:O_MG + 16]
    kq = np.arange(128)
    in_maps = []
    for core in range(8):
        b, half = core // 2, core % 2
        xs = x[b]
        if half == 1:
            xs = xs[::-1]
        m = dict(shared)
        m["x_own"] = f(xs[:T])
        m["x_halo"] = f(xs[T:])
        cols = []
        gb = np.zeros((4, 4), np.float32)
        for d in range(2):
            td = d ^ half
            for kind in range(2):
                cols.append(wg_full[:, td * 8 + kind * 4: td * 8 + kind * 4 + 4])
                gb[:, d * 2 + kind] = gbias[td, kind, :]
        m["w_gate"] = f(np.concatenate(cols, axis=1))
        m["gate_b"] = gb
        bt = np.zeros((128, 16, 3, 128), np.float32)
        for o in range(3):
            rel = (kq[:, None] + (o - 1) * 128) - kq[None, :]
            if half == 1:
                rel = -rel
            bk = _t5_bucket_static(rel)
            bt[:, :, o, :] = rb[bk].transpose(0, 2, 1)
        m["btab"] = f(bt.reshape(128, -1))
        c2 = cst.copy()
        c2[:, 4480:4480 + 128] = m_prev
        c2[:, 4480 + 128:4480 + 256] = m_next
        c2[0:4, 384:4480] = rm
        m["cst"] = c2
        in_maps.append(m)
    key = (tuple(_debug) if _debug else None, _upto)
    if key not in _NC_CACHE:
        _NC_CACHE[key] = build_nc(debug=_debug, upto=_upto)
    nc = _NC_CACHE[key]
    in_maps = [{k: m[k] for k in nc._in_names} for m in in_maps]
    res = run_bass_kernel_spmd(nc, in_maps, core_ids=list(range(8)))
    out = np.zeros((4, 4096, D), np.float32)
    for core in range(8):
        b, half = core // 2, core % 2
        yc = res.results[core].get("y", np.zeros((T, D), np.float32)) if _debug else res.results[core]["y"]
        if half == 0:
            out[b, :T] = yc
        else:
            out[b, T:] = yc[::-1]
    if _debug:
        return out, res
    return out
```

```python
import contextlib
import math
import numpy as np
import concourse.bass as bass
import concourse.mybir as mybir
from concourse.bass_utils import run_bass_kernel_spmd

F32 = mybir.dt.float32
BF16 = mybir.dt.bfloat16
AF = mybir.ActivationFunctionType
ALU = mybir.AluOpType
AX = mybir.AxisListType

T = 2048
D = 2048
NT = 16
EPS = 1e-6
NEG = -1.0e30
O_MQ, O_MK, O_MV, O_MO, O_MG, O_AQ, O_AK, O_AV, O_GM, O_GA = (
    0, 2048, 4096, 6144, 8192, 8208, 10256, 10768, 11280, 13328)
NEB = 128


class Res:
    __slots__ = ("w", "r")

    def __init__(self):
        self.w = None
        self.r = []


class DSem:
    def __init__(self, sem):
        self.sem = sem
        self.val = 0


class _Rec:
    def __init__(self):
        self.call = None

    def __getattr__(self, name):
        def f(*a, **k):
            self.call = (name, a, k)
            return self
        return f


class Prog:
    ENGS = ("pe", "dve", "act", "pool", "sp")

    def __init__(self, nc, esems):
        self.nc = nc
        self.esem = esems
        self.cnt = {e: 0 for e in self.ENGS}
        self.ops = {e: [] for e in self.ENGS}
        self.seen = {e: {} for e in self.ENGS}
        self.nops = 0
        self.dsems = []
        self.fence = {e: [] for e in self.ENGS}

    def barrier(self):
        toks = [(self.esem[e], self.cnt[e], "x") for e in self.ENGS if self.cnt[e] > 0]
        toks += [(d.sem, d.val, "dma") for d in self.dsems if d.val > 0]
        for e in self.ENGS:
            self.fence[e] = list(toks)

    def op(self, eng, fn, reads=(), writes=(), dsem=None, inc=True):
        waits = {}

        def add(tok):
            if tok is None:
                return
            s, v, e = tok
            if e == "pe" and eng == "pe" and dsem is None:
                return
            k = id(s)
            if self.seen[eng].get(k, 0) >= v:
                return
            if k not in waits or waits[k][1] < v:
                waits[k] = (s, v)

        for R in reads:
            add(R.w)
        for R in writes:
            add(R.w)
            for t in R.r:
                add(t)
        if self.fence[eng]:
            for t in self.fence[eng]:
                add(t)
            self.fence[eng] = []
        for k, (s, v) in waits.items():
            self.seen[eng][k] = v
        if dsem is None:
            if inc:
                self.cnt[eng] += 1
                tok = (self.esem[eng], self.cnt[eng], eng)
                incs = (self.esem[eng], 1)
            else:
                tok = (self.esem[eng], self.cnt[eng] + 1, eng)
                incs = None
        else:
            dsem.val += 16
            tok = (dsem.sem, dsem.val, "dma")
            incs = (dsem.sem, 16)
        for R in reads:
            R.r.append(tok)
        for R in writes:
            R.w = tok
            R.r = []
        rec = _Rec()
        fn(rec)
        self.ops[eng].append((list(waits.values()), rec.call, incs))
        self.nops += 1
        return tok

    def finish(self, toks):
        waits = [(s, v) for (s, v, _) in toks if s is not None]
        self.ops["sp"].append((waits, ("nop", (), {}), (self.esem["sp"], 1)))

    def emit(self, block):
        engmap = {"pe": "tensor", "dve": "vector", "act": "scalar", "pool": "gpsimd", "sp": "sync"}
        for e in self.ENGS:
            ops = self.ops[e]

            def body(engine, ops=ops):
                for waits, fn, incs in ops:
                    for s, v in waits:
                        engine.wait_ge(s, v)
                    ins = getattr(engine, fn[0])(*fn[1], **fn[2])
                    if incs is not None:
                        ins.then_inc(incs[0], incs[1])

            getattr(block, engmap[e])(body)


def build_nc(debug=False, upto=None):
    nc = bass.Bass("TRN2", target_bir_lowering=False)

    LEVELS = ["A", "P1", "P", "G", "M", "T", "O", None]
    lvl = LEVELS.index(upto)
    in_names = []
    nc._in_names = in_names

    def din(name, shape, need=0):
        if lvl < need:
            return None
        in_names.append(name)
        return nc.dram_tensor(name, list(shape), F32, kind="ExternalInput").ap()

    dbgset = set(debug) if debug else set()

    def dscr(name, shape, dt):
        return nc.dram_tensor(name, list(shape), dt, kind="ExternalOutput" if name in dbgset else "Internal").ap()

    x_own = din("x_own", (T, D))
    x_halo = din("x_halo", (T, D))
    w_in = din("w_in", (D, 15376))
    w_gate = din("w_gate", (D, 16))
    gate_b = din("gate_b", (4, 4))
    n1g = din("n1g", (128, 16))
    n2g = din("n2g", (128, 16))
    mng = din("mng", (64, D), need=4)
    aqg = din("aqg", (128, 1))
    akg = din("akg", (128, 1))
    sinkb = din("sinkb", (128, 16), need=5)
    btab = din("btab", (128, 16 * 3 * 128), need=5)
    cst = din("cst", (128, 384 + 4096 + 256))
    w_mp = din("w_mp", (D, D), need=6)
    w_ap = din("w_ap", (D, D), need=6)
    w_o = din("w_o", (D, D), need=6)
    w_pq = din("w_pq", (D, D), need=6)
    keysT = din("keysT", (128, 16 * 128), need=6)
    uT = din("uT", (NEB, 128, 16 * 128), need=5)
    pv = din("pv", (NEB, 128, D), need=5)
    y = nc.dram_tensor("y", [T, D], F32, kind="ExternalOutput").ap()

    S_qT = dscr("S_qT", (D, T), BF16)
    S_kT = dscr("S_kT", (D, T), BF16)
    S_k = dscr("S_k", (T, D), BF16)
    S_v = dscr("S_v", (T, D), BF16)
    S_o = dscr("S_o", (T, D), BF16)
    S_kh = dscr("S_kh", (T, D), BF16)
    S_vh = dscr("S_vh", (T, D), BF16)
    S_aqT = dscr("S_aqT", (D, T), BF16)
    S_akT = dscr("S_akT", (512, T + 128), BF16)
    S_av = dscr("S_av", (T + 128, 512), BF16)
    S_gmT = dscr("S_gmT", (D, T), BF16)
    S_gaT = dscr("S_gaT", (D, T), BF16)
    S_g = dscr("S_g", (4, 4, 2 * T), F32)
    S_hf = dscr("S_hf", (T, D), F32)
    S_hm = dscr("S_hm", (T, D), BF16)
    S_haT = dscr("S_haT", (D, T), BF16)
    S_x1 = dscr("S_x1", (T, D), F32)
    S_sub = dscr("S_sub", (T, D), F32)
    S_xn2 = dscr("S_xn2", (NT, 128, 16 * 128), BF16)
    S_UT = dscr("S_UT", (NEB, 128, D), BF16)
    S_V = dscr("S_V", (NEB, 128, D), BF16)

    with contextlib.ExitStack() as top:
        E = top.enter_context
        esems = {e: E(nc.semaphore("es_" + e)) for e in Prog.ENGS}
        P = Prog(nc, esems)
        block = E(nc.Block())
        nds = [0]

        def mkds(st=None):
            nds[0] += 1
            d_ = DSem(top.enter_context(nc.semaphore("ds%d" % nds[0])))
            P.dsems.append(d_)
            return d_

        def sb(st, name, shape, dt):
            return st.enter_context(nc.sbuf_tensor(name, list(shape), dt))

        cst_sb = sb(top, "cst_sb", (128, 128 + 128 + 64 + 64), F32)
        identb = sb(top, "identb", (128, 128), BF16)
        onesb = sb(top, "onesb", (128, 128), BF16)
        g1 = sb(top, "g1", (128, 16), F32)
        g2 = sb(top, "g2", (128, 16), F32)
        Rc = Res()
        dc = mkds()
        P.op("sp", lambda e: e.dma_start(out=cst_sb[:], in_=cst[:, 0:384]), writes=[Rc], dsem=dc)
        P.op("sp", lambda e: e.dma_start(out=g1[:], in_=n1g[:, :]), writes=[Rc], dsem=dc)
        P.op("sp", lambda e: e.dma_start(out=g2[:], in_=n2g[:, :]), writes=[Rc], dsem=dc)
        identf = cst_sb[:, 0:128]
        onesf = cst_sb[:, 128:256]
        maskf = cst_sb[0:64, 256:320]
        maskb = cst_sb[0:64, 320:384]
        Rcb = Res()
        P.op("dve", lambda e: e.tensor_copy(out=identb[:], in_=identf), reads=[Rc], writes=[Rcb])
        P.op("dve", lambda e: e.tensor_copy(out=onesb[:], in_=onesf), reads=[Rc], writes=[Rcb])
        CONST = [Rc, Rcb]

        ps = [E(nc.psum_tensor("ps%d" % i, [128, 512], F32)) for i in range(8)]
        Rps = [Res() for _ in range(8)]
        bank_ctr = [0]

        def nextbank(lo=0, hi=8):
            n = hi - lo
            b = lo + bank_ctr[0] % n
            bank_ctr[0] += 1
            return b

        def barrier_from(res_list):
            B = Res()
            toks = []
            for R in res_list:
                toks += R.r
                if R.w is not None:
                    toks.append(R.w)
            waits = [(s, v) for (s, v, _) in toks]
            P.cnt["sp"] += 1
            tok = (P.esem["sp"], P.cnt["sp"], "sp")
            P.ops["sp"].append((waits, ("nop", (), {}), (P.esem["sp"], 1)))
            B.w = tok
            return B

        def norm_T(st, get_tile, ntiles, xnT, RxnT, gt, tag):
            xt = [sb(st, "xt%s%d" % (tag, i), (128, D), F32) for i in range(2)]
            Rxt = [Res(), Res()]
            dxt = [mkds(st), mkds(st)]
            junk = sb(st, "junk" + tag, (128, D), BF16)
            xs = sb(st, "xs" + tag, (128, D), BF16)
            sm = sb(st, "sm" + tag, (128, 4), F32)
            Rj, Rxs, Rsm = Res(), Res(), Res()
            for t in range(ntiles):
                b = t % 2
                get_tile(t, xt[b], Rxt[b], dxt[b])
                P.op("act", lambda e, b=b: e.activation(out=junk[:], in_=xt[b][:], func=AF.Square, accum_out=sm[:, 0:1]),
                     reads=[Rxt[b]], writes=[Rj, Rsm])
                P.op("act", lambda e: e.activation(out=sm[:, 1:2], in_=sm[:, 0:1], func=AF.Ln, scale=1.0 / D, bias=EPS),
                     reads=[Rsm], writes=[Rsm])
                P.op("act", lambda e: e.activation(out=sm[:, 2:3], in_=sm[:, 1:2], func=AF.Exp, scale=-0.5),
                     reads=[Rsm], writes=[Rsm])
                P.op("dve", lambda e, b=b: e.tensor_scalar(out=xs[:], in0=xt[b][:], scalar1=sm[:, 2:3], scalar2=None, op0=ALU.mult),
                     reads=[Rxt[b], Rsm], writes=[Rxs])
                for g in range(4):
                    bk = nextbank()
                    ptv = ps[bk][:].bitcast(BF16)[:, 0:512].rearrange("p (j n) -> p j n", j=4)
                    for j in range(4):
                        c = g * 4 + j
                        P.op("pe", lambda e, c=c, j=j, ptv=ptv: e.transpose(out=ptv[:, j, :], in_=xs[:, c * 128:(c + 1) * 128], identity=identb[:]),
                             reads=[Rxs] + CONST, writes=[Rps[bk]], inc=(j == 3))
                    P.op("dve", lambda e, g=g, t=t, ptv=ptv: e.tensor_tensor(
                        out=xnT[:, g * 4:(g + 1) * 4, t * 128:(t + 1) * 128], in0=ptv,
                        in1=gt[:, g * 4:(g + 1) * 4].unsqueeze(2).broadcast_to([128, 4, 128]), op=ALU.mult),
                        reads=[Rps[bk]] + CONST, writes=[RxnT[t]])

        with contextlib.ExitStack() as st:
            xnT = sb(st, "xnT", (128, 16, T), BF16)
            RxnT = [Res() for _ in range(NT)]
            wr = [sb(st, "wr%d" % i, (128, 16, 512), BF16) for i in range(3)]
            Rwr = [Res() for _ in range(3)]
            dwr = [mkds(st) for _ in range(3)]
            wctr = [0]
            stg = [sb(st, "stg%d" % i, (128, 8192), BF16) for i in range(2)]
            Rstg = [Res(), Res()]
            dstg = [mkds(st), mkds(st)]
            sctr = [0]
            wg = sb(st, "wg", (128, 16, 16), BF16)
            gb = sb(st, "gb", (4, 4), F32)
            gqs = sb(st, "gqs", (128, 2), F32)
            gst = [sb(st, "gst%d" % i, (4, 512), F32) for i in range(2)]
            Rgst = [Res(), Res()]
            dgst = [mkds(st), mkds(st)]
            sqt = sb(st, "sqt", (128, 512), BF16)
            lnv = sb(st, "lnv", (128, 512), F32)
            Rsq, Rln = Res(), Res()
            Rw0 = Res()
            dw0 = mkds(st)
            dw0p = mkds(st)
            P.op("pool", lambda e: e.dma_start(out=wg[:], in_=w_gate.rearrange("(c p) n -> p c n", p=128)), writes=[Rw0], dsem=dw0p)
            P.op("sp", lambda e: e.dma_start(out=gb[:], in_=gate_b[:, :]), writes=[Rw0], dsem=dw0)
            P.op("sp", lambda e: e.dma_start(out=gqs[:, 0:1], in_=aqg[:, :]), writes=[Rw0], dsem=dw0)
            P.op("sp", lambda e: e.dma_start(out=gqs[:, 1:2], in_=akg[:, :]), writes=[Rw0], dsem=dw0)
            P.op("dve", lambda e: e.tensor_scalar(out=gqs[:, 0:1], in0=gqs[:, 0:1], scalar1=128.0 ** -0.5, scalar2=None, op0=ALU.mult),
                 reads=[Rw0], writes=[Rw0])

            def load_w(wsrc, c0, n):
                s = wctr[0] % 3
                wctr[0] += 1
                P.op("pool", lambda e: e.dma_start(out=wr[s][:, :, 0:n], in_=wsrc[:, c0:c0 + n].rearrange("(c p) n -> p c n", p=128)),
                     writes=[Rwr[s]], dsem=dwr[s])
                return s

            def new_stg():
                s = sctr[0] % 2
                sctr[0] += 1
                return s

            def fm_mm(s, j, tok0, ntok, bk):
                rd = [Rwr[s]] + RxnT[tok0 // 128:(tok0 + ntok + 127) // 128]
                for c in range(16):
                    P.op("pe", lambda e, c=c: e.matmul(ps[bk][:, 0:ntok], lhsT=wr[s][:, c, j * 128:(j + 1) * 128],
                                                         rhs=xnT[:, c, tok0:tok0 + ntok], start=(c == 0), stop=(c == 15)),
                         reads=rd, writes=[Rps[bk]], inc=(c == 15))

            def tm_mm(s, n, t, bk):
                rd = [Rwr[s], RxnT[t]]
                for c in range(16):
                    P.op("pe", lambda e, c=c: e.matmul(ps[bk][:, 0:n], lhsT=xnT[:, c, t * 128:(t + 1) * 128],
                                                         rhs=wr[s][:, c, 0:n], start=(c == 0), stop=(c == 15)),
                         reads=rd, writes=[Rps[bk]], inc=(c == 15))

            evctr = [0]

            def evac_copy(dst, bk, n, wres, func=None):
                if func is not None or evctr[0] % 2 == 0:
                    f = func if func is not None else AF.Copy
                    P.op("act", lambda e: e.activation(out=dst, in_=ps[bk][:, 0:n], func=f), reads=[Rps[bk]], writes=wres)
                else:
                    P.op("dve", lambda e: e.tensor_copy(out=dst, in_=ps[bk][:, 0:n]), reads=[Rps[bk]], writes=wres)
                evctr[0] += 1

            def evac_qknorm(dst, bk, n, gcol, wres):
                P.op("act", lambda e: e.activation(out=sqt[:, 0:n], in_=ps[bk][:, 0:n], func=AF.Square), reads=[Rps[bk]], writes=[Rsq])
                b2 = nextbank()
                P.op("pe", lambda e: e.matmul(ps[b2][:, 0:n], lhsT=onesb[:], rhs=sqt[:, 0:n], start=True, stop=True),
                     reads=[Rsq] + CONST, writes=[Rps[b2]])
                P.op("act", lambda e: e.activation(out=lnv[:, 0:n], in_=ps[b2][:, 0:n], func=AF.Ln, scale=1.0 / 128, bias=EPS),
                     reads=[Rps[b2]], writes=[Rln])
                P.op("act", lambda e: e.activation(out=lnv[:, 0:n], in_=lnv[:, 0:n], func=AF.Exp, scale=-0.5), reads=[Rln], writes=[Rln])
                P.op("dve", lambda e: e.scalar_tensor_tensor(out=dst, in0=ps[bk][:, 0:n], scalar=gqs[:, gcol:gcol + 1], in1=lnv[:, 0:n],
                                                              op0=ALU.mult, op1=ALU.mult), reads=[Rps[bk], Rln, Rw0], writes=wres)

            def fm_block(wsrc, c0, ncol, dstT, row0, kind, ntok=T, tokdst0=0):
                s = load_w(wsrc, c0, ncol)
                g = new_stg()
                nsub = ncol // 128
                sv = stg[g][:, 0:nsub * ntok].rearrange("p (j t) -> p j t", j=nsub)
                for j in range(nsub):
                    for tb in range(0, ntok, 512):
                        n = min(512, ntok - tb)
                        bk = nextbank()
                        fm_mm(s, j, tb, n, bk)
                        dst = sv[:, j, tb:tb + n]
                        if kind == "plain":
                            evac_copy(dst, bk, n, [Rstg[g]])
                        elif kind == "sig":
                            evac_copy(dst, bk, n, [Rstg[g]], func=AF.Sigmoid)
                        elif kind == "qn":
                            evac_qknorm(dst, bk, n, 0, [Rstg[g]])
                        elif kind == "kn":
                            evac_qknorm(dst, bk, n, 1, [Rstg[g]])
                P.op("sp", lambda e: e.dma_start(out=dstT[row0:row0 + ncol, tokdst0:tokdst0 + ntok].rearrange("(j p) t -> p j t", p=128), in_=sv),
                     reads=[Rstg[g]], dsem=dstg[g])

            def tm_block(wsrc, c0, ncol, dst, dcol0, kind, ntiles=NT, tokdst0=0):
                s = load_w(wsrc, c0, ncol)
                g = new_stg()
                sv = stg[g][:, 0:ntiles * ncol].rearrange("p (t n) -> p t n", t=ntiles)
                for t in range(ntiles):
                    bk = nextbank()
                    tm_mm(s, ncol, t, bk)
                    evac_copy(sv[:, t, :], bk, ncol, [Rstg[g]], func=(AF.Sigmoid if kind == "sig" else None))
                P.op("sp", lambda e: e.dma_start(out=dst[tokdst0:tokdst0 + ntiles * 128, dcol0:dcol0 + ncol].rearrange("(t p) n -> p t n", p=128), in_=sv),
                     reads=[Rstg[g]], dsem=dstg[g])

            def gate_block(ggs, tokdst0):
                for tb in range(4):
                    for gg in ggs:
                        bk = nextbank()
                        for c in range(16):
                            P.op("pe", lambda e, c=c: e.matmul(ps[bk][0:4, :], lhsT=wg[:, c, gg * 4:(gg + 1) * 4],
                                                                 rhs=xnT[:, c, tb * 512:(tb + 1) * 512], start=(c == 0), stop=(c == 15)),
                                 reads=[Rw0] + RxnT[tb * 4:tb * 4 + 4], writes=[Rps[bk]], inc=(c == 15))
                        q = (tb * 4 + gg) % 2
                        P.op("act", lambda e: e.activation(out=gst[q][:], in_=ps[bk][0:4, :], func=AF.Identity, bias=gb[:, gg:gg + 1]),
                             reads=[Rps[bk], Rw0], writes=[Rgst[q]])
                        P.op("sp", lambda e: e.dma_start(out=S_g[gg, :, tokdst0 + tb * 512:tokdst0 + (tb + 1) * 512], in_=gst[q][:]),
                             reads=[Rgst[q]], dsem=dgst[q])

            def x_tile_loader(xsrc):
                def get(t, dst, Rd, dsm):
                    P.op("sp", lambda e: e.dma_start(out=dst[:], in_=xsrc[t * 128:(t + 1) * 128, :]), writes=[Rd], dsem=dsm)
                return get

            with contextlib.ExitStack() as st2:
                norm_T(st2, x_tile_loader(x_halo), NT, xnT, RxnT, g1, "h")
                if upto == "A":
                    D_xnT = nc.dram_tensor("D_xnT", [128, 16 * T], BF16, kind="ExternalOutput").ap()
                    dsx = mkds()
                    tk_ = P.op("sp", lambda e: e.dma_start(out=D_xnT[:, :], in_=xnT[:].rearrange("p c t -> p (c t)")), reads=RxnT, dsem=dsx)
                    P.finish([tk_])
                    P.emit(block)
                    return nc
                gate_block([2, 3], T)
                for hb in range(4):
                    tm_block(w_in, O_MK + hb * 512, 512, S_kh, hb * 512, "plain")
                    tm_block(w_in, O_MV + hb * 512, 512, S_vh, hb * 512, "plain")
                fm_block(w_in, O_AK, 512, S_akT, 0, "kn", ntok=128, tokdst0=T)
                tm_block(w_in, O_AV, 512, S_av, 0, "plain", ntiles=1, tokdst0=T)
                if upto == "P1":
                    BP = barrier_from([Rstg[0], Rstg[1], Rgst[0], Rgst[1]])
                    P.finish([BP.w])
                    P.emit(block)
                    return nc
                norm_T(st2, x_tile_loader(x_own), NT, xnT, RxnT, g1, "o")
            gate_block([0, 1, 2, 3], 0)
            for hb in range(4):
                fm_block(w_in, O_MQ + hb * 512, 512, S_qT, hb * 512, "plain")
                fm_block(w_in, O_MK + hb * 512, 512, S_kT, hb * 512, "plain")
                tm_block(w_in, O_MK + hb * 512, 512, S_k, hb * 512, "plain")
                tm_block(w_in, O_MV + hb * 512, 512, S_v, hb * 512, "plain")
                tm_block(w_in, O_MO + hb * 512, 512, S_o, hb * 512, "sig")
                fm_block(w_in, O_AQ + hb * 512, 512, S_aqT, hb * 512, "qn")
                fm_block(w_in, O_GM + hb * 512, 512, S_gmT, hb * 512, "sig")
                fm_block(w_in, O_GA + hb * 512, 512, S_gaT, hb * 512, "sig")
            fm_block(w_in, O_AK, 512, S_akT, 0, "kn")
            tm_block(w_in, O_AV, 512, S_av, 0, "plain")
            PH_P_DONE = [Rstg[0], Rstg[1], Rgst[0], Rgst[1]]
        P.barrier()

        BP = barrier_from(PH_P_DONE)
        out_toks = [BP.w]
        UPTO = upto
        if upto != "P":
            build_rest(nc, P, top, sb, mkds, ps, Rps, nextbank, CONST, BP, locals(), out_toks)
        P.finish(out_toks)
        P.emit(block)
    return nc


def build_rest(nc, P, top, sb, mkds, ps, Rps, nextbank, CONST, BP, L, out_toks):
    identb, onesb, cst_sb = L["identb"], L["onesb"], L["cst_sb"]
    identf, onesf, maskf, maskb = L["identf"], L["onesf"], L["maskf"], L["maskb"]
    cst, g2 = L["cst"], L["g2"]
    S_g, S_qT, S_kT, S_k, S_v, S_o, S_kh, S_vh = (L[k] for k in "S_g S_qT S_kT S_k S_v S_o S_kh S_vh".split())
    S_hf, S_hm, mng = L["S_hf"], L["S_hm"], L["mng"]
    S_UT, S_V, uT, pv = L["S_UT"], L["S_V"], L["uT"], L["pv"]
    L["BU_holder"] = [None]
    barrier_from = L["barrier_from"]
    norm_T = L["norm_T"]

    TSf = sb(top, "TSf", (64, 2, 32, 4), F32)
    TSb = sb(top, "TSb", (64, 96, 4), F32)
    DBf = sb(top, "DBf", (128, 4, 32), F32)
    DBb = sb(top, "DBb", (128, 4, 64), F32)
    RTS = Res()
    with contextlib.ExitStack() as st:
        B = [sb(st, "gB%d" % i, (4, 4096), F32) for i in range(5)]
        RB = [Res() for _ in range(5)]
        dB = [mkds(st) for _ in range(2)]
        rmask = sb(st, "rmask", (4, 4096), F32)
        Rrm = Res()
        drm = mkds(st)
        P.op("sp", lambda e: e.dma_start(out=rmask[:], in_=cst[0:4, 384:384 + 4096]), writes=[Rrm], dsem=drm)
        amax = sb(st, "amax", (4, 64), F32)
        totc = sb(st, "totc", (4, 64), F32)
        Mtab = sb(st, "Mtab", (4, 65), F32)
        MC = sb(st, "MC", (4, 64), F32)
        dd = sb(st, "dd", (4, 64), F32)
        dexp = sb(st, "dexp", (4, 4, 64), F32)
        Rsm = Res()
        LN_S = math.log(512.0 ** -0.5)
        for d in range(2):
            Ltok = T if d == 0 else 2 * T
            nch = Ltok // 64
            li, zf, cum, cx, a = (B[i][:, 0:Ltok] for i in range(5))
            v3 = lambda ap: ap.rearrange("p (c l) -> p c l", l=64)
            P.op("sp", lambda e: e.dma_start(out=li, in_=S_g[d * 2, :, 0:Ltok]), reads=[BP], writes=[RB[0]], dsem=dB[0])
            P.op("sp", lambda e: e.dma_start(out=zf, in_=S_g[d * 2 + 1, :, 0:Ltok]), reads=[BP], writes=[RB[1]], dsem=dB[1])
            P.op("act", lambda e: e.activation(out=zf, in_=zf, func=AF.Exp, scale=-1.0), reads=[RB[1]], writes=[RB[1]])
            P.op("act", lambda e: e.activation(out=zf, in_=zf, func=AF.Ln, bias=1.0), reads=[RB[1]], writes=[RB[1]])
            P.op("dve", lambda e: e.tensor_tensor_scan(out=cum, data0=rmask[:, 0:Ltok], data1=zf, initial=0.0, op0=ALU.mult, op1=ALU.add),
                 reads=[RB[1], Rrm], writes=[RB[2]])
            tot3 = v3(cum)[:, :, 63:64]
            if d == 1:
                P.op("dve", lambda e: e.tensor_tensor(out=cx, in0=zf, in1=cum, op=ALU.subtract), reads=[RB[1], RB[2]], writes=[RB[3]])
                P.op("dve", lambda e: e.tensor_tensor(out=v3(cx), in0=v3(cx), in1=tot3.broadcast_to([4, nch, 64]), op=ALU.add),
                     reads=[RB[3], RB[2]], writes=[RB[3]])
                cxx, Rcx = cx, RB[3]
            else:
                cxx, Rcx = cum, RB[2]
            P.op("dve", lambda e: e.tensor_tensor(out=a, in0=li, in1=cxx, op=ALU.add), reads=[RB[0], Rcx], writes=[RB[4]])
            P.op("dve", lambda e: e.tensor_reduce(out=amax[:, 0:nch], in_=v3(a), axis=AX.X, op=ALU.max), reads=[RB[4]], writes=[Rsm])
            P.op("dve", lambda e: e.tensor_copy(out=totc[:, 0:nch], in_=tot3.rearrange("p c l -> p (c l)")), reads=[RB[2]], writes=[Rsm])
            if d == 0:
                P.op("dve", lambda e: e.memset(Mtab[:, 0:1], 0.0), writes=[Rsm])
                order = [(c, c, c + 1) for c in range(nch)]
            else:
                P.op("dve", lambda e: e.memset(Mtab[:, nch:nch + 1], 0.0), writes=[Rsm])
                order = [(c, c + 1, c) for c in range(nch - 1, -1, -1)]
            for (c, ip, inx) in order:
                P.op("dve", lambda e, c=c, ip=ip: e.tensor_tensor(out=MC[:, c:c + 1], in0=Mtab[:, ip:ip + 1], in1=amax[:, c:c + 1], op=ALU.max),
                     reads=[Rsm], writes=[Rsm])
                P.op("dve", lambda e, c=c, inx=inx: e.tensor_tensor(out=Mtab[:, inx:inx + 1], in0=MC[:, c:c + 1], in1=totc[:, c:c + 1], op=ALU.subtract),
                     reads=[Rsm], writes=[Rsm])
            mprev = Mtab[:, 0:nch] if d == 0 else Mtab[:, 1:nch + 1]
            P.op("dve", lambda e: e.tensor_tensor(out=dd[:, 0:nch], in0=mprev, in1=MC[:, 0:nch], op=ALU.subtract), reads=[Rsm], writes=[Rsm])
            P.op("act", lambda e: e.activation(out=dd[:, 0:nch], in_=dd[:, 0:nch], func=AF.Exp), reads=[Rsm], writes=[Rsm])
            mcb = MC[:, 0:nch].unsqueeze(2).broadcast_to([4, nch, 64])
            P.op("dve", lambda e: e.tensor_tensor(out=v3(zf), in0=v3(a), in1=mcb, op=ALU.subtract), reads=[RB[4], Rsm, RB[1]], writes=[RB[1]])
            P.op("act", lambda e: e.activation(out=zf, in_=zf, func=AF.Exp, bias=LN_S), reads=[RB[1]], writes=[RB[1]])
            P.op("dve", lambda e: e.tensor_tensor(out=v3(li), in0=v3(cxx), in1=mcb, op=ALU.subtract), reads=[Rcx, Rsm, RB[0]], writes=[RB[0]])
            P.op("act", lambda e: e.activation(out=li, in_=li, func=AF.Exp), reads=[RB[0]], writes=[RB[0]])
            bk = nextbank()
            ncols = 0
            for kind, src, Rsrc, nck in ((0, zf, RB[1], nch), (1, li, RB[0], 32)):
                for c in range(nck):
                    col = (kind * nch + c) * 4 if d == 1 else (kind * 32 + c) * 4
                    P.op("pe", lambda e, c=c, col=col, src=src: e.transpose(out=ps[bk][0:64, col:col + 4], in_=src[:, c * 64:(c + 1) * 64], identity=identf[0:4, 0:4]),
                         reads=[Rsrc] + CONST, writes=[Rps[bk]], inc=(c == nck - 1))
                    ncols = max(ncols, col + 4)
            dstTS = TSf[:].rearrange("p k c h -> p (k c h)") if d == 0 else TSb[:].rearrange("p c h -> p (c h)")
            P.op("dve", lambda e, dstTS=dstTS, ncols=ncols: e.tensor_copy(out=dstTS[:, 0:ncols], in_=ps[bk][0:64, 0:ncols]), reads=[Rps[bk]], writes=[RTS])
            P.op("dve", lambda e: e.tensor_tensor(out=dexp[:, :, 0:nch], in0=dd[:, 0:nch].unsqueeze(1).broadcast_to([4, 4, nch]),
                                                   in1=identf[0:4, 0:4].unsqueeze(2).broadcast_to([4, 4, nch]), op=ALU.mult),
                 reads=[Rsm] + CONST, writes=[Rsm])
            bk2 = nextbank()
            P.op("pe", lambda e: e.matmul(ps[bk2][:, 0:4 * nch].rearrange("p (h c) -> p h c", h=4), lhsT=onesf[0:4, :], rhs=dexp[:, :, 0:nch], start=True, stop=True),
                 reads=[Rsm] + CONST, writes=[Rps[bk2]])
            DBd = DBf if d == 0 else DBb
            P.op("dve", lambda e, DBd=DBd: e.tensor_copy(out=DBd[:], in_=ps[bk2][:, 0:4 * nch].rearrange("p (h c) -> p h c", h=4)),
                 reads=[Rps[bk2]], writes=[RTS])

    P.barrier()
    out_toks.append(RTS.w)
    if L["UPTO"] == "G":
        for nm, tl in (("D_TSf", TSf), ("D_TSb", TSb), ("D_DBf", DBf), ("D_DBb", DBb)):
            shp = list(tl[:].shape)
            flat = int(np.prod(shp[1:]))
            dd_ = nc.dram_tensor(nm, [shp[0], flat], F32, kind="ExternalOutput").ap()
            dsx = mkds()
            pat = {4: "p a b c -> p (a b c)", 3: "p a b -> p (a b)"}[len(shp)]
            out_toks.append(P.op("sp", lambda e: e.dma_start(out=dd_[:, :], in_=tl[:].rearrange(pat)), reads=[RTS], dsem=dsx))
        return
    sthm = contextlib.ExitStack()
    Cb_all = sb(sthm, "Cb_all", (128, 4, 4, 512), F32)
    nb_all = sb(sthm, "nb_all", (128, 4, 4), F32)
    RCall = Res()
    pcb = [sb(sthm, "pcb%d" % i, (128, D), BF16) for i in range(4)]
    Rpcb = [Res() for _ in range(4)]
    dpcb = [mkds() for _ in range(4)]
    dpcbo = [mkds() for _ in range(4)]
    RSU = Res()
    pc_state = [0]

    def precast_some(n):
        for _ in range(n):
            k = pc_state[0]
            if k >= 2 * NEB:
                return
            pc_state[0] += 1
            eb, which = k // 2, k % 2
            src, dst = ((uT, S_UT), (pv, S_V))[which]
            i = k % 4
            for hh in range(2):
                P.op("pool", lambda e: e.dma_start(out=pcb[i][:, hh * 1024:(hh + 1) * 1024], in_=src[eb, :, hh * 1024:(hh + 1) * 1024]),
                     writes=[Rpcb[i]], dsem=dpcb[i])
            P.op("sp", lambda e: e.dma_start(out=dst[eb, :, :], in_=pcb[i][:]), reads=[Rpcb[i]], writes=[RSU], dsem=dpcbo[i])

    def make_chain(st, tag, sbank, nbank, light=False):
        ch = dict(
            Cst=sb(st, "Cst" + tag, (128, 4, 512), F32), nst=sb(st, "nst" + tag, (128, 4), F32),
            RC=Res(), RCb=Res(),
            kw=[sb(st, "kw%s%d" % (tag, i), (64, 512), BF16) for i in range(2)], Rkw=[Res(), Res()],
            RSt=[Res(), Res()], Rms=Res(), cc=[0], sbank=sbank, nbank=nbank)
        if not light:
            ch.update(Cb=sb(st, "Cb" + tag, (128, 4, 512), BF16), nb=sb(st, "nb" + tag, (128, 4), BF16),
                      St=[sb(st, "St%s%d" % (tag, i), (64, 64), BF16) for i in range(2)],
                      sm=sb(st, "msm" + tag, (64, 8), F32))
        return ch

    def state_update(ch, kchunk, vchunk, wk, decay, rd):
        i = ch["cc"][0] % 2
        ch["cc"][0] += 1
        kw, Rkw, Cst, nst, RC = ch["kw"], ch["Rkw"], ch["Cst"], ch["nst"], ch["RC"]
        P.op("act", lambda e: e.activation(out=kw[i][:], in_=kchunk, func=AF.Copy, scale=wk), reads=rd + [RTS], writes=[Rkw[i]])
        bn = 7
        for j in range(4):
            P.op("pe", lambda e: e.matmul(ps[bn][:, j:j + 1], lhsT=kw[i][:, j * 128:(j + 1) * 128], rhs=onesb[0:64, 0:1], start=True, stop=True),
                 reads=[Rkw[i]] + CONST, writes=[Rps[bn]], inc=(j == 3))
        P.op("dve", lambda e: e.scalar_tensor_tensor(out=nst[:], in0=nst[:], scalar=decay, in1=ps[bn][:, 0:4], op0=ALU.mult, op1=ALU.add),
             reads=[Rps[bn], RTS, RC], writes=[RC])
        for j in range(4):
            bu = nextbank(4, 7)
            P.op("pe", lambda e: e.matmul(ps[bu][:], lhsT=kw[i][:, j * 128:(j + 1) * 128], rhs=vchunk, start=True, stop=True),
                 reads=[Rkw[i]] + rd, writes=[Rps[bu]])
            P.op("dve", lambda e: e.scalar_tensor_tensor(out=Cst[:, j, :], in0=Cst[:, j, :], scalar=decay, in1=ps[bu][:], op0=ALU.mult, op1=ALU.add),
                 reads=[Rps[bu], RTS, RC], writes=[RC])

    with contextlib.ExitStack() as st:
        chs = [make_chain(st, "h%d" % i, 0, 0, light=True) for i in range(2)]
        kkh = [sb(st, "kkh%d" % i, (64, 32, 512), BF16) for i in range(2)]
        vvh = [sb(st, "vvh%d" % i, (64, 32, 512), BF16) for i in range(2)]
        Rkh = [Res(), Res()]
        dkh = [mkds() for _ in range(4)]
        for hp in range(2):
            for i in range(2):
                h = hp * 2 + i
                P.op("sp", lambda e: e.dma_start(out=kkh[i][:], in_=S_kh[:, h * 512:(h + 1) * 512].rearrange("(c p) d -> p c d", p=64)),
                     reads=[BP], writes=[Rkh[i]], dsem=dkh[2 * i])
                P.op("sp", lambda e: e.dma_start(out=vvh[i][:], in_=S_vh[:, h * 512:(h + 1) * 512].rearrange("(c p) d -> p c d", p=64)),
                     reads=[BP], writes=[Rkh[i]], dsem=dkh[2 * i + 1])
                P.op("dve", lambda e: e.memset(chs[i]["Cst"][:], 0.0), writes=[chs[i]["RC"]])
                P.op("dve", lambda e: e.memset(chs[i]["nst"][:], 0.0), writes=[chs[i]["RC"]])
            for c in range(63, 31, -1):
                hc = c - 32
                for i in range(2):
                    h = hp * 2 + i
                    state_update(chs[i], kkh[i][:, hc, :], vvh[i][:, hc, :], TSb[:, c, h:h + 1], DBb[:, h, c:c + 1], [Rkh[i]])
                precast_some(2)
            for i in range(2):
                h = hp * 2 + i
                P.op("act", lambda e: e.activation(out=Cb_all[:, h], in_=chs[i]["Cst"][:], func=AF.Copy), reads=[chs[i]["RC"]], writes=[RCall])
                P.op("act", lambda e: e.activation(out=nb_all[:, h], in_=chs[i]["nst"][:], func=AF.Copy), reads=[chs[i]["RC"]], writes=[RCall])
    P.barrier()

    with contextlib.ExitStack() as st:
        chs = [make_chain(st, "m0", 0, 1), make_chain(st, "m1", 2, 3)]
        qT = sb(st, "qT", (128, 4, T), BF16)
        kT = sb(st, "kT", (128, 4, T), BF16)
        kk = sb(st, "kk", (64, 32, 512), BF16)
        vv = sb(st, "vv", (64, 32, 512), BF16)
        Rq, Rk = Res(), Res()
        dq = [mkds() for _ in range(4)]
        hst = [sb(st, "hst%d" % i, (64, 512), F32) for i in range(2)]
        Rhst = [Res(), Res()]
        dhst = [mkds(), mkds()]
        hfl = [sb(st, "hfl%d" % i, (64, 512), F32) for i in range(4)]
        Rhfl = [Res() for _ in range(4)]
        dhfl = [mkds() for _ in range(4)]
        osl = [sb(st, "osl%d" % i, (64, 512), BF16) for i in range(4)]
        Rosl = [Res() for _ in range(4)]
        dosl = [mkds() for _ in range(4)]

        def prefetch_fin(h, d, c, slot):
            q = d * 2 + slot
            tk = slice(c * 64, (c + 1) * 64)
            P.op("sp", lambda e: e.dma_start(out=hfl[q][:], in_=S_hf[tk, h * 512:(h + 1) * 512]), reads=[RShf[c]], writes=[Rhfl[q]], dsem=dhfl[q])
            P.op("sp", lambda e: e.dma_start(out=osl[q][:], in_=S_o[tk, h * 512:(h + 1) * 512]), reads=[BP], writes=[Rosl[q]], dsem=dosl[q])
        hmo = [sb(st, "hmo%d" % i, (64, 512), BF16) for i in range(2)]
        Rhmo = [Res(), Res()]
        dhmo = [mkds(), mkds()]
        hjunk = sb(st, "hjunk", (64, 512), BF16)
        Rhj = Res()
        hsum = [sb(st, "hsum%d" % i, (64, 512), F32) for i in range(2)]
        Rhs = [Res(), Res()]
        mg = sb(st, "mgb", (64, 512), F32)
        Rmg = Res()
        dmg = mkds()
        RShf = [Res() for _ in range(32)]
        RShm = Res()

        def chunk_step(h, d, c, second, slot=0):
            ch = chs[d]
            Cst, nst, Cb, nb, RC, RCb, St, RSt, sm, Rms = (ch[k] for k in "Cst nst Cb nb RC RCb St RSt sm Rms".split())
            b_s, b_n = ch["sbank"], ch["nbank"]
            tk = slice(c * 64, (c + 1) * 64)
            if d == 0:
                wk, e2, decay, mk = TSf[:, 0, c, h:h + 1], TSf[:, 1, c, h:h + 1], DBf[:, h, c:c + 1], maskf
            else:
                wk, e2, decay, mk = TSb[:, c, h:h + 1], TSb[:, 64 + c, h:h + 1], DBb[:, h, c:c + 1], maskb
            i = ch["cc"][0] % 2
            P.op("act", lambda e: e.activation(out=Cb[:], in_=Cst[:], func=AF.Copy, scale=decay), reads=[RC, RTS], writes=[RCb])
            P.op("act", lambda e: e.activation(out=nb[:], in_=nst[:], func=AF.Copy, scale=decay), reads=[RC, RTS], writes=[RCb])
            for j in range(4):
                P.op("pe", lambda e: e.matmul(ps[b_s][0:64, 0:64], lhsT=kT[:, j, tk], rhs=qT[:, j, tk], start=(j == 0), stop=(j == 3)),
                     reads=[Rq], writes=[Rps[b_s]], inc=(j == 3))
            P.op("dve", lambda e: e.scalar_tensor_tensor(out=St[i][:], in0=ps[b_s][0:64, 0:64], scalar=wk, in1=mk, op0=ALU.mult, op1=ALU.mult),
                 reads=[Rps[b_s], RTS] + CONST, writes=[RSt[i]])
            for j in range(4):
                P.op("pe", lambda e: e.matmul(ps[b_n][0:64, :], lhsT=qT[:, j, tk], rhs=Cb[:, j, :], start=(j == 0), stop=False),
                     reads=[Rq, RCb], writes=[Rps[b_n]], inc=False)
            P.op("pe", lambda e: e.matmul(ps[b_n][0:64, :], lhsT=St[i][:], rhs=vv[:, c, :], start=False, stop=True),
                 reads=[RSt[i], Rk], writes=[Rps[b_n]])
            for j in range(4):
                P.op("pe", lambda e: e.matmul(ps[b_s][0:64, 64:65], lhsT=qT[:, j, tk], rhs=nb[:, j:j + 1], start=(j == 0), stop=False),
                     reads=[Rq, RCb], writes=[Rps[b_s]], inc=False)
            P.op("pe", lambda e: e.matmul(ps[b_s][0:64, 64:65], lhsT=St[i][:], rhs=onesb[0:64, 0:1], start=False, stop=True),
                 reads=[RSt[i]] + CONST, writes=[Rps[b_s]])
            P.op("act", lambda e: e.activation(out=sm[:, 5:6], in_=ps[b_s][0:64, 64:65], func=AF.Abs), reads=[Rps[b_s]], writes=[Rms])
            P.op("dve", lambda e: e.tensor_scalar(out=sm[:, 0:1], in0=sm[:, 5:6], scalar1=e2, scalar2=None, op0=ALU.max), reads=[Rms, RTS], writes=[Rms])
            P.op("dve", lambda e: e.reciprocal(out=sm[:, 1:2], in_=sm[:, 0:1]), reads=[Rms], writes=[Rms])
            q = d
            if not second:
                P.op("dve", lambda e: e.tensor_scalar(out=hst[q][:], in0=ps[b_n][0:64, :], scalar1=sm[:, 1:2], scalar2=None, op0=ALU.mult),
                     reads=[Rps[b_n], Rms], writes=[Rhst[q]])
                P.op("sp", lambda e: e.dma_start(out=S_hf[tk, h * 512:(h + 1) * 512], in_=hst[q][:]), reads=[Rhst[q]], writes=[RShf[c]], dsem=dhst[q])
            else:
                q4 = d * 2 + slot
                P.op("dve", lambda e: e.scalar_tensor_tensor(out=hsum[q][:], in0=ps[b_n][0:64, :], scalar=sm[:, 1:2], in1=hfl[q4][:], op0=ALU.mult, op1=ALU.add),
                     reads=[Rps[b_n], Rms, Rhfl[q4]], writes=[Rhs[q]])
                P.op("act", lambda e: e.activation(out=hjunk[:], in_=hsum[q][:], func=AF.Square, accum_out=sm[:, 2:3]), reads=[Rhs[q]], writes=[Rhj, Rms])
                P.op("act", lambda e: e.activation(out=sm[:, 3:4], in_=sm[:, 2:3], func=AF.Ln, scale=1.0 / 512, bias=EPS), reads=[Rms], writes=[Rms])
                P.op("act", lambda e: e.activation(out=sm[:, 4:5], in_=sm[:, 3:4], func=AF.Exp, scale=-0.5), reads=[Rms], writes=[Rms])
                P.op("dve", lambda e: e.scalar_tensor_tensor(out=hsum[q][:], in0=hsum[q][:], scalar=sm[:, 4:5], in1=mg[:], op0=ALU.mult, op1=ALU.mult),
                     reads=[Rhs[q], Rms, Rmg], writes=[Rhs[q]])
                P.op("pool", lambda e: e.tensor_tensor(out=hmo[q][:], in0=hsum[q][:], in1=osl[q4][:], op=ALU.mult),
                     reads=[Rhs[q], Rosl[q4]], writes=[Rhmo[q]])
                P.op("sp", lambda e: e.dma_start(out=S_hm[tk, h * 512:(h + 1) * 512], in_=hmo[q][:]), reads=[Rhmo[q]], writes=[RShm], dsem=dhmo[q])
            state_update(ch, kk[:, c, :], vv[:, c, :], wk, decay, [Rk])

        for h in range(4):
            P.op("sp", lambda e: e.dma_start(out=qT[:], in_=S_qT[h * 512:(h + 1) * 512, :].rearrange("(j p) t -> p j t", p=128)),
                 reads=[BP], writes=[Rq], dsem=dq[0])
            P.op("sp", lambda e: e.dma_start(out=kT[:], in_=S_kT[h * 512:(h + 1) * 512, :].rearrange("(j p) t -> p j t", p=128)),
                 reads=[BP], writes=[Rq], dsem=dq[1])
            P.op("sp", lambda e: e.dma_start(out=kk[:], in_=S_k[:, h * 512:(h + 1) * 512].rearrange("(c p) d -> p c d", p=64)),
                 reads=[BP], writes=[Rk], dsem=dq[2])
            P.op("sp", lambda e: e.dma_start(out=vv[:], in_=S_v[:, h * 512:(h + 1) * 512].rearrange("(c p) d -> p c d", p=64)),
                 reads=[BP], writes=[Rk], dsem=dq[3])
            P.op("sp", lambda e: e.dma_start(out=mg[:], in_=mng[:, h * 512:(h + 1) * 512]), writes=[Rmg], dsem=dmg)
            P.op("dve", lambda e: e.memset(chs[0]["Cst"][:], 0.0), writes=[chs[0]["RC"]])
            P.op("dve", lambda e: e.memset(chs[0]["nst"][:], 0.0), writes=[chs[0]["RC"]])
            P.op("act", lambda e: e.activation(out=chs[1]["Cst"][:], in_=Cb_all[:, h], func=AF.Copy), reads=[RCall], writes=[chs[1]["RC"]])
            P.op("act", lambda e: e.activation(out=chs[1]["nst"][:], in_=nb_all[:, h], func=AF.Copy), reads=[RCall], writes=[chs[1]["RC"]])
            for s_ in range(32):
                chunk_step(h, 0, s_, s_ >= 16, s_ % 2)
                chunk_step(h, 1, 31 - s_, s_ >= 16, s_ % 2)
                if 15 <= s_ < 31:
                    prefetch_fin(h, 0, s_ + 1, (s_ + 1) % 2)
                    prefetch_fin(h, 1, 31 - (s_ + 1), (s_ + 1) % 2)
                precast_some(1)
        precast_some(2 * NEB)
        L["BU_holder"][0] = barrier_from([RSU] + Rpcb)
        BM = barrier_from([RShm, Rhmo[0], Rhmo[1]])
    sthm.close()
    P.barrier()
    out_toks.append(BM.w)
    if L["UPTO"] == "M":
        return
    build_tail(nc, P, top, sb, mkds, ps, Rps, nextbank, CONST, BP, BM, L, out_toks)


def build_tail(nc, P, top, sb, mkds, ps, Rps, nextbank, CONST, BP, BM, L, out_toks):
    identb, onesb = L["identb"], L["onesb"]
    g2 = L["g2"]
    S_aqT, S_akT, S_av, S_gmT, S_gaT, S_hm, S_haT, S_x1 = (L[k] for k in "S_aqT S_akT S_av S_gmT S_gaT S_hm S_haT S_x1".split())
    S_UT, S_V, uT, pv, keysT, S_sub, S_xn2 = L["S_UT"], L["S_V"], L["uT"], L["pv"], L["keysT"], L["S_sub"], L["S_xn2"]
    sinkb, btab, x_own, y = L["sinkb"], L["btab"], L["x_own"], L["y"]
    w_mp, w_ap, w_o, w_pq = L["w_mp"], L["w_ap"], L["w_o"], L["w_pq"]
    barrier_from, norm_T = L["barrier_from"], L["norm_T"]

    BU = L["BU_holder"][0]
    with contextlib.ExitStack() as st:
        EB = sb(st, "EB", (128, 16, 3, 128), F32)
        esk = sb(st, "esk", (128, 16), F32)
        REB = Res()
        dEB = mkds(st)
        P.op("sp", lambda e: e.dma_start(out=EB[:].rearrange("p h o q -> p (h o q)"), in_=btab[:, :]), writes=[REB], dsem=dEB)
        P.op("sp", lambda e: e.dma_start(out=esk[:], in_=sinkb[:, :]), writes=[REB], dsem=dEB)
        P.op("act", lambda e: e.activation(out=EB[:].rearrange("p h o q -> p (h o q)"), in_=EB[:].rearrange("p h o q -> p (h o q)"), func=AF.Exp),
             reads=[REB], writes=[REB])
        P.op("act", lambda e: e.activation(out=esk[:], in_=esk[:], func=AF.Exp), reads=[REB], writes=[REB])
        m128 = sb(st, "m128", (128, 2, 128), F32)
        dm = mkds(st)
        P.op("sp", lambda e: e.dma_start(out=m128[:].rearrange("p a q -> p (a q)"), in_=L["cst"][:, 4480:4736]), writes=[REB], dsem=dm)
        for o, a in ((0, 0), (2, 1)):
            P.op("dve", lambda e, o=o, a=a: e.tensor_tensor(out=EB[:, :, o, :], in0=EB[:, :, o, :], in1=m128[:, a, :].unsqueeze(1).broadcast_to([128, 16, 128]), op=ALU.mult),
                 reads=[REB], writes=[REB])
        aqT = sb(st, "aqT", (128, 16, T), BF16)
        akT = sb(st, "akT", (128, 4, T + 128), BF16)
        av = sb(st, "av", (128, 17, 512), BF16)
        Ra = Res()
        da = [mkds(st) for _ in range(3)]
        P.op("sp", lambda e: e.dma_start(out=aqT[:], in_=S_aqT.rearrange("(h p) t -> p h t", p=128)), reads=[BP], writes=[Ra], dsem=da[0])
        P.op("sp", lambda e: e.dma_start(out=akT[:], in_=S_akT.rearrange("(h p) t -> p h t", p=128)), reads=[BP], writes=[Ra], dsem=da[1])
        P.op("sp", lambda e: e.dma_start(out=av[:], in_=S_av.rearrange("(t p) n -> p t n", p=128)), reads=[BP], writes=[Ra], dsem=da[2])
        pex = [sb(st, "pex%d" % i, (128, 512), F32) for i in range(3)]
        Rpex = [Res() for _ in range(3)]
        pT = [sb(st, "pT%d" % i, (128, 512), BF16) for i in range(6)]
        RpT = [Res() for _ in range(6)]
        zt = sb(st, "zt", (128, 512), F32)
        Rz = Res()
        hst = [sb(st, "hag%d" % i, (128, 4, T), BF16) for i in range(2)]
        Rhst = [Res(), Res()]
        dhst = [mkds(st), mkds(st)]
        RSha = Res()
        kk = 0
        for g in range(4):
            for i in range(NT):
                os_ = [o for o in (-1, 0, 1) if i + o >= 0]
                pts = []
                for o in os_:
                    bk = nextbank(0, 4)
                    P.op("pe", lambda e, bk=bk, o=o, i=i: e.matmul(ps[bk][:].rearrange("p (h q) -> p h q", h=4), lhsT=akT[:, g, (i + o) * 128:(i + o + 1) * 128],
                                                                    rhs=aqT[:, 4 * g:4 * g + 4, i * 128:(i + 1) * 128], start=True, stop=True),
                         reads=[Ra], writes=[Rps[bk]])
                    a = kk % 3
                    b = kk % 6
                    kk += 1
                    P.op("act", lambda e, bk=bk, a=a: e.activation(out=pex[a][:], in_=ps[bk][:], func=AF.Exp), reads=[Rps[bk]], writes=[Rpex[a]])
                    P.op("dve", lambda e, a=a, b=b, o=o: e.tensor_tensor(out=pT[b][:].rearrange("p (h q) -> p h q", h=4), in0=pex[a][:].rearrange("p (h q) -> p h q", h=4),
                                                                          in1=EB[:, 4 * g:4 * g + 4, o + 1, :], op=ALU.mult),
                         reads=[Rpex[a], REB], writes=[RpT[b]])
                    pts.append((o, b))
                bo = nextbank(4, 6)
                bz = nextbank(6, 8)
                for n_, (o, b) in enumerate(pts):
                    P.op("pe", lambda e, o=o, b=b, n_=n_, i=i, bo=bo: e.matmul(ps[bo][:], lhsT=av[:, i + o, g * 128:(g + 1) * 128], rhs=pT[b][:], start=(n_ == 0), stop=(n_ == len(pts) - 1)),
                         reads=[Ra, RpT[b]], writes=[Rps[bo]], inc=(n_ == len(pts) - 1))
                for n_, (o, b) in enumerate(pts):
                    P.op("pe", lambda e, b=b, n_=n_, bz=bz: e.matmul(ps[bz][:], lhsT=onesb[:], rhs=pT[b][:], start=(n_ == 0), stop=(n_ == len(pts) - 1)),
                         reads=[RpT[b]] + CONST, writes=[Rps[bz]], inc=(n_ == len(pts) - 1))
                P.op("dve", lambda e, bz=bz: e.tensor_tensor(out=zt[:].rearrange("p (h q) -> p h q", h=4), in0=ps[bz][:].rearrange("p (h q) -> p h q", h=4),
                                                              in1=esk[:, 4 * g:4 * g + 4].unsqueeze(2).broadcast_to([128, 4, 128]), op=ALU.add),
                     reads=[Rps[bz], REB], writes=[Rz])
                P.op("dve", lambda e: e.reciprocal(out=zt[:], in_=zt[:]), reads=[Rz], writes=[Rz])
                P.op("dve", lambda e, bo=bo, i=i: e.tensor_tensor(out=hst[g % 2][:, :, i * 128:(i + 1) * 128], in0=ps[bo][:].rearrange("p (h q) -> p h q", h=4),
                                                                   in1=zt[:].rearrange("p (h q) -> p h q", h=4), op=ALU.mult),
                     reads=[Rps[bo], Rz], writes=[Rhst[g % 2]])
            P.op("sp", lambda e, g=g: e.dma_start(out=S_haT[g * 512:(g + 1) * 512, :].rearrange("(h p) t -> p h t", p=128), in_=hst[g % 2][:]),
                 reads=[Rhst[g % 2]], writes=[RSha], dsem=dhst[g % 2])
        BT = barrier_from([RSha, Rhst[0], Rhst[1]])
    P.barrier()
    out_toks.append(BT.w)
    if L["UPTO"] == "T":
        return

    with contextlib.ExitStack() as st:
        with contextlib.ExitStack() as st2:
            mT = sb(st2, "mT", (128, 16, T), BF16)
            RmT = Res()
            wr = [sb(st2, "owr%d" % i, (128, 16, 512), BF16) for i in range(2)]
            Rwr = [Res(), Res()]
            dwr = [mkds(st2), mkds(st2)]
            wc = [0]

            def load_w(wsrc, c0):
                s = wc[0] % 2
                wc[0] += 1
                P.op("pool", lambda e: e.dma_start(out=wr[s][:], in_=wsrc[:, c0:c0 + 512].rearrange("(c p) n -> p c n", p=128)), writes=[Rwr[s]], dsem=dwr[s])
                return s

            with contextlib.ExitStack() as st3:
                hT = sb(st3, "hT", (128, 16, T), BF16)
                RhT = Res()
                gl = [sb(st3, "gl%d" % i, (128, T), BF16) for i in range(2)]
                Rgl = [Res(), Res()]
                dgl = [mkds(st3), mkds(st3)]
                tmpf = sb(st3, "tmpf", (128, 512), F32)
                Rtf = Res()
                for br in range(2):
                    if br == 0:
                        ht = [sb(st3, "htl%d" % i, (128, D), BF16) for i in range(2)]
                        Rht = [Res(), Res()]
                        dht = [mkds(st3), mkds(st3)]
                        for t in range(NT):
                            b = t % 2
                            P.op("sp", lambda e, b=b, t=t: e.dma_start(out=ht[b][:], in_=S_hm[t * 128:(t + 1) * 128, :]), reads=[BM], writes=[Rht[b]], dsem=dht[b])
                            for gq in range(4):
                                bk = nextbank()
                                ptv = ps[bk][:].bitcast(BF16)[:, 0:512].rearrange("p (j n) -> p j n", j=4)
                                for j in range(4):
                                    c = gq * 4 + j
                                    P.op("pe", lambda e, c=c, j=j, ptv=ptv, b=b: e.transpose(out=ptv[:, j, :], in_=ht[b][:, c * 128:(c + 1) * 128], identity=identb[:]),
                                         reads=[Rht[b]] + CONST, writes=[Rps[bk]], inc=(j == 3))
                                if gq % 2 == 0:
                                    P.op("act", lambda e, gq=gq, t=t, ptv=ptv: e.activation(out=hT[:, gq * 4:(gq + 1) * 4, t * 128:(t + 1) * 128], in_=ptv, func=AF.Copy),
                                         reads=[Rps[bk]], writes=[RhT])
                                else:
                                    P.op("dve", lambda e, gq=gq, t=t, ptv=ptv: e.tensor_copy(out=hT[:, gq * 4:(gq + 1) * 4, t * 128:(t + 1) * 128], in_=ptv),
                                         reads=[Rps[bk]], writes=[RhT])
                        wsrc, gsrc = w_mp, S_gmT
                    else:
                        dhT = mkds(st3)
                        P.op("sp", lambda e: e.dma_start(out=hT[:], in_=S_haT.rearrange("(c p) t -> p c t", p=128)), reads=[BT], writes=[RhT], dsem=dhT)
                        wsrc, gsrc = w_ap, S_gaT
                    for cb_ in range(4):
                        s = load_w(wsrc, cb_ * 512)
                        for j in range(4):
                            jj = cb_ * 4 + j
                            q = jj % 2
                            P.op("sp", lambda e, q=q, jj=jj, gsrc=gsrc: e.dma_start(out=gl[q][:], in_=gsrc[jj * 128:(jj + 1) * 128, :]), reads=[BP], writes=[Rgl[q]], dsem=dgl[q])
                            for tb in range(4):
                                bk = nextbank()
                                for c in range(16):
                                    P.op("pe", lambda e, c=c, j=j, tb=tb, bk=bk, s=s: e.matmul(ps[bk][:], lhsT=wr[s][:, c, j * 128:(j + 1) * 128], rhs=hT[:, c, tb * 512:(tb + 1) * 512],
                                                                                                 start=(c == 0), stop=(c == 15)),
                                         reads=[Rwr[s], RhT], writes=[Rps[bk]], inc=(c == 15))
                                if br == 0:
                                    P.op("dve", lambda e, jj=jj, tb=tb, bk=bk, q=q: e.tensor_tensor(out=mT[:, jj, tb * 512:(tb + 1) * 512], in0=ps[bk][:], in1=gl[q][:, tb * 512:(tb + 1) * 512], op=ALU.mult),
                                         reads=[Rps[bk], Rgl[q]], writes=[RmT])
                                else:
                                    P.op("dve", lambda e, tb=tb, bk=bk, q=q: e.tensor_tensor(out=tmpf[:], in0=ps[bk][:], in1=gl[q][:, tb * 512:(tb + 1) * 512], op=ALU.mult),
                                         reads=[Rps[bk], Rgl[q]], writes=[Rtf])
                                    P.op("pool", lambda e, jj=jj, tb=tb: e.tensor_tensor(out=mT[:, jj, tb * 512:(tb + 1) * 512], in0=mT[:, jj, tb * 512:(tb + 1) * 512], in1=tmpf[:], op=ALU.add),
                                         reads=[Rtf, RmT], writes=[RmT])
            P.barrier()
            xl = [sb(st2, "xl%d" % i, (128, 512), F32) for i in range(2)]
            Rxl = [Res(), Res()]
            dxl = [mkds(st2), mkds(st2)]
            x1s = [sb(st2, "x1s%d" % i, (128, 512), F32) for i in range(2)]
            Rx1s = [Res(), Res()]
            dx1s = [mkds(st2), mkds(st2)]
            RSx1 = Res()
            kx = 0
            for cb_ in range(4):
                s = load_w(w_o, cb_ * 512)
                for t in range(NT):
                    b = kx % 2
                    kx += 1
                    P.op("sp", lambda e, b=b, t=t, cb_=cb_: e.dma_start(out=xl[b][:], in_=x_own[t * 128:(t + 1) * 128, cb_ * 512:(cb_ + 1) * 512]), writes=[Rxl[b]], dsem=dxl[b])
                    bk = nextbank()
                    for c in range(16):
                        P.op("pe", lambda e, c=c, t=t, bk=bk, s=s: e.matmul(ps[bk][:], lhsT=mT[:, c, t * 128:(t + 1) * 128], rhs=wr[s][:, c, :], start=(c == 0), stop=(c == 15)),
                             reads=[RmT, Rwr[s]], writes=[Rps[bk]], inc=(c == 15))
                    P.op("dve", lambda e, b=b, bk=bk: e.tensor_tensor(out=x1s[b][:], in0=ps[bk][:], in1=xl[b][:], op=ALU.add),
                         reads=[Rps[bk], Rxl[b]], writes=[Rx1s[b]])
                    P.op("sp", lambda e, b=b, t=t, cb_=cb_: e.dma_start(out=S_x1[t * 128:(t + 1) * 128, cb_ * 512:(cb_ + 1) * 512], in_=x1s[b][:]),
                         reads=[Rx1s[b]], writes=[RSx1], dsem=dx1s[b])
            BX = barrier_from([RSx1] + Rx1s)
        P.barrier()
        stx = contextlib.ExitStack()
        xn2T = sb(stx, "xn2T", (128, 16, T), BF16)
        Rxn2 = [Res() for _ in range(NT)]
        with contextlib.ExitStack() as st2:
            def get_x1(t, dst, Rd, dsm):
                P.op("sp", lambda e: e.dma_start(out=dst[:], in_=S_x1[t * 128:(t + 1) * 128, :]), reads=[BX], writes=[Rd], dsem=dsm)
            norm_T(st2, get_x1, NT, xn2T, Rxn2, g2, "2")
        P.barrier()

        with contextlib.ExitStack() as stq:
            pqT = sb(stq, "pqT", (128, 16, T), BF16)
            RpqT = Res()
            with contextlib.ExitStack() as st2:
                wr = [sb(st2, "qwr%d" % i, (128, 16, 512), BF16) for i in range(2)]
                Rwr = [Res(), Res()]
                dwr = [mkds(st2), mkds(st2)]
                for cb_ in range(4):
                    s = cb_ % 2
                    P.op("pool", lambda e, s=s, cb_=cb_: e.dma_start(out=wr[s][:], in_=w_pq[:, cb_ * 512:(cb_ + 1) * 512].rearrange("(c p) n -> p c n", p=128)), writes=[Rwr[s]], dsem=dwr[s])
                    for j in range(4):
                        for tb in range(4):
                            bk = nextbank()
                            for c in range(16):
                                P.op("pe", lambda e, c=c, j=j, tb=tb, bk=bk, s=s: e.matmul(ps[bk][:], lhsT=wr[s][:, c, j * 128:(j + 1) * 128], rhs=xn2T[:, c, tb * 512:(tb + 1) * 512], start=(c == 0), stop=(c == 15)),
                                     reads=[Rwr[s]] + Rxn2[tb * 4:tb * 4 + 4], writes=[Rps[bk]], inc=(c == 15))
                            if (j + tb) % 2 == 0:
                                P.op("act", lambda e, j=j, tb=tb, bk=bk, cb_=cb_: e.activation(out=pqT[:, cb_ * 4 + j, tb * 512:(tb + 1) * 512], in_=ps[bk][:], func=AF.Copy), reads=[Rps[bk]], writes=[RpqT])
                            else:
                                P.op("dve", lambda e, j=j, tb=tb, bk=bk, cb_=cb_: e.tensor_copy(out=pqT[:, cb_ * 4 + j, tb * 512:(tb + 1) * 512], in_=ps[bk][:]), reads=[Rps[bk]], writes=[RpqT])
            P.barrier()
            kTb = sb(stq, "kTb", (128, 16, 128), BF16)
            RkT = Res()
            dkT = mkds(stq)
            P.op("pool", lambda e: e.dma_start(out=kTb[:].rearrange("p a n -> p (a n)"), in_=keysT[:, :]), writes=[RkT], dsem=dkT)
            subs = [sb(stq, "subs%d" % i, (128, 16, 128), F32) for i in range(2)]
            Rsubs = [Res(), Res()]
            dsubs = [mkds(stq), mkds(stq)]
            RSsub = Res()
            for t in range(NT):
                tk = slice(t * 128, (t + 1) * 128)
                b = t % 2
                for qd in range(4):
                    bk = nextbank()
                    for r in range(4):
                        hp = qd * 4 + r
                        P.op("pe", lambda e, hp=hp, r=r, bk=bk, tk=tk: e.matmul(ps[bk][:, r * 128:(r + 1) * 128], lhsT=pqT[:, hp, tk], rhs=kTb[:, hp, :], start=True, stop=True),
                             reads=[RpqT, RkT], writes=[Rps[bk]], inc=(r == 3))
                    P.op("act", lambda e, qd=qd, bk=bk, b=b: e.activation(out=subs[b][:, qd * 4:(qd + 1) * 4, :], in_=ps[bk][:].rearrange("p (r n) -> p r n", r=4), func=AF.Copy),
                         reads=[Rps[bk]], writes=[Rsubs[b]])
                P.op("sp", lambda e, b=b, tk=tk: e.dma_start(out=S_sub[tk, :], in_=subs[b][:].rearrange("p a n -> p (a n)")), reads=[Rsubs[b]], writes=[RSsub], dsem=dsubs[b])
            BS = barrier_from([RSsub] + Rsubs)
        RSxn2 = Res()
        dxd = mkds(st)
        for t in range(NT):
            P.op("sp", lambda e: e.dma_start(out=S_xn2[t, :, :].rearrange("p (c n) -> p c n", c=16), in_=xn2T[:, :, t * 128:(t + 1) * 128]), reads=[Rxn2[t]], writes=[RSxn2], dsem=dxd)
        BXN = barrier_from([RSxn2])
        stx.close()
        P.barrier()
        out_toks.append(BS.w)
        out_toks.append(BX.w)
        if L["UPTO"] == "O":
            return
        sub = sb(st, "sub", (128, 16, 128), F32)
        tmp = sb(st, "ptmp", (128, 16, 128), F32)
        sv = sb(st, "psv", (128, 16, 16), F32)
        cand = sb(st, "cand", (128, 8, 256), F32)
        tmp2 = tmp[:].rearrange("p (h two) n -> p h (two n)", two=2)
        c1 = sb(st, "c1", (128, 8, 16), F32)
        dmat = sb(st, "pdm", (128, 8, 16), F32)
        sc = sb(st, "psc", (128, 8, 7), F32)
        dg = sb(st, "pdg", (128, 8, 128), BF16)
        Rdg = Res()
        b3 = sb(st, "b3", (128, 8, 128), F32)
        Rsub, Rsv, Rc1, Rsc, Rb3 = (Res() for _ in range(5))
        REg = [[Res(), Res(), Res()], [Res(), Res(), Res()]]
        RE = REg[0] + REg[1]
        RG = [Res() for _ in range(4)]
        Ebuf = [tmp[:, 8 * i:8 * (i + 1), :] for i in range(2)]
        Gp = [cand[:].rearrange("p h n -> p (h n)").bitcast(BF16)[:, 1024 * i:1024 * (i + 1)] for i in range(4)]
        dsub = mkds(st)
        hraw = [sb(st, "hraw%d" % i, (128, NEB, 128), BF16) for i in range(2)]
        Rh = [Res(), Res()]
        NRING = 4
        ub = [sb(st, "ub%d" % i, (128, 2, 16, 128), BF16) for i in range(NRING)]
        Rub = [Res() for _ in range(NRING)]
        dub = [mkds(st) for _ in range(NRING)]
        vb = [sb(st, "vb%d" % i, (128, 2, D), BF16) for i in range(NRING)]
        Rvb = [Res() for _ in range(NRING)]
        dvb = [mkds(st) for _ in range(NRING)]
        xt2 = [sb(st, "xt2_%d" % i, (128, 16, 128), BF16) for i in range(2)]
        Rxt2 = [Res(), Res()]
        dxt2 = [mkds(st), mkds(st)]
        ut_next = [0]
        v_next = [0]

        def issue_ut(upto):
            while ut_next[0] < min(upto, NT * 64):
                g = ut_next[0]
                ut_next[0] += 1
                p_ = g % 64
                i = g % NRING
                P.op("sp", lambda e: e.dma_start(out=ub[i][:].rearrange("p b c n -> p b (c n)"), in_=S_UT[2 * p_:2 * p_ + 2, :, :].rearrange("b p n -> p b n")),
                     reads=[BU], writes=[Rub[i]], dsem=dub[i])

        def issue_v(upto):
            while v_next[0] < min(upto, NT * 64):
                g = v_next[0]
                v_next[0] += 1
                p_ = g % 64
                i = g % NRING
                P.op("sp", lambda e: e.dma_start(out=vb[i][:], in_=S_V[2 * p_:2 * p_ + 2, :, :].rearrange("b p n -> p b n")), reads=[BU], writes=[Rvb[i]], dsem=dvb[i])

        def load_xt2(tt):
            P.op("sp", lambda e: e.dma_start(out=xt2[tt % 2][:].rearrange("p c n -> p (c n)"), in_=S_xn2[tt, :, :]), reads=[BXN], writes=[Rxt2[tt % 2]], dsem=dxt2[tt % 2])

        MARGIN = 2.0e-3
        Eb4 = sb(st, "Eb4", (128, 4, 128), F32)
        Ea4 = sb(st, "Ea4", (128, 4, 128), F32)
        REab = Res()
        hstg = [sb(st, "hstg%d" % i, (128, 256), BF16) for i in range(2)]
        Rhstg = [Res(), Res()]
        At = [sb(st, "At%d" % i, (128, 128), BF16) for i in range(3)]
        RAt = [Res() for _ in range(3)]
        x1l = [sb(st, "x1l%d" % i, (128, 256), F32) for i in range(1)] * 2
        Rx1l = [Res()] * 2
        dx1l = [mkds(st)] * 2
        yo = [sb(st, "yo%d" % i, (128, 256), F32) for i in range(1)] * 2
        Ryo = [Res()] * 2
        dyo = [mkds(st)] * 2
        sv4 = sv[:].rearrange("p (h two) k -> p h two k", two=2)
        sub4 = sub[:].rearrange("p (h two) n -> p h two n", two=2)
        ke = 0

        hb_bank = {}

        def h_mm(tt, eb):
            g = tt * 64 + eb // 2
            issue_ut(g + NRING)
            iu = g % NRING
            bk = nextbank(4, 6)
            hb_bank[eb] = bk
            for c in range(16):
                P.op("pe", lambda e: e.matmul(ps[bk][:, 0:256].rearrange("p (b n) -> p b n", b=2), lhsT=xt2[tt % 2][:, c, :], rhs=ub[iu][:, :, c, :], start=(c == 0), stop=(c == 15)),
                     reads=[Rub[iu], Rxt2[tt % 2]], writes=[Rps[bk]], inc=(c == 15))

        def h_cp1(tt, eb):
            bk = hb_bank[eb]
            k = (eb // 2) % 2
            P.op("dve", lambda e: e.tensor_copy(out=hstg[k][:], in_=ps[bk][:, 0:256]), reads=[Rps[bk]], writes=[Rhstg[k]])

        def h_tr(tt, eb):
            bk = hb_bank[eb]
            k = (eb // 2) % 2
            tv = ps[bk][:].bitcast(BF16)[:, 512:768]
            for j in range(2):
                P.op("pe", lambda e: e.transpose(out=tv[:, j * 128:(j + 1) * 128], in_=hstg[k][:, j * 128:(j + 1) * 128], identity=identb[:]),
                     reads=[Rhstg[k]] + CONST, writes=[Rps[bk]], inc=(j == 1))

        def h_cp2(tt, eb):
            bk = hb_bank[eb]
            tv = ps[bk][:].bitcast(BF16)[:, 512:768]
            P.op("act", lambda e: e.activation(out=hraw[tt % 2][:, eb:eb + 2, :], in_=tv.rearrange("p (b n) -> p b n", b=2), func=AF.Copy), reads=[Rps[bk]], writes=[Rh[tt % 2]])

        def h_gelu(tt):
            hv = hraw[tt % 2][:].rearrange("p a n -> p (a n)")
            for qq in range(4):
                P.op("act", lambda e: e.activation(out=hv[:, qq * 4096:(qq + 1) * 4096], in_=hv[:, qq * 4096:(qq + 1) * 4096], func=AF.Gelu),
                     reads=[Rh[tt % 2]], writes=[Rh[tt % 2]])

        load_xt2(0)
        for eb in range(0, NEB, 2):
            h_mm(0, eb)
            h_cp1(0, eb)
            h_tr(0, eb)
            h_cp2(0, eb)
        h_gelu(0)
        for t in range(NT):
            tk = slice(t * 128, (t + 1) * 128)
            if t == 0:
                P.op("sp", lambda e, tk=tk: e.dma_start(out=sub[:].rearrange("p a n -> p (a n)"), in_=S_sub[tk, :]), reads=[BS], writes=[Rsub], dsem=dsub)
            for hp in range(16):
                P.op("dve", lambda e, hp=hp: e.max(out=sv[:, hp, 0:8], in_=sub[:, hp, :]), reads=[Rsub], writes=[Rsv])
                P.op("dve", lambda e, hp=hp: e.match_replace(out=tmp[:, hp, :], in_to_replace=sv[:, hp, 0:8], in_values=sub[:, hp, :], imm_value=NEG), reads=[Rsub, Rsv], writes=RE)
                P.op("dve", lambda e, hp=hp: e.max(out=sv[:, hp, 8:16], in_=tmp[:, hp, :]), reads=RE, writes=[Rsv])
            P.op("dve", lambda e: e.tensor_tensor(out=cand[:].rearrange("p h (a b) -> p h a b", a=16), in0=sv4[:, :, 0, :].unsqueeze(3).broadcast_to([128, 8, 16, 16]),
                                                   in1=sv4[:, :, 1, :].unsqueeze(2).broadcast_to([128, 8, 16, 16]), op=ALU.add), reads=[Rsv], writes=RG)
            for h in range(8):
                P.op("dve", lambda e, h=h: e.max(out=c1[:, h, 0:8], in_=cand[:, h, :]), reads=RG, writes=[Rc1])
                P.op("dve", lambda e, h=h: e.match_replace(out=tmp2[:, h, :], in_to_replace=c1[:, h, 0:8], in_values=cand[:, h, :], imm_value=NEG), reads=RG + [Rc1], writes=RE)
                P.op("dve", lambda e, h=h: e.max(out=c1[:, h, 8:16], in_=tmp2[:, h, :]), reads=RE, writes=[Rc1])
            P.op("dve", lambda e: e.tensor_tensor(out=dmat[:], in0=c1[:], in1=c1[:, :, 0:1].broadcast_to([128, 8, 16]), op=ALU.subtract), reads=[Rc1], writes=[Rsc])
            P.op("act", lambda e: e.activation(out=dmat[:], in_=dmat[:], func=AF.Exp), reads=[Rsc], writes=[Rsc])
            P.op("dve", lambda e: e.tensor_reduce(out=sc[:, :, 0], in_=dmat[:], axis=AX.X, op=ALU.add), reads=[Rsc], writes=[Rsc])
            P.op("act", lambda e: e.activation(out=sc[:, :, 1], in_=sc[:, :, 0], func=AF.Ln), reads=[Rsc], writes=[Rsc])
            P.op("dve", lambda e: e.scalar_tensor_tensor(out=sc[:, :, 2], in0=c1[:, :, 0], scalar=-1.0, in1=sc[:, :, 1], op0=ALU.mult, op1=ALU.subtract), reads=[Rsc, Rc1], writes=[Rsc])
            P.op("dve", lambda e: e.tensor_tensor(out=sc[:, :, 3], in0=c1[:, :, 15], in1=sc[:, :, 2], op=ALU.add), reads=[Rsc, Rc1], writes=[Rsc])
            P.op("act", lambda e: e.activation(out=sc[:, :, 3], in_=sc[:, :, 3], func=AF.Exp, bias=-MARGIN), reads=[Rsc], writes=[Rsc])
            P.op("dve", lambda e: e.tensor_scalar(out=sc[:, :, 4], in0=c1[:, :, 15], scalar1=-1.0, scalar2=MARGIN, op0=ALU.mult, op1=ALU.add), reads=[Rc1, Rsc], writes=[Rsc])
            P.op("dve", lambda e: e.tensor_tensor(out=b3[:], in0=sub4[:, :, 0, :], in1=sc[:, :, 4:5].broadcast_to([128, 8, 128]), op=ALU.add), reads=[Rsub, Rsc], writes=[Rb3])
            for h in range(8):
                P.op("pool", lambda e: e.tensor_scalar(out=dg[:, h, :], in0=identb[:], scalar1=sc[:, h, 3:4], scalar2=1.0, op0=ALU.mult, op1=ALU.mult),
                     reads=[Rsc] + CONST, writes=[Rdg])
            P.op("dve", lambda e: e.tensor_scalar(out=sc[:, :, 5], in0=sv4[:, :, 1, 0], scalar1=-1.0, scalar2=None, op0=ALU.mult), reads=[Rsv, Rsc], writes=[Rsc])
            P.op("dve", lambda e: e.tensor_tensor(out=sc[:, :, 6], in0=sc[:, :, 4], in1=sv4[:, :, 1, 0], op=ALU.add), reads=[Rsv, Rsc], writes=[Rsc])
            for h in range(4, 8):
                P.op("act", lambda e: e.activation(out=Eb4[:, h - 4, :], in_=sub4[:, h, 1, :], func=AF.Exp, bias=sc[:, h, 5:6]), reads=[Rsub, Rsc], writes=[REab])
                P.op("act", lambda e: e.activation(out=Ea4[:, h - 4, :], in_=sub4[:, h, 0, :], func=AF.Exp, bias=sc[:, h, 6:7]), reads=[Rsub, Rsc], writes=[REab])
            gel = hraw[t % 2]
            Rgel = Rh[t % 2]
            pend_out = None
            for eb in range(NEB):
                if eb == 0 and t + 1 < NT:
                    load_xt2(t + 1)
                if eb % 2 == 1 or eb == 0:
                    issue_v(t * 64 + eb // 2 + NRING)
                es = eb % 2
                gs = eb % 4
                for h in range(4):
                    P.op("act", lambda e: e.activation(out=Ebuf[es][:, h, :], in_=sub4[:, h, 1, :], func=AF.Exp, bias=b3[:, h, eb:eb + 1]),
                         reads=[Rsub, Rb3], writes=[REg[es][0]])
                for h in (4, 5):
                    P.op("pool", lambda e: e.tensor_scalar(out=Ebuf[es][:, h, :], in0=Eb4[:, h - 4, :], scalar1=Ea4[:, h - 4, eb:eb + 1], scalar2=1.0, op0=ALU.mult, op1=ALU.mult),
                         reads=[REab], writes=[REg[es][1]])
                for h in (6, 7):
                    P.op("dve", lambda e: e.tensor_scalar(out=Ebuf[es][:, h, :], in0=Eb4[:, h - 4, :], scalar1=Ea4[:, h - 4, eb:eb + 1], scalar2=None, op0=ALU.mult),
                         reads=[REab], writes=[REg[es][2]])
                P.op("dve", lambda e: e.scalar_tensor_tensor(out=Gp[gs], in0=Ebuf[es].rearrange("p h n -> p (h n)"), scalar=1.0, in1=Ebuf[es].rearrange("p h n -> p (h n)"),
                                                              op0=ALU.is_ge, op1=ALU.mult),
                     reads=REg[es], writes=[RG[gs]])
                if t + 1 < NT:
                    if eb % 2 == 0:
                        h_mm(t + 1, eb)
                    else:
                        h_tr(t + 1, eb - 1)
                bg = nextbank(6, 8)
                for h in range(8):
                    P.op("pe", lambda e: e.matmul(ps[bg][:, 0:128], lhsT=Gp[gs][:, h * 128:(h + 1) * 128], rhs=dg[:, h, :], start=(h == 0), stop=(h == 7)),
                         reads=[RG[gs], Rdg], writes=[Rps[bg]], inc=(h == 7))
                if pend_out is not None:
                    pend_out()
                ia = eb % 3
                P.op("dve", lambda e: e.tensor_tensor(out=At[ia][:], in0=ps[bg][:, 0:128], in1=gel[:, eb, :], op=ALU.mult),
                     reads=[Rps[bg], Rgel], writes=[RAt[ia]])
                if t + 1 < NT:
                    if eb % 2 == 0:
                        h_cp1(t + 1, eb)
                    else:
                        h_cp2(t + 1, eb - 1)

                def mk_out(eb=eb, ia=ia, t=t):
                    iv2 = (t * 64 + eb // 2) % NRING
                    for db in range(4):
                        P.op("pe", lambda e: e.matmul(ps[db][:], lhsT=At[ia][:], rhs=vb[iv2][:, eb % 2, db * 512:(db + 1) * 512], start=(eb == 0), stop=(eb == NEB - 1)),
                             reads=[RAt[ia], Rvb[iv2]], writes=[Rps[db]], inc=(db == 3))
                pend_out = mk_out
            pend_out()
            if t + 1 < NT:
                h_gelu(t + 1)
            if t + 1 < NT:
                P.op("sp", lambda e: e.dma_start(out=sub[:].rearrange("p a n -> p (a n)"), in_=S_sub[(t + 1) * 128:(t + 2) * 128, :]), reads=[BS], writes=[Rsub], dsem=dsub)
            x1big = tmp[:].rearrange("p a n -> p (a n)")
            ybig = cand[:].rearrange("p h n -> p (h n)")
            P.op("sp", lambda e: e.dma_start(out=x1big, in_=S_x1[tk, :]), reads=[BX], writes=RE, dsem=dx1l[0])
            for db in range(4):
                P.op("dve", lambda e: e.tensor_tensor(out=ybig[:, db * 512:(db + 1) * 512], in0=ps[db][:], in1=x1big[:, db * 512:(db + 1) * 512], op=ALU.add),
                     reads=[Rps[db]] + RE, writes=RG)
            tok = P.op("sp", lambda e: e.dma_start(out=y[tk, :], in_=ybig), reads=RG, dsem=dyo[0])
            out_toks.append(tok)


def _t5_bucket_static(rel):
    half, max_exact = 16, 8
    ret = np.where(rel > 0, half, 0)
    n = np.abs(rel)
    nf = np.maximum(n, 1).astype(np.float32)
    large = max_exact + (np.log(nf / max_exact) / math.log(128 / max_exact) * (half - max_exact)).astype(np.int32)
    large = np.minimum(large, half - 1)
    return ret + np.where(n < max_exact, n, large)


def _consts():
    c = np.zeros((128, 384 + 4096 + 256), np.float32)
    c[:, 0:128] = np.eye(128, dtype=np.float32)
    c[:, 128:256] = 1.0
    s = np.arange(64)
    c[0:64, 256:320] = (s[:, None] <= s[None, :]).astype(np.float32)
    c[0:64, 320:384] = (s[:, None] >= s[None, :]).astype(np.float32)
    big = np.ones((128, 4096), np.float32)
    k = np.arange(128)
    m_prev = (k[:, None] >= k[None, :]).astype(np.float32)
    m_next = (k[:, None] <= k[None, :]).astype(np.float32)
    c[:, 384:4480] = big
    return c, m_prev, m_next


_NC_CACHE = {}


def kernel(x, norm1_g, w_in, mlstm_gate_b, mlstm_norm_g, w_m_proj, attn_q_norm_g, attn_k_norm_g,
           attn_sink, rel_bias, w_a_proj, w_out, norm2_g, peer_wq, peer_keys, peer_u, peer_v, _debug=False, _upto=None):
    f = lambda a: np.ascontiguousarray(np.asarray(a, dtype=np.float32))
    x, w_in = f(x), f(w_in)
    cst, m_prev, m_next = _consts()
    rm = np.ones((4, 4096), np.float32)
    rm[:, ::64] = 0.0
    shared = {}
    shared["n1g"] = f(np.asarray(norm1_g).reshape(16, 128).T)
    shared["n2g"] = f(np.asarray(norm2_g).reshape(16, 128).T)
    shared["mng"] = f(np.broadcast_to(np.asarray(mlstm_norm_g).reshape(1, D), (64, D)))
    shared["aqg"] = f(np.asarray(attn_q_norm_g).reshape(128, 1))
    shared["akg"] = f(np.asarray(attn_k_norm_g).reshape(128, 1))
    shared["sinkb"] = f(np.broadcast_to(np.asarray(attn_sink).reshape(1, 16), (128, 16)))
    shared["w_in"] = w_in
    shared["w_mp"] = f(w_m_proj)
    shared["w_ap"] = f(w_a_proj)
    shared["w_o"] = f(w_out)
    shared["w_pq"] = f(peer_wq)
    shared["keysT"] = f(np.asarray(peer_keys).reshape(16, 128, 128).transpose(2, 0, 1).reshape(128, 16 * 128))
    shared["uT"] = f(np.asarray(peer_u).reshape(NEB, 128, 16, 128).transpose(0, 3, 2, 1).reshape(NEB, 128, 16 * 128))
    shared["pv"] = f(np.asarray(peer_v).reshape(NEB, 128, D))
    rb = np.asarray(rel_bias, dtype=np.float32)
    gbias = np.asarray(mlstm_gate_b, dtype=np.float32)
    wg_full = w_in[:, O_MG:O_MG + 16]
    kq = np.arange(128)
    in_maps = []
    for core in range(8):
        b, half = core // 2, core % 2
        xs = x[b]
        if half == 1:
            xs = xs[::-1]
        m = dict(shared)
        m["x_own"] = f(xs[:T])
        m["x_halo"] = f(xs[T:])
        cols = []
        gb = np.zeros((4, 4), np.float32)
        for d in range(2):
            td = d ^ half
            for kind in range(2):
                cols.append(wg_full[:, td * 8 + kind * 4: td * 8 + kind * 4 + 4])
                gb[:, d * 2 + kind] = gbias[td, kind, :]
        m["w_gate"] = f(np.concatenate(cols, axis=1))
        m["gate_b"] = gb
        bt = np.zeros((128, 16, 3, 128), np.float32)
        for o in range(3):
            rel = (kq[:, None] + (o - 1) * 128) - kq[None, :]
            if half == 1:
                rel = -rel
            bk = _t5_bucket_static(rel)
            bt[:, :, o, :] = rb[bk].transpose(0, 2, 1)
        m["btab"] = f(bt.reshape(128, -1))
        c2 = cst.copy()
        c2[:, 4480:4480 + 128] = m_prev
        c2[:, 4480 + 128:4480 + 256] = m_next
        c2[0:4, 384:4480] = rm
        m["cst"] = c2
        in_maps.append(m)
    key = (tuple(_debug) if _debug else None, _upto)
    if key not in _NC_CACHE:
        _NC_CACHE[key] = build_nc(debug=_debug, upto=_upto)
    nc = _NC_CACHE[key]
    in_maps = [{k: m[k] for k in nc._in_names} for m in in_maps]
    res = run_bass_kernel_spmd(nc, in_maps, core_ids=list(range(8)))
    out = np.zeros((4, 4096, D), np.float32)
    for core in range(8):
        b, half = core // 2, core % 2
        yc = res.results[core].get("y", np.zeros((T, D), np.float32)) if _debug else res.results[core]["y"]
        if half == 0:
            out[b, :T] = yc
        else:
            out[b, T:] = yc[::-1]
    if _debug:
        return out, res
    return out
```
